# Optimizing a Trainium2 kernel written in Bass

```python
import jax
import jax.numpy as jnp
from jax import lax
import numpy as np


D_MODEL = 1024
BATCH = 4
SEQ = 4096
DEPTH = 1

N_HEADS = 8
N_KV_HEADS = 2
HEAD_DIM = 64
ATTN_WIDTH = N_HEADS * HEAD_DIM
KV_WIDTH = N_KV_HEADS * HEAD_DIM
WINDOW = 128
ATTN_BLOCK = 128
CONV_CH = 512
CONV_WIDTH = 31
Q_END = ATTN_WIDTH
K_END = Q_END + KV_WIDTH
V_END = K_END + KV_WIDTH
CONV_END = V_END + 2 * CONV_CH
IN_WIDTH = CONV_END + 2 * D_MODEL
N_EXPERTS = 32
TOP_K = 4
D_EXPERT = D_MODEL
SWIGLU_ALPHA = 1.702
SWIGLU_LIMIT = 7.0
MOE_BLOCK = 128
LN_EPS = 1e-5
DEEPNORM_ALPHA = (2 * DEPTH) ** 0.25
DEEPNORM_BETA = (8 * DEPTH) ** -0.25

kernel_name = "hybrid_swa_sink_conformer_moe_deepnorm"


def layer_norm(x, g, b):
    xf = x.astype(jnp.float32)
    mu = jnp.mean(xf, axis=-1, keepdims=True)
    var = jnp.mean(jnp.square(xf - mu), axis=-1, keepdims=True)
    y = (xf - mu) * lax.rsqrt(var + LN_EPS) * g.astype(jnp.float32) + b.astype(jnp.float32)
    return y.astype(x.dtype)


def band_mask(n_blocks):
    qi = np.arange(ATTN_BLOCK)[:, None]
    kj = np.arange(2 * ATTN_BLOCK)[None, :]
    delta = qi + ATTN_BLOCK - kj
    band = (delta >= 0) & (delta < WINDOW)
    blk = np.arange(n_blocks)[:, None, None]
    return band[None] & ((blk > 0) | (kj >= ATTN_BLOCK)[None])


def sliding_window_sink_attention(q, k, v, sinks):
    B, S, _ = q.shape
    nb = S // ATTN_BLOCK
    G = N_HEADS // N_KV_HEADS
    qb = q.reshape(B, nb, ATTN_BLOCK, N_KV_HEADS, G, HEAD_DIM)
    kb = k.reshape(B, nb, ATTN_BLOCK, N_KV_HEADS, HEAD_DIM)
    vb = v.reshape(B, nb, ATTN_BLOCK, N_KV_HEADS, HEAD_DIM)
    pad = ((0, 0), (1, 0), (0, 0), (0, 0), (0, 0))
    kk = jnp.concatenate([jnp.pad(kb[:, :-1], pad), kb], axis=2)
    vv = jnp.concatenate([jnp.pad(vb[:, :-1], pad), vb], axis=2)
    s = jnp.einsum('bnqhgd,bnkhd->bnhgqk', qb, kk,
                   preferred_element_type=jnp.float32) * (HEAD_DIM ** -0.5)
    mask = band_mask(nb)
    s = jnp.where(mask[None, :, None, None], s, -1e30)
    sink = sinks.astype(jnp.float32).reshape(N_KV_HEADS, G)[None, None, :, :, None, None]
    sink = jnp.broadcast_to(sink, s.shape[:-1] + (1,))
    p = jax.nn.softmax(jnp.concatenate([s, sink], axis=-1), axis=-1)[..., :-1]
    o = jnp.einsum('bnhgqk,bnkhd->bnqhgd', p.astype(v.dtype), vv)
    return o.reshape(B, S, ATTN_WIDTH)


def conformer_conv(c_in, conv_w, conv_b, ln_g, ln_b, w_br, b_br):
    a = c_in[..., :CONV_CH] * jax.nn.sigmoid(c_in[..., CONV_CH:])
    y = lax.conv_general_dilated(
        a, conv_w[:, None, :].astype(a.dtype), window_strides=(1,),
        padding=[(CONV_WIDTH - 1, 0)], dimension_numbers=('NWC', 'WIO', 'NWC'),
        feature_group_count=CONV_CH) + conv_b
    y = jax.nn.silu(layer_norm(y, ln_g, ln_b))
    return y @ w_br + b_br


def clamped_swiglu(up):
    x_glu = jnp.minimum(up[..., ::2], SWIGLU_LIMIT)
    x_lin = jnp.clip(up[..., 1::2], -SWIGLU_LIMIT, SWIGLU_LIMIT)
    return x_glu * jax.nn.sigmoid(SWIGLU_ALPHA * x_glu) * (x_lin + 1.0)


def moe_ffn(h, w_router, b_router, w_up, b_up, w_down, b_down):
    T, D = h.shape
    logits = (h @ w_router + b_router).astype(jnp.float32)
    top_val, top_idx = lax.top_k(logits, TOP_K)
    gates = jax.nn.softmax(top_val, axis=-1)
    A = T * TOP_K
    e_flat = top_idx.reshape(A)
    tok_flat = jnp.arange(A, dtype=jnp.int32) // TOP_K
    g_flat = gates.reshape(A)
    order = jnp.argsort(e_flat, stable=True)
    e_sorted = e_flat[order]
    counts = jnp.zeros((N_EXPERTS,), jnp.int32).at[e_flat].add(1)
    padded = (counts + MOE_BLOCK - 1) // MOE_BLOCK * MOE_BLOCK
    pad_end = jnp.cumsum(padded)
    pad_start = pad_end - padded
    start = jnp.cumsum(counts) - counts
    rank = jnp.arange(A, dtype=jnp.int32) - start[e_sorted]
    dest = pad_start[e_sorted] + rank
    P = A + N_EXPERTS * MOE_BLOCK
    n_blk = P // MOE_BLOCK
    slot_tok = jnp.zeros((P,), jnp.int32).at[dest].set(tok_flat[order])
    slot_gate = jnp.zeros((P,), jnp.float32).at[dest].set(g_flat[order])
    blk_expert = jnp.minimum(
        jnp.searchsorted(pad_end, jnp.arange(n_blk, dtype=jnp.int32) * MOE_BLOCK, side='right'),
        N_EXPERTS - 1)
    xs = h[slot_tok].reshape(n_blk, MOE_BLOCK, D)

    def expert_block(args):
        xb, e = args
        act = clamped_swiglu(xb @ w_up[e] + b_up[e])
        return act @ w_down[e] + b_down[e]

    ys = lax.map(expert_block, (xs, blk_expert)).reshape(P, D)
    return jnp.zeros_like(h).at[slot_tok].add(ys * slot_gate[:, None].astype(ys.dtype))


def setup_inputs(seed: int = 0) -> dict:
    key = jax.random.key(seed)
    ks = jax.random.split(key, 24)

    def nrm(k, shape, scale):
        return jax.random.normal(k, shape, jnp.float32) * scale

    x = nrm(ks[0], (BATCH, SEQ, D_MODEL), 1.0)
    col_scale = jnp.ones((IN_WIDTH,), jnp.float32).at[K_END:V_END].set(DEEPNORM_BETA)
    w_in = nrm(ks[1], (DEPTH, D_MODEL, IN_WIDTH), D_MODEL ** -0.5) * col_scale
    b_in = nrm(ks[2], (DEPTH, IN_WIDTH), 0.02)
    attn_sinks = nrm(ks[3], (DEPTH, N_HEADS), 0.5)
    w_attn_br = nrm(ks[4], (DEPTH, ATTN_WIDTH, D_MODEL), ATTN_WIDTH ** -0.5 * DEEPNORM_BETA)
    conv_w = nrm(ks[5], (DEPTH, CONV_WIDTH, CONV_CH), CONV_WIDTH ** -0.5)
    conv_b = nrm(ks[6], (DEPTH, CONV_CH), 0.02)
    conv_ln_g = 1.0 + nrm(ks[7], (DEPTH, CONV_CH), 0.02)
    conv_ln_b = nrm(ks[8], (DEPTH, CONV_CH), 0.02)
    w_conv_br = nrm(ks[9], (DEPTH, CONV_CH, D_MODEL), CONV_CH ** -0.5 * DEEPNORM_BETA)
    b_conv_br = nrm(ks[10], (DEPTH, D_MODEL), 0.02)
    w_o = nrm(ks[11], (DEPTH, D_MODEL, D_MODEL), D_MODEL ** -0.5 * DEEPNORM_BETA)
    ln1_g = 1.0 + nrm(ks[12], (DEPTH, D_MODEL), 0.02)
    ln1_b = nrm(ks[13], (DEPTH, D_MODEL), 0.02)
    w_router = nrm(ks[14], (DEPTH, D_MODEL, N_EXPERTS), D_MODEL ** -0.5)
    b_router = nrm(ks[15], (DEPTH, N_EXPERTS), 0.01)
    w_up = nrm(ks[16], (DEPTH, N_EXPERTS, D_MODEL, 2 * D_EXPERT), D_MODEL ** -0.5)
    b_up = nrm(ks[17], (DEPTH, N_EXPERTS, 2 * D_EXPERT), 0.02)
    w_down = nrm(ks[18], (DEPTH, N_EXPERTS, D_EXPERT, D_MODEL), D_EXPERT ** -0.5 * DEEPNORM_BETA)
    b_down = nrm(ks[19], (DEPTH, N_EXPERTS, D_MODEL), 0.02)
    ln2_g = 1.0 + nrm(ks[20], (DEPTH, D_MODEL), 0.02)
    ln2_b = nrm(ks[21], (DEPTH, D_MODEL), 0.02)
    return {"x": x, "w_in": w_in, "b_in": b_in, "attn_sinks": attn_sinks, "w_attn_br": w_attn_br,
            "conv_w": conv_w, "conv_b": conv_b, "conv_ln_g": conv_ln_g, "conv_ln_b": conv_ln_b,
            "w_conv_br": w_conv_br, "b_conv_br": b_conv_br, "w_o": w_o, "ln1_g": ln1_g, "ln1_b": ln1_b,
            "w_router": w_router, "b_router": b_router, "w_up": w_up, "b_up": b_up,
            "w_down": w_down, "b_down": b_down, "ln2_g": ln2_g, "ln2_b": ln2_b}


def reference(x, w_in, b_in, attn_sinks, w_attn_br, conv_w, conv_b, conv_ln_g, conv_ln_b,
              w_conv_br, b_conv_br, w_o, ln1_g, ln1_b, w_router, b_router, w_up, b_up,
              w_down, b_down, ln2_g, ln2_b):
    B, S, D = x.shape
    for l in range(DEPTH):
        u = x @ w_in[l] + b_in[l]
        q = u[..., :Q_END]
        k = u[..., Q_END:K_END]
        v = u[..., K_END:V_END]
        c_in = u[..., V_END:CONV_END]
        gate = jax.nn.sigmoid(u[..., CONV_END:])
        y_attn = sliding_window_sink_attention(q, k, v, attn_sinks[l]) @ w_attn_br[l]
        y_conv = conformer_conv(c_in, conv_w[l], conv_b[l], conv_ln_g[l], conv_ln_b[l],
                                w_conv_br[l], b_conv_br[l])
        merged = gate[..., :D] * y_attn + gate[..., D:] * y_conv
        x = layer_norm(DEEPNORM_ALPHA * x + merged @ w_o[l], ln1_g[l], ln1_b[l])
        h = x.reshape(B * S, D)
        y_moe = moe_ffn(h, w_router[l], b_router[l], w_up[l], b_up[l], w_down[l], b_down[l])
        x = layer_norm(DEEPNORM_ALPHA * x + y_moe.reshape(B, S, D), ln2_g[l], ln2_b[l])
    return x
```

```python
from contextlib import ExitStack
import numpy as np
import concourse.bass as bass
import concourse.mybir as mybir
from concourse.bass_utils import run_bass_kernel_spmd

F32 = mybir.dt.float32
BF16 = mybir.dt.bfloat16
I32 = mybir.dt.int32
U32 = mybir.dt.uint32
AF = mybir.ActivationFunctionType
ALU = mybir.AluOpType
ENGS = ["tensor", "vector", "scalar", "gpsimd", "sync"]


class Buf:
    __slots__ = ("name", "writers", "readers")

    def __init__(self, name):
        self.name = name
        self.writers = []
        self.readers = []


class Op:
    __slots__ = ("eng", "fn", "deps", "marked", "is_dma", "sem", "val")

    def __init__(self, eng, fn, is_dma=False, sem=None):
        self.eng = eng
        self.fn = fn
        self.deps = []
        self.marked = False
        self.is_dma = is_dma
        self.sem = sem
        self.val = None


class DSem:
    def __init__(self, name, group=False):
        self.name = name
        self.count = 0
        self.handle = None
        self.group = group


def _prune(lst, op):
    if op.is_dma:
        out = [o for o in lst if not (o.is_dma and o.sem is op.sem)]
    else:
        out = [o for o in lst if o.is_dma or o.eng != op.eng]
    out.append(op)
    return out


class Sched:
    def __init__(self, nc, tag):
        self.nc = nc
        self.tag = tag
        self.ops = {e: [] for e in ENGS}
        self.dsems = []
        self.dma_ops = []

    def dsem(self, name, group=False):
        s = DSem(name, group)
        self.dsems.append(s)
        return s

    def op(self, eng, fn, reads=(), writes=(), dma_sem=None):
        o = Op(eng, fn, is_dma=dma_sem is not None, sem=dma_sem)
        deps = []
        for b in reads:
            deps.extend(b.writers)
        for b in writes:
            deps.extend(b.writers)
            deps.extend(b.readers)
        seen = set()
        for d in deps:
            if id(d) in seen:
                continue
            seen.add(id(d))
            if (not d.is_dma) and (not o.is_dma) and d.eng == "tensor" and eng == "tensor":
                continue
            if d.is_dma and o.is_dma and d.sem is o.sem:
                continue
            o.deps.append(d)
            d.marked = True
        if o.is_dma:
            dma_sem.count += 16
            o.val = dma_sem.count
            o.marked = True
            self.dma_ops.append(o)
        for b in reads:
            b.readers = _prune(b.readers, o)
        for b in writes:
            b.writers = [o]
            b.readers = []
        self.ops[eng].append(o)
        return o

    def emit(self):
        nc = self.nc
        with ExitStack() as st:
            esem = {e: st.enter_context(nc.semaphore(self.tag + "_s_" + e)) for e in ENGS}
            for s in self.dsems:
                s.handle = st.enter_context(nc.semaphore(self.tag + "_d_" + s.name))
            for e in ENGS:
                c = 0
                for o in self.ops[e]:
                    if not o.is_dma and o.marked:
                        c += 1
                        o.val = c
            finals = {}
            for o in self.dma_ops:
                finals[id(o.sem)] = o
            block = st.enter_context(nc.Block())

            def run(e, eng):
                waited = {}
                for o in self.ops[e]:
                    for d in o.deps:
                        if d.is_dma:
                            key, h = id(d.sem), d.sem.handle
                            dv = d.sem.count if d.sem.group else d.val
                        else:
                            key, h = d.eng, esem[d.eng]
                            dv = d.val
                        if waited.get(key, 0) >= dv:
                            continue
                        waited[key] = dv
                        eng.wait_ge(h, dv)
                    inst = o.fn(eng)
                    if o.is_dma:
                        inst.then_inc(o.sem.handle, 16)
                    elif o.marked:
                        inst.then_inc(esem[e], 1)
                if e == "sync":
                    for d in finals.values():
                        eng.wait_ge(d.sem.handle, d.val)

            @block.tensor
            def _(eng):
                run("tensor", eng)

            @block.vector
            def _(eng):
                run("vector", eng)

            @block.scalar
            def _(eng):
                run("scalar", eng)

            @block.gpsimd
            def _(eng):
                run("gpsimd", eng)

            @block.sync
            def _(eng):
                run("sync", eng)


def make_cfg(D=1024, NH=8, CC=512, NE=32, T=2048, CAP=384, ST=128, NCORES=8, B=4, repl=True,
             alpha=2 ** 0.25):
    c = dict(D=D, NH=NH, CC=CC, NE=NE, T=T, CAP=CAP, ST=ST, NCORES=NCORES, B=B, repl=repl, alpha=alpha)
    c["G"] = NH // 2
    c["AW"] = NH * 64
    c["QE"] = c["AW"]
    c["KE"] = c["QE"] + 128
    c["VE"] = c["KE"] + 128
    c["CE"] = c["VE"] + 2 * CC
    c["INW"] = c["CE"] + 2 * D
    c["KC"] = D // 128
    c["AC"] = c["AW"] // 128
    c["CCH"] = CC // 128
    c["FC"] = D // 128
    c["W2"] = min(D, 512)
    c["NH2"] = D // c["W2"]
    c["NBLK"] = T // 128
    c["NB"] = CAP // 128
    c["EPC"] = NE // NCORES
    c["SEQ"] = T * (NCORES // B)
    return c


def build(cfg):
    D, NH, CC, NE, T, CAP, ST = (cfg[k] for k in ["D", "NH", "CC", "NE", "T", "CAP", "ST"])
    G, AW, QE, KE, VE, CE, INW = (cfg[k] for k in ["G", "AW", "QE", "KE", "VE", "CE", "INW"])
    KC, AC, CCH, FC, W2, NH2, NBLK, NB = (cfg[k] for k in ["KC", "AC", "CCH", "FC", "W2", "NH2", "NBLK", "NB"])
    alpha = float(cfg["alpha"])
    NU = INW // 128
    GW = G * 128
    PADL = 32
    NEW = NE if cfg["repl"] else cfg["EPC"]
    BIG = 4.0e6
    EPS = 1e-5

    nc = bass.Bass("TRN2", target_bir_lowering=False)

    def din(name, shape, dt=F32):
        return nc.dram_tensor(name, list(shape), dt, kind="ExternalInput").ap()

    xT = din("xT", [D, 128 + T])
    xtok = din("xtok", [T, D])
    masks = din("masks", [128, 3, GW])
    flag = din("flag", [128, 1])
    w_in = din("w_in", [D, INW])
    b_in_t = din("b_in_t", [128, NU])
    b_v = din("b_v", [1, 128])
    sinks_b = din("sinks_b", [128, NH])
    w_ab = din("w_ab", [AW, D])
    cw_t = din("cw_t", [128, CCH, 31])
    cvec = din("cvec", [128, 3, CCH])
    w_cb = din("w_cb", [CC, D])
    bcb_t = din("bcb_t", [128, KC])
    w_o = din("w_o", [D, D])
    lnb = din("lnb", [128, 4, D])
    w_r = din("w_r", [D, NE])
    b_r = din("b_r", [1, NE])
    w_up = din("w_up", [NEW, D, 2 * D])
    b_up_t = din("b_up_t", [128, NE, 2 * FC])
    w_dn = din("w_dn", [NEW, D, D])
    b_dn = din("b_dn", [NE, D])
    consts = din("consts", [128, 3, 128])
    erow = din("erow", [128, 2, NE])
    out = nc.dram_tensor("out", [T, D], F32, kind="ExternalOutput").ap()
    xs_d = nc.dram_tensor("xs_d", [NE * CAP, D], BF16, kind="Internal").ap()
    ys_d = nc.dram_tensor("ys_d", [NE * CAP, D], F32, kind="Internal").ap()
    x1_d = nc.dram_tensor("x1_d", [T, D], F32, kind="Internal").ap()
    if not cfg["repl"]:
        EPC = cfg["EPC"]
        wl_up = nc.dram_tensor("wl_up", [EPC * D, 2 * D], BF16, kind="Internal").ap()
        wa_up = nc.dram_tensor("wa_up", [NE * D, 2 * D], BF16, kind="Internal").ap()
        wl_dn = nc.dram_tensor("wl_dn", [EPC * D, D], BF16, kind="Internal").ap()
        wa_dn = nc.dram_tensor("wa_dn", [NE * D, D], BF16, kind="Internal").ap()

    regs = {}

    def breg(e, phase):
        if phase not in regs:
            regs[phase] = e.to_reg(NE * CAP - 1)
        return regs[phase]

    with ExitStack() as pst:
        def sbp(name, shape, dt):
            return pst.enter_context(nc.sbuf_tensor(name, list(shape), dt))

        dest_i = sbp("dest_i", [128, NBLK, 4], I32)
        gate = sbp("gate", [128, NBLK, 4], F32)
        ident_f = sbp("ident_f", [128, 128], F32)
        ident_b = sbp("ident_b", [128, 128], BF16)
        ones_b = sbp("ones_b", [128, 128], BF16)
        ones_f = sbp("ones_f", [128, 128], F32)
        lnbc = sbp("lnbc", [128, 2, D], F32)
        bdn_b = sbp("bdn_b", [NE, D], BF16)
        selT = sbp("selT", [NE, NE, 128], BF16)
        bup = sbp("bup", [128, NE, 2 * FC], F32)
        ps = [pst.enter_context(nc.psum_tensor("ps%d" % i, [128, 512], F32)) for i in range(8)]
        B_dest, B_gate, B_xs, B_ys, B_x1d = Buf("dest"), Buf("gate"), Buf("xs"), Buf("ys"), Buf("x1d")
        B_const = Buf("const")

        S = Sched(nc, "m")
        PB = [Buf("ps%d" % i) for i in range(8)]
        with ExitStack() as st:
            def sb(name, shape, dt):
                return st.enter_context(nc.sbuf_tensor(name, list(shape), dt))

            w_in_bf = sb("w_in_bf", [128, KC, INW], BF16)
            w_ab_bf = sb("w_ab_bf", [128, AC, D], BF16)
            w_cb_bf = sb("w_cb_bf", [128, CCH, D], BF16)
            w_o_bf = sb("w_o_bf", [128, KC, D], BF16)
            diag = sb("diag", [128, 31, 128], BF16)
            w_r_f = sb("w_r_f", [128, KC, NE], F32)
            b_r_f = sb("b_r_f", [1, NE], F32)
            bin_t = sb("bin_t", [128, NU], F32)
            bv_b = sb("bv_b", [1, 128], BF16)
            esink = sb("esink", [128, NH], F32)
            cw = sb("cw", [128, CCH, 31], F32)
            cv = sb("cv", [128, 3, CCH], F32)
            bcb = sb("bcb", [128, KC], F32)
            mk = sb("mk", [128, 3, GW], BF16)
            flg = sb("flg", [128, 1], F32)
            cst = sb("cst", [128, 3, 128], F32)
            ltri_b = sb("ltri_b", [128, 128], BF16)
            onesm_f = sb("onesm_f", [128, 128], F32)
            er = sb("er", [128, 2, NE], F32)
            run_c = sb("run_c", [128, NE], F32)
            xT_bf = [sb("xT_bf%d" % i, [128, KC, ST], BF16) for i in range(2)]
            qT = sb("qT", [128, AC, ST], BF16)
            kT = sb("kT", [128, 128 + ST], BF16)
            Vr = sb("Vr", [128, 1 + ST // 128, 2, 65], BF16)
            aT = sb("aT", [128, CCH, PADL + ST], BF16)
            sgt = sb("sgt", [128, ST], F32)
            Pp = [sb("Pp%d" % i, [128, GW], BF16) for i in range(2)]
            Pc = [sb("Pc%d" % i, [128, GW], BF16) for i in range(2)]
            den = sb("den", [128, G], F32)
            o_n = sb("o_n", [128, AW], BF16)
            oT = sb("oT", [128, AC, ST], BF16)
            yb = sb("yb", [128, CCH, ST], F32)
            sq = sb("sq", [128, CCH, ST], F32)
            mean_sb = sb("mean_sb", [128, ST], F32)
            var_sb = sb("var_sb", [128, ST], F32)
            tmpc = sb("tmpc", [128, ST], F32)
            sT = sb("sT", [128, CCH, ST], BF16)
            ga = [sb("ga%d" % i, [128, ST], F32) for i in range(2)]
            gb = [sb("gb%d" % i, [128, ST], F32) for i in range(2)]
            t1 = sb("t1", [128, ST], F32)
            t2 = sb("t2", [128, ST], F32)
            mg = sb("mg", [128, KC, ST], BF16)
            xt = [sb("xt%d" % i, [128, D], F32) for i in range(1)]
            z = sb("z", [128, D], F32)
            nrm = sb("nrm", [128, D], F32)
            x1 = [sb("x1_%d" % i, [128, D], F32) for i in range(1)]
            x1b = [sb("x1b%d" % i, [128, D], BF16) for i in range(1)]
            x1T = sb("x1T", [128, KC, 128], F32)
            stt = sb("stt", [128, max(NH2, 1) * 6], F32)
            mv = sb("mv", [128, 2], F32)
            sm = sb("sm", [128, 8], F32)
            lg = sb("lg", [128, NE], F32)
            top8 = sb("top8", [128, 8], F32)
            tidx = sb("tidx", [128, 8], U32)
            ef = sb("ef", [128, 4], F32)
            ex4 = sb("ex4", [128, 4], F32)
            mskb = sb("mskb", [128, NE], BF16)
            pos = sb("pos", [128, NE], F32)
            ovf = sb("ovf", [128, NE], F32)
            junk = sb("junk", [128, NE], F32)
            dest_f = sb("dest_f", [128, 4], F32)

            Bn = {}

            def Bf(n):
                if n not in Bn:
                    Bn[n] = Buf(n)
                return Bn[n]

            dc = S.dsem("c", group=True)
            dw = S.dsem("w", group=True)

            def ld(eng, o_ap, i_ap, bufname, sem=dc, **kw):
                S.op(eng, lambda e: e.dma_start(out=o_ap, in_=i_ap, **kw), writes=[Bf(bufname)], dma_sem=sem)

            ld("sync", cst[:, :, :], consts, "cst")
            ld("sync", er[:, :, :], erow, "er")
            ld("sync", lnbc[:, :, :], lnb[:, 0:2, :], "lnbc")
            ld("sync", bup[:, :, :], b_up_t, "bup")
            ld("sync", w_r_f[:, :, :], w_r.rearrange("(kc p) n -> p kc n", p=128), "w_r")
            ld("sync", b_r_f[:, :], b_r, "b_r")
            ld("sync", bin_t[:, :], b_in_t, "bin")
            ld("sync", esink[:, :], sinks_b, "esink")
            ld("sync", cw[:, :, :], cw_t, "cw")
            ld("sync", cv[:, :, :], cvec, "cv")
            ld("sync", bcb[:, :], bcb_t, "bcb")
            ld("gpsimd", mk[:, :, :], masks, "mk")
            ld("sync", flg[:, :], flag, "flg")
            ld("gpsimd", bv_b[:, :], b_v, "bv")
            ld("gpsimd", bdn_b[:, :], b_dn, "bdn")
            HW = INW // 2
            for kc in range(KC):
                for hh in range(2):
                    ld("gpsimd", w_in_bf[:, kc, hh * HW:(hh + 1) * HW], w_in[kc * 128:(kc + 1) * 128, hh * HW:(hh + 1) * HW], "w_in", sem=dw)
            for ac in range(AC):
                ld("gpsimd", w_ab_bf[:, ac, :], w_ab[ac * 128:(ac + 1) * 128, :], "w_ab", sem=dw)
            for c in range(CCH):
                ld("gpsimd", w_cb_bf[:, c, :], w_cb[c * 128:(c + 1) * 128, :], "w_cb", sem=dw)
            for kc in range(KC):
                ld("gpsimd", w_o_bf[:, kc, :], w_o[kc * 128:(kc + 1) * 128, :], "w_o", sem=dw)

            V = lambda fn, r=(), w=(): S.op("vector", fn, reads=r, writes=w)
            A = lambda fn, r=(), w=(): S.op("scalar", fn, reads=r, writes=w)
            P = lambda fn, r=(), w=(): S.op("gpsimd", fn, reads=r, writes=w)
            TE = lambda fn, r=(), w=(): S.op("tensor", fn, reads=r, writes=w)

            if not cfg["repl"]:
                dwl = S.dsem("wl", group=True)
                dag = S.dsem("ag", group=True)
                for i in range(EPC):
                    for kc in range(KC):
                        r0 = i * D + kc * 128
                        S.op("gpsimd", lambda e, i=i, kc=kc, r0=r0: e.dma_start(out=wl_up[r0:r0 + 128, :], in_=w_up[i, kc * 128:(kc + 1) * 128, :]),
                             writes=[Bf("wl")], dma_sem=dwl)
                        S.op("gpsimd", lambda e, i=i, kc=kc, r0=r0: e.dma_start(out=wl_dn[r0:r0 + 128, :], in_=w_dn[i, kc * 128:(kc + 1) * 128, :]),
                             writes=[Bf("wl")], dma_sem=dwl)

            def issue_ag():
                grp = [list(range(cfg["NCORES"]))]
                S.op("gpsimd", lambda e: e.collective_compute("AllGather", ALU.bypass, replica_groups=grp, ins=[wl_up[:, :]], outs=[wa_up[:, :]]),
                     reads=[Bf("wl")], writes=[Bf("wa")], dma_sem=dag)
                S.op("gpsimd", lambda e: e.collective_compute("AllGather", ALU.bypass, replica_groups=grp, ins=[wl_dn[:, :]], outs=[wa_dn[:, :]]),
                     reads=[Bf("wl")], writes=[Bf("wa")], dma_sem=dag)

            V(lambda e: e.tensor_copy(out=ident_f[:, :], in_=cst[:, 0, :]), [Bf("cst")], [Bf("ident_f")])
            V(lambda e: e.tensor_copy(out=ident_b[:, :], in_=cst[:, 0, :]), [Bf("cst")], [Bf("ident_b")])
            V(lambda e: e.tensor_copy(out=ltri_b[:, :], in_=cst[:, 1, :]), [Bf("cst")], [Bf("ltri")])
            V(lambda e: e.tensor_copy(out=ones_b[:, :], in_=cst[:, 2, :]), [Bf("cst")], [Bf("ones_b")])
            V(lambda e: e.tensor_copy(out=ones_f[:, :], in_=cst[:, 2, :]), [Bf("cst")], [Bf("ones_f")])
            V(lambda e: e.tensor_scalar(out=onesm_f[:, :], in0=cst[:, 2, :], scalar1=1.0 / CC, scalar2=None, op0=ALU.mult),
              [Bf("cst")], [Bf("onesm")])
            V(lambda e: e.tensor_copy(out=selT[:, :, :], in_=cst[0:NE, 0, 0:NE].unsqueeze(2).to_broadcast([NE, NE, 128])), [Bf("cst")], [Bf("selT")])
            A(lambda e: e.activation(out=esink[:, :], in_=esink[:, :], func=AF.Exp), [Bf("esink")], [Bf("esink")])
            P(lambda e: e.memset(Vr[:, :, :, :], 1.0), [], [Bf("Vr")])
            P(lambda e: e.memset(run_c[:, :], 0.0), [], [Bf("run")])
            P(lambda e: e.memset(aT[:, :, :], 0.0), [], [Bf("aT")])
            P(lambda e: e.memset(kT[:, :], 0.0), [], [Bf("kT")])

            rot = [0]

            def nbank(pool=(0, 1, 2, 3)):
                rot[0] += 1
                return pool[rot[0] % len(pool)]

            dx = [S.dsem("x0"), S.dsem("x1")]
            dxt = [S.dsem("xt0"), S.dsem("xt1")]
            dx1 = [S.dsem("x1s0"), S.dsem("x1s1")]
            dsc = [S.dsem("sc0"), S.dsem("sc1")]

            NST = T // ST
            sizes = [128] + [ST] * NST
            tau0 = 0
            for s, n in enumerate(sizes):
                xs_ = s % 2
                XB = xT_bf[xs_]
                BX = Bf("xT_bf%d" % xs_)
                for kc in range(KC):
                    S.op("gpsimd", lambda e, kc=kc, XB=XB, tau0=tau0, n=n: e.dma_start(
                        out=XB[:, kc, 0:n], in_=xT[kc * 128:(kc + 1) * 128, tau0:tau0 + n]), writes=[BX], dma_sem=dx[xs_])

                def proj(ch, n=n, XB=XB, BX=BX):
                    b = nbank()
                    for kc in range(KC):
                        TE(lambda e, kc=kc, b=b, ch=ch: e.matmul(ps[b][:, 0:n], lhsT=w_in_bf[:, kc, ch * 128:(ch + 1) * 128],
                                                                 rhs=XB[:, kc, 0:n], start=(kc == 0), stop=(kc == KC - 1)),
                           [Bf("w_in"), BX], [PB[b]])
                    return b

                if s > 0:
                    for c in range(AC):
                        b = proj(c)
                        A(lambda e, b=b, c=c, n=n: e.activation(out=qT[:, c, 0:n], in_=ps[b][:, 0:n], func=AF.Identity,
                                                                 bias=bin_t[:, c:c + 1], scale=1.0), [PB[b], Bf("bin")], [Bf("qT")])
                b = proj(AC)
                A(lambda e, b=b, n=n: e.activation(out=kT[:, 128:128 + n], in_=ps[b][:, 0:n], func=AF.Identity,
                                                   bias=bin_t[:, AC:AC + 1], scale=1.0), [PB[b], Bf("bin")], [Bf("kT")])
                for bb in range(n // 128):
                    b = nbank()
                    for kc in range(KC):
                        TE(lambda e, kc=kc, b=b, bb=bb, XB=XB: e.matmul(ps[b][:, 0:128], lhsT=XB[:, kc, bb * 128:(bb + 1) * 128],
                                                                        rhs=w_in_bf[:, kc, KE:VE], start=(kc == 0), stop=False),
                           [Bf("w_in"), BX], [PB[b]])
                    TE(lambda e, b=b: e.matmul(ps[b][:, 0:128], lhsT=ones_b[0:1, :], rhs=bv_b[0:1, :], start=False, stop=True),
                       [Bf("ones_b"), Bf("bv")], [PB[b]])
                    V(lambda e, b=b, bb=bb: e.tensor_copy(out=Vr[:, 1 + bb, :, 0:64], in_=ps[b][:, 0:128].rearrange("p (g d) -> p g d", g=2)),
                      [PB[b]], [Bf("Vr")])
                for c in range(CCH):
                    bg = proj(AC + 2 + CCH + c)
                    A(lambda e, bg=bg, c=c, n=n: e.activation(out=sgt[:, 0:n], in_=ps[bg][:, 0:n], func=AF.Sigmoid,
                                                               bias=bin_t[:, AC + 2 + CCH + c:AC + 3 + CCH + c], scale=1.0),
                      [PB[bg], Bf("bin")], [Bf("sgt")])
                    ba = proj(AC + 2 + c)
                    V(lambda e, ba=ba, c=c, n=n: e.scalar_tensor_tensor(out=aT[:, c, PADL:PADL + n], in0=ps[ba][:, 0:n],
                                                                        scalar=bin_t[:, AC + 2 + c:AC + 3 + c], in1=sgt[:, 0:n],
                                                                        op0=ALU.add, op1=ALU.mult),
                      [PB[ba], Bf("bin"), Bf("sgt")], [Bf("aT")])

                if s > 0:
                    for qb in range(n // 128):
                        gq = (s - 1) * (ST // 128) + qb
                        mprev = 2 if gq == 0 else 1
                        for g in range(2):
                            rows = slice(64 * g, 64 * g + 64)
                            bP, bC, bO = 2 + 2 * g, 3 + 2 * g, 6 + g
                            pp, pc = Pp[g], Pc[g]
                            TE(lambda e, rows=rows, bP=bP, qb=qb: e.matmul(ps[bP][:, 0:GW], lhsT=kT[rows, qb * 128:qb * 128 + 128],
                                                                          rhs=qT[rows, :, qb * 128:(qb + 1) * 128], start=True, stop=True),
                               [Bf("kT"), Bf("qT")], [PB[bP]])
                            TE(lambda e, rows=rows, bC=bC, qb=qb: e.matmul(ps[bC][:, 0:GW], lhsT=kT[rows, 128 + qb * 128:256 + qb * 128],
                                                                          rhs=qT[rows, :, qb * 128:(qb + 1) * 128], start=True, stop=True),
                               [Bf("kT"), Bf("qT")], [PB[bC]])
                            A(lambda e, bP=bP, pp=pp: e.activation(out=pp[:, :], in_=ps[bP][:, 0:GW], func=AF.Exp, scale=0.125),
                              [PB[bP]], [Bf("Pp%d" % g)])
                            A(lambda e, bC=bC, pc=pc: e.activation(out=pc[:, :], in_=ps[bC][:, 0:GW], func=AF.Exp, scale=0.125),
                              [PB[bC]], [Bf("Pc%d" % g)])
                            P(lambda e, pp=pp, mprev=mprev: e.tensor_tensor(out=pp[:, :], in0=pp[:, :], in1=mk[:, mprev, :], op=ALU.mult),
                              [Bf("Pp%d" % g), Bf("mk")], [Bf("Pp%d" % g)])
                            P(lambda e, pc=pc: e.tensor_tensor(out=pc[:, :], in0=pc[:, :], in1=mk[:, 0, :], op=ALU.mult),
                              [Bf("Pc%d" % g), Bf("mk")], [Bf("Pc%d" % g)])
                            for c in range(G):
                                TE(lambda e, c=c, bO=bO, pp=pp, qb=qb, g=g: e.matmul(ps[bO][:, c * 65:(c + 1) * 65], lhsT=pp[:, c * 128:(c + 1) * 128],
                                                                                    rhs=Vr[:, qb, g, :], start=True, stop=False),
                                   [Bf("Pp%d" % g), Bf("Vr")], [PB[bO]])
                                TE(lambda e, c=c, bO=bO, pc=pc, qb=qb, g=g: e.matmul(ps[bO][:, c * 65:(c + 1) * 65], lhsT=pc[:, c * 128:(c + 1) * 128],
                                                                                    rhs=Vr[:, qb + 1, g, :], start=False, stop=True),
                                   [Bf("Pc%d" % g), Bf("Vr")], [PB[bO]])
                            o3 = ps[bO][:, 0:G * 65].rearrange("p (c d) -> p c d", c=G)
                            V(lambda e, o3=o3, g=g: e.tensor_tensor(out=den[:, :], in0=o3[:, :, 64], in1=esink[:, g * G:(g + 1) * G], op=ALU.add),
                              [PB[bO], Bf("esink")], [Bf("den")])
                            V(lambda e: e.reciprocal(out=den[:, :], in_=den[:, :]), [Bf("den")], [Bf("den")])
                            V(lambda e, o3=o3, g=g: e.tensor_tensor(out=o_n[:, g * G * 64:(g + 1) * G * 64].rearrange("p (c d) -> p c d", c=G),
                                                                    in0=o3[:, :, 0:64], in1=den[:, :].unsqueeze(2).to_broadcast([128, G, 64]), op=ALU.mult),
                              [PB[bO], Bf("den")], [Bf("o_n")])
                        b = nbank((0, 1))
                        pbf = ps[b][:, :].bitcast(BF16)
                        for ac in range(AC):
                            TE(lambda e, ac=ac, pbf=pbf: e.transpose(out=pbf[:, ac * 128:(ac + 1) * 128], in_=o_n[:, ac * 128:(ac + 1) * 128], identity=ident_b[:, :]),
                               [Bf("o_n"), Bf("ident_b")], [PB[b]])
                        A(lambda e, pbf=pbf, qb=qb: e.activation(out=oT[:, :, qb * 128:(qb + 1) * 128], in_=pbf[:, 0:AC * 128].rearrange("p (a t) -> p a t", a=AC),
                                                                 func=AF.Copy), [PB[b]], [Bf("oT")])
                    for c in range(CCH):
                        b = nbank((0, 1))
                        for j in range(31):
                            V(lambda e, j=j, c=c: e.tensor_scalar(out=diag[:, j, :], in0=cst[:, 0, :], scalar1=cw[:, c, j:j + 1],
                                                                  scalar2=None, op0=ALU.mult), [Bf("cst"), Bf("cw")], [Bf("diag")])
                        for j in range(31):
                            TE(lambda e, j=j, c=c, b=b, n=n: e.matmul(ps[b][:, 0:n], lhsT=diag[:, j, :], rhs=aT[:, c, PADL - 30 + j:PADL - 30 + j + n],
                                                                      start=(j == 0), stop=(j == 30)), [Bf("diag"), Bf("aT")], [PB[b]])
                        A(lambda e, c=c, b=b, n=n: e.activation(out=yb[:, c, 0:n], in_=ps[b][:, 0:n], func=AF.Identity, bias=cv[:, 0, c:c + 1], scale=1.0),
                          [PB[b], Bf("cv")], [Bf("yb")])
                        A(lambda e, c=c, b=b, n=n: e.activation(out=sq[:, c, 0:n], in_=ps[b][:, 0:n], func=AF.Square, bias=cv[:, 0, c:c + 1], scale=1.0),
                          [PB[b], Bf("cv")], [Bf("sq")])
                    for c in range(CCH):
                        TE(lambda e, c=c, n=n: e.matmul(ps[2][:, 0:n], lhsT=onesm_f[:, :], rhs=yb[:, c, 0:n], start=(c == 0), stop=(c == CCH - 1)),
                           [Bf("onesm"), Bf("yb")], [PB[2]])
                    for c in range(CCH):
                        TE(lambda e, c=c, n=n: e.matmul(ps[3][:, 0:n], lhsT=onesm_f[:, :], rhs=sq[:, c, 0:n], start=(c == 0), stop=(c == CCH - 1)),
                           [Bf("onesm"), Bf("sq")], [PB[3]])
                    A(lambda e, n=n: e.activation(out=mean_sb[:, 0:n], in_=ps[2][:, 0:n], func=AF.Copy), [PB[2]], [Bf("mean")])
                    V(lambda e, n=n: e.tensor_tensor(out=var_sb[:, 0:n], in0=mean_sb[:, 0:n], in1=mean_sb[:, 0:n], op=ALU.mult), [Bf("mean")], [Bf("var")])
                    V(lambda e, n=n: e.tensor_tensor(out=var_sb[:, 0:n], in0=ps[3][:, 0:n], in1=var_sb[:, 0:n], op=ALU.subtract), [PB[3], Bf("var")], [Bf("var")])
                    V(lambda e, n=n: e.tensor_scalar(out=var_sb[:, 0:n], in0=var_sb[:, 0:n], scalar1=EPS, scalar2=None, op0=ALU.add), [Bf("var")], [Bf("var")])
                    A(lambda e, n=n: e.activation(out=var_sb[:, 0:n], in_=var_sb[:, 0:n], func=AF.Sqrt), [Bf("var")], [Bf("var")])
                    V(lambda e, n=n: e.reciprocal(out=var_sb[:, 0:n], in_=var_sb[:, 0:n]), [Bf("var")], [Bf("var")])
                    for c in range(CCH):
                        V(lambda e, c=c, n=n: e.tensor_tensor(out=tmpc[:, 0:n], in0=yb[:, c, 0:n], in1=mean_sb[:, 0:n], op=ALU.subtract),
                          [Bf("yb"), Bf("mean")], [Bf("tmpc")])
                        V(lambda e, n=n: e.tensor_tensor(out=tmpc[:, 0:n], in0=tmpc[:, 0:n], in1=var_sb[:, 0:n], op=ALU.mult),
                          [Bf("tmpc"), Bf("var")], [Bf("tmpc")])
                        A(lambda e, c=c, n=n: e.activation(out=sT[:, c, 0:n], in_=tmpc[:, 0:n], func=AF.Silu, scale=cv[:, 1, c:c + 1], bias=cv[:, 2, c:c + 1]),
                          [Bf("tmpc"), Bf("cv")], [Bf("sT")])
                    for j in range(KC):
                        bs = (0, 1, 2, 3) if j % 2 == 0 else (4, 5, 6, 7)
                        bA, bGA, bB, bGB = bs
                        gaj, gbj = ga[j % 2], gb[j % 2]
                        for ac in range(AC):
                            TE(lambda e, ac=ac, j=j, bA=bA, n=n: e.matmul(ps[bA][:, 0:n], lhsT=w_ab_bf[:, ac, j * 128:(j + 1) * 128], rhs=oT[:, ac, 0:n],
                                                                          start=(ac == 0), stop=(ac == AC - 1)), [Bf("w_ab"), Bf("oT")], [PB[bA]])
                        for kc in range(KC):
                            TE(lambda e, kc=kc, j=j, bGA=bGA, n=n, XB=XB: e.matmul(ps[bGA][:, 0:n], lhsT=w_in_bf[:, kc, CE + j * 128:CE + (j + 1) * 128], rhs=XB[:, kc, 0:n],
                                                                                  start=(kc == 0), stop=(kc == KC - 1)), [Bf("w_in"), BX], [PB[bGA]])
                        for c in range(CCH):
                            TE(lambda e, c=c, j=j, bB=bB, n=n: e.matmul(ps[bB][:, 0:n], lhsT=w_cb_bf[:, c, j * 128:(j + 1) * 128], rhs=sT[:, c, 0:n],
                                                                        start=(c == 0), stop=(c == CCH - 1)), [Bf("w_cb"), Bf("sT")], [PB[bB]])
                        for kc in range(KC):
                            TE(lambda e, kc=kc, j=j, bGB=bGB, n=n, XB=XB: e.matmul(ps[bGB][:, 0:n], lhsT=w_in_bf[:, kc, CE + D + j * 128:CE + D + (j + 1) * 128], rhs=XB[:, kc, 0:n],
                                                                                  start=(kc == 0), stop=(kc == KC - 1)), [Bf("w_in"), BX], [PB[bGB]])
                        ua = CE // 128 + j
                        ub = CE // 128 + KC + j
                        A(lambda e, bGA=bGA, gaj=gaj, ua=ua, n=n: e.activation(out=gaj[:, 0:n], in_=ps[bGA][:, 0:n], func=AF.Sigmoid, bias=bin_t[:, ua:ua + 1], scale=1.0),
                          [PB[bGA], Bf("bin")], [Bf("ga%d" % (j % 2))])
                        A(lambda e, bGB=bGB, gbj=gbj, ub=ub, n=n: e.activation(out=gbj[:, 0:n], in_=ps[bGB][:, 0:n], func=AF.Sigmoid, bias=bin_t[:, ub:ub + 1], scale=1.0),
                          [PB[bGB], Bf("bin")], [Bf("gb%d" % (j % 2))])
                        V(lambda e, bA=bA, gaj=gaj, n=n: e.tensor_tensor(out=t1[:, 0:n], in0=ps[bA][:, 0:n], in1=gaj[:, 0:n], op=ALU.mult),
                          [PB[bA], Bf("ga%d" % (j % 2))], [Bf("t1")])
                        V(lambda e, bB=bB, gbj=gbj, j=j, n=n: e.scalar_tensor_tensor(out=t2[:, 0:n], in0=ps[bB][:, 0:n], scalar=bcb[:, j:j + 1], in1=gbj[:, 0:n],
                                                                                    op0=ALU.add, op1=ALU.mult), [PB[bB], Bf("gb%d" % (j % 2)), Bf("bcb")], [Bf("t2")])
                        V(lambda e, j=j, n=n: e.tensor_tensor(out=mg[:, j, 0:n], in0=t1[:, 0:n], in1=t2[:, 0:n], op=ALU.add), [Bf("t1"), Bf("t2")], [Bf("mg")])
                    for bb in range(n // 128):
                        blk = (s - 1) * (ST // 128) + bb
                        t0 = blk * 128
                        sl = 0
                        xtt, x1t, x1bt = xt[sl], x1[sl], x1b[sl]
                        S.op("sync", lambda e, xtt=xtt, t0=t0: e.dma_start(out=xtt[:, :], in_=xtok[t0:t0 + 128, :]), writes=[Bf("xt%d" % sl)], dma_sem=dxt[sl])
                        zb = (0, 1) if blk % 2 == 0 else (2, 3)
                        for h in range(NH2):
                            b = zb[h % 2]
                            for kc in range(KC):
                                TE(lambda e, kc=kc, b=b, h=h, bb=bb: e.matmul(ps[b][:, 0:W2], lhsT=mg[:, kc, bb * 128:(bb + 1) * 128], rhs=w_o_bf[:, kc, h * W2:(h + 1) * W2],
                                                                            start=(kc == 0), stop=(kc == KC - 1)), [Bf("mg"), Bf("w_o")], [PB[b]])
                            V(lambda e, b=b, h=h, xtt=xtt: e.scalar_tensor_tensor(out=z[:, h * W2:(h + 1) * W2], in0=xtt[:, h * W2:(h + 1) * W2], scalar=alpha,
                                                                                 in1=ps[b][:, 0:W2], op0=ALU.mult, op1=ALU.add), [PB[b], Bf("xt%d" % sl)], [Bf("z")])

                        def layer_norm(src, srcB, dst, dstB, gi):
                            for h in range(NH2):
                                V(lambda e, h=h: e.bn_stats(out=stt[:, h * 6:(h + 1) * 6], in_=src[:, h * W2:(h + 1) * W2]), [srcB], [Bf("stt")])
                            V(lambda e: e.bn_aggr(out=mv[:, :], in_=stt[:, 0:NH2 * 6]), [Bf("stt")], [Bf("mv")])
                            V(lambda e: e.tensor_scalar(out=sm[:, 0:1], in0=mv[:, 1:2], scalar1=EPS, scalar2=None, op0=ALU.add), [Bf("mv")], [Bf("sm")])
                            A(lambda e: e.activation(out=sm[:, 1:2], in_=sm[:, 0:1], func=AF.Sqrt), [Bf("sm")], [Bf("sm")])
                            V(lambda e: e.reciprocal(out=sm[:, 2:3], in_=sm[:, 1:2]), [Bf("sm")], [Bf("sm")])
                            V(lambda e: e.scalar_tensor_tensor(out=sm[:, 3:4], in0=mv[:, 0:1], scalar=-1.0, in1=sm[:, 2:3], op0=ALU.mult, op1=ALU.mult),
                              [Bf("mv"), Bf("sm")], [Bf("sm")])
                            A(lambda e: e.activation(out=nrm[:, :], in_=src[:, :], func=AF.Identity, scale=sm[:, 2:3], bias=sm[:, 3:4]), [srcB, Bf("sm")], [Bf("nrm")])
                            P(lambda e: e.tensor_tensor(out=nrm[:, :], in0=nrm[:, :], in1=lnbc[:, gi, :], op=ALU.mult), [Bf("nrm"), Bf("lnbc")], [Bf("nrm")])
                            P(lambda e: e.tensor_tensor(out=dst[:, :], in0=nrm[:, :], in1=lnbc[:, gi + 1, :], op=ALU.add), [Bf("nrm"), Bf("lnbc")], [dstB])

                        layer_norm(z, Bf("z"), x1t, Bf("x1_%d" % sl), 0)
                        S.op("sync", lambda e, x1t=x1t, t0=t0: e.dma_start(out=x1_d[t0:t0 + 128, :], in_=x1t[:, :]), reads=[Bf("x1_%d" % sl)], writes=[B_x1d], dma_sem=dx1[sl])
                        A(lambda e, x1t=x1t, x1bt=x1bt: e.activation(out=x1bt[:, :], in_=x1t[:, :], func=AF.Copy), [Bf("x1_%d" % sl)], [Bf("x1b%d" % sl)])
                        for kc in range(KC):
                            b = 4 + (kc // 4) % 2
                            TE(lambda e, kc=kc, b=b, x1t=x1t: e.transpose(out=ps[b][:, (kc % 4) * 128:(kc % 4 + 1) * 128], in_=x1t[:, kc * 128:(kc + 1) * 128], identity=ident_f[:, :]),
                               [Bf("x1_%d" % sl), Bf("ident_f")], [PB[b]])
                            if kc % 4 == 3 or kc == KC - 1:
                                k0 = (kc // 4) * 4
                                nk = kc - k0 + 1
                                V(lambda e, b=b, k0=k0, nk=nk: e.tensor_copy(out=x1T[:, k0:k0 + nk, :], in_=ps[b][:, 0:nk * 128].rearrange("p (k t) -> p k t", k=nk)),
                                  [PB[b]], [Bf("x1T")])
                        for kc in range(KC):
                            TE(lambda e, kc=kc: e.matmul(ps[6][:, 0:NE], lhsT=x1T[:, kc, :], rhs=w_r_f[:, kc, :], start=(kc == 0), stop=False),
                               [Bf("x1T"), Bf("w_r")], [PB[6]])
                        TE(lambda e: e.matmul(ps[6][:, 0:NE], lhsT=ones_f[0:1, :], rhs=b_r_f[0:1, :], start=False, stop=True), [Bf("ones_f"), Bf("b_r")], [PB[6]])
                        V(lambda e: e.tensor_copy(out=lg[:, :], in_=ps[6][:, 0:NE]), [PB[6]], [Bf("lg")])
                        V(lambda e: e.max(out=top8[:, :], in_=lg[:, :]), [Bf("lg")], [Bf("top8")])
                        V(lambda e: e.max_index(out=tidx[:, :], in_max=top8[:, :], in_values=lg[:, :]), [Bf("lg"), Bf("top8")], [Bf("tidx")])
                        V(lambda e: e.tensor_scalar(out=sm[:, 4:5], in0=top8[:, 0:1], scalar1=-1.0, scalar2=None, op0=ALU.mult), [Bf("top8")], [Bf("sm2")])
                        A(lambda e: e.activation(out=ex4[:, :], in_=top8[:, 0:4], func=AF.Exp, bias=sm[:, 4:5], scale=1.0), [Bf("top8"), Bf("sm2")], [Bf("ex4")])
                        V(lambda e: e.tensor_reduce(out=sm[:, 5:6], in_=ex4[:, :], axis=mybir.AxisListType.X, op=ALU.add), [Bf("ex4")], [Bf("sm3")])
                        V(lambda e: e.reciprocal(out=sm[:, 6:7], in_=sm[:, 5:6]), [Bf("sm3")], [Bf("sm3")])
                        V(lambda e, blk=blk: e.tensor_scalar(out=gate[:, blk, :], in0=ex4[:, :], scalar1=sm[:, 6:7], scalar2=None, op0=ALU.mult),
                          [Bf("ex4"), Bf("sm3")], [B_gate])
                        V(lambda e: e.tensor_scalar(out=mskb[:, :], in0=lg[:, :], scalar1=top8[:, 3:4], scalar2=None, op0=ALU.is_ge), [Bf("lg"), Bf("top8")], [Bf("mskb")])
                        TE(lambda e: e.matmul(ps[7][:, 0:NE], lhsT=ltri_b[:, :], rhs=mskb[:, :], start=True, stop=True), [Bf("ltri"), Bf("mskb")], [PB[7]])
                        TE(lambda e: e.matmul(ps[7][:, 64:64 + NE], lhsT=ones_b[:, :], rhs=mskb[:, :], start=True, stop=True), [Bf("ones_b"), Bf("mskb")], [PB[7]])
                        V(lambda e: e.tensor_tensor(out=pos[:, :], in0=ps[7][:, 0:NE], in1=run_c[:, :], op=ALU.add), [PB[7], Bf("run")], [Bf("pos")])
                        V(lambda e: e.tensor_tensor(out=run_c[:, :], in0=ps[7][:, 64:64 + NE], in1=run_c[:, :], op=ALU.add), [PB[7], Bf("run")], [Bf("run")])
                        V(lambda e: e.tensor_scalar(out=ovf[:, :], in0=pos[:, :], scalar1=float(CAP), scalar2=BIG, op0=ALU.is_ge, op1=ALU.mult), [Bf("pos")], [Bf("ovf")])
                        V(lambda e: e.tensor_tensor(out=pos[:, :], in0=pos[:, :], in1=er[:, 1, :], op=ALU.add), [Bf("pos"), Bf("er")], [Bf("pos")])
                        V(lambda e: e.tensor_tensor(out=pos[:, :], in0=pos[:, :], in1=ovf[:, :], op=ALU.add), [Bf("pos"), Bf("ovf")], [Bf("pos")])
                        V(lambda e: e.tensor_copy(out=ef[:, :], in_=tidx[:, 0:4]), [Bf("tidx")], [Bf("ef")])
                        for k in range(4):
                            V(lambda e, k=k: e.scalar_tensor_tensor(out=junk[:, :], in0=er[:, 0, :], scalar=ef[:, k:k + 1], in1=pos[:, :], op0=ALU.is_equal, op1=ALU.mult,
                                                                   accum_out=dest_f[:, k:k + 1]), [Bf("er"), Bf("ef"), Bf("pos")], [Bf("junk"), Bf("dest_f")])
                        V(lambda e, blk=blk: e.tensor_copy(out=dest_i[:, blk, :], in_=dest_f[:, :]), [Bf("dest_f")], [B_dest])
                        for k in range(4):
                            S.op("gpsimd", lambda e, k=k, blk=blk, x1bt=x1bt: e.indirect_dma_start(
                                out=xs_d[:, :], out_offset=bass.IndirectOffsetOnAxis(ap=dest_i[:, blk, k:k + 1], axis=0), in_=x1bt[:, :], in_offset=None,
                                bounds_check=breg(e, 1), oob_is_err=False), reads=[Bf("x1b%d" % sl), B_dest], writes=[], dma_sem=dsc[sl])
                P(lambda e, n=n: e.tensor_copy(out=kT[:, 0:128], in_=kT[:, n:n + 128]), [Bf("kT")], [Bf("kT")])
                P(lambda e, n=n: e.tensor_copy(out=Vr[:, 0, :, 0:64], in_=Vr[:, n // 128, :, 0:64]), [Bf("Vr")], [Bf("Vr")])
                P(lambda e, n=n: e.tensor_copy(out=aT[:, :, 0:PADL], in_=aT[:, :, n:n + PADL]), [Bf("aT")], [Bf("aT")])
                if s == 0:
                    P(lambda e: e.tensor_scalar(out=aT[:, :, 0:PADL], in0=aT[:, :, 0:PADL], scalar1=flg[:, 0:1], scalar2=None, op0=ALU.mult),
                      [Bf("aT"), Bf("flg")], [Bf("aT")])
                tau0 += n
                if (not cfg["repl"]) and s == min(2, len(sizes) - 1):
                    issue_ag()
            S.emit()

        S = Sched(nc, "e")
        PB = [Buf("ps%d" % i) for i in range(8)]
        with ExitStack() as st:
            def sb(name, shape, dt):
                return st.enter_context(nc.sbuf_tensor(name, list(shape), dt))

            wu = [sb("wu%d" % i, [128, KC, 2 * D], BF16) for i in range(2)]
            wd = [sb("wd%d" % i, [128, FC, D], BF16) for i in range(2)]
            xs_t = [sb("xs_t%d" % i, [128, NB, D], BF16) for i in range(2)]
            xsT = [sb("xsT%d" % i, [128, KC, CAP], BF16) for i in range(2)]
            actT = [sb("actT%d" % i, [128, FC, CAP], BF16) for i in range(2)]
            xg = [sb("xg%d" % i, [128, CAP], F32) for i in range(2)]
            sg = [sb("sg%d" % i, [128, CAP], F32) for i in range(2)]
            xl = [sb("xl%d" % i, [128, CAP], F32) for i in range(2)]
            ys_t = [sb("ys_t%d" % i, [128, D], F32) for i in range(2)]
            Bn = {}

            def Bf(n):
                if n not in Bn:
                    Bn[n] = Buf(n)
                return Bn[n]

            V = lambda fn, r=(), w=(): S.op("vector", fn, reads=r, writes=w)
            A = lambda fn, r=(), w=(): S.op("scalar", fn, reads=r, writes=w)
            P = lambda fn, r=(), w=(): S.op("gpsimd", fn, reads=r, writes=w)
            TE = lambda fn, r=(), w=(): S.op("tensor", fn, reads=r, writes=w)
            dwu = [S.dsem("wu0"), S.dsem("wu1")]
            dwd = [S.dsem("wd0"), S.dsem("wd1")]
            dxs = [S.dsem("xs0"), S.dsem("xs1")]
            dys = [S.dsem("ys0"), S.dsem("ys1")]

            def load_w(e_):
                sl = e_ % 2
                if not cfg["repl"]:
                    S.op("sync", lambda e, sl=sl, e_=e_: e.dma_start(out=wu[sl][:, :, :], in_=wa_up[e_ * D:(e_ + 1) * D, :].rearrange("(kc p) f -> p kc f", p=128)),
                         writes=[Bf("wu%d" % sl)], dma_sem=dwu[sl])
                    S.op("sync", lambda e, sl=sl, e_=e_: e.dma_start(out=wd[sl][:, :, :], in_=wa_dn[e_ * D:(e_ + 1) * D, :].rearrange("(kc p) f -> p kc f", p=128)),
                         writes=[Bf("wd%d" % sl)], dma_sem=dwd[sl])
                    return
                for kc in range(KC):
                    S.op("gpsimd", lambda e, kc=kc, sl=sl, e_=e_: e.dma_start(out=wu[sl][:, kc, :], in_=w_up[e_, kc * 128:(kc + 1) * 128, :]),
                         writes=[Bf("wu%d" % sl)], dma_sem=dwu[sl])
                for fc in range(FC):
                    S.op("gpsimd", lambda e, fc=fc, sl=sl, e_=e_: e.dma_start(out=wd[sl][:, fc, :], in_=w_dn[e_, fc * 128:(fc + 1) * 128, :]),
                         writes=[Bf("wd%d" % sl)], dma_sem=dwd[sl])

            def load_xs(e_):
                sl = e_ % 2
                S.op("sync", lambda e, sl=sl, e_=e_: e.dma_start(out=xs_t[sl][:, :, :], in_=xs_d[e_ * CAP:(e_ + 1) * CAP, :].rearrange("(b p) d -> p b d", p=128)),
                     reads=[B_xs], writes=[Bf("xs_t%d" % sl)], dma_sem=dxs[sl])

            load_w(0)
            load_xs(0)
            cnt = [0]
            for e_ in range(NE):
                sl = e_ % 2
                if e_ + 1 < NE:
                    load_w(e_ + 1)
                    load_xs(e_ + 1)
                Bwu, Bwd = Bf("wu%d" % sl), Bf("wd%d" % sl)
                for nb in range(NB):
                    b = 6 + nb % 2
                    pbf = ps[b][:, :].bitcast(BF16)
                    for kc in range(KC):
                        TE(lambda e, kc=kc, nb=nb, pbf=pbf, sl=sl: e.transpose(out=pbf[:, kc * 128:(kc + 1) * 128], in_=xs_t[sl][:, nb, kc * 128:(kc + 1) * 128], identity=ident_b[:, :]),
                           [Bf("xs_t%d" % sl)], [PB[b]])
                    cp = A if nb % 2 == 0 else V
                    if nb % 2 == 0:
                        A(lambda e, pbf=pbf, nb=nb, sl=sl: e.activation(out=xsT[sl][:, :, nb * 128:(nb + 1) * 128], in_=pbf[:, 0:KC * 128].rearrange("p (k t) -> p k t", k=KC), func=AF.Copy),
                          [PB[b]], [Bf("xsT%d" % sl)])
                    else:
                        V(lambda e, pbf=pbf, nb=nb, sl=sl: e.tensor_copy(out=xsT[sl][:, :, nb * 128:(nb + 1) * 128], in_=pbf[:, 0:KC * 128].rearrange("p (k t) -> p k t", k=KC)),
                          [PB[b]], [Bf("xsT%d" % sl)])
                for fp in range(FC):
                    cnt[0] += 1
                    pr = cnt[0] % 2
                    bG, bL = (0, 1) if pr == 0 else (2, 3)
                    for kc in range(KC):
                        TE(lambda e, kc=kc, fp=fp, bG=bG, sl=sl: e.matmul(ps[bG][:, 0:CAP], lhsT=wu[sl][:, kc, fp * 128:(fp + 1) * 128], rhs=xsT[sl][:, kc, :],
                                                                         start=(kc == 0), stop=(kc == KC - 1)), [Bwu, Bf("xsT%d" % sl)], [PB[bG]])
                    for kc in range(KC):
                        TE(lambda e, kc=kc, fp=fp, bL=bL, sl=sl: e.matmul(ps[bL][:, 0:CAP], lhsT=wu[sl][:, kc, D + fp * 128:D + (fp + 1) * 128], rhs=xsT[sl][:, kc, :],
                                                                         start=(kc == 0), stop=(kc == KC - 1)), [Bwu, Bf("xsT%d" % sl)], [PB[bL]])
                    V(lambda e, bG=bG, e_=e_, fp=fp, pr=pr: e.tensor_scalar(out=xg[pr][:, :], in0=ps[bG][:, 0:CAP], scalar1=bup[:, e_, fp:fp + 1], scalar2=7.0, op0=ALU.add, op1=ALU.min),
                      [PB[bG]], [Bf("xg%d" % pr)])
                    A(lambda e, pr=pr: e.activation(out=sg[pr][:, :], in_=xg[pr][:, :], func=AF.Sigmoid, scale=1.702), [Bf("xg%d" % pr)], [Bf("sg%d" % pr)])
                    V(lambda e, bL=bL, e_=e_, fp=fp, pr=pr: e.tensor_scalar(out=xl[pr][:, :], in0=ps[bL][:, 0:CAP], scalar1=bup[:, e_, FC + fp:FC + fp + 1], scalar2=7.0, op0=ALU.add, op1=ALU.min),
                      [PB[bL]], [Bf("xl%d" % pr)])
                    P(lambda e, pr=pr: e.tensor_scalar(out=xl[pr][:, :], in0=xl[pr][:, :], scalar1=-7.0, scalar2=1.0, op0=ALU.max, op1=ALU.add), [Bf("xl%d" % pr)], [Bf("xl%d" % pr)])
                    P(lambda e, pr=pr: e.tensor_tensor(out=xg[pr][:, :], in0=xg[pr][:, :], in1=sg[pr][:, :], op=ALU.mult), [Bf("xg%d" % pr), Bf("sg%d" % pr)], [Bf("xg%d" % pr)])
                    V(lambda e, pr=pr, fp=fp, sl=sl: e.tensor_tensor(out=actT[sl][:, fp, :], in0=xg[pr][:, :], in1=xl[pr][:, :], op=ALU.mult),
                      [Bf("xg%d" % pr), Bf("xl%d" % pr)], [Bf("actT%d" % sl)])
                for nb in range(NB):
                    ysl = (e_ * NB + nb) % 2
                    for h in range(NH2):
                        b = 4 + h % 2
                        for fc in range(FC):
                            TE(lambda e, fc=fc, nb=nb, h=h, b=b, sl=sl: e.matmul(ps[b][:, 0:W2], lhsT=actT[sl][:, fc, nb * 128:(nb + 1) * 128], rhs=wd[sl][:, fc, h * W2:(h + 1) * W2],
                                                                                start=(fc == 0), stop=False), [Bf("actT%d" % sl), Bwd], [PB[b]])
                        TE(lambda e, h=h, b=b, e_=e_: e.matmul(ps[b][:, 0:W2], lhsT=selT[:, e_, :], rhs=bdn_b[:, h * W2:(h + 1) * W2], start=False, stop=True),
                           [], [PB[b]])
                        if h % 2 == 0:
                            A(lambda e, b=b, h=h, ysl=ysl: e.activation(out=ys_t[ysl][:, h * W2:(h + 1) * W2], in_=ps[b][:, 0:W2], func=AF.Copy), [PB[b]], [Bf("ys_t%d" % ysl)])
                        else:
                            V(lambda e, b=b, h=h, ysl=ysl: e.tensor_copy(out=ys_t[ysl][:, h * W2:(h + 1) * W2], in_=ps[b][:, 0:W2]), [PB[b]], [Bf("ys_t%d" % ysl)])
                    r0 = e_ * CAP + nb * 128
                    S.op("sync", lambda e, ysl=ysl, r0=r0: e.dma_start(out=ys_d[r0:r0 + 128, :], in_=ys_t[ysl][:, :]), reads=[Bf("ys_t%d" % ysl)], writes=[], dma_sem=dys[ysl])
            S.emit()

        S = Sched(nc, "c")
        with ExitStack() as st:
            def sb(name, shape, dt):
                return st.enter_context(nc.sbuf_tensor(name, list(shape), dt))

            x1c = [sb("x1c%d" % i, [128, D], F32) for i in range(2)]
            yk = [[sb("yk%d_%d" % (i, k), [128, D], F32) for k in range(4)] for i in range(2)]
            acc = sb("acc", [128, D], F32)
            nrm = sb("nrm2", [128, D], F32)
            res = [sb("res%d" % i, [128, D], F32) for i in range(2)]
            stt = sb("stt2", [128, max(NH2, 1) * 6], F32)
            mv = sb("mv2", [128, 2], F32)
            sm = sb("sm2", [128, 8], F32)
            Bn = {}

            def Bf(n):
                if n not in Bn:
                    Bn[n] = Buf(n)
                return Bn[n]

            V = lambda fn, r=(), w=(): S.op("vector", fn, reads=r, writes=w)
            A = lambda fn, r=(), w=(): S.op("scalar", fn, reads=r, writes=w)
            P = lambda fn, r=(), w=(): S.op("gpsimd", fn, reads=r, writes=w)
            S.op("sync", lambda e: e.dma_start(out=lnbc[:, :, :], in_=lnb[:, 2:4, :]), writes=[Bf("lnbc")], dma_sem=S.dsem("ln2", group=True))
            dxc = [S.dsem("xc0"), S.dsem("xc1")]
            dyk = [S.dsem("yk0"), S.dsem("yk1")]
            dout = [S.dsem("o0"), S.dsem("o1")]
            for blk in range(NBLK):
                sl = blk % 2
                t0 = blk * 128
                S.op("sync", lambda e, sl=sl, t0=t0: e.dma_start(out=x1c[sl][:, :], in_=x1_d[t0:t0 + 128, :]), writes=[Bf("x1c%d" % sl)], dma_sem=dxc[sl])
                for k in range(4):
                    S.op("gpsimd", lambda e, k=k, sl=sl, blk=blk: e.indirect_dma_start(
                        out=yk[sl][k][:, :], out_offset=None, in_=ys_d[:, :], in_offset=bass.IndirectOffsetOnAxis(ap=dest_i[:, blk, k:k + 1], axis=0),
                        bounds_check=breg(e, 3), oob_is_err=False), writes=[Bf("yk%d_%d" % (sl, k))], dma_sem=dyk[sl])
                V(lambda e, sl=sl: e.tensor_scalar(out=acc[:, :], in0=x1c[sl][:, :], scalar1=alpha, scalar2=None, op0=ALU.mult), [Bf("x1c%d" % sl)], [Bf("acc")])
                for k in range(4):
                    V(lambda e, k=k, sl=sl, blk=blk: e.scalar_tensor_tensor(out=acc[:, :], in0=yk[sl][k][:, :], scalar=gate[:, blk, k:k + 1], in1=acc[:, :], op0=ALU.mult, op1=ALU.add),
                      [Bf("yk%d_%d" % (sl, k)), Bf("acc")], [Bf("acc")])
                for h in range(NH2):
                    V(lambda e, h=h: e.bn_stats(out=stt[:, h * 6:(h + 1) * 6], in_=acc[:, h * W2:(h + 1) * W2]), [Bf("acc")], [Bf("stt")])
                V(lambda e: e.bn_aggr(out=mv[:, :], in_=stt[:, 0:NH2 * 6]), [Bf("stt")], [Bf("mv")])
                V(lambda e: e.tensor_scalar(out=sm[:, 0:1], in0=mv[:, 1:2], scalar1=EPS, scalar2=None, op0=ALU.add), [Bf("mv")], [Bf("sm")])
                A(lambda e: e.activation(out=sm[:, 1:2], in_=sm[:, 0:1], func=AF.Sqrt), [Bf("sm")], [Bf("sm")])
                V(lambda e: e.reciprocal(out=sm[:, 2:3], in_=sm[:, 1:2]), [Bf("sm")], [Bf("sm")])
                V(lambda e: e.scalar_tensor_tensor(out=sm[:, 3:4], in0=mv[:, 0:1], scalar=-1.0, in1=sm[:, 2:3], op0=ALU.mult, op1=ALU.mult), [Bf("mv"), Bf("sm")], [Bf("sm")])
                A(lambda e: e.activation(out=nrm[:, :], in_=acc[:, :], func=AF.Identity, scale=sm[:, 2:3], bias=sm[:, 3:4]), [Bf("acc"), Bf("sm")], [Bf("nrm")])
                P(lambda e: e.tensor_tensor(out=nrm[:, :], in0=nrm[:, :], in1=lnbc[:, 0, :], op=ALU.mult), [Bf("nrm"), Bf("lnbc")], [Bf("nrm")])
                P(lambda e, sl=sl: e.tensor_tensor(out=res[sl][:, :], in0=nrm[:, :], in1=lnbc[:, 1, :], op=ALU.add), [Bf("nrm"), Bf("lnbc")], [Bf("res%d" % sl)])
                S.op("sync", lambda e, sl=sl, t0=t0: e.dma_start(out=out[t0:t0 + 128, :], in_=res[sl][:, :]), reads=[Bf("res%d" % sl)], writes=[], dma_sem=dout[sl])
            S.emit()
    return nc


def prep_inputs(cfg, x, w_in, b_in, attn_sinks, w_attn_br, conv_w, conv_b, conv_ln_g, conv_ln_b,
                w_conv_br, b_conv_br, w_o, ln1_g, ln1_b, w_router, b_router, w_up, b_up,
                w_down, b_down, ln2_g, ln2_b):
    D, NH, CC, NE, T, CAP = (cfg[k] for k in ["D", "NH", "CC", "NE", "T", "CAP"])
    G, AW, QE, KE, VE, CE, INW, KC, CCH, FC = (cfg[k] for k in ["G", "AW", "QE", "KE", "VE", "CE", "INW", "KC", "CCH", "FC"])
    NC_ = cfg["NCORES"]
    CPB = NC_ // cfg["B"]
    f = lambda a: np.ascontiguousarray(np.asarray(a, dtype=np.float32))
    x = f(x)
    w_in0, b_in0 = f(w_in)[0], f(b_in)[0]
    qperm = np.array([(g * G + c) * 64 + j for c in range(G) for g in range(2) for j in range(64)])
    perm = np.concatenate([qperm, np.arange(QE, INW)])
    w_in_p = np.ascontiguousarray(w_in0[:, perm])
    b_in_p = b_in0[perm]
    b_in_t = np.ascontiguousarray(b_in_p.reshape(INW // 128, 128).T)
    b_v = np.ascontiguousarray(b_in_p[KE:VE].reshape(1, 128))
    sinks_b = np.ascontiguousarray(np.broadcast_to(f(attn_sinks)[0][None, :], (128, NH)))
    cw_t = np.ascontiguousarray(f(conv_w)[0].T.reshape(CCH, 128, 31).transpose(1, 0, 2))
    pp = lambda v: v.reshape(-1, 128).T
    cvec = np.ascontiguousarray(np.stack([pp(f(conv_b)[0]), pp(f(conv_ln_g)[0]), pp(f(conv_ln_b)[0])], axis=1))
    bcb_t = np.ascontiguousarray(pp(f(b_conv_br)[0]))
    lnb = np.ascontiguousarray(np.broadcast_to(np.stack([f(ln1_g)[0], f(ln1_b)[0], f(ln2_g)[0], f(ln2_b)[0]])[None], (128, 4, D)))
    w_up0 = f(w_up)[0]
    w_up_p = np.concatenate([w_up0[:, :, 0::2], w_up0[:, :, 1::2]], axis=2)
    b_up0 = f(b_up)[0]
    b_up_p = np.concatenate([b_up0[:, 0::2], b_up0[:, 1::2]], axis=1)
    b_up_t = np.ascontiguousarray(b_up_p.reshape(NE, 2 * FC, 128).transpose(2, 0, 1))
    w_dn0 = f(w_down)[0]
    b_dn = np.ascontiguousarray(f(b_down)[0])
    ident = np.eye(128, dtype=np.float32)
    ltri = np.triu(np.ones((128, 128), np.float32), 1)
    consts = np.ascontiguousarray(np.stack([ident, ltri, np.ones((128, 128), np.float32)], axis=1))
    erow = np.ascontiguousarray(np.broadcast_to(np.stack([np.arange(NE, dtype=np.float32), np.arange(NE, dtype=np.float32) * CAP])[None], (128, 2, NE)))
    kk = np.arange(128)[:, None]
    qq = np.arange(128)[None, :]
    m_cur = (kk <= qq).astype(np.float32)
    m_prev = (kk > qq).astype(np.float32)
    shared = dict(w_in=w_in_p, b_in_t=b_in_t, b_v=b_v, sinks_b=sinks_b, w_ab=f(w_attn_br)[0], cw_t=cw_t, cvec=cvec,
                  w_cb=f(w_conv_br)[0], bcb_t=bcb_t, w_o=f(w_o)[0], lnb=lnb, w_r=f(w_router)[0], b_r=f(b_router)[0].reshape(1, NE),
                  b_up_t=b_up_t, b_dn=b_dn, consts=consts, erow=erow)
    maps = []
    for c in range(NC_):
        b, h = c // CPB, c % CPB
        st = h * T
        xT = np.zeros((D, 128 + T), np.float32)
        xT[:, 128:] = x[b, st:st + T].T
        if h > 0:
            xT[:, :128] = x[b, st - 128:st].T
        fl = 1.0 if h > 0 else 0.0
        masks = np.stack([np.tile(m_cur, (1, G)), np.tile(m_prev, (1, G)), np.tile(m_prev * fl, (1, G))], axis=1)
        m = dict(shared)
        m.update(xT=xT, xtok=np.ascontiguousarray(x[b, st:st + T]), masks=np.ascontiguousarray(masks),
                 flag=np.full((128, 1), fl, np.float32))
        if cfg["repl"]:
            m.update(w_up=w_up_p, w_dn=w_dn0)
        else:
            E = cfg["EPC"]
            m.update(w_up=np.ascontiguousarray(w_up_p[c * E:(c + 1) * E]), w_dn=np.ascontiguousarray(w_dn0[c * E:(c + 1) * E]))
        maps.append(m)
    return maps


def run_cfg(cfg, inputs, trace=False):
    maps = prep_inputs(cfg, **inputs)
    nc = build(cfg)
    res = run_bass_kernel_spmd(nc, maps, core_ids=list(range(cfg["NCORES"])), trace=trace)
    T, D, B = cfg["T"], cfg["D"], cfg["B"]
    CPB = cfg["NCORES"] // B
    outv = np.zeros((B, CPB * T, D), np.float32)
    for c in range(cfg["NCORES"]):
        b, h = c // CPB, c % CPB
        outv[b, h * T:(h + 1) * T] = res.results[c]["out"]
    return outv, res


def kernel(**inputs):
    cfg = make_cfg()
    outv, _ = run_cfg(cfg, inputs)
    return outv
```

```python
from contextlib import ExitStack
import numpy as np
import concourse.bass as bass
import concourse.mybir as mybir
from concourse.bass_utils import run_bass_kernel_spmd

F32 = mybir.dt.float32
BF16 = mybir.dt.bfloat16
I32 = mybir.dt.int32
U32 = mybir.dt.uint32
AF = mybir.ActivationFunctionType
ALU = mybir.AluOpType
ENGS = ["tensor", "vector", "scalar", "gpsimd", "sync"]


class Buf:
    __slots__ = ("name", "writers", "readers")

    def __init__(self, name):
        self.name = name
        self.writers = []
        self.readers = []


class Op:
    __slots__ = ("eng", "fn", "deps", "marked", "is_dma", "sem", "val")

    def __init__(self, eng, fn, is_dma=False, sem=None):
        self.eng = eng
        self.fn = fn
        self.deps = []
        self.marked = False
        self.is_dma = is_dma
        self.sem = sem
        self.val = None


class DSem:
    def __init__(self, name, group=False, inc=16):
        self.name = name
        self.count = 0
        self.handle = None
        self.group = group
        self.inc = inc


def _prune(lst, op):
    if op.is_dma:
        out = [o for o in lst if not (o.is_dma and o.sem is op.sem)]
    else:
        out = [o for o in lst if o.is_dma or o.eng != op.eng]
    out.append(op)
    return out


class Sched:
    def __init__(self, nc, tag):
        self.nc = nc
        self.tag = tag
        self.ops = {e: [] for e in ENGS}
        self.dsems = []
        self.dma_ops = []

    def dsem(self, name, group=False, inc=16):
        s = DSem(name, group, inc)
        self.dsems.append(s)
        return s

    def op(self, eng, fn, reads=(), writes=(), dma_sem=None):
        o = Op(eng, fn, is_dma=dma_sem is not None, sem=dma_sem)
        deps = []
        for b in reads:
            deps.extend(b.writers)
        for b in writes:
            deps.extend(b.writers)
            deps.extend(b.readers)
        seen = set()
        for d in deps:
            if id(d) in seen:
                continue
            seen.add(id(d))
            if (not d.is_dma) and (not o.is_dma) and d.eng == "tensor" and eng == "tensor":
                continue
            if d.is_dma and o.is_dma and d.sem is o.sem:
                continue
            o.deps.append(d)
            d.marked = True
        if o.is_dma:
            dma_sem.count += dma_sem.inc
            o.val = dma_sem.count
            o.marked = True
            self.dma_ops.append(o)
        for b in reads:
            b.readers = _prune(b.readers, o)
        for b in writes:
            b.writers = [o]
            b.readers = []
        self.ops[eng].append(o)
        return o

    def emit(self):
        nc = self.nc
        with ExitStack() as st:
            esem = {e: st.enter_context(nc.semaphore(self.tag + "_s_" + e)) for e in ENGS}
            for s in self.dsems:
                s.handle = st.enter_context(nc.semaphore(self.tag + "_d_" + s.name))
            for e in ENGS:
                c = 0
                for o in self.ops[e]:
                    if not o.is_dma and o.marked:
                        c += 1
                        o.val = c
            finals = {}
            for o in self.dma_ops:
                finals[id(o.sem)] = o
            block = st.enter_context(nc.Block())

            def run(e, eng):
                waited = {}
                for o in self.ops[e]:
                    for d in o.deps:
                        if d.is_dma:
                            key, h = id(d.sem), d.sem.handle
                            dv = d.sem.count if d.sem.group else d.val
                        else:
                            key, h = d.eng, esem[d.eng]
                            dv = d.val
                        if waited.get(key, 0) >= dv:
                            continue
                        waited[key] = dv
                        eng.wait_ge(h, dv)
                    inst = o.fn(eng)
                    if o.is_dma:
                        if o.sem.inc == 16:
                            inst.then_inc(o.sem.handle, 16)
                        else:
                            inst.then_inc(o.sem.handle)
                    elif o.marked:
                        inst.then_inc(esem[e], 1)
                if e == "sync":
                    for d in finals.values():
                        eng.wait_ge(d.sem.handle, d.val)

            @block.tensor
            def _(eng):
                run("tensor", eng)

            @block.vector
            def _(eng):
                run("vector", eng)

            @block.scalar
            def _(eng):
                run("scalar", eng)

            @block.gpsimd
            def _(eng):
                run("gpsimd", eng)

            @block.sync
            def _(eng):
                run("sync", eng)


def make_cfg(D=1024, NH=8, CC=512, NE=32, T=2048, CAP=384, ST=128, NCORES=8, B=4, repl=True,
             alpha=2 ** 0.25):
    c = dict(D=D, NH=NH, CC=CC, NE=NE, T=T, CAP=CAP, ST=ST, NCORES=NCORES, B=B, repl=repl, alpha=alpha)
    c["G"] = NH // 2
    c["AW"] = NH * 64
    c["QE"] = c["AW"]
    c["KE"] = c["QE"] + 128
    c["VE"] = c["KE"] + 128
    c["CE"] = c["VE"] + 2 * CC
    c["INW"] = c["CE"] + 2 * D
    c["KC"] = D // 128
    c["AC"] = c["AW"] // 128
    c["CCH"] = CC // 128
    c["FC"] = D // 128
    c["W2"] = min(D, 512)
    c["NH2"] = D // c["W2"]
    c["NBLK"] = T // 128
    c["NB"] = CAP // 128
    c["EPC"] = NE // NCORES
    c["SEQ"] = T * (NCORES // B)
    return c


def build(cfg):
    D, NH, CC, NE, T, CAP, ST = (cfg[k] for k in ["D", "NH", "CC", "NE", "T", "CAP", "ST"])
    G, AW, QE, KE, VE, CE, INW = (cfg[k] for k in ["G", "AW", "QE", "KE", "VE", "CE", "INW"])
    KC, AC, CCH, FC, W2, NH2, NBLK, NB = (cfg[k] for k in ["KC", "AC", "CCH", "FC", "W2", "NH2", "NBLK", "NB"])
    alpha = float(cfg["alpha"])
    NU = INW // 128
    GW = G * 128
    PADL = 32
    NEW = NE if cfg["repl"] else cfg["EPC"]
    BIG = 4.0e6
    EPS = 1e-5

    nc = bass.Bass("TRN2", target_bir_lowering=False)

    def din(name, shape, dt=F32):
        return nc.dram_tensor(name, list(shape), dt, kind="ExternalInput").ap()

    xT = din("xT", [D, 128 + T])
    xtok = din("xtok", [T, D])
    masks = din("masks", [128, 3, GW])
    flag = din("flag", [128, 1])
    w_in = din("w_in", [D, INW])
    b_in_t = din("b_in_t", [128, NU])
    b_v = din("b_v", [1, 128])
    sinks_b = din("sinks_b", [128, NH])
    w_ab = din("w_ab", [AW, D])
    cw_t = din("cw_t", [128, CCH, 31])
    cvec = din("cvec", [128, 3, CCH])
    w_cb = din("w_cb", [CC, D])
    bcb_t = din("bcb_t", [128, KC])
    w_o = din("w_o", [D, D])
    lnb = din("lnb", [128, 4, D])
    w_r = din("w_r", [D, NE])
    b_r = din("b_r", [1, NE])
    w_up = din("w_up", [NEW, D, 2 * D])
    b_up_t = din("b_up_t", [128, NE, 2 * FC])
    w_dn = din("w_dn", [NEW, D, D])
    b_dn = din("b_dn", [NE, D])
    consts = din("consts", [128, 3, 128])
    erow = din("erow", [128, 2, NE])
    out = nc.dram_tensor("out", [T, D], F32, kind="ExternalOutput").ap()
    xs_d = nc.dram_tensor("xs_d", [NE * CAP, D], BF16, kind="Internal").ap()
    ys_d = nc.dram_tensor("ys_d", [NE * CAP, D], F32, kind="Internal").ap()
    x1_d = nc.dram_tensor("x1_d", [T, D], F32, kind="Internal").ap()
    if not cfg["repl"]:
        EPC = cfg["EPC"]
        wl_up_t = nc.dram_tensor("wl_up", [EPC * D, 2 * D], BF16)
        wa_up_t = nc.dram_tensor("wa_up", [NE * D, 2 * D], BF16)
        wl_dn_t = nc.dram_tensor("wl_dn", [EPC * D, D], BF16)
        wa_dn_t = nc.dram_tensor("wa_dn", [NE * D, D], BF16)
        wl_up, wa_up, wl_dn, wa_dn = wl_up_t.ap(), wa_up_t.ap(), wl_dn_t.ap(), wa_dn_t.ap()

    regs = {}

    def breg(e, phase):
        if phase not in regs:
            regs[phase] = e.to_reg(NE * CAP - 1)
        return regs[phase]

    with ExitStack() as pst:
        def sbp(name, shape, dt):
            return pst.enter_context(nc.sbuf_tensor(name, list(shape), dt))

        dest_i = sbp("dest_i", [128, NBLK, 4], I32)
        gate = sbp("gate", [128, NBLK, 4], F32)
        ident_f = sbp("ident_f", [128, 128], F32)
        ident_b = sbp("ident_b", [128, 128], BF16)
        ones_b = sbp("ones_b", [128, 128], BF16)
        ones_f = sbp("ones_f", [128, 128], F32)
        lnbc = sbp("lnbc", [128, 2, D], F32)
        bdn_b = sbp("bdn_b", [NE, D], BF16)
        selT = sbp("selT", [NE, NE, 128], BF16)
        bup = sbp("bup", [128, NE, 2 * FC], F32)
        ps = [pst.enter_context(nc.psum_tensor("ps%d" % i, [128, 512], F32)) for i in range(8)]
        B_dest, B_gate, B_xs, B_ys, B_x1d = Buf("dest"), Buf("gate"), Buf("xs"), Buf("ys"), Buf("x1d")
        B_const = Buf("const")

        S = Sched(nc, "m")
        PB = [Buf("ps%d" % i) for i in range(8)]
        with ExitStack() as st:
            def sb(name, shape, dt):
                return st.enter_context(nc.sbuf_tensor(name, list(shape), dt))

            w_in_bf = sb("w_in_bf", [128, KC, INW], BF16)
            w_ab_bf = sb("w_ab_bf", [128, AC, D], BF16)
            w_cb_bf = sb("w_cb_bf", [128, CCH, D], BF16)
            w_o_bf = sb("w_o_bf", [128, KC, D], BF16)
            diag2 = [sb("diag%d" % i, [128, 31, 128], BF16) for i in range(2)]
            w_r_f = sb("w_r_f", [128, KC, NE], F32)
            b_r_f = sb("b_r_f", [1, NE], F32)
            bin_t = sb("bin_t", [128, NU], F32)
            bv_b = sb("bv_b", [1, 128], BF16)
            esink = sb("esink", [128, NH], F32)
            cw = sb("cw", [128, CCH, 31], F32)
            cv = sb("cv", [128, 3, CCH], F32)
            bcb = sb("bcb", [128, KC], F32)
            mk = sb("mk", [128, 3, GW], BF16)
            flg = sb("flg", [128, 1], F32)
            cst = sb("cst", [128, 3, 128], F32)
            ltri_b = sb("ltri_b", [128, 128], BF16)
            onesm_f = sb("onesm_f", [128, 128], F32)
            er = sb("er", [128, 2, NE], F32)
            run_c = sb("run_c", [128, NE], F32)
            xT_bf = [sb("xT_bf%d" % i, [128, KC, ST], BF16) for i in range(2)]
            qT = sb("qT", [128, AC, ST], BF16)
            kT = sb("kT", [128, 128 + ST], BF16)
            Vr = sb("Vr", [128, 1 + ST // 128, 2, 65], BF16)
            aT = sb("aT", [128, CCH, PADL + ST], BF16)
            sgt = sb("sgt", [128, ST], F32)
            Pp = [sb("Pp%d" % i, [128, GW], BF16) for i in range(2)]
            Pc = [sb("Pc%d" % i, [128, GW], BF16) for i in range(2)]
            den = sb("den", [128, G], F32)
            o_n = sb("o_n", [128, AW], BF16)
            oT = sb("oT", [128, AC, ST], BF16)
            yb = sb("yb", [128, CCH, ST], F32)
            sq = sb("sq", [128, CCH, ST], F32)
            mean_sb = sb("mean_sb", [128, ST], F32)
            var_sb = sb("var_sb", [128, ST], F32)
            tmpc = sb("tmpc", [128, ST], F32)
            sT = sb("sT", [128, CCH, ST], BF16)
            ga = [sb("ga%d" % i, [128, ST], F32) for i in range(2)]
            gb = [sb("gb%d" % i, [128, ST], F32) for i in range(2)]
            t1 = sb("t1", [128, ST], F32)
            t2 = sb("t2", [128, ST], F32)
            mg = sb("mg", [128, KC, ST], BF16)
            xt = [sb("xt%d" % i, [128, D], F32) for i in range(1)]
            z = sb("z", [128, D], F32)
            nrm = sb("nrm", [128, D], F32)
            x1 = [sb("x1_%d" % i, [128, D], F32) for i in range(1)]
            x1b = [sb("x1b%d" % i, [128, D], BF16) for i in range(1)]
            x1T = sb("x1T", [128, KC, 128], F32)
            stt = sb("stt", [128, max(NH2, 1) * 6], F32)
            mv = sb("mv", [128, 2], F32)
            sm = sb("sm", [128, 8], F32)
            lg = sb("lg", [128, NE], F32)
            top8 = sb("top8", [128, 8], F32)
            tidx = sb("tidx", [128, 8], U32)
            ef = sb("ef", [128, 4], F32)
            ex4 = sb("ex4", [128, 4], F32)
            mskb = sb("mskb", [128, NE], BF16)
            pos = sb("pos", [128, NE], F32)
            ovf = sb("ovf", [128, NE], F32)
            junk = sb("junk", [128, NE], F32)
            dest_f = sb("dest_f", [128, 4], F32)

            Bn = {}

            def Bf(n):
                if n not in Bn:
                    Bn[n] = Buf(n)
                return Bn[n]

            dc = S.dsem("c", group=True)
            dw = S.dsem("w", group=True)

            def ld(eng, o_ap, i_ap, bufname, sem=dc, **kw):
                S.op(eng, lambda e: e.dma_start(out=o_ap, in_=i_ap, **kw), writes=[Bf(bufname)], dma_sem=sem)

            ld("sync", cst[:, :, :], consts, "cst")
            ld("sync", er[:, :, :], erow, "er")
            ld("sync", lnbc[:, :, :], lnb[:, 0:2, :], "lnbc")
            ld("sync", bup[:, :, :], b_up_t, "bup")
            ld("sync", w_r_f[:, :, :], w_r.rearrange("(kc p) n -> p kc n", p=128), "w_r")
            ld("sync", b_r_f[:, :], b_r, "b_r")
            ld("sync", bin_t[:, :], b_in_t, "bin")
            ld("sync", esink[:, :], sinks_b, "esink")
            ld("sync", cw[:, :, :], cw_t, "cw")
            ld("sync", cv[:, :, :], cvec, "cv")
            ld("sync", bcb[:, :], bcb_t, "bcb")
            ld("gpsimd", mk[:, :, :], masks, "mk")
            ld("sync", flg[:, :], flag, "flg")
            ld("gpsimd", bv_b[:, :], b_v, "bv")
            ld("gpsimd", bdn_b[:, :], b_dn, "bdn")
            HW = INW // 2
            for kc in range(KC):
                for hh in range(2):
                    ld("gpsimd", w_in_bf[:, kc, hh * HW:(hh + 1) * HW], w_in[kc * 128:(kc + 1) * 128, hh * HW:(hh + 1) * HW], "w_in", sem=dw)
            for ac in range(AC):
                ld("gpsimd", w_ab_bf[:, ac, :], w_ab[ac * 128:(ac + 1) * 128, :], "w_ab", sem=dw)
            for c in range(CCH):
                ld("gpsimd", w_cb_bf[:, c, :], w_cb[c * 128:(c + 1) * 128, :], "w_cb", sem=dw)
            for kc in range(KC):
                ld("gpsimd", w_o_bf[:, kc, :], w_o[kc * 128:(kc + 1) * 128, :], "w_o", sem=dw)

            V = lambda fn, r=(), w=(): S.op("vector", fn, reads=r, writes=w)
            A = lambda fn, r=(), w=(): S.op("scalar", fn, reads=r, writes=w)
            P = lambda fn, r=(), w=(): S.op("gpsimd", fn, reads=r, writes=w)
            TE = lambda fn, r=(), w=(): S.op("tensor", fn, reads=r, writes=w)

            cast_jobs = []
            if not cfg["repl"]:
                dwl = S.dsem("wl", group=True)
                dag = S.dsem("ag", group=True, inc=1)
                for i in range(EPC):
                    for kc in range(KC):
                        cast_jobs.append((0, i, kc))
                        cast_jobs.append((1, i, kc))

            def issue_cast(job, jn):
                which, i, kc = job
                r0 = i * D + kc * 128
                if which == 0:
                    S.op("gpsimd", lambda e: e.dma_start(out=wl_up[r0:r0 + 128, :], in_=w_up[i, kc * 128:(kc + 1) * 128, :]), writes=[Bf("wl")], dma_sem=dwl)
                else:
                    S.op("gpsimd", lambda e: e.dma_start(out=wl_dn[r0:r0 + 128, :], in_=w_dn[i, kc * 128:(kc + 1) * 128, :]), writes=[Bf("wl")], dma_sem=dwl)

            def issue_ag():
                grp = [list(range(cfg["NCORES"]))]
                S.op("gpsimd", lambda e: e.collective_compute("AllGather", ALU.bypass, replica_groups=grp, ins=[wl_up_t.ap().opt()], outs=[wa_up_t.ap().opt()]),
                     reads=[Bf("wl")], writes=[Bf("wa")], dma_sem=dag)
                S.op("gpsimd", lambda e: e.collective_compute("AllGather", ALU.bypass, replica_groups=grp, ins=[wl_dn_t.ap().opt()], outs=[wa_dn_t.ap().opt()]),
                     reads=[Bf("wl")], writes=[Bf("wa")], dma_sem=dag)

            V(lambda e: e.tensor_copy(out=ident_f[:, :], in_=cst[:, 0, :]), [Bf("cst")], [Bf("ident_f")])
            V(lambda e: e.tensor_copy(out=ident_b[:, :], in_=cst[:, 0, :]), [Bf("cst")], [Bf("ident_b")])
            V(lambda e: e.tensor_copy(out=ltri_b[:, :], in_=cst[:, 1, :]), [Bf("cst")], [Bf("ltri")])
            V(lambda e: e.tensor_copy(out=ones_b[:, :], in_=cst[:, 2, :]), [Bf("cst")], [Bf("ones_b")])
            V(lambda e: e.tensor_copy(out=ones_f[:, :], in_=cst[:, 2, :]), [Bf("cst")], [Bf("ones_f")])
            V(lambda e: e.tensor_scalar(out=onesm_f[:, :], in0=cst[:, 2, :], scalar1=1.0 / CC, scalar2=None, op0=ALU.mult),
              [Bf("cst")], [Bf("onesm")])
            V(lambda e: e.tensor_copy(out=selT[:, :, :], in_=cst[0:NE, 0, 0:NE].unsqueeze(2).to_broadcast([NE, NE, 128])), [Bf("cst")], [Bf("selT")])
            A(lambda e: e.activation(out=esink[:, :], in_=esink[:, :], func=AF.Exp), [Bf("esink")], [Bf("esink")])
            P(lambda e: e.memset(Vr[:, :, :, :], 1.0), [], [Bf("Vr")])
            P(lambda e: e.memset(run_c[:, :], 0.0), [], [Bf("run")])
            P(lambda e: e.memset(aT[:, :, :], 0.0), [], [Bf("aT")])
            P(lambda e: e.memset(kT[:, :], 0.0), [], [Bf("kT")])

            rot = [0]

            def nbank(pool=(0, 1, 2, 3)):
                rot[0] += 1
                return pool[rot[0] % len(pool)]

            dx = [S.dsem("x0"), S.dsem("x1")]
            dxt = [S.dsem("xt0"), S.dsem("xt1")]
            dx1 = [S.dsem("x1s0"), S.dsem("x1s1")]
            dsc = [S.dsem("sc0"), S.dsem("sc1")]

            NST = T // ST
            sizes = [128] + [ST] * NST
            tau0 = 0
            def body(s, n, tau0):
                    xs_ = s % 2
                    XB = xT_bf[xs_]
                    BX = Bf("xT_bf%d" % xs_)
                    for kc in range(KC):
                        S.op("gpsimd", lambda e, kc=kc, XB=XB, tau0=tau0, n=n: e.dma_start(
                            out=XB[:, kc, 0:n], in_=xT[kc * 128:(kc + 1) * 128, tau0:tau0 + n]), writes=[BX], dma_sem=dx[xs_])

                    def proj(ch, n=n, XB=XB, BX=BX):
                        b = nbank()
                        for kc in range(KC):
                            TE(lambda e, kc=kc, b=b, ch=ch: e.matmul(ps[b][:, 0:n], lhsT=w_in_bf[:, kc, ch * 128:(ch + 1) * 128],
                                                                     rhs=XB[:, kc, 0:n], start=(kc == 0), stop=(kc == KC - 1)),
                               [Bf("w_in"), BX], [PB[b]])
                        return b

                    if s > 0:
                        for c in range(AC):
                            b = proj(c)
                            A(lambda e, b=b, c=c, n=n: e.activation(out=qT[:, c, 0:n], in_=ps[b][:, 0:n], func=AF.Identity,
                                                                     bias=bin_t[:, c:c + 1], scale=1.0), [PB[b], Bf("bin")], [Bf("qT")])
                    b = proj(AC)
                    A(lambda e, b=b, n=n: e.activation(out=kT[:, 128:128 + n], in_=ps[b][:, 0:n], func=AF.Identity,
                                                       bias=bin_t[:, AC:AC + 1], scale=1.0), [PB[b], Bf("bin")], [Bf("kT")])
                    for bb in range(n // 128):
                        b = nbank()
                        for kc in range(KC):
                            TE(lambda e, kc=kc, b=b, bb=bb, XB=XB: e.matmul(ps[b][:, 0:128], lhsT=XB[:, kc, bb * 128:(bb + 1) * 128],
                                                                            rhs=w_in_bf[:, kc, KE:VE], start=(kc == 0), stop=False),
                               [Bf("w_in"), BX], [PB[b]])
                        TE(lambda e, b=b: e.matmul(ps[b][:, 0:128], lhsT=ones_b[0:1, :], rhs=bv_b[0:1, :], start=False, stop=True),
                           [Bf("ones_b"), Bf("bv")], [PB[b]])
                        V(lambda e, b=b, bb=bb: e.tensor_copy(out=Vr[:, 1 + bb, :, 0:64], in_=ps[b][:, 0:128].rearrange("p (g d) -> p g d", g=2)),
                          [PB[b]], [Bf("Vr")])
                    for c in range(CCH):
                        bg = proj(AC + 2 + CCH + c)
                        A(lambda e, bg=bg, c=c, n=n: e.activation(out=sgt[:, 0:n], in_=ps[bg][:, 0:n], func=AF.Sigmoid,
                                                                   bias=bin_t[:, AC + 2 + CCH + c:AC + 3 + CCH + c], scale=1.0),
                          [PB[bg], Bf("bin")], [Bf("sgt")])
                        ba = proj(AC + 2 + c)
                        V(lambda e, ba=ba, c=c, n=n: e.scalar_tensor_tensor(out=aT[:, c, PADL:PADL + n], in0=ps[ba][:, 0:n],
                                                                            scalar=bin_t[:, AC + 2 + c:AC + 3 + c], in1=sgt[:, 0:n],
                                                                            op0=ALU.add, op1=ALU.mult),
                          [PB[ba], Bf("bin"), Bf("sgt")], [Bf("aT")])

                    if s > 0:
                        for qb in range(n // 128):
                            gq = (s - 1) * (ST // 128) + qb
                            mprev = 2 if gq == 0 else 1
                            for g in range(2):
                                rows = slice(64 * g, 64 * g + 64)
                                bP, bC, bO = 2 + 2 * g, 3 + 2 * g, 6 + g
                                pp, pc = Pp[g], Pc[g]
                                TE(lambda e, rows=rows, bP=bP, qb=qb: e.matmul(ps[bP][:, 0:GW], lhsT=kT[rows, qb * 128:qb * 128 + 128],
                                                                              rhs=qT[rows, :, qb * 128:(qb + 1) * 128], start=True, stop=True),
                                   [Bf("kT"), Bf("qT")], [PB[bP]])
                                TE(lambda e, rows=rows, bC=bC, qb=qb: e.matmul(ps[bC][:, 0:GW], lhsT=kT[rows, 128 + qb * 128:256 + qb * 128],
                                                                              rhs=qT[rows, :, qb * 128:(qb + 1) * 128], start=True, stop=True),
                                   [Bf("kT"), Bf("qT")], [PB[bC]])
                                A(lambda e, bP=bP, pp=pp: e.activation(out=pp[:, :], in_=ps[bP][:, 0:GW], func=AF.Exp, scale=0.125),
                                  [PB[bP]], [Bf("Pp%d" % g)])
                                A(lambda e, bC=bC, pc=pc: e.activation(out=pc[:, :], in_=ps[bC][:, 0:GW], func=AF.Exp, scale=0.125),
                                  [PB[bC]], [Bf("Pc%d" % g)])
                                P(lambda e, pp=pp, mprev=mprev: e.tensor_tensor(out=pp[:, :], in0=pp[:, :], in1=mk[:, mprev, :], op=ALU.mult),
                                  [Bf("Pp%d" % g), Bf("mk")], [Bf("Pp%d" % g)])
                                P(lambda e, pc=pc: e.tensor_tensor(out=pc[:, :], in0=pc[:, :], in1=mk[:, 0, :], op=ALU.mult),
                                  [Bf("Pc%d" % g), Bf("mk")], [Bf("Pc%d" % g)])
                                for c in range(G):
                                    TE(lambda e, c=c, bO=bO, pp=pp, qb=qb, g=g: e.matmul(ps[bO][:, c * 65:(c + 1) * 65], lhsT=pp[:, c * 128:(c + 1) * 128],
                                                                                        rhs=Vr[:, qb, g, :], start=True, stop=False),
                                       [Bf("Pp%d" % g), Bf("Vr")], [PB[bO]])
                                    TE(lambda e, c=c, bO=bO, pc=pc, qb=qb, g=g: e.matmul(ps[bO][:, c * 65:(c + 1) * 65], lhsT=pc[:, c * 128:(c + 1) * 128],
                                                                                        rhs=Vr[:, qb + 1, g, :], start=False, stop=True),
                                       [Bf("Pc%d" % g), Bf("Vr")], [PB[bO]])
                                o3 = ps[bO][:, 0:G * 65].rearrange("p (c d) -> p c d", c=G)
                                V(lambda e, o3=o3, g=g: e.tensor_tensor(out=den[:, :], in0=o3[:, :, 64], in1=esink[:, g * G:(g + 1) * G], op=ALU.add),
                                  [PB[bO], Bf("esink")], [Bf("den")])
                                V(lambda e: e.reciprocal(out=den[:, :], in_=den[:, :]), [Bf("den")], [Bf("den")])
                                V(lambda e, o3=o3, g=g: e.tensor_tensor(out=o_n[:, g * G * 64:(g + 1) * G * 64].rearrange("p (c d) -> p c d", c=G),
                                                                        in0=o3[:, :, 0:64], in1=den[:, :].unsqueeze(2).to_broadcast([128, G, 64]), op=ALU.mult),
                                  [PB[bO], Bf("den")], [Bf("o_n")])
                            b = nbank((0, 1))
                            pbf = ps[b][:, :].bitcast(BF16)
                            for ac in range(AC):
                                TE(lambda e, ac=ac, pbf=pbf: e.transpose(out=pbf[:, ac * 128:(ac + 1) * 128], in_=o_n[:, ac * 128:(ac + 1) * 128], identity=ident_b[:, :]),
                                   [Bf("o_n"), Bf("ident_b")], [PB[b]])
                            A(lambda e, pbf=pbf, qb=qb: e.activation(out=oT[:, :, qb * 128:(qb + 1) * 128], in_=pbf[:, 0:AC * 128].rearrange("p (a t) -> p a t", a=AC),
                                                                     func=AF.Copy), [PB[b]], [Bf("oT")])
                        for c in range(CCH):
                            b = nbank((0, 1))
                            dg = diag2[c % 2]
                            dgB = Bf("diag%d" % (c % 2))
                            for j in range(31):
                                eng_ = V if j % 2 == 0 else P
                                eng_(lambda e, j=j, c=c, dg=dg: e.tensor_scalar(out=dg[:, j, :], in0=cst[:, 0, :], scalar1=cw[:, c, j:j + 1],
                                                                                scalar2=1.0, op0=ALU.mult, op1=ALU.mult), [Bf("cst"), Bf("cw")], [dgB])
                            for j in range(31):
                                TE(lambda e, j=j, c=c, b=b, n=n, dg=dg: e.matmul(ps[b][:, 0:n], lhsT=dg[:, j, :], rhs=aT[:, c, PADL - 30 + j:PADL - 30 + j + n],
                                                                                 start=(j == 0), stop=(j == 30)), [dgB, Bf("aT")], [PB[b]])
                            A(lambda e, c=c, b=b, n=n: e.activation(out=yb[:, c, 0:n], in_=ps[b][:, 0:n], func=AF.Identity, bias=cv[:, 0, c:c + 1], scale=1.0),
                              [PB[b], Bf("cv")], [Bf("yb")])
                            A(lambda e, c=c, b=b, n=n: e.activation(out=sq[:, c, 0:n], in_=ps[b][:, 0:n], func=AF.Square, bias=cv[:, 0, c:c + 1], scale=1.0),
                              [PB[b], Bf("cv")], [Bf("sq")])
                        for c in range(CCH):
                            TE(lambda e, c=c, n=n: e.matmul(ps[2][:, 0:n], lhsT=onesm_f[:, :], rhs=yb[:, c, 0:n], start=(c == 0), stop=(c == CCH - 1)),
                               [Bf("onesm"), Bf("yb")], [PB[2]])
                        for c in range(CCH):
                            TE(lambda e, c=c, n=n: e.matmul(ps[3][:, 0:n], lhsT=onesm_f[:, :], rhs=sq[:, c, 0:n], start=(c == 0), stop=(c == CCH - 1)),
                               [Bf("onesm"), Bf("sq")], [PB[3]])
                        A(lambda e, n=n: e.activation(out=mean_sb[:, 0:n], in_=ps[2][:, 0:n], func=AF.Copy), [PB[2]], [Bf("mean")])
                        V(lambda e, n=n: e.tensor_tensor(out=var_sb[:, 0:n], in0=mean_sb[:, 0:n], in1=mean_sb[:, 0:n], op=ALU.mult), [Bf("mean")], [Bf("var")])
                        V(lambda e, n=n: e.tensor_tensor(out=var_sb[:, 0:n], in0=ps[3][:, 0:n], in1=var_sb[:, 0:n], op=ALU.subtract), [PB[3], Bf("var")], [Bf("var")])
                        V(lambda e, n=n: e.tensor_scalar(out=var_sb[:, 0:n], in0=var_sb[:, 0:n], scalar1=EPS, scalar2=None, op0=ALU.add), [Bf("var")], [Bf("var")])
                        A(lambda e, n=n: e.activation(out=var_sb[:, 0:n], in_=var_sb[:, 0:n], func=AF.Sqrt), [Bf("var")], [Bf("var")])
                        V(lambda e, n=n: e.reciprocal(out=var_sb[:, 0:n], in_=var_sb[:, 0:n]), [Bf("var")], [Bf("var")])
                        for c in range(CCH):
                            V(lambda e, c=c, n=n: e.tensor_tensor(out=tmpc[:, 0:n], in0=yb[:, c, 0:n], in1=mean_sb[:, 0:n], op=ALU.subtract),
                              [Bf("yb"), Bf("mean")], [Bf("tmpc")])
                            V(lambda e, n=n: e.tensor_tensor(out=tmpc[:, 0:n], in0=tmpc[:, 0:n], in1=var_sb[:, 0:n], op=ALU.mult),
                              [Bf("tmpc"), Bf("var")], [Bf("tmpc")])
                            A(lambda e, c=c, n=n: e.activation(out=sT[:, c, 0:n], in_=tmpc[:, 0:n], func=AF.Silu, scale=cv[:, 1, c:c + 1], bias=cv[:, 2, c:c + 1]),
                              [Bf("tmpc"), Bf("cv")], [Bf("sT")])
                    P(lambda e, n=n: e.tensor_copy(out=kT[:, 0:128], in_=kT[:, n:n + 128]), [Bf("kT")], [Bf("kT")])
                    P(lambda e, n=n: e.tensor_copy(out=Vr[:, 0, :, 0:64], in_=Vr[:, n // 128, :, 0:64]), [Bf("Vr")], [Bf("Vr")])
                    P(lambda e, n=n: e.tensor_copy(out=aT[:, :, 0:PADL], in_=aT[:, :, n:n + PADL]), [Bf("aT")], [Bf("aT")])
                    if s == 0:
                        P(lambda e: e.tensor_scalar(out=aT[:, :, 0:PADL], in0=aT[:, :, 0:PADL], scalar1=flg[:, 0:1], scalar2=None, op0=ALU.mult),
                          [Bf("aT"), Bf("flg")], [Bf("aT")])
                    yield "A"
                    if s == 0:
                        return
                    for j in range(KC):
                        bs = (0, 1, 2, 3) if j % 2 == 0 else (4, 5, 6, 7)
                        bA, bGA, bB, bGB = bs
                        gaj, gbj = ga[j % 2], gb[j % 2]
                        for ac in range(AC):
                            TE(lambda e, ac=ac, j=j, bA=bA, n=n: e.matmul(ps[bA][:, 0:n], lhsT=w_ab_bf[:, ac, j * 128:(j + 1) * 128], rhs=oT[:, ac, 0:n],
                                                                          start=(ac == 0), stop=(ac == AC - 1)), [Bf("w_ab"), Bf("oT")], [PB[bA]])
                        for kc in range(KC):
                            TE(lambda e, kc=kc, j=j, bGA=bGA, n=n, XB=XB: e.matmul(ps[bGA][:, 0:n], lhsT=w_in_bf[:, kc, CE + j * 128:CE + (j + 1) * 128], rhs=XB[:, kc, 0:n],
                                                                                  start=(kc == 0), stop=(kc == KC - 1)), [Bf("w_in"), BX], [PB[bGA]])
                        for c in range(CCH):
                            TE(lambda e, c=c, j=j, bB=bB, n=n: e.matmul(ps[bB][:, 0:n], lhsT=w_cb_bf[:, c, j * 128:(j + 1) * 128], rhs=sT[:, c, 0:n],
                                                                        start=(c == 0), stop=(c == CCH - 1)), [Bf("w_cb"), Bf("sT")], [PB[bB]])
                        for kc in range(KC):
                            TE(lambda e, kc=kc, j=j, bGB=bGB, n=n, XB=XB: e.matmul(ps[bGB][:, 0:n], lhsT=w_in_bf[:, kc, CE + D + j * 128:CE + D + (j + 1) * 128], rhs=XB[:, kc, 0:n],
                                                                                  start=(kc == 0), stop=(kc == KC - 1)), [Bf("w_in"), BX], [PB[bGB]])
                        ua = CE // 128 + j
                        ub = CE // 128 + KC + j
                        A(lambda e, bGA=bGA, gaj=gaj, ua=ua, n=n: e.activation(out=gaj[:, 0:n], in_=ps[bGA][:, 0:n], func=AF.Sigmoid, bias=bin_t[:, ua:ua + 1], scale=1.0),
                          [PB[bGA], Bf("bin")], [Bf("ga%d" % (j % 2))])
                        A(lambda e, bGB=bGB, gbj=gbj, ub=ub, n=n: e.activation(out=gbj[:, 0:n], in_=ps[bGB][:, 0:n], func=AF.Sigmoid, bias=bin_t[:, ub:ub + 1], scale=1.0),
                          [PB[bGB], Bf("bin")], [Bf("gb%d" % (j % 2))])
                        V(lambda e, bA=bA, gaj=gaj, n=n: e.tensor_tensor(out=t1[:, 0:n], in0=ps[bA][:, 0:n], in1=gaj[:, 0:n], op=ALU.mult),
                          [PB[bA], Bf("ga%d" % (j % 2))], [Bf("t1")])
                        V(lambda e, bB=bB, gbj=gbj, j=j, n=n: e.scalar_tensor_tensor(out=t2[:, 0:n], in0=ps[bB][:, 0:n], scalar=bcb[:, j:j + 1], in1=gbj[:, 0:n],
                                                                                    op0=ALU.add, op1=ALU.mult), [PB[bB], Bf("gb%d" % (j % 2)), Bf("bcb")], [Bf("t2")])
                        V(lambda e, j=j, n=n: e.tensor_tensor(out=mg[:, j, 0:n], in0=t1[:, 0:n], in1=t2[:, 0:n], op=ALU.add), [Bf("t1"), Bf("t2")], [Bf("mg")])
                    yield "B"
                    for bb in range(n // 128):
                        blk = (s - 1) * (ST // 128) + bb
                        t0 = blk * 128
                        sl = 0
                        xtt, x1t, x1bt = xt[sl], x1[sl], x1b[sl]
                        S.op("sync", lambda e, xtt=xtt, t0=t0: e.dma_start(out=xtt[:, :], in_=xtok[t0:t0 + 128, :]), writes=[Bf("xt%d" % sl)], dma_sem=dxt[sl])
                        zb = (0, 1) if blk % 2 == 0 else (2, 3)
                        for h in range(NH2):
                            b = zb[h % 2]
                            for kc in range(KC):
                                TE(lambda e, kc=kc, b=b, h=h, bb=bb: e.matmul(ps[b][:, 0:W2], lhsT=mg[:, kc, bb * 128:(bb + 1) * 128], rhs=w_o_bf[:, kc, h * W2:(h + 1) * W2],
                                                                            start=(kc == 0), stop=(kc == KC - 1)), [Bf("mg"), Bf("w_o")], [PB[b]])
                            V(lambda e, b=b, h=h, xtt=xtt: e.scalar_tensor_tensor(out=z[:, h * W2:(h + 1) * W2], in0=xtt[:, h * W2:(h + 1) * W2], scalar=alpha,
                                                                                 in1=ps[b][:, 0:W2], op0=ALU.mult, op1=ALU.add), [PB[b], Bf("xt%d" % sl)], [Bf("z")])

                        def layer_norm(src, srcB, dst, dstB, gi):
                            for h in range(NH2):
                                V(lambda e, h=h: e.bn_stats(out=stt[:, h * 6:(h + 1) * 6], in_=src[:, h * W2:(h + 1) * W2]), [srcB], [Bf("stt")])
                            V(lambda e: e.bn_aggr(out=mv[:, :], in_=stt[:, 0:NH2 * 6]), [Bf("stt")], [Bf("mv")])
                            V(lambda e: e.tensor_scalar(out=sm[:, 0:1], in0=mv[:, 1:2], scalar1=EPS, scalar2=None, op0=ALU.add), [Bf("mv")], [Bf("sm")])
                            A(lambda e: e.activation(out=sm[:, 1:2], in_=sm[:, 0:1], func=AF.Sqrt), [Bf("sm")], [Bf("sm")])
                            V(lambda e: e.reciprocal(out=sm[:, 2:3], in_=sm[:, 1:2]), [Bf("sm")], [Bf("sm")])
                            V(lambda e: e.scalar_tensor_tensor(out=sm[:, 3:4], in0=mv[:, 0:1], scalar=-1.0, in1=sm[:, 2:3], op0=ALU.mult, op1=ALU.mult),
                              [Bf("mv"), Bf("sm")], [Bf("sm")])
                            A(lambda e: e.activation(out=nrm[:, :], in_=src[:, :], func=AF.Identity, scale=sm[:, 2:3], bias=sm[:, 3:4]), [srcB, Bf("sm")], [Bf("nrm")])
                            P(lambda e: e.tensor_tensor(out=nrm[:, :], in0=nrm[:, :], in1=lnbc[:, gi, :], op=ALU.mult), [Bf("nrm"), Bf("lnbc")], [Bf("nrm")])
                            P(lambda e: e.tensor_tensor(out=dst[:, :], in0=nrm[:, :], in1=lnbc[:, gi + 1, :], op=ALU.add), [Bf("nrm"), Bf("lnbc")], [dstB])

                        layer_norm(z, Bf("z"), x1t, Bf("x1_%d" % sl), 0)
                        S.op("sync", lambda e, x1t=x1t, t0=t0: e.dma_start(out=x1_d[t0:t0 + 128, :], in_=x1t[:, :]), reads=[Bf("x1_%d" % sl)], writes=[B_x1d], dma_sem=dx1[sl])
                        A(lambda e, x1t=x1t, x1bt=x1bt: e.activation(out=x1bt[:, :], in_=x1t[:, :], func=AF.Copy), [Bf("x1_%d" % sl)], [Bf("x1b%d" % sl)])
                        yield "C1"
                        for kc in range(KC):
                            b = 4 + (kc // 4) % 2
                            TE(lambda e, kc=kc, b=b, x1t=x1t: e.transpose(out=ps[b][:, (kc % 4) * 128:(kc % 4 + 1) * 128], in_=x1t[:, kc * 128:(kc + 1) * 128], identity=ident_f[:, :]),
                               [Bf("x1_%d" % sl), Bf("ident_f")], [PB[b]])
                            if kc % 4 == 3 or kc == KC - 1:
                                k0 = (kc // 4) * 4
                                nk = kc - k0 + 1
                                V(lambda e, b=b, k0=k0, nk=nk: e.tensor_copy(out=x1T[:, k0:k0 + nk, :], in_=ps[b][:, 0:nk * 128].rearrange("p (k t) -> p k t", k=nk)),
                                  [PB[b]], [Bf("x1T")])
                        for kc in range(KC):
                            TE(lambda e, kc=kc: e.matmul(ps[6][:, 0:NE], lhsT=x1T[:, kc, :], rhs=w_r_f[:, kc, :], start=(kc == 0), stop=False),
                               [Bf("x1T"), Bf("w_r")], [PB[6]])
                        TE(lambda e: e.matmul(ps[6][:, 0:NE], lhsT=ones_f[0:1, :], rhs=b_r_f[0:1, :], start=False, stop=True), [Bf("ones_f"), Bf("b_r")], [PB[6]])
                        V(lambda e: e.tensor_copy(out=lg[:, :], in_=ps[6][:, 0:NE]), [PB[6]], [Bf("lg")])
                        V(lambda e: e.max(out=top8[:, :], in_=lg[:, :]), [Bf("lg")], [Bf("top8")])
                        V(lambda e: e.max_index(out=tidx[:, :], in_max=top8[:, :], in_values=lg[:, :]), [Bf("lg"), Bf("top8")], [Bf("tidx")])
                        V(lambda e: e.tensor_scalar(out=sm[:, 4:5], in0=top8[:, 0:1], scalar1=-1.0, scalar2=None, op0=ALU.mult), [Bf("top8")], [Bf("sm2")])
                        A(lambda e: e.activation(out=ex4[:, :], in_=top8[:, 0:4], func=AF.Exp, bias=sm[:, 4:5], scale=1.0), [Bf("top8"), Bf("sm2")], [Bf("ex4")])
                        V(lambda e: e.tensor_reduce(out=sm[:, 5:6], in_=ex4[:, :], axis=mybir.AxisListType.X, op=ALU.add), [Bf("ex4")], [Bf("sm3")])
                        V(lambda e: e.reciprocal(out=sm[:, 6:7], in_=sm[:, 5:6]), [Bf("sm3")], [Bf("sm3")])
                        V(lambda e, blk=blk: e.tensor_scalar(out=gate[:, blk, :], in0=ex4[:, :], scalar1=sm[:, 6:7], scalar2=None, op0=ALU.mult),
                          [Bf("ex4"), Bf("sm3")], [B_gate])
                        V(lambda e: e.tensor_scalar(out=mskb[:, :], in0=lg[:, :], scalar1=top8[:, 3:4], scalar2=None, op0=ALU.is_ge), [Bf("lg"), Bf("top8")], [Bf("mskb")])
                        yield "C2"
                        TE(lambda e: e.matmul(ps[7][:, 0:NE], lhsT=ltri_b[:, :], rhs=mskb[:, :], start=True, stop=True), [Bf("ltri"), Bf("mskb")], [PB[7]])
                        TE(lambda e: e.matmul(ps[7][:, 64:64 + NE], lhsT=ones_b[:, :], rhs=mskb[:, :], start=True, stop=True), [Bf("ones_b"), Bf("mskb")], [PB[7]])
                        V(lambda e: e.tensor_tensor(out=pos[:, :], in0=ps[7][:, 0:NE], in1=run_c[:, :], op=ALU.add), [PB[7], Bf("run")], [Bf("pos")])
                        V(lambda e: e.tensor_tensor(out=run_c[:, :], in0=ps[7][:, 64:64 + NE], in1=run_c[:, :], op=ALU.add), [PB[7], Bf("run")], [Bf("run")])
                        V(lambda e: e.tensor_scalar(out=ovf[:, :], in0=pos[:, :], scalar1=float(CAP), scalar2=BIG, op0=ALU.is_ge, op1=ALU.mult), [Bf("pos")], [Bf("ovf")])
                        V(lambda e: e.tensor_tensor(out=pos[:, :], in0=pos[:, :], in1=er[:, 1, :], op=ALU.add), [Bf("pos"), Bf("er")], [Bf("pos")])
                        V(lambda e: e.tensor_tensor(out=pos[:, :], in0=pos[:, :], in1=ovf[:, :], op=ALU.add), [Bf("pos"), Bf("ovf")], [Bf("pos")])
                        V(lambda e: e.tensor_copy(out=ef[:, :], in_=tidx[:, 0:4]), [Bf("tidx")], [Bf("ef")])
                        for k in range(4):
                            V(lambda e, k=k: e.scalar_tensor_tensor(out=junk[:, :], in0=er[:, 0, :], scalar=ef[:, k:k + 1], in1=pos[:, :], op0=ALU.is_equal, op1=ALU.mult,
                                                                   accum_out=dest_f[:, k:k + 1]), [Bf("er"), Bf("ef"), Bf("pos")], [Bf("junk"), Bf("dest_f")])
                        V(lambda e, blk=blk: e.tensor_copy(out=dest_i[:, blk, :], in_=dest_f[:, :]), [Bf("dest_f")], [B_dest])
                        for k in range(4):
                            S.op("gpsimd", lambda e, k=k, blk=blk, x1bt=x1bt: e.indirect_dma_start(
                                out=xs_d[:, :], out_offset=bass.IndirectOffsetOnAxis(ap=dest_i[:, blk, k:k + 1], axis=0), in_=x1bt[:, :], in_offset=None,
                                bounds_check=breg(e, 1), oob_is_err=False), reads=[Bf("x1b%d" % sl), B_dest], writes=[], dma_sem=dsc[sl])

            gens = []
            tau0 = 0
            for s, n in enumerate(sizes):
                gens.append(body(s, n, tau0))
                tau0 += n

            def adv(i):
                if 0 <= i < len(gens):
                    try:
                        next(gens[i])
                    except StopIteration:
                        pass

            for s in range(len(sizes) + 3):
                adv(s)
                adv(s - 2)
                adv(s - 1)
                adv(s)
                adv(s - 1)
                if cast_jobs and s < len(sizes):
                    ncast = len(cast_jobs)
                    nsp = min(4, NST)
                    per = (ncast + nsp - 1) // nsp
                    if 1 <= s <= nsp:
                        for jn in range((s - 1) * per, min(s * per, ncast)):
                            issue_cast(cast_jobs[jn], jn)
                    if s == min(6, len(sizes) - 1):
                        issue_ag()
            for g_ in gens:
                for _ in g_:
                    pass
            S.emit()

        S = Sched(nc, "e")
        PB = [Buf("ps%d" % i) for i in range(8)]
        with ExitStack() as st:
            def sb(name, shape, dt):
                return st.enter_context(nc.sbuf_tensor(name, list(shape), dt))

            wu = [sb("wu%d" % i, [128, KC, 2 * D], BF16) for i in range(2)]
            wd = [sb("wd%d" % i, [128, FC, D], BF16) for i in range(2)]
            xs_t = [sb("xs_t%d" % i, [128, NB, D], BF16) for i in range(2)]
            xsT = [sb("xsT%d" % i, [128, KC, CAP], BF16) for i in range(2)]
            actT = [sb("actT%d" % i, [128, FC, CAP], BF16) for i in range(2)]
            xg = [sb("xg%d" % i, [128, CAP], F32) for i in range(2)]
            sg = [sb("sg%d" % i, [128, CAP], F32) for i in range(2)]
            xl = [sb("xl%d" % i, [128, CAP], F32) for i in range(2)]
            ys_t = [sb("ys_t%d" % i, [128, D], F32) for i in range(2)]
            Bn = {}

            def Bf(n):
                if n not in Bn:
                    Bn[n] = Buf(n)
                return Bn[n]

            V = lambda fn, r=(), w=(): S.op("vector", fn, reads=r, writes=w)
            A = lambda fn, r=(), w=(): S.op("scalar", fn, reads=r, writes=w)
            P = lambda fn, r=(), w=(): S.op("gpsimd", fn, reads=r, writes=w)
            TE = lambda fn, r=(), w=(): S.op("tensor", fn, reads=r, writes=w)
            dwu = [S.dsem("wu0"), S.dsem("wu1")]
            dwd = [S.dsem("wd0"), S.dsem("wd1")]
            dxs = [S.dsem("xs0"), S.dsem("xs1")]
            dys = [S.dsem("ys0"), S.dsem("ys1")]

            def load_w(e_):
                sl = e_ % 2
                if not cfg["repl"]:
                    S.op("sync", lambda e, sl=sl, e_=e_: e.dma_start(out=wu[sl][:, :, :], in_=wa_up[e_ * D:(e_ + 1) * D, :].rearrange("(kc p) f -> p kc f", p=128)),
                         writes=[Bf("wu%d" % sl)], dma_sem=dwu[sl])
                    S.op("sync", lambda e, sl=sl, e_=e_: e.dma_start(out=wd[sl][:, :, :], in_=wa_dn[e_ * D:(e_ + 1) * D, :].rearrange("(kc p) f -> p kc f", p=128)),
                         writes=[Bf("wd%d" % sl)], dma_sem=dwd[sl])
                    return
                nsp = 2 if KC >= 2 else 1
                kh = KC // nsp
                for h in range(nsp):
                    S.op("gpsimd", lambda e, h=h, sl=sl, e_=e_: e.dma_start(out=wu[sl][:, h * kh:(h + 1) * kh, :],
                                                                          in_=w_up[e_, h * kh * 128:(h + 1) * kh * 128, :].rearrange("(kc p) f -> p kc f", p=128)),
                         writes=[Bf("wu%d" % sl)], dma_sem=dwu[sl])
                S.op("gpsimd", lambda e, sl=sl, e_=e_: e.dma_start(out=wd[sl][:, :, :], in_=w_dn[e_, :, :].rearrange("(fc p) d -> p fc d", p=128)),
                     writes=[Bf("wd%d" % sl)], dma_sem=dwd[sl])

            def load_xs(e_):
                sl = e_ % 2
                S.op("sync", lambda e, sl=sl, e_=e_: e.dma_start(out=xs_t[sl][:, :, :], in_=xs_d[e_ * CAP:(e_ + 1) * CAP, :].rearrange("(b p) d -> p b d", p=128)),
                     reads=[B_xs], writes=[Bf("xs_t%d" % sl)], dma_sem=dxs[sl])

            load_w(0)
            load_xs(0)
            cnt = [0]
            for e_ in range(NE):
                sl = e_ % 2
                if e_ + 1 < NE:
                    load_w(e_ + 1)
                    load_xs(e_ + 1)
                Bwu, Bwd = Bf("wu%d" % sl), Bf("wd%d" % sl)
                for nb in range(NB):
                    b = 6 + nb % 2
                    pbf = ps[b][:, :].bitcast(BF16)
                    for kc in range(KC):
                        TE(lambda e, kc=kc, nb=nb, pbf=pbf, sl=sl: e.transpose(out=pbf[:, kc * 128:(kc + 1) * 128], in_=xs_t[sl][:, nb, kc * 128:(kc + 1) * 128], identity=ident_b[:, :]),
                           [Bf("xs_t%d" % sl)], [PB[b]])
                    cp = A if nb % 2 == 0 else V
                    if nb % 2 == 0:
                        A(lambda e, pbf=pbf, nb=nb, sl=sl: e.activation(out=xsT[sl][:, :, nb * 128:(nb + 1) * 128], in_=pbf[:, 0:KC * 128].rearrange("p (k t) -> p k t", k=KC), func=AF.Copy),
                          [PB[b]], [Bf("xsT%d" % sl)])
                    else:
                        V(lambda e, pbf=pbf, nb=nb, sl=sl: e.tensor_copy(out=xsT[sl][:, :, nb * 128:(nb + 1) * 128], in_=pbf[:, 0:KC * 128].rearrange("p (k t) -> p k t", k=KC)),
                          [PB[b]], [Bf("xsT%d" % sl)])
                for fp in range(FC):
                    cnt[0] += 1
                    pr = cnt[0] % 2
                    bG, bL = (0, 1) if pr == 0 else (2, 3)
                    for kc in range(KC):
                        TE(lambda e, kc=kc, fp=fp, bG=bG, sl=sl: e.matmul(ps[bG][:, 0:CAP], lhsT=wu[sl][:, kc, fp * 128:(fp + 1) * 128], rhs=xsT[sl][:, kc, :],
                                                                         start=(kc == 0), stop=(kc == KC - 1)), [Bwu, Bf("xsT%d" % sl)], [PB[bG]])
                    for kc in range(KC):
                        TE(lambda e, kc=kc, fp=fp, bL=bL, sl=sl: e.matmul(ps[bL][:, 0:CAP], lhsT=wu[sl][:, kc, D + fp * 128:D + (fp + 1) * 128], rhs=xsT[sl][:, kc, :],
                                                                         start=(kc == 0), stop=(kc == KC - 1)), [Bwu, Bf("xsT%d" % sl)], [PB[bL]])
                    V(lambda e, bG=bG, e_=e_, fp=fp, pr=pr: e.tensor_scalar(out=xg[pr][:, :], in0=ps[bG][:, 0:CAP], scalar1=bup[:, e_, fp:fp + 1], scalar2=7.0, op0=ALU.add, op1=ALU.min),
                      [PB[bG]], [Bf("xg%d" % pr)])
                    A(lambda e, pr=pr: e.activation(out=sg[pr][:, :], in_=xg[pr][:, :], func=AF.Sigmoid, scale=1.702), [Bf("xg%d" % pr)], [Bf("sg%d" % pr)])
                    V(lambda e, bL=bL, e_=e_, fp=fp, pr=pr: e.tensor_scalar(out=xl[pr][:, :], in0=ps[bL][:, 0:CAP], scalar1=bup[:, e_, FC + fp:FC + fp + 1], scalar2=7.0, op0=ALU.add, op1=ALU.min),
                      [PB[bL]], [Bf("xl%d" % pr)])
                    P(lambda e, pr=pr: e.tensor_scalar(out=xl[pr][:, :], in0=xl[pr][:, :], scalar1=7.0, scalar2=-7.0, op0=ALU.min, op1=ALU.max), [Bf("xl%d" % pr)], [Bf("xl%d" % pr)])
                    P(lambda e, pr=pr: e.tensor_tensor(out=xg[pr][:, :], in0=xg[pr][:, :], in1=sg[pr][:, :], op=ALU.mult), [Bf("xg%d" % pr), Bf("sg%d" % pr)], [Bf("xg%d" % pr)])
                    V(lambda e, pr=pr, fp=fp, sl=sl: e.scalar_tensor_tensor(out=actT[sl][:, fp, :], in0=xl[pr][:, :], scalar=1.0, in1=xg[pr][:, :], op0=ALU.add, op1=ALU.mult),
                      [Bf("xg%d" % pr), Bf("xl%d" % pr)], [Bf("actT%d" % sl)])
                for nb in range(NB):
                    ysl = (e_ * NB + nb) % 2
                    for h in range(NH2):
                        b = 4 + h % 2
                        for fc in range(FC):
                            TE(lambda e, fc=fc, nb=nb, h=h, b=b, sl=sl: e.matmul(ps[b][:, 0:W2], lhsT=actT[sl][:, fc, nb * 128:(nb + 1) * 128], rhs=wd[sl][:, fc, h * W2:(h + 1) * W2],
                                                                                start=(fc == 0), stop=False), [Bf("actT%d" % sl), Bwd], [PB[b]])
                        TE(lambda e, h=h, b=b, e_=e_: e.matmul(ps[b][:, 0:W2], lhsT=selT[:, e_, :], rhs=bdn_b[:, h * W2:(h + 1) * W2], start=False, stop=True),
                           [], [PB[b]])
                        if h % 2 == 0:
                            A(lambda e, b=b, h=h, ysl=ysl: e.activation(out=ys_t[ysl][:, h * W2:(h + 1) * W2], in_=ps[b][:, 0:W2], func=AF.Copy), [PB[b]], [Bf("ys_t%d" % ysl)])
                        else:
                            V(lambda e, b=b, h=h, ysl=ysl: e.tensor_copy(out=ys_t[ysl][:, h * W2:(h + 1) * W2], in_=ps[b][:, 0:W2]), [PB[b]], [Bf("ys_t%d" % ysl)])
                    r0 = e_ * CAP + nb * 128
                    S.op("sync", lambda e, ysl=ysl, r0=r0: e.dma_start(out=ys_d[r0:r0 + 128, :], in_=ys_t[ysl][:, :]), reads=[Bf("ys_t%d" % ysl)], writes=[], dma_sem=dys[ysl])
            S.emit()

        S = Sched(nc, "c")
        with ExitStack() as st:
            def sb(name, shape, dt):
                return st.enter_context(nc.sbuf_tensor(name, list(shape), dt))

            x1c = [sb("x1c%d" % i, [128, D], F32) for i in range(2)]
            yk = [[sb("yk%d_%d" % (i, k), [128, D], F32) for k in range(4)] for i in range(2)]
            acc = sb("acc", [128, D], F32)
            nrm = sb("nrm2", [128, D], F32)
            res = [sb("res%d" % i, [128, D], F32) for i in range(2)]
            stt = sb("stt2", [128, max(NH2, 1) * 6], F32)
            mv = sb("mv2", [128, 2], F32)
            sm = sb("sm2", [128, 8], F32)
            Bn = {}

            def Bf(n):
                if n not in Bn:
                    Bn[n] = Buf(n)
                return Bn[n]

            V = lambda fn, r=(), w=(): S.op("vector", fn, reads=r, writes=w)
            A = lambda fn, r=(), w=(): S.op("scalar", fn, reads=r, writes=w)
            P = lambda fn, r=(), w=(): S.op("gpsimd", fn, reads=r, writes=w)
            S.op("sync", lambda e: e.dma_start(out=lnbc[:, :, :], in_=lnb[:, 2:4, :]), writes=[Bf("lnbc")], dma_sem=S.dsem("ln2", group=True))
            dxc = [S.dsem("xc0"), S.dsem("xc1")]
            dyk = [S.dsem("yk0"), S.dsem("yk1")]
            dout = [S.dsem("o0"), S.dsem("o1")]
            for blk in range(NBLK):
                sl = blk % 2
                t0 = blk * 128
                S.op("sync", lambda e, sl=sl, t0=t0: e.dma_start(out=x1c[sl][:, :], in_=x1_d[t0:t0 + 128, :]), writes=[Bf("x1c%d" % sl)], dma_sem=dxc[sl])
                for k in range(4):
                    S.op("gpsimd", lambda e, k=k, sl=sl, blk=blk: e.indirect_dma_start(
                        out=yk[sl][k][:, :], out_offset=None, in_=ys_d[:, :], in_offset=bass.IndirectOffsetOnAxis(ap=dest_i[:, blk, k:k + 1], axis=0),
                        bounds_check=breg(e, 3), oob_is_err=False), writes=[Bf("yk%d_%d" % (sl, k))], dma_sem=dyk[sl])
                V(lambda e, sl=sl: e.tensor_scalar(out=acc[:, :], in0=x1c[sl][:, :], scalar1=alpha, scalar2=None, op0=ALU.mult), [Bf("x1c%d" % sl)], [Bf("acc")])
                for k in range(4):
                    V(lambda e, k=k, sl=sl, blk=blk: e.scalar_tensor_tensor(out=acc[:, :], in0=yk[sl][k][:, :], scalar=gate[:, blk, k:k + 1], in1=acc[:, :], op0=ALU.mult, op1=ALU.add),
                      [Bf("yk%d_%d" % (sl, k)), Bf("acc")], [Bf("acc")])
                for h in range(NH2):
                    V(lambda e, h=h: e.bn_stats(out=stt[:, h * 6:(h + 1) * 6], in_=acc[:, h * W2:(h + 1) * W2]), [Bf("acc")], [Bf("stt")])
                V(lambda e: e.bn_aggr(out=mv[:, :], in_=stt[:, 0:NH2 * 6]), [Bf("stt")], [Bf("mv")])
                V(lambda e: e.tensor_scalar(out=sm[:, 0:1], in0=mv[:, 1:2], scalar1=EPS, scalar2=None, op0=ALU.add), [Bf("mv")], [Bf("sm")])
                A(lambda e: e.activation(out=sm[:, 1:2], in_=sm[:, 0:1], func=AF.Sqrt), [Bf("sm")], [Bf("sm")])
                V(lambda e: e.reciprocal(out=sm[:, 2:3], in_=sm[:, 1:2]), [Bf("sm")], [Bf("sm")])
                V(lambda e: e.scalar_tensor_tensor(out=sm[:, 3:4], in0=mv[:, 0:1], scalar=-1.0, in1=sm[:, 2:3], op0=ALU.mult, op1=ALU.mult), [Bf("mv"), Bf("sm")], [Bf("sm")])
                A(lambda e: e.activation(out=nrm[:, :], in_=acc[:, :], func=AF.Identity, scale=sm[:, 2:3], bias=sm[:, 3:4]), [Bf("acc"), Bf("sm")], [Bf("nrm")])
                P(lambda e: e.tensor_tensor(out=nrm[:, :], in0=nrm[:, :], in1=lnbc[:, 0, :], op=ALU.mult), [Bf("nrm"), Bf("lnbc")], [Bf("nrm")])
                P(lambda e, sl=sl: e.tensor_tensor(out=res[sl][:, :], in0=nrm[:, :], in1=lnbc[:, 1, :], op=ALU.add), [Bf("nrm"), Bf("lnbc")], [Bf("res%d" % sl)])
                S.op("sync", lambda e, sl=sl, t0=t0: e.dma_start(out=out[t0:t0 + 128, :], in_=res[sl][:, :]), reads=[Bf("res%d" % sl)], writes=[], dma_sem=dout[sl])
            S.emit()
    return nc


def prep_inputs(cfg, x, w_in, b_in, attn_sinks, w_attn_br, conv_w, conv_b, conv_ln_g, conv_ln_b,
                w_conv_br, b_conv_br, w_o, ln1_g, ln1_b, w_router, b_router, w_up, b_up,
                w_down, b_down, ln2_g, ln2_b):
    D, NH, CC, NE, T, CAP = (cfg[k] for k in ["D", "NH", "CC", "NE", "T", "CAP"])
    G, AW, QE, KE, VE, CE, INW, KC, CCH, FC = (cfg[k] for k in ["G", "AW", "QE", "KE", "VE", "CE", "INW", "KC", "CCH", "FC"])
    NC_ = cfg["NCORES"]
    CPB = NC_ // cfg["B"]
    f = lambda a: np.ascontiguousarray(np.asarray(a, dtype=np.float32))
    x = f(x)
    w_in0, b_in0 = f(w_in)[0], f(b_in)[0]
    qperm = np.array([(g * G + c) * 64 + j for c in range(G) for g in range(2) for j in range(64)])
    perm = np.concatenate([qperm, np.arange(QE, INW)])
    w_in_p = np.ascontiguousarray(w_in0[:, perm])
    b_in_p = b_in0[perm]
    b_in_t = np.ascontiguousarray(b_in_p.reshape(INW // 128, 128).T)
    b_v = np.ascontiguousarray(b_in_p[KE:VE].reshape(1, 128))
    sinks_b = np.ascontiguousarray(np.broadcast_to(f(attn_sinks)[0][None, :], (128, NH)))
    cw_t = np.ascontiguousarray(f(conv_w)[0].T.reshape(CCH, 128, 31).transpose(1, 0, 2))
    pp = lambda v: v.reshape(-1, 128).T
    cvec = np.ascontiguousarray(np.stack([pp(f(conv_b)[0]), pp(f(conv_ln_g)[0]), pp(f(conv_ln_b)[0])], axis=1))
    bcb_t = np.ascontiguousarray(pp(f(b_conv_br)[0]))
    lnb = np.ascontiguousarray(np.broadcast_to(np.stack([f(ln1_g)[0], f(ln1_b)[0], f(ln2_g)[0], f(ln2_b)[0]])[None], (128, 4, D)))
    w_up0 = f(w_up)[0]
    w_up_p = np.concatenate([w_up0[:, :, 0::2], w_up0[:, :, 1::2]], axis=2)
    b_up0 = f(b_up)[0]
    b_up_p = np.concatenate([b_up0[:, 0::2], b_up0[:, 1::2]], axis=1)
    b_up_t = np.ascontiguousarray(b_up_p.reshape(NE, 2 * FC, 128).transpose(2, 0, 1))
    w_dn0 = f(w_down)[0]
    b_dn = np.ascontiguousarray(f(b_down)[0])
    ident = np.eye(128, dtype=np.float32)
    ltri = np.triu(np.ones((128, 128), np.float32), 1)
    consts = np.ascontiguousarray(np.stack([ident, ltri, np.ones((128, 128), np.float32)], axis=1))
    erow = np.ascontiguousarray(np.broadcast_to(np.stack([np.arange(NE, dtype=np.float32), np.arange(NE, dtype=np.float32) * CAP])[None], (128, 2, NE)))
    kk = np.arange(128)[:, None]
    qq = np.arange(128)[None, :]
    m_cur = (kk <= qq).astype(np.float32)
    m_prev = (kk > qq).astype(np.float32)
    shared = dict(w_in=w_in_p, b_in_t=b_in_t, b_v=b_v, sinks_b=sinks_b, w_ab=f(w_attn_br)[0], cw_t=cw_t, cvec=cvec,
                  w_cb=f(w_conv_br)[0], bcb_t=bcb_t, w_o=f(w_o)[0], lnb=lnb, w_r=f(w_router)[0], b_r=f(b_router)[0].reshape(1, NE),
                  b_up_t=b_up_t, b_dn=b_dn, consts=consts, erow=erow)
    maps = []
    for c in range(NC_):
        b, h = c // CPB, c % CPB
        st = h * T
        xT = np.zeros((D, 128 + T), np.float32)
        xT[:, 128:] = x[b, st:st + T].T
        if h > 0:
            xT[:, :128] = x[b, st - 128:st].T
        fl = 1.0 if h > 0 else 0.0
        masks = np.stack([np.tile(m_cur, (1, G)), np.tile(m_prev, (1, G)), np.tile(m_prev * fl, (1, G))], axis=1)
        m = dict(shared)
        m.update(xT=xT, xtok=np.ascontiguousarray(x[b, st:st + T]), masks=np.ascontiguousarray(masks),
                 flag=np.full((128, 1), fl, np.float32))
        if cfg["repl"]:
            m.update(w_up=w_up_p, w_dn=w_dn0)
        else:
            E = cfg["EPC"]
            m.update(w_up=np.ascontiguousarray(w_up_p[c * E:(c + 1) * E]), w_dn=np.ascontiguousarray(w_dn0[c * E:(c + 1) * E]))
        maps.append(m)
    return maps


def run_cfg(cfg, inputs, trace=False):
    maps = prep_inputs(cfg, **inputs)
    nc = build(cfg)
    res = run_bass_kernel_spmd(nc, maps, core_ids=list(range(cfg["NCORES"])), trace=trace)
    T, D, B = cfg["T"], cfg["D"], cfg["B"]
    CPB = cfg["NCORES"] // B
    outv = np.zeros((B, CPB * T, D), np.float32)
    for c in range(cfg["NCORES"]):
        b, h = c // CPB, c % CPB
        outv[b, h * T:(h + 1) * T] = res.results[c]["out"]
    return outv, res


def kernel(**inputs):
    cfg = make_cfg()
    outv, _ = run_cfg(cfg, inputs)
    return outv
```

```python
from contextlib import ExitStack
import numpy as np
import concourse.bass as bass
import concourse.mybir as mybir
from concourse.bass_utils import run_bass_kernel_spmd

F32 = mybir.dt.float32
BF16 = mybir.dt.bfloat16
I32 = mybir.dt.int32
U32 = mybir.dt.uint32
AF = mybir.ActivationFunctionType
ALU = mybir.AluOpType
ENGS = ["tensor", "vector", "scalar", "gpsimd", "sync"]


class Buf:
    __slots__ = ("name", "writers", "readers")

    def __init__(self, name):
        self.name = name
        self.writers = []
        self.readers = []


class Op:
    __slots__ = ("eng", "fn", "deps", "marked", "is_dma", "sem", "val")

    def __init__(self, eng, fn, is_dma=False, sem=None):
        self.eng = eng
        self.fn = fn
        self.deps = []
        self.marked = False
        self.is_dma = is_dma
        self.sem = sem
        self.val = None


class DSem:
    def __init__(self, name, group=False, inc=16):
        self.name = name
        self.count = 0
        self.handle = None
        self.group = group
        self.inc = inc


def _prune(lst, op):
    if op.is_dma:
        out = [o for o in lst if not (o.is_dma and o.sem is op.sem)]
    else:
        out = [o for o in lst if o.is_dma or o.eng != op.eng]
    out.append(op)
    return out


class Sched:
    def __init__(self, nc, tag):
        self.nc = nc
        self.tag = tag
        self.ops = {e: [] for e in ENGS}
        self.dsems = []
        self.dma_ops = []

    def dsem(self, name, group=False, inc=16):
        s = DSem(name, group, inc)
        self.dsems.append(s)
        return s

    def op(self, eng, fn, reads=(), writes=(), dma_sem=None):
        o = Op(eng, fn, is_dma=dma_sem is not None, sem=dma_sem)
        deps = []
        for b in reads:
            deps.extend(b.writers)
        for b in writes:
            deps.extend(b.writers)
            deps.extend(b.readers)
        seen = set()
        for d in deps:
            if id(d) in seen:
                continue
            seen.add(id(d))
            if (not d.is_dma) and (not o.is_dma) and d.eng == "tensor" and eng == "tensor":
                continue
            if d.is_dma and o.is_dma and d.sem is o.sem:
                continue
            o.deps.append(d)
            d.marked = True
        if o.is_dma:
            dma_sem.count += dma_sem.inc
            o.val = dma_sem.count
            o.marked = True
            self.dma_ops.append(o)
        for b in reads:
            b.readers = _prune(b.readers, o)
        for b in writes:
            b.writers = [o]
            b.readers = []
        self.ops[eng].append(o)
        return o

    def emit(self):
        nc = self.nc
        with ExitStack() as st:
            esem = {e: st.enter_context(nc.semaphore(self.tag + "_s_" + e)) for e in ENGS}
            for s in self.dsems:
                s.handle = st.enter_context(nc.semaphore(self.tag + "_d_" + s.name))
            for e in ENGS:
                c = 0
                for o in self.ops[e]:
                    if not o.is_dma and o.marked:
                        c += 1
                        o.val = c
            finals = {}
            for o in self.dma_ops:
                finals[id(o.sem)] = o
            block = st.enter_context(nc.Block())

            def run(e, eng):
                waited = {}
                for o in self.ops[e]:
                    for d in o.deps:
                        if d.is_dma:
                            key, h = id(d.sem), d.sem.handle
                            dv = d.sem.count if d.sem.group else d.val
                        else:
                            key, h = d.eng, esem[d.eng]
                            dv = d.val
                        if waited.get(key, 0) >= dv:
                            continue
                        waited[key] = dv
                        eng.wait_ge(h, dv)
                    inst = o.fn(eng)
                    if o.is_dma:
                        if o.sem.inc == 16:
                            inst.then_inc(o.sem.handle, 16)
                        else:
                            inst.then_inc(o.sem.handle)
                    elif o.marked:
                        inst.then_inc(esem[e], 1)
                if e == "sync":
                    for d in finals.values():
                        eng.wait_ge(d.sem.handle, d.val)

            @block.tensor
            def _(eng):
                run("tensor", eng)

            @block.vector
            def _(eng):
                run("vector", eng)

            @block.scalar
            def _(eng):
                run("scalar", eng)

            @block.gpsimd
            def _(eng):
                run("gpsimd", eng)

            @block.sync
            def _(eng):
                run("sync", eng)


def make_cfg(D=1024, NH=8, CC=512, NE=32, T=2048, CAP=384, ST=256, NCORES=8, B=4, repl=True,
             alpha=2 ** 0.25):
    c = dict(D=D, NH=NH, CC=CC, NE=NE, T=T, CAP=CAP, ST=ST, NCORES=NCORES, B=B, repl=repl, alpha=alpha)
    c["G"] = NH // 2
    c["AW"] = NH * 64
    c["QE"] = c["AW"]
    c["KE"] = c["QE"] + 128
    c["VE"] = c["KE"] + 128
    c["CE"] = c["VE"] + 2 * CC
    c["INW"] = c["CE"] + 2 * D
    c["KC"] = D // 128
    c["AC"] = c["AW"] // 128
    c["CCH"] = CC // 128
    c["FC"] = D // 128
    c["W2"] = min(D, 512)
    c["NH2"] = D // c["W2"]
    c["NBLK"] = T // 128
    c["NB"] = CAP // 128
    c["EPC"] = NE // NCORES
    c["SEQ"] = T * (NCORES // B)
    return c


def build(cfg):
    D, NH, CC, NE, T, CAP, ST = (cfg[k] for k in ["D", "NH", "CC", "NE", "T", "CAP", "ST"])
    G, AW, QE, KE, VE, CE, INW = (cfg[k] for k in ["G", "AW", "QE", "KE", "VE", "CE", "INW"])
    KC, AC, CCH, FC, W2, NH2, NBLK, NB = (cfg[k] for k in ["KC", "AC", "CCH", "FC", "W2", "NH2", "NBLK", "NB"])
    alpha = float(cfg["alpha"])
    NU = INW // 128
    GW = G * 128
    PADL = 32
    NEW = NE if cfg["repl"] else cfg["EPC"]
    BIG = 4.0e6
    EPS = 1e-5

    nc = bass.Bass("TRN2", target_bir_lowering=False)

    def din(name, shape, dt=F32):
        return nc.dram_tensor(name, list(shape), dt, kind="ExternalInput").ap()

    xT = din("xT", [D, 128 + T])
    xtok = din("xtok", [T, D])
    masks = din("masks", [128, 3, GW])
    flag = din("flag", [128, 1])
    w_in = din("w_in", [D, INW])
    b_in_t = din("b_in_t", [128, NU])
    b_v = din("b_v", [1, 128])
    sinks_b = din("sinks_b", [128, NH])
    w_ab = din("w_ab", [AW, D])
    cw_t = din("cw_t", [128, CCH, 31])
    cvec = din("cvec", [128, 3, CCH])
    w_cb = din("w_cb", [CC, D])
    bcb_t = din("bcb_t", [128, KC])
    w_o = din("w_o", [D, D])
    lnb = din("lnb", [128, 4, D])
    w_r = din("w_r", [D, NE])
    b_r = din("b_r", [1, NE])
    w_up = din("w_up", [NEW, D, 2 * D])
    b_up_t = din("b_up_t", [128, NE, 2 * FC])
    w_dn = din("w_dn", [NEW, D, D])
    b_dn = din("b_dn", [NE, D])
    consts = din("consts", [128, 3, 128])
    erow = din("erow", [128, 2, NE])
    out = nc.dram_tensor("out", [T, D], F32, kind="ExternalOutput").ap()
    xs_d = nc.dram_tensor("xs_d", [NE * CAP, D], BF16, kind="Internal").ap()
    ys_d = nc.dram_tensor("ys_d", [NE * CAP, D], F32, kind="Internal").ap()
    x1_d = nc.dram_tensor("x1_d", [T, D], F32, kind="Internal").ap()
    if not cfg["repl"]:
        EPC = cfg["EPC"]
        wl_up_t = nc.dram_tensor("wl_up", [EPC * D, 2 * D], BF16)
        wa_up_t = nc.dram_tensor("wa_up", [NE * D, 2 * D], BF16)
        wl_dn_t = nc.dram_tensor("wl_dn", [EPC * D, D], BF16)
        wa_dn_t = nc.dram_tensor("wa_dn", [NE * D, D], BF16)
        wl_up, wa_up, wl_dn, wa_dn = wl_up_t.ap(), wa_up_t.ap(), wl_dn_t.ap(), wa_dn_t.ap()

    regs = {}

    def breg(e, phase):
        if phase not in regs:
            regs[phase] = e.to_reg(NE * CAP - 1)
        return regs[phase]

    with ExitStack() as pst:
        def sbp(name, shape, dt):
            return pst.enter_context(nc.sbuf_tensor(name, list(shape), dt))

        dest_i = sbp("dest_i", [128, NBLK, 4], I32)
        gate = sbp("gate", [128, NBLK, 4], F32)
        ident_f = sbp("ident_f", [128, 128], F32)
        ident_b = sbp("ident_b", [128, 128], BF16)
        ones_b = sbp("ones_b", [128, 128], BF16)
        ones_f = sbp("ones_f", [128, 128], F32)
        lnbc = sbp("lnbc", [128, 2, D], F32)
        bdn_b = sbp("bdn_b", [NE, D], BF16)
        selT = sbp("selT", [NE, NE, 128], BF16)
        bup = sbp("bup", [128, NE, 2 * FC], F32)
        ps = [pst.enter_context(nc.psum_tensor("ps%d" % i, [128, 512], F32)) for i in range(8)]
        B_dest, B_gate, B_xs, B_ys, B_x1d = Buf("dest"), Buf("gate"), Buf("xs"), Buf("ys"), Buf("x1d")
        B_const = Buf("const")

        S = Sched(nc, "m")
        PB = [Buf("ps%d" % i) for i in range(8)]
        with ExitStack() as st:
            def sb(name, shape, dt):
                return st.enter_context(nc.sbuf_tensor(name, list(shape), dt))

            w_in_bf = sb("w_in_bf", [128, KC, INW], BF16)
            w_ab_bf = sb("w_ab_bf", [128, AC, D], BF16)
            w_cb_bf = sb("w_cb_bf", [128, CCH, D], BF16)
            w_o_bf = sb("w_o_bf", [128, KC, D], BF16)
            diag2 = [sb("diag%d" % i, [128, 31, 128], BF16) for i in range(2)]
            w_r_f = sb("w_r_f", [128, KC, NE], F32)
            b_r_f = sb("b_r_f", [1, NE], F32)
            bin_t = sb("bin_t", [128, NU], F32)
            bv_b = sb("bv_b", [1, 128], BF16)
            esink = sb("esink", [128, NH], F32)
            cw = sb("cw", [128, CCH, 31], F32)
            cv = sb("cv", [128, 3, CCH], F32)
            bcb = sb("bcb", [128, KC], F32)
            mk = sb("mk", [128, 3, GW], BF16)
            flg = sb("flg", [128, 1], F32)
            cst = sb("cst", [128, 3, 128], F32)
            ltri_b = sb("ltri_b", [128, 128], BF16)
            onesm_f = sb("onesm_f", [128, 128], F32)
            er = sb("er", [128, 2, NE], F32)
            run_c = sb("run_c", [128, NE], F32)
            xT_bf = [sb("xT_bf%d" % i, [128, KC, ST], BF16) for i in range(2)]
            qT = sb("qT", [128, AC, ST], BF16)
            kT = sb("kT", [128, 128 + ST], BF16)
            Vr = sb("Vr", [128, 1 + ST // 128, 2, 65], BF16)
            aT = sb("aT", [128, CCH, PADL + ST], BF16)
            sgt = sb("sgt", [128, ST], F32)
            Pp = [sb("Pp%d" % i, [128, GW], BF16) for i in range(2)]
            Pc = [sb("Pc%d" % i, [128, GW], BF16) for i in range(2)]
            den = sb("den", [128, G], F32)
            o_n = sb("o_n", [128, AW], BF16)
            oT = sb("oT", [128, AC, ST], BF16)
            yb = sb("yb", [128, CCH, ST], F32)
            sq = sb("sq", [128, CCH, ST], F32)
            mean_sb = sb("mean_sb", [128, ST], F32)
            var_sb = sb("var_sb", [128, ST], F32)
            tmpc = sb("tmpc", [128, ST], F32)
            sT = sb("sT", [128, CCH, ST], BF16)
            ga = [sb("ga%d" % i, [128, ST], F32) for i in range(2)]
            gb = [sb("gb%d" % i, [128, ST], F32) for i in range(2)]
            t1 = sb("t1", [128, ST], F32)
            t2 = sb("t2", [128, ST], F32)
            mg = sb("mg", [128, KC, ST], BF16)
            xt = [sb("xt%d" % i, [128, D], F32) for i in range(1)]
            z = sb("z", [128, D], F32)
            nrm = sb("nrm", [128, D], F32)
            x1 = [sb("x1_%d" % i, [128, D], F32) for i in range(1)]
            x1b = [sb("x1b%d" % i, [128, D], BF16) for i in range(1)]
            x1T = sb("x1T", [128, KC, 128], F32)
            stt = sb("stt", [128, max(NH2, 1) * 6], F32)
            mv = sb("mv", [128, 2], F32)
            sm = sb("sm", [128, 8], F32)
            lg = sb("lg", [128, NE], F32)
            top8 = sb("top8", [128, 8], F32)
            tidx = sb("tidx", [128, 8], U32)
            ef = sb("ef", [128, 4], F32)
            ex4 = sb("ex4", [128, 4], F32)
            mskb = sb("mskb", [128, NE], BF16)
            pos = sb("pos", [128, NE], F32)
            ovf = sb("ovf", [128, NE], F32)
            junk = sb("junk", [128, NE], F32)
            dest_f = sb("dest_f", [128, 4], F32)

            Bn = {}

            def Bf(n):
                if n not in Bn:
                    Bn[n] = Buf(n)
                return Bn[n]

            dc = S.dsem("c", group=True)
            dw = S.dsem("w", group=True)

            def ld(eng, o_ap, i_ap, bufname, sem=dc, **kw):
                S.op(eng, lambda e: e.dma_start(out=o_ap, in_=i_ap, **kw), writes=[Bf(bufname)], dma_sem=sem)

            ld("sync", cst[:, :, :], consts, "cst")
            ld("sync", er[:, :, :], erow, "er")
            ld("sync", lnbc[:, :, :], lnb[:, 0:2, :], "lnbc")
            ld("sync", bup[:, :, :], b_up_t, "bup")
            ld("sync", w_r_f[:, :, :], w_r.rearrange("(kc p) n -> p kc n", p=128), "w_r")
            ld("sync", b_r_f[:, :], b_r, "b_r")
            ld("sync", bin_t[:, :], b_in_t, "bin")
            ld("sync", esink[:, :], sinks_b, "esink")
            ld("sync", cw[:, :, :], cw_t, "cw")
            ld("sync", cv[:, :, :], cvec, "cv")
            ld("sync", bcb[:, :], bcb_t, "bcb")
            ld("gpsimd", mk[:, :, :], masks, "mk")
            ld("sync", flg[:, :], flag, "flg")
            ld("gpsimd", bv_b[:, :], b_v, "bv")
            ld("gpsimd", bdn_b[:, :], b_dn, "bdn")
            HW = INW // 2
            for kc in range(KC):
                for hh in range(2):
                    ld("gpsimd", w_in_bf[:, kc, hh * HW:(hh + 1) * HW], w_in[kc * 128:(kc + 1) * 128, hh * HW:(hh + 1) * HW], "w_in", sem=dw)
            for ac in range(AC):
                ld("gpsimd", w_ab_bf[:, ac, :], w_ab[ac * 128:(ac + 1) * 128, :], "w_ab", sem=dw)
            for c in range(CCH):
                ld("gpsimd", w_cb_bf[:, c, :], w_cb[c * 128:(c + 1) * 128, :], "w_cb", sem=dw)
            for kc in range(KC):
                ld("gpsimd", w_o_bf[:, kc, :], w_o[kc * 128:(kc + 1) * 128, :], "w_o", sem=dw)

            V = lambda fn, r=(), w=(): S.op("vector", fn, reads=r, writes=w)
            A = lambda fn, r=(), w=(): S.op("scalar", fn, reads=r, writes=w)
            P = lambda fn, r=(), w=(): S.op("gpsimd", fn, reads=r, writes=w)
            TE = lambda fn, r=(), w=(): S.op("tensor", fn, reads=r, writes=w)

            cast_jobs = []
            if not cfg["repl"]:
                dwl = S.dsem("wl", group=True)
                dag = S.dsem("ag", group=True, inc=1)
                for i in range(EPC):
                    for kc in range(KC):
                        cast_jobs.append((0, i, kc))
                        cast_jobs.append((1, i, kc))

            def issue_cast(job, jn):
                which, i, kc = job
                r0 = i * D + kc * 128
                if which == 0:
                    S.op("gpsimd", lambda e: e.dma_start(out=wl_up[r0:r0 + 128, :], in_=w_up[i, kc * 128:(kc + 1) * 128, :]), writes=[Bf("wl")], dma_sem=dwl)
                else:
                    S.op("gpsimd", lambda e: e.dma_start(out=wl_dn[r0:r0 + 128, :], in_=w_dn[i, kc * 128:(kc + 1) * 128, :]), writes=[Bf("wl")], dma_sem=dwl)

            def issue_ag():
                grp = [list(range(cfg["NCORES"]))]
                S.op("gpsimd", lambda e: e.collective_compute("AllGather", ALU.bypass, replica_groups=grp, ins=[wl_up_t.ap().opt()], outs=[wa_up_t.ap().opt()]),
                     reads=[Bf("wl")], writes=[Bf("wa")], dma_sem=dag)
                S.op("gpsimd", lambda e: e.collective_compute("AllGather", ALU.bypass, replica_groups=grp, ins=[wl_dn_t.ap().opt()], outs=[wa_dn_t.ap().opt()]),
                     reads=[Bf("wl")], writes=[Bf("wa")], dma_sem=dag)

            V(lambda e: e.tensor_copy(out=ident_f[:, :], in_=cst[:, 0, :]), [Bf("cst")], [Bf("ident_f")])
            V(lambda e: e.tensor_copy(out=ident_b[:, :], in_=cst[:, 0, :]), [Bf("cst")], [Bf("ident_b")])
            V(lambda e: e.tensor_copy(out=ltri_b[:, :], in_=cst[:, 1, :]), [Bf("cst")], [Bf("ltri")])
            V(lambda e: e.tensor_copy(out=ones_b[:, :], in_=cst[:, 2, :]), [Bf("cst")], [Bf("ones_b")])
            V(lambda e: e.tensor_copy(out=ones_f[:, :], in_=cst[:, 2, :]), [Bf("cst")], [Bf("ones_f")])
            V(lambda e: e.tensor_scalar(out=onesm_f[:, :], in0=cst[:, 2, :], scalar1=1.0 / CC, scalar2=None, op0=ALU.mult),
              [Bf("cst")], [Bf("onesm")])
            V(lambda e: e.tensor_copy(out=selT[:, :, :], in_=cst[0:NE, 0, 0:NE].unsqueeze(2).to_broadcast([NE, NE, 128])), [Bf("cst")], [Bf("selT")])
            A(lambda e: e.activation(out=esink[:, :], in_=esink[:, :], func=AF.Exp), [Bf("esink")], [Bf("esink")])
            P(lambda e: e.memset(Vr[:, :, :, :], 1.0), [], [Bf("Vr")])
            P(lambda e: e.memset(run_c[:, :], 0.0), [], [Bf("run")])
            P(lambda e: e.memset(aT[:, :, :], 0.0), [], [Bf("aT")])
            P(lambda e: e.memset(kT[:, :], 0.0), [], [Bf("kT")])

            rot = [0]

            def nbank(pool=(0, 1, 2, 3)):
                rot[0] += 1
                return pool[rot[0] % len(pool)]

            dx = [S.dsem("x0"), S.dsem("x1")]
            dxt = [S.dsem("xt0"), S.dsem("xt1")]
            dx1 = [S.dsem("x1s0"), S.dsem("x1s1")]
            dsc = [S.dsem("sc0"), S.dsem("sc1")]

            NST = T // ST
            sizes = [128] + [ST] * NST
            tau0 = 0
            def body(s, n, tau0):
                    xs_ = s % 2
                    XB = xT_bf[xs_]
                    BX = Bf("xT_bf%d" % xs_)
                    for kc in range(KC):
                        S.op("gpsimd", lambda e, kc=kc, XB=XB, tau0=tau0, n=n: e.dma_start(
                            out=XB[:, kc, 0:n], in_=xT[kc * 128:(kc + 1) * 128, tau0:tau0 + n]), writes=[BX], dma_sem=dx[xs_])

                    def proj(ch, n=n, XB=XB, BX=BX):
                        b = nbank()
                        for kc in range(KC):
                            TE(lambda e, kc=kc, b=b, ch=ch: e.matmul(ps[b][:, 0:n], lhsT=w_in_bf[:, kc, ch * 128:(ch + 1) * 128],
                                                                     rhs=XB[:, kc, 0:n], start=(kc == 0), stop=(kc == KC - 1)),
                               [Bf("w_in"), BX], [PB[b]])
                        return b

                    if s > 0:
                        for c in range(AC):
                            b = proj(c)
                            A(lambda e, b=b, c=c, n=n: e.activation(out=qT[:, c, 0:n], in_=ps[b][:, 0:n], func=AF.Identity,
                                                                     bias=bin_t[:, c:c + 1], scale=1.0), [PB[b], Bf("bin")], [Bf("qT")])
                    b = proj(AC)
                    A(lambda e, b=b, n=n: e.activation(out=kT[:, 128:128 + n], in_=ps[b][:, 0:n], func=AF.Identity,
                                                       bias=bin_t[:, AC:AC + 1], scale=1.0), [PB[b], Bf("bin")], [Bf("kT")])
                    for bb in range(n // 128):
                        b = nbank()
                        for kc in range(KC):
                            TE(lambda e, kc=kc, b=b, bb=bb, XB=XB: e.matmul(ps[b][:, 0:128], lhsT=XB[:, kc, bb * 128:(bb + 1) * 128],
                                                                            rhs=w_in_bf[:, kc, KE:VE], start=(kc == 0), stop=False),
                               [Bf("w_in"), BX], [PB[b]])
                        TE(lambda e, b=b: e.matmul(ps[b][:, 0:128], lhsT=ones_b[0:1, :], rhs=bv_b[0:1, :], start=False, stop=True),
                           [Bf("ones_b"), Bf("bv")], [PB[b]])
                        V(lambda e, b=b, bb=bb: e.tensor_copy(out=Vr[:, 1 + bb, :, 0:64], in_=ps[b][:, 0:128].rearrange("p (g d) -> p g d", g=2)),
                          [PB[b]], [Bf("Vr")])
                    for c in range(CCH):
                        bg = proj(AC + 2 + CCH + c)
                        A(lambda e, bg=bg, c=c, n=n: e.activation(out=sgt[:, 0:n], in_=ps[bg][:, 0:n], func=AF.Sigmoid,
                                                                   bias=bin_t[:, AC + 2 + CCH + c:AC + 3 + CCH + c], scale=1.0),
                          [PB[bg], Bf("bin")], [Bf("sgt")])
                        ba = proj(AC + 2 + c)
                        V(lambda e, ba=ba, c=c, n=n: e.scalar_tensor_tensor(out=aT[:, c, PADL:PADL + n], in0=ps[ba][:, 0:n],
                                                                            scalar=bin_t[:, AC + 2 + c:AC + 3 + c], in1=sgt[:, 0:n],
                                                                            op0=ALU.add, op1=ALU.mult),
                          [PB[ba], Bf("bin"), Bf("sgt")], [Bf("aT")])

                    yield "A1"
                    if s > 0:
                        for qb in range(n // 128):
                            gq = (s - 1) * (ST // 128) + qb
                            mprev = 2 if gq == 0 else 1
                            for g in range(2):
                                rows = slice(64 * g, 64 * g + 64)
                                bP, bC, bO = 2 + 2 * g, 3 + 2 * g, 6 + g
                                pp, pc = Pp[g], Pc[g]
                                TE(lambda e, rows=rows, bP=bP, qb=qb: e.matmul(ps[bP][:, 0:GW], lhsT=kT[rows, qb * 128:qb * 128 + 128],
                                                                              rhs=qT[rows, :, qb * 128:(qb + 1) * 128], start=True, stop=True),
                                   [Bf("kT"), Bf("qT")], [PB[bP]])
                                TE(lambda e, rows=rows, bC=bC, qb=qb: e.matmul(ps[bC][:, 0:GW], lhsT=kT[rows, 128 + qb * 128:256 + qb * 128],
                                                                              rhs=qT[rows, :, qb * 128:(qb + 1) * 128], start=True, stop=True),
                                   [Bf("kT"), Bf("qT")], [PB[bC]])
                                A(lambda e, bP=bP, pp=pp: e.activation(out=pp[:, :], in_=ps[bP][:, 0:GW], func=AF.Exp, scale=0.125),
                                  [PB[bP]], [Bf("Pp%d" % g)])
                                A(lambda e, bC=bC, pc=pc: e.activation(out=pc[:, :], in_=ps[bC][:, 0:GW], func=AF.Exp, scale=0.125),
                                  [PB[bC]], [Bf("Pc%d" % g)])
                                P(lambda e, pp=pp, mprev=mprev: e.tensor_tensor(out=pp[:, :], in0=pp[:, :], in1=mk[:, mprev, :], op=ALU.mult),
                                  [Bf("Pp%d" % g), Bf("mk")], [Bf("Pp%d" % g)])
                                P(lambda e, pc=pc: e.tensor_tensor(out=pc[:, :], in0=pc[:, :], in1=mk[:, 0, :], op=ALU.mult),
                                  [Bf("Pc%d" % g), Bf("mk")], [Bf("Pc%d" % g)])
                                for c in range(G):
                                    TE(lambda e, c=c, bO=bO, pp=pp, qb=qb, g=g: e.matmul(ps[bO][:, c * 65:(c + 1) * 65], lhsT=pp[:, c * 128:(c + 1) * 128],
                                                                                        rhs=Vr[:, qb, g, :], start=True, stop=False),
                                       [Bf("Pp%d" % g), Bf("Vr")], [PB[bO]])
                                    TE(lambda e, c=c, bO=bO, pc=pc, qb=qb, g=g: e.matmul(ps[bO][:, c * 65:(c + 1) * 65], lhsT=pc[:, c * 128:(c + 1) * 128],
                                                                                        rhs=Vr[:, qb + 1, g, :], start=False, stop=True),
                                       [Bf("Pc%d" % g), Bf("Vr")], [PB[bO]])
                                o3 = ps[bO][:, 0:G * 65].rearrange("p (c d) -> p c d", c=G)
                                V(lambda e, o3=o3, g=g: e.tensor_tensor(out=den[:, :], in0=o3[:, :, 64], in1=esink[:, g * G:(g + 1) * G], op=ALU.add),
                                  [PB[bO], Bf("esink")], [Bf("den")])
                                V(lambda e: e.reciprocal(out=den[:, :], in_=den[:, :]), [Bf("den")], [Bf("den")])
                                V(lambda e, o3=o3, g=g: e.tensor_tensor(out=o_n[:, g * G * 64:(g + 1) * G * 64].rearrange("p (c d) -> p c d", c=G),
                                                                        in0=o3[:, :, 0:64], in1=den[:, :].unsqueeze(2).to_broadcast([128, G, 64]), op=ALU.mult),
                                  [PB[bO], Bf("den")], [Bf("o_n")])
                            b = nbank((0, 1))
                            pbf = ps[b][:, :].bitcast(BF16)
                            for ac in range(AC):
                                TE(lambda e, ac=ac, pbf=pbf: e.transpose(out=pbf[:, ac * 128:(ac + 1) * 128], in_=o_n[:, ac * 128:(ac + 1) * 128], identity=ident_b[:, :]),
                                   [Bf("o_n"), Bf("ident_b")], [PB[b]])
                            A(lambda e, pbf=pbf, qb=qb: e.activation(out=oT[:, :, qb * 128:(qb + 1) * 128], in_=pbf[:, 0:AC * 128].rearrange("p (a t) -> p a t", a=AC),
                                                                     func=AF.Copy), [PB[b]], [Bf("oT")])
                    yield "A2"
                    if s > 0:
                        for c in range(CCH):
                            b = nbank((0, 1))
                            dg = diag2[c % 2]
                            dgB = Bf("diag%d" % (c % 2))
                            for j in range(31):
                                eng_ = V if j % 2 == 0 else P
                                eng_(lambda e, j=j, c=c, dg=dg: e.tensor_scalar(out=dg[:, j, :], in0=cst[:, 0, :], scalar1=cw[:, c, j:j + 1],
                                                                                scalar2=1.0, op0=ALU.mult, op1=ALU.mult), [Bf("cst"), Bf("cw")], [dgB])
                            for j in range(31):
                                TE(lambda e, j=j, c=c, b=b, n=n, dg=dg: e.matmul(ps[b][:, 0:n], lhsT=dg[:, j, :], rhs=aT[:, c, PADL - 30 + j:PADL - 30 + j + n],
                                                                                 start=(j == 0), stop=(j == 30)), [dgB, Bf("aT")], [PB[b]])
                            A(lambda e, c=c, b=b, n=n: e.activation(out=yb[:, c, 0:n], in_=ps[b][:, 0:n], func=AF.Identity, bias=cv[:, 0, c:c + 1], scale=1.0),
                              [PB[b], Bf("cv")], [Bf("yb")])
                            A(lambda e, c=c, b=b, n=n: e.activation(out=sq[:, c, 0:n], in_=ps[b][:, 0:n], func=AF.Square, bias=cv[:, 0, c:c + 1], scale=1.0),
                              [PB[b], Bf("cv")], [Bf("sq")])
                        for c in range(CCH):
                            TE(lambda e, c=c, n=n: e.matmul(ps[2][:, 0:n], lhsT=onesm_f[:, :], rhs=yb[:, c, 0:n], start=(c == 0), stop=(c == CCH - 1)),
                               [Bf("onesm"), Bf("yb")], [PB[2]])
                        for c in range(CCH):
                            TE(lambda e, c=c, n=n: e.matmul(ps[3][:, 0:n], lhsT=onesm_f[:, :], rhs=sq[:, c, 0:n], start=(c == 0), stop=(c == CCH - 1)),
                               [Bf("onesm"), Bf("sq")], [PB[3]])
                        A(lambda e, n=n: e.activation(out=mean_sb[:, 0:n], in_=ps[2][:, 0:n], func=AF.Copy), [PB[2]], [Bf("mean")])
                        V(lambda e, n=n: e.tensor_tensor(out=var_sb[:, 0:n], in0=mean_sb[:, 0:n], in1=mean_sb[:, 0:n], op=ALU.mult), [Bf("mean")], [Bf("var")])
                        V(lambda e, n=n: e.tensor_tensor(out=var_sb[:, 0:n], in0=ps[3][:, 0:n], in1=var_sb[:, 0:n], op=ALU.subtract), [PB[3], Bf("var")], [Bf("var")])
                        V(lambda e, n=n: e.tensor_scalar(out=var_sb[:, 0:n], in0=var_sb[:, 0:n], scalar1=EPS, scalar2=None, op0=ALU.add), [Bf("var")], [Bf("var")])
                        A(lambda e, n=n: e.activation(out=var_sb[:, 0:n], in_=var_sb[:, 0:n], func=AF.Sqrt), [Bf("var")], [Bf("var")])
                        V(lambda e, n=n: e.reciprocal(out=var_sb[:, 0:n], in_=var_sb[:, 0:n]), [Bf("var")], [Bf("var")])
                        for c in range(CCH):
                            V(lambda e, c=c, n=n: e.tensor_tensor(out=tmpc[:, 0:n], in0=yb[:, c, 0:n], in1=mean_sb[:, 0:n], op=ALU.subtract),
                              [Bf("yb"), Bf("mean")], [Bf("tmpc")])
                            V(lambda e, n=n: e.tensor_tensor(out=tmpc[:, 0:n], in0=tmpc[:, 0:n], in1=var_sb[:, 0:n], op=ALU.mult),
                              [Bf("tmpc"), Bf("var")], [Bf("tmpc")])
                            A(lambda e, c=c, n=n: e.activation(out=sT[:, c, 0:n], in_=tmpc[:, 0:n], func=AF.Silu, scale=cv[:, 1, c:c + 1], bias=cv[:, 2, c:c + 1]),
                              [Bf("tmpc"), Bf("cv")], [Bf("sT")])
                    P(lambda e, n=n: e.tensor_copy(out=kT[:, 0:128], in_=kT[:, n:n + 128]), [Bf("kT")], [Bf("kT")])
                    P(lambda e, n=n: e.tensor_copy(out=Vr[:, 0, :, 0:64], in_=Vr[:, n // 128, :, 0:64]), [Bf("Vr")], [Bf("Vr")])
                    P(lambda e, n=n: e.tensor_copy(out=aT[:, :, 0:PADL], in_=aT[:, :, n:n + PADL]), [Bf("aT")], [Bf("aT")])
                    if s == 0:
                        P(lambda e: e.tensor_scalar(out=aT[:, :, 0:PADL], in0=aT[:, :, 0:PADL], scalar1=flg[:, 0:1], scalar2=None, op0=ALU.mult),
                          [Bf("aT"), Bf("flg")], [Bf("aT")])
                    yield "A"
                    if s == 0:
                        return
                    for j in range(KC):
                        bs = (0, 1, 2, 3) if j % 2 == 0 else (4, 5, 6, 7)
                        bA, bGA, bB, bGB = bs
                        gaj, gbj = ga[j % 2], gb[j % 2]
                        for ac in range(AC):
                            TE(lambda e, ac=ac, j=j, bA=bA, n=n: e.matmul(ps[bA][:, 0:n], lhsT=w_ab_bf[:, ac, j * 128:(j + 1) * 128], rhs=oT[:, ac, 0:n],
                                                                          start=(ac == 0), stop=(ac == AC - 1)), [Bf("w_ab"), Bf("oT")], [PB[bA]])
                        for kc in range(KC):
                            TE(lambda e, kc=kc, j=j, bGA=bGA, n=n, XB=XB: e.matmul(ps[bGA][:, 0:n], lhsT=w_in_bf[:, kc, CE + j * 128:CE + (j + 1) * 128], rhs=XB[:, kc, 0:n],
                                                                                  start=(kc == 0), stop=(kc == KC - 1)), [Bf("w_in"), BX], [PB[bGA]])
                        for c in range(CCH):
                            TE(lambda e, c=c, j=j, bB=bB, n=n: e.matmul(ps[bB][:, 0:n], lhsT=w_cb_bf[:, c, j * 128:(j + 1) * 128], rhs=sT[:, c, 0:n],
                                                                        start=(c == 0), stop=(c == CCH - 1)), [Bf("w_cb"), Bf("sT")], [PB[bB]])
                        for kc in range(KC):
                            TE(lambda e, kc=kc, j=j, bGB=bGB, n=n, XB=XB: e.matmul(ps[bGB][:, 0:n], lhsT=w_in_bf[:, kc, CE + D + j * 128:CE + D + (j + 1) * 128], rhs=XB[:, kc, 0:n],
                                                                                  start=(kc == 0), stop=(kc == KC - 1)), [Bf("w_in"), BX], [PB[bGB]])
                        ua = CE // 128 + j
                        ub = CE // 128 + KC + j
                        A(lambda e, bGA=bGA, gaj=gaj, ua=ua, n=n: e.activation(out=gaj[:, 0:n], in_=ps[bGA][:, 0:n], func=AF.Sigmoid, bias=bin_t[:, ua:ua + 1], scale=1.0),
                          [PB[bGA], Bf("bin")], [Bf("ga%d" % (j % 2))])
                        A(lambda e, bGB=bGB, gbj=gbj, ub=ub, n=n: e.activation(out=gbj[:, 0:n], in_=ps[bGB][:, 0:n], func=AF.Sigmoid, bias=bin_t[:, ub:ub + 1], scale=1.0),
                          [PB[bGB], Bf("bin")], [Bf("gb%d" % (j % 2))])
                        V(lambda e, bA=bA, gaj=gaj, n=n: e.tensor_tensor(out=t1[:, 0:n], in0=ps[bA][:, 0:n], in1=gaj[:, 0:n], op=ALU.mult),
                          [PB[bA], Bf("ga%d" % (j % 2))], [Bf("t1")])
                        V(lambda e, bB=bB, gbj=gbj, j=j, n=n: e.scalar_tensor_tensor(out=t2[:, 0:n], in0=ps[bB][:, 0:n], scalar=bcb[:, j:j + 1], in1=gbj[:, 0:n],
                                                                                    op0=ALU.add, op1=ALU.mult), [PB[bB], Bf("gb%d" % (j % 2)), Bf("bcb")], [Bf("t2")])
                        V(lambda e, j=j, n=n: e.tensor_tensor(out=mg[:, j, 0:n], in0=t1[:, 0:n], in1=t2[:, 0:n], op=ALU.add), [Bf("t1"), Bf("t2")], [Bf("mg")])
                    yield "B"
                    for bb in range(n // 128):
                        blk = (s - 1) * (ST // 128) + bb
                        t0 = blk * 128
                        sl = 0
                        xtt, x1t, x1bt = xt[sl], x1[sl], x1b[sl]
                        S.op("sync", lambda e, xtt=xtt, t0=t0: e.dma_start(out=xtt[:, :], in_=xtok[t0:t0 + 128, :]), writes=[Bf("xt%d" % sl)], dma_sem=dxt[sl])
                        zb = (0, 1) if blk % 2 == 0 else (2, 3)
                        for h in range(NH2):
                            b = zb[h % 2]
                            for kc in range(KC):
                                TE(lambda e, kc=kc, b=b, h=h, bb=bb: e.matmul(ps[b][:, 0:W2], lhsT=mg[:, kc, bb * 128:(bb + 1) * 128], rhs=w_o_bf[:, kc, h * W2:(h + 1) * W2],
                                                                            start=(kc == 0), stop=(kc == KC - 1)), [Bf("mg"), Bf("w_o")], [PB[b]])
                            V(lambda e, b=b, h=h, xtt=xtt: e.scalar_tensor_tensor(out=z[:, h * W2:(h + 1) * W2], in0=xtt[:, h * W2:(h + 1) * W2], scalar=alpha,
                                                                                 in1=ps[b][:, 0:W2], op0=ALU.mult, op1=ALU.add), [PB[b], Bf("xt%d" % sl)], [Bf("z")])

                        def layer_norm(src, srcB, dst, dstB, gi):
                            for h in range(NH2):
                                V(lambda e, h=h: e.bn_stats(out=stt[:, h * 6:(h + 1) * 6], in_=src[:, h * W2:(h + 1) * W2]), [srcB], [Bf("stt")])
                            V(lambda e: e.bn_aggr(out=mv[:, :], in_=stt[:, 0:NH2 * 6]), [Bf("stt")], [Bf("mv")])
                            V(lambda e: e.tensor_scalar(out=sm[:, 0:1], in0=mv[:, 1:2], scalar1=EPS, scalar2=None, op0=ALU.add), [Bf("mv")], [Bf("sm")])
                            A(lambda e: e.activation(out=sm[:, 1:2], in_=sm[:, 0:1], func=AF.Sqrt), [Bf("sm")], [Bf("sm")])
                            V(lambda e: e.reciprocal(out=sm[:, 2:3], in_=sm[:, 1:2]), [Bf("sm")], [Bf("sm")])
                            V(lambda e: e.scalar_tensor_tensor(out=sm[:, 3:4], in0=mv[:, 0:1], scalar=-1.0, in1=sm[:, 2:3], op0=ALU.mult, op1=ALU.mult),
                              [Bf("mv"), Bf("sm")], [Bf("sm")])
                            A(lambda e: e.activation(out=nrm[:, :], in_=src[:, :], func=AF.Identity, scale=sm[:, 2:3], bias=sm[:, 3:4]), [srcB, Bf("sm")], [Bf("nrm")])
                            P(lambda e: e.tensor_tensor(out=nrm[:, :], in0=nrm[:, :], in1=lnbc[:, gi, :], op=ALU.mult), [Bf("nrm"), Bf("lnbc")], [Bf("nrm")])
                            P(lambda e: e.tensor_tensor(out=dst[:, :], in0=nrm[:, :], in1=lnbc[:, gi + 1, :], op=ALU.add), [Bf("nrm"), Bf("lnbc")], [dstB])

                        layer_norm(z, Bf("z"), x1t, Bf("x1_%d" % sl), 0)
                        S.op("sync", lambda e, x1t=x1t, t0=t0: e.dma_start(out=x1_d[t0:t0 + 128, :], in_=x1t[:, :]), reads=[Bf("x1_%d" % sl)], writes=[B_x1d], dma_sem=dx1[sl])
                        A(lambda e, x1t=x1t, x1bt=x1bt: e.activation(out=x1bt[:, :], in_=x1t[:, :], func=AF.Copy), [Bf("x1_%d" % sl)], [Bf("x1b%d" % sl)])
                        yield "C1"
                        for kc in range(KC):
                            b = 4 + (kc // 4) % 2
                            TE(lambda e, kc=kc, b=b, x1t=x1t: e.transpose(out=ps[b][:, (kc % 4) * 128:(kc % 4 + 1) * 128], in_=x1t[:, kc * 128:(kc + 1) * 128], identity=ident_f[:, :]),
                               [Bf("x1_%d" % sl), Bf("ident_f")], [PB[b]])
                            if kc % 4 == 3 or kc == KC - 1:
                                k0 = (kc // 4) * 4
                                nk = kc - k0 + 1
                                V(lambda e, b=b, k0=k0, nk=nk: e.tensor_copy(out=x1T[:, k0:k0 + nk, :], in_=ps[b][:, 0:nk * 128].rearrange("p (k t) -> p k t", k=nk)),
                                  [PB[b]], [Bf("x1T")])
                        for kc in range(KC):
                            TE(lambda e, kc=kc: e.matmul(ps[6][:, 0:NE], lhsT=x1T[:, kc, :], rhs=w_r_f[:, kc, :], start=(kc == 0), stop=False),
                               [Bf("x1T"), Bf("w_r")], [PB[6]])
                        TE(lambda e: e.matmul(ps[6][:, 0:NE], lhsT=ones_f[0:1, :], rhs=b_r_f[0:1, :], start=False, stop=True), [Bf("ones_f"), Bf("b_r")], [PB[6]])
                        V(lambda e: e.tensor_copy(out=lg[:, :], in_=ps[6][:, 0:NE]), [PB[6]], [Bf("lg")])
                        V(lambda e: e.max(out=top8[:, :], in_=lg[:, :]), [Bf("lg")], [Bf("top8")])
                        V(lambda e: e.max_index(out=tidx[:, :], in_max=top8[:, :], in_values=lg[:, :]), [Bf("lg"), Bf("top8")], [Bf("tidx")])
                        V(lambda e: e.tensor_scalar(out=sm[:, 4:5], in0=top8[:, 0:1], scalar1=-1.0, scalar2=None, op0=ALU.mult), [Bf("top8")], [Bf("sm2")])
                        A(lambda e: e.activation(out=ex4[:, :], in_=top8[:, 0:4], func=AF.Exp, bias=sm[:, 4:5], scale=1.0), [Bf("top8"), Bf("sm2")], [Bf("ex4")])
                        V(lambda e: e.tensor_reduce(out=sm[:, 5:6], in_=ex4[:, :], axis=mybir.AxisListType.X, op=ALU.add), [Bf("ex4")], [Bf("sm3")])
                        V(lambda e: e.reciprocal(out=sm[:, 6:7], in_=sm[:, 5:6]), [Bf("sm3")], [Bf("sm3")])
                        V(lambda e, blk=blk: e.tensor_scalar(out=gate[:, blk, :], in0=ex4[:, :], scalar1=sm[:, 6:7], scalar2=None, op0=ALU.mult),
                          [Bf("ex4"), Bf("sm3")], [B_gate])
                        V(lambda e: e.tensor_scalar(out=mskb[:, :], in0=lg[:, :], scalar1=top8[:, 3:4], scalar2=None, op0=ALU.is_ge), [Bf("lg"), Bf("top8")], [Bf("mskb")])
                        yield "C2"
                        TE(lambda e: e.matmul(ps[7][:, 0:NE], lhsT=ltri_b[:, :], rhs=mskb[:, :], start=True, stop=True), [Bf("ltri"), Bf("mskb")], [PB[7]])
                        TE(lambda e: e.matmul(ps[7][:, 64:64 + NE], lhsT=ones_b[:, :], rhs=mskb[:, :], start=True, stop=True), [Bf("ones_b"), Bf("mskb")], [PB[7]])
                        V(lambda e: e.tensor_tensor(out=pos[:, :], in0=ps[7][:, 0:NE], in1=run_c[:, :], op=ALU.add), [PB[7], Bf("run")], [Bf("pos")])
                        V(lambda e: e.tensor_tensor(out=run_c[:, :], in0=ps[7][:, 64:64 + NE], in1=run_c[:, :], op=ALU.add), [PB[7], Bf("run")], [Bf("run")])
                        V(lambda e: e.tensor_scalar(out=ovf[:, :], in0=pos[:, :], scalar1=float(CAP), scalar2=BIG, op0=ALU.is_ge, op1=ALU.mult), [Bf("pos")], [Bf("ovf")])
                        V(lambda e: e.tensor_tensor(out=pos[:, :], in0=pos[:, :], in1=er[:, 1, :], op=ALU.add), [Bf("pos"), Bf("er")], [Bf("pos")])
                        V(lambda e: e.tensor_tensor(out=pos[:, :], in0=pos[:, :], in1=ovf[:, :], op=ALU.add), [Bf("pos"), Bf("ovf")], [Bf("pos")])
                        V(lambda e: e.tensor_copy(out=ef[:, :], in_=tidx[:, 0:4]), [Bf("tidx")], [Bf("ef")])
                        for k in range(4):
                            V(lambda e, k=k: e.scalar_tensor_tensor(out=junk[:, :], in0=er[:, 0, :], scalar=ef[:, k:k + 1], in1=pos[:, :], op0=ALU.is_equal, op1=ALU.mult,
                                                                   accum_out=dest_f[:, k:k + 1]), [Bf("er"), Bf("ef"), Bf("pos")], [Bf("junk"), Bf("dest_f")])
                        V(lambda e, blk=blk: e.tensor_copy(out=dest_i[:, blk, :], in_=dest_f[:, :]), [Bf("dest_f")], [B_dest])
                        for k in range(4):
                            S.op("gpsimd", lambda e, k=k, blk=blk, x1bt=x1bt: e.indirect_dma_start(
                                out=xs_d[:, :], out_offset=bass.IndirectOffsetOnAxis(ap=dest_i[:, blk, k:k + 1], axis=0), in_=x1bt[:, :], in_offset=None,
                                bounds_check=breg(e, 1), oob_is_err=False), reads=[Bf("x1b%d" % sl), B_dest], writes=[], dma_sem=dsc[sl])

            gens = []
            tau0 = 0
            for s, n in enumerate(sizes):
                gens.append(body(s, n, tau0))
                tau0 += n

            def adv(i):
                if 0 <= i < len(gens):
                    try:
                        next(gens[i])
                    except StopIteration:
                        pass

            for s in range(len(sizes) + 3):
                adv(s)
                adv(s - 2)
                adv(s - 1)
                adv(s)
                adv(s - 1)
                adv(s)
                adv(s - 1)
                adv(s)
                adv(s - 1)
                if cast_jobs and s < len(sizes):
                    ncast = len(cast_jobs)
                    nsp = min(4, NST)
                    per = (ncast + nsp - 1) // nsp
                    if 1 <= s <= nsp:
                        for jn in range((s - 1) * per, min(s * per, ncast)):
                            issue_cast(cast_jobs[jn], jn)
                    if s == min(6, len(sizes) - 1):
                        issue_ag()
            for g_ in gens:
                for _ in g_:
                    pass
            S.emit()

        S = Sched(nc, "e")
        PB = [Buf("ps%d" % i) for i in range(8)]
        with ExitStack() as st:
            def sb(name, shape, dt):
                return st.enter_context(nc.sbuf_tensor(name, list(shape), dt))

            wu = [sb("wu%d" % i, [128, KC, 2 * D], BF16) for i in range(2)]
            wd = [sb("wd%d" % i, [128, FC, D], BF16) for i in range(2)]
            xs_t = [sb("xs_t%d" % i, [128, NB, D], BF16) for i in range(2)]
            xsT = [sb("xsT%d" % i, [128, KC, CAP], BF16) for i in range(2)]
            actT = [sb("actT%d" % i, [128, FC, CAP], BF16) for i in range(2)]
            xg = [sb("xg%d" % i, [128, CAP], F32) for i in range(2)]
            sg = [sb("sg%d" % i, [128, CAP], F32) for i in range(2)]
            xl = [sb("xl%d" % i, [128, CAP], F32) for i in range(2)]
            ys_t = [sb("ys_t%d" % i, [128, D], F32) for i in range(2)]
            Bn = {}

            def Bf(n):
                if n not in Bn:
                    Bn[n] = Buf(n)
                return Bn[n]

            V = lambda fn, r=(), w=(): S.op("vector", fn, reads=r, writes=w)
            A = lambda fn, r=(), w=(): S.op("scalar", fn, reads=r, writes=w)
            P = lambda fn, r=(), w=(): S.op("gpsimd", fn, reads=r, writes=w)
            TE = lambda fn, r=(), w=(): S.op("tensor", fn, reads=r, writes=w)
            dwu = [S.dsem("wu0"), S.dsem("wu1")]
            dwd = [S.dsem("wd0"), S.dsem("wd1")]
            dxs = [S.dsem("xs0"), S.dsem("xs1")]
            dys = [S.dsem("ys0"), S.dsem("ys1")]

            def load_w(e_):
                sl = e_ % 2
                if not cfg["repl"]:
                    S.op("sync", lambda e, sl=sl, e_=e_: e.dma_start(out=wu[sl][:, :, :], in_=wa_up[e_ * D:(e_ + 1) * D, :].rearrange("(kc p) f -> p kc f", p=128)),
                         writes=[Bf("wu%d" % sl)], dma_sem=dwu[sl])
                    S.op("sync", lambda e, sl=sl, e_=e_: e.dma_start(out=wd[sl][:, :, :], in_=wa_dn[e_ * D:(e_ + 1) * D, :].rearrange("(kc p) f -> p kc f", p=128)),
                         writes=[Bf("wd%d" % sl)], dma_sem=dwd[sl])
                    return
                nsp = 2 if KC >= 2 else 1
                kh = KC // nsp
                for h in range(nsp):
                    S.op("gpsimd", lambda e, h=h, sl=sl, e_=e_: e.dma_start(out=wu[sl][:, h * kh:(h + 1) * kh, :],
                                                                          in_=w_up[e_, h * kh * 128:(h + 1) * kh * 128, :].rearrange("(kc p) f -> p kc f", p=128)),
                         writes=[Bf("wu%d" % sl)], dma_sem=dwu[sl])
                S.op("gpsimd", lambda e, sl=sl, e_=e_: e.dma_start(out=wd[sl][:, :, :], in_=w_dn[e_, :, :].rearrange("(fc p) d -> p fc d", p=128)),
                     writes=[Bf("wd%d" % sl)], dma_sem=dwd[sl])

            def load_xs(e_):
                sl = e_ % 2
                S.op("sync", lambda e, sl=sl, e_=e_: e.dma_start(out=xs_t[sl][:, :, :], in_=xs_d[e_ * CAP:(e_ + 1) * CAP, :].rearrange("(b p) d -> p b d", p=128)),
                     reads=[B_xs], writes=[Bf("xs_t%d" % sl)], dma_sem=dxs[sl])

            load_w(0)
            load_xs(0)
            cnt = [0]
            for e_ in range(NE):
                sl = e_ % 2
                if e_ + 1 < NE:
                    load_w(e_ + 1)
                    load_xs(e_ + 1)
                Bwu, Bwd = Bf("wu%d" % sl), Bf("wd%d" % sl)
                for nb in range(NB):
                    b = 6 + nb % 2
                    pbf = ps[b][:, :].bitcast(BF16)
                    for kc in range(KC):
                        TE(lambda e, kc=kc, nb=nb, pbf=pbf, sl=sl: e.transpose(out=pbf[:, kc * 128:(kc + 1) * 128], in_=xs_t[sl][:, nb, kc * 128:(kc + 1) * 128], identity=ident_b[:, :]),
                           [Bf("xs_t%d" % sl)], [PB[b]])
                    cp = A if nb % 2 == 0 else V
                    if nb % 2 == 0:
                        A(lambda e, pbf=pbf, nb=nb, sl=sl: e.activation(out=xsT[sl][:, :, nb * 128:(nb + 1) * 128], in_=pbf[:, 0:KC * 128].rearrange("p (k t) -> p k t", k=KC), func=AF.Copy),
                          [PB[b]], [Bf("xsT%d" % sl)])
                    else:
                        V(lambda e, pbf=pbf, nb=nb, sl=sl: e.tensor_copy(out=xsT[sl][:, :, nb * 128:(nb + 1) * 128], in_=pbf[:, 0:KC * 128].rearrange("p (k t) -> p k t", k=KC)),
                          [PB[b]], [Bf("xsT%d" % sl)])
                for fp in range(FC):
                    cnt[0] += 1
                    pr = cnt[0] % 2
                    bG, bL = (0, 1) if pr == 0 else (2, 3)
                    for kc in range(KC):
                        TE(lambda e, kc=kc, fp=fp, bG=bG, sl=sl: e.matmul(ps[bG][:, 0:CAP], lhsT=wu[sl][:, kc, fp * 128:(fp + 1) * 128], rhs=xsT[sl][:, kc, :],
                                                                         start=(kc == 0), stop=(kc == KC - 1)), [Bwu, Bf("xsT%d" % sl)], [PB[bG]])
                    for kc in range(KC):
                        TE(lambda e, kc=kc, fp=fp, bL=bL, sl=sl: e.matmul(ps[bL][:, 0:CAP], lhsT=wu[sl][:, kc, D + fp * 128:D + (fp + 1) * 128], rhs=xsT[sl][:, kc, :],
                                                                         start=(kc == 0), stop=(kc == KC - 1)), [Bwu, Bf("xsT%d" % sl)], [PB[bL]])
                    V(lambda e, bG=bG, e_=e_, fp=fp, pr=pr: e.tensor_scalar(out=xg[pr][:, :], in0=ps[bG][:, 0:CAP], scalar1=bup[:, e_, fp:fp + 1], scalar2=7.0, op0=ALU.add, op1=ALU.min),
                      [PB[bG]], [Bf("xg%d" % pr)])
                    A(lambda e, pr=pr: e.activation(out=sg[pr][:, :], in_=xg[pr][:, :], func=AF.Sigmoid, scale=1.702), [Bf("xg%d" % pr)], [Bf("sg%d" % pr)])
                    V(lambda e, bL=bL, e_=e_, fp=fp, pr=pr: e.tensor_scalar(out=xl[pr][:, :], in0=ps[bL][:, 0:CAP], scalar1=bup[:, e_, FC + fp:FC + fp + 1], scalar2=7.0, op0=ALU.add, op1=ALU.min),
                      [PB[bL]], [Bf("xl%d" % pr)])
                    P(lambda e, pr=pr: e.tensor_scalar(out=xl[pr][:, :], in0=xl[pr][:, :], scalar1=7.0, scalar2=-7.0, op0=ALU.min, op1=ALU.max), [Bf("xl%d" % pr)], [Bf("xl%d" % pr)])
                    P(lambda e, pr=pr: e.tensor_tensor(out=xg[pr][:, :], in0=xg[pr][:, :], in1=sg[pr][:, :], op=ALU.mult), [Bf("xg%d" % pr), Bf("sg%d" % pr)], [Bf("xg%d" % pr)])
                    V(lambda e, pr=pr, fp=fp, sl=sl: e.scalar_tensor_tensor(out=actT[sl][:, fp, :], in0=xl[pr][:, :], scalar=1.0, in1=xg[pr][:, :], op0=ALU.add, op1=ALU.mult),
                      [Bf("xg%d" % pr), Bf("xl%d" % pr)], [Bf("actT%d" % sl)])
                for nb in range(NB):
                    ysl = (e_ * NB + nb) % 2
                    for h in range(NH2):
                        b = 4 + h % 2
                        for fc in range(FC):
                            TE(lambda e, fc=fc, nb=nb, h=h, b=b, sl=sl: e.matmul(ps[b][:, 0:W2], lhsT=actT[sl][:, fc, nb * 128:(nb + 1) * 128], rhs=wd[sl][:, fc, h * W2:(h + 1) * W2],
                                                                                start=(fc == 0), stop=False), [Bf("actT%d" % sl), Bwd], [PB[b]])
                        TE(lambda e, h=h, b=b, e_=e_: e.matmul(ps[b][:, 0:W2], lhsT=selT[:, e_, :], rhs=bdn_b[:, h * W2:(h + 1) * W2], start=False, stop=True),
                           [], [PB[b]])
                        if h % 2 == 0:
                            A(lambda e, b=b, h=h, ysl=ysl: e.activation(out=ys_t[ysl][:, h * W2:(h + 1) * W2], in_=ps[b][:, 0:W2], func=AF.Copy), [PB[b]], [Bf("ys_t%d" % ysl)])
                        else:
                            V(lambda e, b=b, h=h, ysl=ysl: e.tensor_copy(out=ys_t[ysl][:, h * W2:(h + 1) * W2], in_=ps[b][:, 0:W2]), [PB[b]], [Bf("ys_t%d" % ysl)])
                    r0 = e_ * CAP + nb * 128
                    S.op("sync", lambda e, ysl=ysl, r0=r0: e.dma_start(out=ys_d[r0:r0 + 128, :], in_=ys_t[ysl][:, :]), reads=[Bf("ys_t%d" % ysl)], writes=[], dma_sem=dys[ysl])
            S.emit()

        S = Sched(nc, "c")
        with ExitStack() as st:
            def sb(name, shape, dt):
                return st.enter_context(nc.sbuf_tensor(name, list(shape), dt))

            x1c = [sb("x1c%d" % i, [128, D], F32) for i in range(2)]
            yk = [[sb("yk%d_%d" % (i, k), [128, D], F32) for k in range(4)] for i in range(2)]
            acc = sb("acc", [128, D], F32)
            nrm = sb("nrm2", [128, D], F32)
            res = [sb("res%d" % i, [128, D], F32) for i in range(2)]
            stt = sb("stt2", [128, max(NH2, 1) * 6], F32)
            mv = sb("mv2", [128, 2], F32)
            sm = sb("sm2", [128, 8], F32)
            Bn = {}

            def Bf(n):
                if n not in Bn:
                    Bn[n] = Buf(n)
                return Bn[n]

            V = lambda fn, r=(), w=(): S.op("vector", fn, reads=r, writes=w)
            A = lambda fn, r=(), w=(): S.op("scalar", fn, reads=r, writes=w)
            P = lambda fn, r=(), w=(): S.op("gpsimd", fn, reads=r, writes=w)
            S.op("sync", lambda e: e.dma_start(out=lnbc[:, :, :], in_=lnb[:, 2:4, :]), writes=[Bf("lnbc")], dma_sem=S.dsem("ln2", group=True))
            dxc = [S.dsem("xc0"), S.dsem("xc1")]
            dyk = [S.dsem("yk0"), S.dsem("yk1")]
            dout = [S.dsem("o0"), S.dsem("o1")]
            for blk in range(NBLK):
                sl = blk % 2
                t0 = blk * 128
                S.op("sync", lambda e, sl=sl, t0=t0: e.dma_start(out=x1c[sl][:, :], in_=x1_d[t0:t0 + 128, :]), writes=[Bf("x1c%d" % sl)], dma_sem=dxc[sl])
                for k in range(4):
                    S.op("gpsimd", lambda e, k=k, sl=sl, blk=blk: e.indirect_dma_start(
                        out=yk[sl][k][:, :], out_offset=None, in_=ys_d[:, :], in_offset=bass.IndirectOffsetOnAxis(ap=dest_i[:, blk, k:k + 1], axis=0),
                        bounds_check=breg(e, 3), oob_is_err=False), writes=[Bf("yk%d_%d" % (sl, k))], dma_sem=dyk[sl])
                V(lambda e, sl=sl: e.tensor_scalar(out=acc[:, :], in0=x1c[sl][:, :], scalar1=alpha, scalar2=None, op0=ALU.mult), [Bf("x1c%d" % sl)], [Bf("acc")])
                for k in range(4):
                    V(lambda e, k=k, sl=sl, blk=blk: e.scalar_tensor_tensor(out=acc[:, :], in0=yk[sl][k][:, :], scalar=gate[:, blk, k:k + 1], in1=acc[:, :], op0=ALU.mult, op1=ALU.add),
                      [Bf("yk%d_%d" % (sl, k)), Bf("acc")], [Bf("acc")])
                for h in range(NH2):
                    V(lambda e, h=h: e.bn_stats(out=stt[:, h * 6:(h + 1) * 6], in_=acc[:, h * W2:(h + 1) * W2]), [Bf("acc")], [Bf("stt")])
                V(lambda e: e.bn_aggr(out=mv[:, :], in_=stt[:, 0:NH2 * 6]), [Bf("stt")], [Bf("mv")])
                V(lambda e: e.tensor_scalar(out=sm[:, 0:1], in0=mv[:, 1:2], scalar1=EPS, scalar2=None, op0=ALU.add), [Bf("mv")], [Bf("sm")])
                A(lambda e: e.activation(out=sm[:, 1:2], in_=sm[:, 0:1], func=AF.Sqrt), [Bf("sm")], [Bf("sm")])
                V(lambda e: e.reciprocal(out=sm[:, 2:3], in_=sm[:, 1:2]), [Bf("sm")], [Bf("sm")])
                V(lambda e: e.scalar_tensor_tensor(out=sm[:, 3:4], in0=mv[:, 0:1], scalar=-1.0, in1=sm[:, 2:3], op0=ALU.mult, op1=ALU.mult), [Bf("mv"), Bf("sm")], [Bf("sm")])
                A(lambda e: e.activation(out=nrm[:, :], in_=acc[:, :], func=AF.Identity, scale=sm[:, 2:3], bias=sm[:, 3:4]), [Bf("acc"), Bf("sm")], [Bf("nrm")])
                P(lambda e: e.tensor_tensor(out=nrm[:, :], in0=nrm[:, :], in1=lnbc[:, 0, :], op=ALU.mult), [Bf("nrm"), Bf("lnbc")], [Bf("nrm")])
                P(lambda e, sl=sl: e.tensor_tensor(out=res[sl][:, :], in0=nrm[:, :], in1=lnbc[:, 1, :], op=ALU.add), [Bf("nrm"), Bf("lnbc")], [Bf("res%d" % sl)])
                S.op("sync", lambda e, sl=sl, t0=t0: e.dma_start(out=out[t0:t0 + 128, :], in_=res[sl][:, :]), reads=[Bf("res%d" % sl)], writes=[], dma_sem=dout[sl])
            S.emit()
    return nc


def prep_inputs(cfg, x, w_in, b_in, attn_sinks, w_attn_br, conv_w, conv_b, conv_ln_g, conv_ln_b,
                w_conv_br, b_conv_br, w_o, ln1_g, ln1_b, w_router, b_router, w_up, b_up,
                w_down, b_down, ln2_g, ln2_b):
    D, NH, CC, NE, T, CAP = (cfg[k] for k in ["D", "NH", "CC", "NE", "T", "CAP"])
    G, AW, QE, KE, VE, CE, INW, KC, CCH, FC = (cfg[k] for k in ["G", "AW", "QE", "KE", "VE", "CE", "INW", "KC", "CCH", "FC"])
    NC_ = cfg["NCORES"]
    CPB = NC_ // cfg["B"]
    f = lambda a: np.ascontiguousarray(np.asarray(a, dtype=np.float32))
    x = f(x)
    w_in0, b_in0 = f(w_in)[0], f(b_in)[0]
    qperm = np.array([(g * G + c) * 64 + j for c in range(G) for g in range(2) for j in range(64)])
    perm = np.concatenate([qperm, np.arange(QE, INW)])
    w_in_p = np.ascontiguousarray(w_in0[:, perm])
    b_in_p = b_in0[perm]
    b_in_t = np.ascontiguousarray(b_in_p.reshape(INW // 128, 128).T)
    b_v = np.ascontiguousarray(b_in_p[KE:VE].reshape(1, 128))
    sinks_b = np.ascontiguousarray(np.broadcast_to(f(attn_sinks)[0][None, :], (128, NH)))
    cw_t = np.ascontiguousarray(f(conv_w)[0].T.reshape(CCH, 128, 31).transpose(1, 0, 2))
    pp = lambda v: v.reshape(-1, 128).T
    cvec = np.ascontiguousarray(np.stack([pp(f(conv_b)[0]), pp(f(conv_ln_g)[0]), pp(f(conv_ln_b)[0])], axis=1))
    bcb_t = np.ascontiguousarray(pp(f(b_conv_br)[0]))
    lnb = np.ascontiguousarray(np.broadcast_to(np.stack([f(ln1_g)[0], f(ln1_b)[0], f(ln2_g)[0], f(ln2_b)[0]])[None], (128, 4, D)))
    w_up0 = f(w_up)[0]
    w_up_p = np.concatenate([w_up0[:, :, 0::2], w_up0[:, :, 1::2]], axis=2)
    b_up0 = f(b_up)[0]
    b_up_p = np.concatenate([b_up0[:, 0::2], b_up0[:, 1::2]], axis=1)
    b_up_t = np.ascontiguousarray(b_up_p.reshape(NE, 2 * FC, 128).transpose(2, 0, 1))
    w_dn0 = f(w_down)[0]
    b_dn = np.ascontiguousarray(f(b_down)[0])
    ident = np.eye(128, dtype=np.float32)
    ltri = np.triu(np.ones((128, 128), np.float32), 1)
    consts = np.ascontiguousarray(np.stack([ident, ltri, np.ones((128, 128), np.float32)], axis=1))
    erow = np.ascontiguousarray(np.broadcast_to(np.stack([np.arange(NE, dtype=np.float32), np.arange(NE, dtype=np.float32) * CAP])[None], (128, 2, NE)))
    kk = np.arange(128)[:, None]
    qq = np.arange(128)[None, :]
    m_cur = (kk <= qq).astype(np.float32)
    m_prev = (kk > qq).astype(np.float32)
    shared = dict(w_in=w_in_p, b_in_t=b_in_t, b_v=b_v, sinks_b=sinks_b, w_ab=f(w_attn_br)[0], cw_t=cw_t, cvec=cvec,
                  w_cb=f(w_conv_br)[0], bcb_t=bcb_t, w_o=f(w_o)[0], lnb=lnb, w_r=f(w_router)[0], b_r=f(b_router)[0].reshape(1, NE),
                  b_up_t=b_up_t, b_dn=b_dn, consts=consts, erow=erow)
    maps = []
    for c in range(NC_):
        b, h = c // CPB, c % CPB
        st = h * T
        xT = np.zeros((D, 128 + T), np.float32)
        xT[:, 128:] = x[b, st:st + T].T
        if h > 0:
            xT[:, :128] = x[b, st - 128:st].T
        fl = 1.0 if h > 0 else 0.0
        masks = np.stack([np.tile(m_cur, (1, G)), np.tile(m_prev, (1, G)), np.tile(m_prev * fl, (1, G))], axis=1)
        m = dict(shared)
        m.update(xT=xT, xtok=np.ascontiguousarray(x[b, st:st + T]), masks=np.ascontiguousarray(masks),
                 flag=np.full((128, 1), fl, np.float32))
        if cfg["repl"]:
            m.update(w_up=w_up_p, w_dn=w_dn0)
        else:
            E = cfg["EPC"]
            m.update(w_up=np.ascontiguousarray(w_up_p[c * E:(c + 1) * E]), w_dn=np.ascontiguousarray(w_dn0[c * E:(c + 1) * E]))
        maps.append(m)
    return maps


def run_cfg(cfg, inputs, trace=False):
    maps = prep_inputs(cfg, **inputs)
    nc = build(cfg)
    res = run_bass_kernel_spmd(nc, maps, core_ids=list(range(cfg["NCORES"])), trace=trace)
    T, D, B = cfg["T"], cfg["D"], cfg["B"]
    CPB = cfg["NCORES"] // B
    outv = np.zeros((B, CPB * T, D), np.float32)
    for c in range(cfg["NCORES"]):
        b, h = c // CPB, c % CPB
        outv[b, h * T:(h + 1) * T] = res.results[c]["out"]
    return outv, res


def kernel(**inputs):
    cfg = make_cfg()
    outv, _ = run_cfg(cfg, inputs)
    return outv
```

```python
from contextlib import ExitStack
import numpy as np
import concourse.bass as bass
import concourse.mybir as mybir
from concourse.bass_utils import run_bass_kernel_spmd

F32 = mybir.dt.float32
BF16 = mybir.dt.bfloat16
I32 = mybir.dt.int32
U32 = mybir.dt.uint32
AF = mybir.ActivationFunctionType
ALU = mybir.AluOpType
ENGS = ["tensor", "vector", "scalar", "gpsimd", "sync"]


class Buf:
    __slots__ = ("name", "writers", "readers")

    def __init__(self, name):
        self.name = name
        self.writers = []
        self.readers = []


class Op:
    __slots__ = ("eng", "fn", "deps", "marked", "is_dma", "sem", "val")

    def __init__(self, eng, fn, is_dma=False, sem=None):
        self.eng = eng
        self.fn = fn
        self.deps = []
        self.marked = False
        self.is_dma = is_dma
        self.sem = sem
        self.val = None


class DSem:
    def __init__(self, name, group=False, inc=16):
        self.name = name
        self.count = 0
        self.handle = None
        self.group = group
        self.inc = inc


def _prune(lst, op):
    if op.is_dma:
        out = [o for o in lst if not (o.is_dma and o.sem is op.sem)]
    else:
        out = [o for o in lst if o.is_dma or o.eng != op.eng]
    out.append(op)
    return out


class Sched:
    def __init__(self, nc, tag):
        self.nc = nc
        self.tag = tag
        self.ops = {e: [] for e in ENGS}
        self.dsems = []
        self.dma_ops = []

    def dsem(self, name, group=False, inc=16):
        s = DSem(name, group, inc)
        self.dsems.append(s)
        return s

    def op(self, eng, fn, reads=(), writes=(), dma_sem=None):
        o = Op(eng, fn, is_dma=dma_sem is not None, sem=dma_sem)
        deps = []
        for b in reads:
            deps.extend(b.writers)
        for b in writes:
            deps.extend(b.writers)
            deps.extend(b.readers)
        seen = set()
        for d in deps:
            if id(d) in seen:
                continue
            seen.add(id(d))
            if (not d.is_dma) and (not o.is_dma) and d.eng == "tensor" and eng == "tensor":
                continue
            if d.is_dma and o.is_dma and d.sem is o.sem:
                continue
            o.deps.append(d)
            d.marked = True
        if o.is_dma:
            dma_sem.count += dma_sem.inc
            o.val = dma_sem.count
            o.marked = True
            self.dma_ops.append(o)
        for b in reads:
            b.readers = _prune(b.readers, o)
        for b in writes:
            b.writers = [o]
            b.readers = []
        self.ops[eng].append(o)
        return o

    def emit(self):
        nc = self.nc
        with ExitStack() as st:
            esem = {e: st.enter_context(nc.semaphore(self.tag + "_s_" + e)) for e in ENGS}
            for s in self.dsems:
                s.handle = st.enter_context(nc.semaphore(self.tag + "_d_" + s.name))
            for e in ENGS:
                c = 0
                for o in self.ops[e]:
                    if not o.is_dma and o.marked:
                        c += 1
                        o.val = c
            finals = {}
            for o in self.dma_ops:
                finals[id(o.sem)] = o
            block = st.enter_context(nc.Block())

            def run(e, eng):
                waited = {}
                for o in self.ops[e]:
                    for d in o.deps:
                        if d.is_dma:
                            key, h = id(d.sem), d.sem.handle
                            dv = d.sem.count if d.sem.group else d.val
                        else:
                            key, h = d.eng, esem[d.eng]
                            dv = d.val
                        if waited.get(key, 0) >= dv:
                            continue
                        waited[key] = dv
                        eng.wait_ge(h, dv)
                    inst = o.fn(eng)
                    if o.is_dma:
                        if o.sem.inc == 16:
                            inst.then_inc(o.sem.handle, 16)
                        else:
                            inst.then_inc(o.sem.handle)
                    elif o.marked:
                        inst.then_inc(esem[e], 1)
                if e == "sync":
                    for d in finals.values():
                        eng.wait_ge(d.sem.handle, d.val)

            @block.tensor
            def _(eng):
                run("tensor", eng)

            @block.vector
            def _(eng):
                run("vector", eng)

            @block.scalar
            def _(eng):
                run("scalar", eng)

            @block.gpsimd
            def _(eng):
                run("gpsimd", eng)

            @block.sync
            def _(eng):
                run("sync", eng)


def make_cfg(D=1024, NH=8, CC=512, NE=32, T=2048, CAP=384, ST=256, NCORES=8, B=4, repl=True,
             alpha=2 ** 0.25):
    c = dict(D=D, NH=NH, CC=CC, NE=NE, T=T, CAP=CAP, ST=ST, NCORES=NCORES, B=B, repl=repl, alpha=alpha)
    c["G"] = NH // 2
    c["AW"] = NH * 64
    c["QE"] = c["AW"]
    c["KE"] = c["QE"] + 128
    c["VE"] = c["KE"] + 128
    c["CE"] = c["VE"] + 2 * CC
    c["INW"] = c["CE"] + 2 * D
    c["KC"] = D // 128
    c["AC"] = c["AW"] // 128
    c["CCH"] = CC // 128
    c["FC"] = D // 128
    c["W2"] = min(D, 512)
    c["NH2"] = D // c["W2"]
    c["NBLK"] = T // 128
    c["NB"] = CAP // 128
    c["EPC"] = NE // NCORES
    c["SEQ"] = T * (NCORES // B)
    return c


def build(cfg):
    D, NH, CC, NE, T, CAP, ST = (cfg[k] for k in ["D", "NH", "CC", "NE", "T", "CAP", "ST"])
    G, AW, QE, KE, VE, CE, INW = (cfg[k] for k in ["G", "AW", "QE", "KE", "VE", "CE", "INW"])
    KC, AC, CCH, FC, W2, NH2, NBLK, NB = (cfg[k] for k in ["KC", "AC", "CCH", "FC", "W2", "NH2", "NBLK", "NB"])
    alpha = float(cfg["alpha"])
    NU = INW // 128
    GW = G * 128
    PADL = 32
    NEW = NE if cfg["repl"] else cfg["EPC"]
    BIG = 4.0e6
    EPS = 1e-5

    nc = bass.Bass("TRN2", target_bir_lowering=False)

    def din(name, shape, dt=F32):
        return nc.dram_tensor(name, list(shape), dt, kind="ExternalInput").ap()

    xT = din("xT", [D, 128 + T])
    xtok = din("xtok", [T, D])
    masks = din("masks", [128, 3, GW])
    flag = din("flag", [128, 1])
    w_in = din("w_in", [D, INW])
    b_in_t = din("b_in_t", [128, NU])
    b_v = din("b_v", [1, 128])
    sinks_b = din("sinks_b", [128, NH])
    w_ab = din("w_ab", [AW, D])
    cw_t = din("cw_t", [128, CCH, 31])
    cvec = din("cvec", [128, 3, CCH])
    w_cb = din("w_cb", [CC, D])
    bcb_t = din("bcb_t", [128, KC])
    w_o = din("w_o", [D, D])
    lnb = din("lnb", [128, 4, D])
    w_r = din("w_r", [D, NE])
    b_r = din("b_r", [1, NE])
    w_up = din("w_up", [NEW, D, 2 * D])
    b_up_t = din("b_up_t", [128, NE, 2 * FC])
    w_dn = din("w_dn", [NEW, D, D])
    b_dn = din("b_dn", [NE, D])
    consts = din("consts", [128, 3, 128])
    erow = din("erow", [128, 2, NE])
    out = nc.dram_tensor("out", [T, D], F32, kind="ExternalOutput").ap()
    xs_d = nc.dram_tensor("xs_d", [NE * CAP, D], BF16, kind="Internal").ap()
    ys_d = nc.dram_tensor("ys_d", [NE * CAP, D], F32, kind="Internal").ap()
    x1_d = nc.dram_tensor("x1_d", [T, D], F32, kind="Internal").ap()
    if not cfg["repl"]:
        EPC = cfg["EPC"]
        wl_up_t = nc.dram_tensor("wl_up", [EPC * D, 2 * D], BF16)
        wa_up_t = nc.dram_tensor("wa_up", [NE * D, 2 * D], BF16)
        wl_dn_t = nc.dram_tensor("wl_dn", [EPC * D, D], BF16)
        wa_dn_t = nc.dram_tensor("wa_dn", [NE * D, D], BF16)
        wl_up, wa_up, wl_dn, wa_dn = wl_up_t.ap(), wa_up_t.ap(), wl_dn_t.ap(), wa_dn_t.ap()

    regs = {}

    def breg(e, phase):
        if phase not in regs:
            regs[phase] = e.to_reg(NE * CAP - 1)
        return regs[phase]

    with ExitStack() as pst:
        def sbp(name, shape, dt):
            return pst.enter_context(nc.sbuf_tensor(name, list(shape), dt))

        dest_i = sbp("dest_i", [128, NBLK, 4], I32)
        gate = sbp("gate", [128, NBLK, 4], F32)
        ident_f = sbp("ident_f", [128, 128], F32)
        ident_b = sbp("ident_b", [128, 128], BF16)
        ones_b = sbp("ones_b", [128, 128], BF16)
        ones_f = sbp("ones_f", [128, 128], F32)
        lnbc = sbp("lnbc", [128, 2, D], F32)
        bdn_b = sbp("bdn_b", [NE, D], BF16)
        selT = sbp("selT", [NE, NE, 128], BF16)
        bup = sbp("bup", [128, NE, 2 * FC], F32)
        ps = [pst.enter_context(nc.psum_tensor("ps%d" % i, [128, 512], F32)) for i in range(8)]
        B_dest, B_gate, B_xs, B_ys, B_x1d = Buf("dest"), Buf("gate"), Buf("xs"), Buf("ys"), Buf("x1d")
        B_const = Buf("const")

        S = Sched(nc, "m")
        PB = [Buf("ps%d" % i) for i in range(8)]
        with ExitStack() as st:
            def sb(name, shape, dt):
                return st.enter_context(nc.sbuf_tensor(name, list(shape), dt))

            w_in_bf = sb("w_in_bf", [128, KC, INW], BF16)
            w_ab_bf = sb("w_ab_bf", [128, AC, D], BF16)
            w_cb_bf = sb("w_cb_bf", [128, CCH, D], BF16)
            w_o_bf = sb("w_o_bf", [128, KC, D], BF16)
            diag2 = [sb("diag%d" % i, [128, 31, 128], BF16) for i in range(2)]
            w_r_f = sb("w_r_f", [128, KC, NE], F32)
            b_r_f = sb("b_r_f", [1, NE], F32)
            bin_t = sb("bin_t", [128, NU], F32)
            bv_b = sb("bv_b", [1, 128], BF16)
            esink = sb("esink", [128, NH], F32)
            cw = sb("cw", [128, CCH, 31], F32)
            cv = sb("cv", [128, 3, CCH], F32)
            bcb = sb("bcb", [128, KC], F32)
            mk = sb("mk", [128, 3, GW], BF16)
            flg = sb("flg", [128, 1], F32)
            cst = sb("cst", [128, 3, 128], F32)
            ltri_b = sb("ltri_b", [128, 128], BF16)
            onesm_f = sb("onesm_f", [128, 128], F32)
            er = sb("er", [128, 2, NE], F32)
            run_c = sb("run_c", [128, NE], F32)
            xT_bf = [sb("xT_bf%d" % i, [128, KC, ST], BF16) for i in range(2)]
            qT = sb("qT", [128, AC, ST], BF16)
            kT = sb("kT", [128, 128 + ST], BF16)
            Vr = sb("Vr", [128, 1 + ST // 128, 2, 65], BF16)
            aT = sb("aT", [128, CCH, PADL + ST], BF16)
            sgt = sb("sgt", [128, ST], F32)
            Pp = [sb("Pp%d" % i, [128, GW], BF16) for i in range(2)]
            Pc = [sb("Pc%d" % i, [128, GW], BF16) for i in range(2)]
            den = sb("den", [128, G], F32)
            o_n = sb("o_n", [128, AW], BF16)
            oT = sb("oT", [128, AC, ST], BF16)
            yb = sb("yb", [128, CCH, ST], F32)
            sq = sb("sq", [128, CCH, ST], F32)
            mean_sb = sb("mean_sb", [128, ST], F32)
            var_sb = sb("var_sb", [128, ST], F32)
            tmpc = sb("tmpc", [128, ST], F32)
            sT = sb("sT", [128, CCH, ST], BF16)
            ga = [sb("ga%d" % i, [128, ST], F32) for i in range(2)]
            gb = [sb("gb%d" % i, [128, ST], F32) for i in range(2)]
            t1 = sb("t1", [128, ST], F32)
            t2 = sb("t2", [128, ST], F32)
            mg = sb("mg", [128, KC, ST], BF16)
            xt = [sb("xt%d" % i, [128, D], F32) for i in range(1)]
            z = sb("z", [128, D], F32)
            nrm = sb("nrm", [128, D], F32)
            x1 = [sb("x1_%d" % i, [128, D], F32) for i in range(1)]
            x1b = [sb("x1b%d" % i, [128, D], BF16) for i in range(1)]
            x1T = sb("x1T", [128, KC, 128], F32)
            stt = sb("stt", [128, max(NH2, 1) * 6], F32)
            mv = sb("mv", [128, 2], F32)
            sm = sb("sm", [128, 8], F32)
            lg = sb("lg", [128, NE], F32)
            top8 = sb("top8", [128, 8], F32)
            tidx = sb("tidx", [128, 8], U32)
            ef = sb("ef", [128, 4], F32)
            ex4 = sb("ex4", [128, 4], F32)
            mskb = sb("mskb", [128, NE], BF16)
            pos = sb("pos", [128, NE], F32)
            ovf = sb("ovf", [128, NE], F32)
            junk = sb("junk", [128, NE], F32)
            dest_f = sb("dest_f", [128, 4], F32)

            Bn = {}

            def Bf(n):
                if n not in Bn:
                    Bn[n] = Buf(n)
                return Bn[n]

            dc = S.dsem("c", group=True)
            dw = S.dsem("w", group=True)

            def ld(eng, o_ap, i_ap, bufname, sem=dc, **kw):
                S.op(eng, lambda e: e.dma_start(out=o_ap, in_=i_ap, **kw), writes=[Bf(bufname)], dma_sem=sem)

            ld("sync", cst[:, :, :], consts, "cst")
            ld("sync", er[:, :, :], erow, "er")
            ld("sync", lnbc[:, :, :], lnb[:, 0:2, :], "lnbc")
            ld("sync", bup[:, :, :], b_up_t, "bup")
            ld("sync", w_r_f[:, :, :], w_r.rearrange("(kc p) n -> p kc n", p=128), "w_r")
            ld("sync", b_r_f[:, :], b_r, "b_r")
            ld("sync", bin_t[:, :], b_in_t, "bin")
            ld("sync", esink[:, :], sinks_b, "esink")
            ld("sync", cw[:, :, :], cw_t, "cw")
            ld("sync", cv[:, :, :], cvec, "cv")
            ld("sync", bcb[:, :], bcb_t, "bcb")
            ld("gpsimd", mk[:, :, :], masks, "mk")
            ld("sync", flg[:, :], flag, "flg")
            ld("gpsimd", bv_b[:, :], b_v, "bv")
            ld("gpsimd", bdn_b[:, :], b_dn, "bdn")
            HW = INW // 2
            for kc in range(KC):
                for hh in range(2):
                    ld("gpsimd", w_in_bf[:, kc, hh * HW:(hh + 1) * HW], w_in[kc * 128:(kc + 1) * 128, hh * HW:(hh + 1) * HW], "w_in", sem=dw)
            for ac in range(AC):
                ld("gpsimd", w_ab_bf[:, ac, :], w_ab[ac * 128:(ac + 1) * 128, :], "w_ab", sem=dw)
            for c in range(CCH):
                ld("gpsimd", w_cb_bf[:, c, :], w_cb[c * 128:(c + 1) * 128, :], "w_cb", sem=dw)
            for kc in range(KC):
                ld("gpsimd", w_o_bf[:, kc, :], w_o[kc * 128:(kc + 1) * 128, :], "w_o", sem=dw)

            V = lambda fn, r=(), w=(): S.op("vector", fn, reads=r, writes=w)
            A = lambda fn, r=(), w=(): S.op("scalar", fn, reads=r, writes=w)
            P = lambda fn, r=(), w=(): S.op("gpsimd", fn, reads=r, writes=w)
            TE = lambda fn, r=(), w=(): S.op("tensor", fn, reads=r, writes=w)

            cast_jobs = []
            if not cfg["repl"]:
                dwl = S.dsem("wl", group=True)
                dag = S.dsem("ag", group=True, inc=1)
                for i in range(EPC):
                    for kc in range(KC):
                        cast_jobs.append((0, i, kc))
                        cast_jobs.append((1, i, kc))

            def issue_cast(job, jn):
                which, i, kc = job
                r0 = i * D + kc * 128
                if which == 0:
                    S.op("gpsimd", lambda e: e.dma_start(out=wl_up[r0:r0 + 128, :], in_=w_up[i, kc * 128:(kc + 1) * 128, :]), writes=[Bf("wl")], dma_sem=dwl)
                else:
                    S.op("gpsimd", lambda e: e.dma_start(out=wl_dn[r0:r0 + 128, :], in_=w_dn[i, kc * 128:(kc + 1) * 128, :]), writes=[Bf("wl")], dma_sem=dwl)

            def issue_ag():
                grp = [list(range(cfg["NCORES"]))]
                S.op("gpsimd", lambda e: e.collective_compute("AllGather", ALU.bypass, replica_groups=grp, ins=[wl_up_t.ap().opt()], outs=[wa_up_t.ap().opt()]),
                     reads=[Bf("wl")], writes=[Bf("wa")], dma_sem=dag)
                S.op("gpsimd", lambda e: e.collective_compute("AllGather", ALU.bypass, replica_groups=grp, ins=[wl_dn_t.ap().opt()], outs=[wa_dn_t.ap().opt()]),
                     reads=[Bf("wl")], writes=[Bf("wa")], dma_sem=dag)

            V(lambda e: e.tensor_copy(out=ident_f[:, :], in_=cst[:, 0, :]), [Bf("cst")], [Bf("ident_f")])
            V(lambda e: e.tensor_copy(out=ident_b[:, :], in_=cst[:, 0, :]), [Bf("cst")], [Bf("ident_b")])
            V(lambda e: e.tensor_copy(out=ltri_b[:, :], in_=cst[:, 1, :]), [Bf("cst")], [Bf("ltri")])
            V(lambda e: e.tensor_copy(out=ones_b[:, :], in_=cst[:, 2, :]), [Bf("cst")], [Bf("ones_b")])
            V(lambda e: e.tensor_copy(out=ones_f[:, :], in_=cst[:, 2, :]), [Bf("cst")], [Bf("ones_f")])
            V(lambda e: e.tensor_scalar(out=onesm_f[:, :], in0=cst[:, 2, :], scalar1=1.0 / CC, scalar2=None, op0=ALU.mult),
              [Bf("cst")], [Bf("onesm")])
            V(lambda e: e.tensor_copy(out=selT[:, :, :], in_=cst[0:NE, 0, 0:NE].unsqueeze(2).to_broadcast([NE, NE, 128])), [Bf("cst")], [Bf("selT")])
            A(lambda e: e.activation(out=esink[:, :], in_=esink[:, :], func=AF.Exp), [Bf("esink")], [Bf("esink")])
            P(lambda e: e.memset(Vr[:, :, :, :], 1.0), [], [Bf("Vr")])
            P(lambda e: e.memset(run_c[:, :], 0.0), [], [Bf("run")])
            P(lambda e: e.memset(aT[:, :, :], 0.0), [], [Bf("aT")])
            P(lambda e: e.memset(kT[:, :], 0.0), [], [Bf("kT")])

            rot = [0]

            def nbank(pool=(0, 1, 2, 3)):
                rot[0] += 1
                return pool[rot[0] % len(pool)]

            dx = [S.dsem("x0"), S.dsem("x1")]
            dxt = [S.dsem("xt0"), S.dsem("xt1")]
            dx1 = [S.dsem("x1s0"), S.dsem("x1s1")]
            dsc = [S.dsem("sc0"), S.dsem("sc1")]

            NST = T // ST
            sizes = [128] + [ST] * NST
            tau0 = 0
            def body(s, n, tau0):
                    xs_ = s % 2
                    XB = xT_bf[xs_]
                    BX = Bf("xT_bf%d" % xs_)
                    for kc in range(KC):
                        S.op("gpsimd", lambda e, kc=kc, XB=XB, tau0=tau0, n=n: e.dma_start(
                            out=XB[:, kc, 0:n], in_=xT[kc * 128:(kc + 1) * 128, tau0:tau0 + n]), writes=[BX], dma_sem=dx[xs_])

                    def proj(ch, n=n, XB=XB, BX=BX):
                        b = nbank()
                        for kc in range(KC):
                            TE(lambda e, kc=kc, b=b, ch=ch: e.matmul(ps[b][:, 0:n], lhsT=w_in_bf[:, kc, ch * 128:(ch + 1) * 128],
                                                                     rhs=XB[:, kc, 0:n], start=(kc == 0), stop=(kc == KC - 1)),
                               [Bf("w_in"), BX], [PB[b]])
                        return b

                    if s > 0:
                        for c in range(AC):
                            b = proj(c)
                            A(lambda e, b=b, c=c, n=n: e.activation(out=qT[:, c, 0:n], in_=ps[b][:, 0:n], func=AF.Identity,
                                                                     bias=bin_t[:, c:c + 1], scale=1.0), [PB[b], Bf("bin")], [Bf("qT")])
                    b = proj(AC)
                    A(lambda e, b=b, n=n: e.activation(out=kT[:, 128:128 + n], in_=ps[b][:, 0:n], func=AF.Identity,
                                                       bias=bin_t[:, AC:AC + 1], scale=1.0), [PB[b], Bf("bin")], [Bf("kT")])
                    for bb in range(n // 128):
                        b = nbank()
                        for kc in range(KC):
                            TE(lambda e, kc=kc, b=b, bb=bb, XB=XB: e.matmul(ps[b][:, 0:128], lhsT=XB[:, kc, bb * 128:(bb + 1) * 128],
                                                                            rhs=w_in_bf[:, kc, KE:VE], start=(kc == 0), stop=False),
                               [Bf("w_in"), BX], [PB[b]])
                        TE(lambda e, b=b: e.matmul(ps[b][:, 0:128], lhsT=ones_b[0:1, :], rhs=bv_b[0:1, :], start=False, stop=True),
                           [Bf("ones_b"), Bf("bv")], [PB[b]])
                        A(lambda e, b=b, bb=bb: e.activation(out=Vr[:, 1 + bb, :, 0:64], in_=ps[b][:, 0:128].rearrange("p (g d) -> p g d", g=2), func=AF.Copy),
                          [PB[b]], [Bf("Vr")])
                    for c in range(CCH):
                        bg = proj(AC + 2 + CCH + c)
                        A(lambda e, bg=bg, c=c, n=n: e.activation(out=sgt[:, 0:n], in_=ps[bg][:, 0:n], func=AF.Sigmoid,
                                                                   bias=bin_t[:, AC + 2 + CCH + c:AC + 3 + CCH + c], scale=1.0),
                          [PB[bg], Bf("bin")], [Bf("sgt")])
                        ba = proj(AC + 2 + c)
                        V(lambda e, ba=ba, c=c, n=n: e.scalar_tensor_tensor(out=aT[:, c, PADL:PADL + n], in0=ps[ba][:, 0:n],
                                                                            scalar=bin_t[:, AC + 2 + c:AC + 3 + c], in1=sgt[:, 0:n],
                                                                            op0=ALU.add, op1=ALU.mult),
                          [PB[ba], Bf("bin"), Bf("sgt")], [Bf("aT")])

                    yield "A1"
                    if s > 0:
                        for qb in range(n // 128):
                            gq = (s - 1) * (ST // 128) + qb
                            mprev = 2 if gq == 0 else 1
                            for g in range(2):
                                rows = slice(64 * g, 64 * g + 64)
                                bP, bC, bO = 2 + 2 * g, 3 + 2 * g, 6 + g
                                pp, pc = Pp[g], Pc[g]
                                TE(lambda e, rows=rows, bP=bP, qb=qb: e.matmul(ps[bP][:, 0:GW], lhsT=kT[rows, qb * 128:qb * 128 + 128],
                                                                              rhs=qT[rows, :, qb * 128:(qb + 1) * 128], start=True, stop=True),
                                   [Bf("kT"), Bf("qT")], [PB[bP]])
                                TE(lambda e, rows=rows, bC=bC, qb=qb: e.matmul(ps[bC][:, 0:GW], lhsT=kT[rows, 128 + qb * 128:256 + qb * 128],
                                                                              rhs=qT[rows, :, qb * 128:(qb + 1) * 128], start=True, stop=True),
                                   [Bf("kT"), Bf("qT")], [PB[bC]])
                                A(lambda e, bP=bP, pp=pp: e.activation(out=pp[:, :], in_=ps[bP][:, 0:GW], func=AF.Exp, scale=0.125),
                                  [PB[bP]], [Bf("Pp%d" % g)])
                                A(lambda e, bC=bC, pc=pc: e.activation(out=pc[:, :], in_=ps[bC][:, 0:GW], func=AF.Exp, scale=0.125),
                                  [PB[bC]], [Bf("Pc%d" % g)])
                                P(lambda e, pp=pp, mprev=mprev: e.tensor_tensor(out=pp[:, :], in0=pp[:, :], in1=mk[:, mprev, :], op=ALU.mult),
                                  [Bf("Pp%d" % g), Bf("mk")], [Bf("Pp%d" % g)])
                                P(lambda e, pc=pc: e.tensor_tensor(out=pc[:, :], in0=pc[:, :], in1=mk[:, 0, :], op=ALU.mult),
                                  [Bf("Pc%d" % g), Bf("mk")], [Bf("Pc%d" % g)])
                                for c in range(G):
                                    TE(lambda e, c=c, bO=bO, pp=pp, qb=qb, g=g: e.matmul(ps[bO][:, c * 65:(c + 1) * 65], lhsT=pp[:, c * 128:(c + 1) * 128],
                                                                                        rhs=Vr[:, qb, g, :], start=True, stop=False),
                                       [Bf("Pp%d" % g), Bf("Vr")], [PB[bO]])
                                    TE(lambda e, c=c, bO=bO, pc=pc, qb=qb, g=g: e.matmul(ps[bO][:, c * 65:(c + 1) * 65], lhsT=pc[:, c * 128:(c + 1) * 128],
                                                                                        rhs=Vr[:, qb + 1, g, :], start=False, stop=True),
                                       [Bf("Pc%d" % g), Bf("Vr")], [PB[bO]])
                                o3 = ps[bO][:, 0:G * 65].rearrange("p (c d) -> p c d", c=G)
                                V(lambda e, o3=o3, g=g: e.tensor_tensor(out=den[:, :], in0=o3[:, :, 64], in1=esink[:, g * G:(g + 1) * G], op=ALU.add),
                                  [PB[bO], Bf("esink")], [Bf("den")])
                                V(lambda e: e.reciprocal(out=den[:, :], in_=den[:, :]), [Bf("den")], [Bf("den")])
                                V(lambda e, o3=o3, g=g: e.tensor_tensor(out=o_n[:, g * G * 64:(g + 1) * G * 64].rearrange("p (c d) -> p c d", c=G),
                                                                        in0=o3[:, :, 0:64], in1=den[:, :].unsqueeze(2).to_broadcast([128, G, 64]), op=ALU.mult),
                                  [PB[bO], Bf("den")], [Bf("o_n")])
                            b = nbank((0, 1))
                            pbf = ps[b][:, :].bitcast(BF16)
                            for ac in range(AC):
                                TE(lambda e, ac=ac, pbf=pbf: e.transpose(out=pbf[:, ac * 128:(ac + 1) * 128], in_=o_n[:, ac * 128:(ac + 1) * 128], identity=ident_b[:, :]),
                                   [Bf("o_n"), Bf("ident_b")], [PB[b]])
                            A(lambda e, pbf=pbf, qb=qb: e.activation(out=oT[:, :, qb * 128:(qb + 1) * 128], in_=pbf[:, 0:AC * 128].rearrange("p (a t) -> p a t", a=AC),
                                                                     func=AF.Copy), [PB[b]], [Bf("oT")])
                    yield "A2"
                    if s > 0:
                        for c in range(CCH):
                            b = nbank((0, 1))
                            dg = diag2[c % 2]
                            for j in range(31):
                                eng_ = V if j % 3 == 0 else P
                                eng_(lambda e, j=j, c=c, dg=dg: e.tensor_scalar(out=dg[:, j, :], in0=cst[:, 0, :], scalar1=cw[:, c, j:j + 1],
                                                                                scalar2=1.0, op0=ALU.mult, op1=ALU.mult), [Bf("cst"), Bf("cw")], [Bf("diag%d_%d" % (c % 2, j))])
                            for j in range(31):
                                TE(lambda e, j=j, c=c, b=b, n=n, dg=dg: e.matmul(ps[b][:, 0:n], lhsT=dg[:, j, :], rhs=aT[:, c, PADL - 30 + j:PADL - 30 + j + n],
                                                                                 start=(j == 0), stop=(j == 30)), [Bf("diag%d_%d" % (c % 2, j)), Bf("aT")], [PB[b]])
                            A(lambda e, c=c, b=b, n=n: e.activation(out=yb[:, c, 0:n], in_=ps[b][:, 0:n], func=AF.Identity, bias=cv[:, 0, c:c + 1], scale=1.0),
                              [PB[b], Bf("cv")], [Bf("yb")])
                            A(lambda e, c=c, b=b, n=n: e.activation(out=sq[:, c, 0:n], in_=ps[b][:, 0:n], func=AF.Square, bias=cv[:, 0, c:c + 1], scale=1.0),
                              [PB[b], Bf("cv")], [Bf("sq")])
                        for c in range(CCH):
                            TE(lambda e, c=c, n=n: e.matmul(ps[2][:, 0:n], lhsT=onesm_f[:, :], rhs=yb[:, c, 0:n], start=(c == 0), stop=(c == CCH - 1)),
                               [Bf("onesm"), Bf("yb")], [PB[2]])
                        for c in range(CCH):
                            TE(lambda e, c=c, n=n: e.matmul(ps[3][:, 0:n], lhsT=onesm_f[:, :], rhs=sq[:, c, 0:n], start=(c == 0), stop=(c == CCH - 1)),
                               [Bf("onesm"), Bf("sq")], [PB[3]])
                        A(lambda e, n=n: e.activation(out=mean_sb[:, 0:n], in_=ps[2][:, 0:n], func=AF.Copy), [PB[2]], [Bf("mean")])
                        V(lambda e, n=n: e.tensor_tensor(out=var_sb[:, 0:n], in0=mean_sb[:, 0:n], in1=mean_sb[:, 0:n], op=ALU.mult), [Bf("mean")], [Bf("var")])
                        V(lambda e, n=n: e.tensor_tensor(out=var_sb[:, 0:n], in0=ps[3][:, 0:n], in1=var_sb[:, 0:n], op=ALU.subtract), [PB[3], Bf("var")], [Bf("var")])
                        V(lambda e, n=n: e.tensor_scalar(out=var_sb[:, 0:n], in0=var_sb[:, 0:n], scalar1=EPS, scalar2=None, op0=ALU.add), [Bf("var")], [Bf("var")])
                        A(lambda e, n=n: e.activation(out=var_sb[:, 0:n], in_=var_sb[:, 0:n], func=AF.Sqrt), [Bf("var")], [Bf("var")])
                        V(lambda e, n=n: e.reciprocal(out=var_sb[:, 0:n], in_=var_sb[:, 0:n]), [Bf("var")], [Bf("var")])
                        for c in range(CCH):
                            V(lambda e, c=c, n=n: e.tensor_tensor(out=tmpc[:, 0:n], in0=yb[:, c, 0:n], in1=mean_sb[:, 0:n], op=ALU.subtract),
                              [Bf("yb"), Bf("mean")], [Bf("tmpc")])
                            V(lambda e, n=n: e.tensor_tensor(out=tmpc[:, 0:n], in0=tmpc[:, 0:n], in1=var_sb[:, 0:n], op=ALU.mult),
                              [Bf("tmpc"), Bf("var")], [Bf("tmpc")])
                            A(lambda e, c=c, n=n: e.activation(out=sT[:, c, 0:n], in_=tmpc[:, 0:n], func=AF.Silu, scale=cv[:, 1, c:c + 1], bias=cv[:, 2, c:c + 1]),
                              [Bf("tmpc"), Bf("cv")], [Bf("sT")])
                    P(lambda e, n=n: e.tensor_copy(out=kT[:, 0:128], in_=kT[:, n:n + 128]), [Bf("kT")], [Bf("kT")])
                    P(lambda e, n=n: e.tensor_copy(out=Vr[:, 0, :, 0:64], in_=Vr[:, n // 128, :, 0:64]), [Bf("Vr")], [Bf("Vr")])
                    P(lambda e, n=n: e.tensor_copy(out=aT[:, :, 0:PADL], in_=aT[:, :, n:n + PADL]), [Bf("aT")], [Bf("aT")])
                    if s == 0:
                        P(lambda e: e.tensor_scalar(out=aT[:, :, 0:PADL], in0=aT[:, :, 0:PADL], scalar1=flg[:, 0:1], scalar2=None, op0=ALU.mult),
                          [Bf("aT"), Bf("flg")], [Bf("aT")])
                    yield "A"
                    if s == 0:
                        return
                    for j in range(KC):
                        bs = (0, 1, 2, 3) if j % 2 == 0 else (4, 5, 6, 7)
                        bA, bGA, bB, bGB = bs
                        gaj, gbj = ga[j % 2], gb[j % 2]
                        for ac in range(AC):
                            TE(lambda e, ac=ac, j=j, bA=bA, n=n: e.matmul(ps[bA][:, 0:n], lhsT=w_ab_bf[:, ac, j * 128:(j + 1) * 128], rhs=oT[:, ac, 0:n],
                                                                          start=(ac == 0), stop=(ac == AC - 1)), [Bf("w_ab"), Bf("oT")], [PB[bA]])
                        for kc in range(KC):
                            TE(lambda e, kc=kc, j=j, bGA=bGA, n=n, XB=XB: e.matmul(ps[bGA][:, 0:n], lhsT=w_in_bf[:, kc, CE + j * 128:CE + (j + 1) * 128], rhs=XB[:, kc, 0:n],
                                                                                  start=(kc == 0), stop=(kc == KC - 1)), [Bf("w_in"), BX], [PB[bGA]])
                        for c in range(CCH):
                            TE(lambda e, c=c, j=j, bB=bB, n=n: e.matmul(ps[bB][:, 0:n], lhsT=w_cb_bf[:, c, j * 128:(j + 1) * 128], rhs=sT[:, c, 0:n],
                                                                        start=(c == 0), stop=(c == CCH - 1)), [Bf("w_cb"), Bf("sT")], [PB[bB]])
                        for kc in range(KC):
                            TE(lambda e, kc=kc, j=j, bGB=bGB, n=n, XB=XB: e.matmul(ps[bGB][:, 0:n], lhsT=w_in_bf[:, kc, CE + D + j * 128:CE + D + (j + 1) * 128], rhs=XB[:, kc, 0:n],
                                                                                  start=(kc == 0), stop=(kc == KC - 1)), [Bf("w_in"), BX], [PB[bGB]])
                        ua = CE // 128 + j
                        ub = CE // 128 + KC + j
                        A(lambda e, bGA=bGA, gaj=gaj, ua=ua, n=n: e.activation(out=gaj[:, 0:n], in_=ps[bGA][:, 0:n], func=AF.Sigmoid, bias=bin_t[:, ua:ua + 1], scale=1.0),
                          [PB[bGA], Bf("bin")], [Bf("ga%d" % (j % 2))])
                        A(lambda e, bGB=bGB, gbj=gbj, ub=ub, n=n: e.activation(out=gbj[:, 0:n], in_=ps[bGB][:, 0:n], func=AF.Sigmoid, bias=bin_t[:, ub:ub + 1], scale=1.0),
                          [PB[bGB], Bf("bin")], [Bf("gb%d" % (j % 2))])
                        V(lambda e, bA=bA, gaj=gaj, n=n: e.tensor_tensor(out=t1[:, 0:n], in0=ps[bA][:, 0:n], in1=gaj[:, 0:n], op=ALU.mult),
                          [PB[bA], Bf("ga%d" % (j % 2))], [Bf("t1")])
                        V(lambda e, bB=bB, gbj=gbj, j=j, n=n: e.scalar_tensor_tensor(out=t2[:, 0:n], in0=ps[bB][:, 0:n], scalar=bcb[:, j:j + 1], in1=gbj[:, 0:n],
                                                                                    op0=ALU.add, op1=ALU.mult), [PB[bB], Bf("gb%d" % (j % 2)), Bf("bcb")], [Bf("t2")])
                        V(lambda e, j=j, n=n: e.tensor_tensor(out=mg[:, j, 0:n], in0=t1[:, 0:n], in1=t2[:, 0:n], op=ALU.add), [Bf("t1"), Bf("t2")], [Bf("mg")])
                    yield "B"
                    for bb in range(n // 128):
                        blk = (s - 1) * (ST // 128) + bb
                        t0 = blk * 128
                        sl = 0
                        xtt, x1t, x1bt = xt[sl], x1[sl], x1b[sl]
                        S.op("sync", lambda e, xtt=xtt, t0=t0: e.dma_start(out=xtt[:, :], in_=xtok[t0:t0 + 128, :]), writes=[Bf("xt%d" % sl)], dma_sem=dxt[sl])
                        zb = (0, 1) if blk % 2 == 0 else (2, 3)
                        for h in range(NH2):
                            b = zb[h % 2]
                            for kc in range(KC):
                                TE(lambda e, kc=kc, b=b, h=h, bb=bb: e.matmul(ps[b][:, 0:W2], lhsT=mg[:, kc, bb * 128:(bb + 1) * 128], rhs=w_o_bf[:, kc, h * W2:(h + 1) * W2],
                                                                            start=(kc == 0), stop=(kc == KC - 1)), [Bf("mg"), Bf("w_o")], [PB[b]])
                            V(lambda e, b=b, h=h, xtt=xtt: e.scalar_tensor_tensor(out=z[:, h * W2:(h + 1) * W2], in0=xtt[:, h * W2:(h + 1) * W2], scalar=alpha,
                                                                                 in1=ps[b][:, 0:W2], op0=ALU.mult, op1=ALU.add), [PB[b], Bf("xt%d" % sl)], [Bf("z")])

                        def layer_norm(src, srcB, dst, dstB, gi):
                            for h in range(NH2):
                                V(lambda e, h=h: e.bn_stats(out=stt[:, h * 6:(h + 1) * 6], in_=src[:, h * W2:(h + 1) * W2]), [srcB], [Bf("stt")])
                            V(lambda e: e.bn_aggr(out=mv[:, :], in_=stt[:, 0:NH2 * 6]), [Bf("stt")], [Bf("mv")])
                            V(lambda e: e.tensor_scalar(out=sm[:, 0:1], in0=mv[:, 1:2], scalar1=EPS, scalar2=None, op0=ALU.add), [Bf("mv")], [Bf("sm")])
                            A(lambda e: e.activation(out=sm[:, 1:2], in_=sm[:, 0:1], func=AF.Sqrt), [Bf("sm")], [Bf("sm")])
                            V(lambda e: e.reciprocal(out=sm[:, 2:3], in_=sm[:, 1:2]), [Bf("sm")], [Bf("sm")])
                            V(lambda e: e.scalar_tensor_tensor(out=sm[:, 3:4], in0=mv[:, 0:1], scalar=-1.0, in1=sm[:, 2:3], op0=ALU.mult, op1=ALU.mult),
                              [Bf("mv"), Bf("sm")], [Bf("sm")])
                            A(lambda e: e.activation(out=nrm[:, :], in_=src[:, :], func=AF.Identity, scale=sm[:, 2:3], bias=sm[:, 3:4]), [srcB, Bf("sm")], [Bf("nrm")])
                            P(lambda e: e.tensor_tensor(out=nrm[:, :], in0=nrm[:, :], in1=lnbc[:, gi, :], op=ALU.mult), [Bf("nrm"), Bf("lnbc")], [Bf("nrm")])
                            P(lambda e: e.tensor_tensor(out=dst[:, :], in0=nrm[:, :], in1=lnbc[:, gi + 1, :], op=ALU.add), [Bf("nrm"), Bf("lnbc")], [dstB])

                        layer_norm(z, Bf("z"), x1t, Bf("x1_%d" % sl), 0)
                        S.op("sync", lambda e, x1t=x1t, t0=t0: e.dma_start(out=x1_d[t0:t0 + 128, :], in_=x1t[:, :]), reads=[Bf("x1_%d" % sl)], writes=[B_x1d], dma_sem=dx1[sl])
                        A(lambda e, x1t=x1t, x1bt=x1bt: e.activation(out=x1bt[:, :], in_=x1t[:, :], func=AF.Copy), [Bf("x1_%d" % sl)], [Bf("x1b%d" % sl)])
                        yield "C1"
                        for kc in range(KC):
                            b = 4 + (kc // 4) % 2
                            TE(lambda e, kc=kc, b=b, x1t=x1t: e.transpose(out=ps[b][:, (kc % 4) * 128:(kc % 4 + 1) * 128], in_=x1t[:, kc * 128:(kc + 1) * 128], identity=ident_f[:, :]),
                               [Bf("x1_%d" % sl), Bf("ident_f")], [PB[b]])
                            if kc % 4 == 3 or kc == KC - 1:
                                k0 = (kc // 4) * 4
                                nk = kc - k0 + 1
                                A(lambda e, b=b, k0=k0, nk=nk: e.activation(out=x1T[:, k0:k0 + nk, :], in_=ps[b][:, 0:nk * 128].rearrange("p (k t) -> p k t", k=nk), func=AF.Copy),
                                  [PB[b]], [Bf("x1T")])
                        for kc in range(KC):
                            TE(lambda e, kc=kc: e.matmul(ps[6][:, 0:NE], lhsT=x1T[:, kc, :], rhs=w_r_f[:, kc, :], start=(kc == 0), stop=False),
                               [Bf("x1T"), Bf("w_r")], [PB[6]])
                        TE(lambda e: e.matmul(ps[6][:, 0:NE], lhsT=ones_f[0:1, :], rhs=b_r_f[0:1, :], start=False, stop=True), [Bf("ones_f"), Bf("b_r")], [PB[6]])
                        V(lambda e: e.tensor_copy(out=lg[:, :], in_=ps[6][:, 0:NE]), [PB[6]], [Bf("lg")])
                        V(lambda e: e.max(out=top8[:, :], in_=lg[:, :]), [Bf("lg")], [Bf("top8")])
                        V(lambda e: e.max_index(out=tidx[:, :], in_max=top8[:, :], in_values=lg[:, :]), [Bf("lg"), Bf("top8")], [Bf("tidx")])
                        V(lambda e: e.tensor_scalar(out=sm[:, 4:5], in0=top8[:, 0:1], scalar1=-1.0, scalar2=None, op0=ALU.mult), [Bf("top8")], [Bf("sm2")])
                        A(lambda e: e.activation(out=ex4[:, :], in_=top8[:, 0:4], func=AF.Exp, bias=sm[:, 4:5], scale=1.0), [Bf("top8"), Bf("sm2")], [Bf("ex4")])
                        V(lambda e: e.tensor_reduce(out=sm[:, 5:6], in_=ex4[:, :], axis=mybir.AxisListType.X, op=ALU.add), [Bf("ex4")], [Bf("sm3")])
                        V(lambda e: e.reciprocal(out=sm[:, 6:7], in_=sm[:, 5:6]), [Bf("sm3")], [Bf("sm3")])
                        V(lambda e, blk=blk: e.tensor_scalar(out=gate[:, blk, :], in0=ex4[:, :], scalar1=sm[:, 6:7], scalar2=None, op0=ALU.mult),
                          [Bf("ex4"), Bf("sm3")], [B_gate])
                        V(lambda e: e.tensor_scalar(out=mskb[:, :], in0=lg[:, :], scalar1=top8[:, 3:4], scalar2=None, op0=ALU.is_ge), [Bf("lg"), Bf("top8")], [Bf("mskb")])
                        yield "C2"
                        TE(lambda e: e.matmul(ps[7][:, 0:NE], lhsT=ltri_b[:, :], rhs=mskb[:, :], start=True, stop=True), [Bf("ltri"), Bf("mskb")], [PB[7]])
                        TE(lambda e: e.matmul(ps[7][:, 64:64 + NE], lhsT=ones_b[:, :], rhs=mskb[:, :], start=True, stop=True), [Bf("ones_b"), Bf("mskb")], [PB[7]])
                        V(lambda e: e.tensor_tensor(out=pos[:, :], in0=ps[7][:, 0:NE], in1=run_c[:, :], op=ALU.add), [PB[7], Bf("run")], [Bf("pos")])
                        V(lambda e: e.tensor_tensor(out=run_c[:, :], in0=ps[7][:, 64:64 + NE], in1=run_c[:, :], op=ALU.add), [PB[7], Bf("run")], [Bf("run")])
                        V(lambda e: e.tensor_scalar(out=ovf[:, :], in0=pos[:, :], scalar1=float(CAP), scalar2=BIG, op0=ALU.is_ge, op1=ALU.mult), [Bf("pos")], [Bf("ovf")])
                        V(lambda e: e.tensor_tensor(out=pos[:, :], in0=pos[:, :], in1=er[:, 1, :], op=ALU.add), [Bf("pos"), Bf("er")], [Bf("pos")])
                        V(lambda e: e.tensor_tensor(out=pos[:, :], in0=pos[:, :], in1=ovf[:, :], op=ALU.add), [Bf("pos"), Bf("ovf")], [Bf("pos")])
                        V(lambda e: e.tensor_copy(out=ef[:, :], in_=tidx[:, 0:4]), [Bf("tidx")], [Bf("ef")])
                        for k in range(4):
                            V(lambda e, k=k: e.scalar_tensor_tensor(out=junk[:, :], in0=er[:, 0, :], scalar=ef[:, k:k + 1], in1=pos[:, :], op0=ALU.is_equal, op1=ALU.mult,
                                                                   accum_out=dest_f[:, k:k + 1]), [Bf("er"), Bf("ef"), Bf("pos")], [Bf("junk"), Bf("dest_f")])
                        V(lambda e, blk=blk: e.tensor_copy(out=dest_i[:, blk, :], in_=dest_f[:, :]), [Bf("dest_f")], [B_dest])
                        for k in range(4):
                            S.op("gpsimd", lambda e, k=k, blk=blk, x1bt=x1bt: e.indirect_dma_start(
                                out=xs_d[:, :], out_offset=bass.IndirectOffsetOnAxis(ap=dest_i[:, blk, k:k + 1], axis=0), in_=x1bt[:, :], in_offset=None,
                                bounds_check=breg(e, 1), oob_is_err=False), reads=[Bf("x1b%d" % sl), B_dest], writes=[], dma_sem=dsc[sl])

            gens = []
            tau0 = 0
            for s, n in enumerate(sizes):
                gens.append(body(s, n, tau0))
                tau0 += n

            def adv(i):
                if 0 <= i < len(gens):
                    try:
                        next(gens[i])
                    except StopIteration:
                        pass

            for s in range(len(sizes) + 3):
                adv(s)
                adv(s - 2)
                adv(s - 1)
                adv(s)
                adv(s - 1)
                adv(s)
                adv(s - 1)
                adv(s)
                adv(s - 1)
                if cast_jobs and s < len(sizes):
                    ncast = len(cast_jobs)
                    nsp = min(4, NST)
                    per = (ncast + nsp - 1) // nsp
                    if 1 <= s <= nsp:
                        for jn in range((s - 1) * per, min(s * per, ncast)):
                            issue_cast(cast_jobs[jn], jn)
                    if s == min(6, len(sizes) - 1):
                        issue_ag()
            for g_ in gens:
                for _ in g_:
                    pass
            S.emit()

        S = Sched(nc, "e")
        PB = [Buf("ps%d" % i) for i in range(8)]
        with ExitStack() as st:
            def sb(name, shape, dt):
                return st.enter_context(nc.sbuf_tensor(name, list(shape), dt))

            wu = [sb("wu%d" % i, [128, KC, 2 * D], BF16) for i in range(2)]
            wd = [sb("wd%d" % i, [128, FC, D], BF16) for i in range(2)]
            xs_t = [sb("xs_t%d" % i, [128, NB, D], BF16) for i in range(2)]
            xsT = [sb("xsT%d" % i, [128, KC, CAP], BF16) for i in range(2)]
            actT = [sb("actT%d" % i, [128, FC, CAP], BF16) for i in range(2)]
            xg = [sb("xg%d" % i, [128, CAP], F32) for i in range(2)]
            sg = [sb("sg%d" % i, [128, CAP], F32) for i in range(2)]
            xl = [sb("xl%d" % i, [128, CAP], F32) for i in range(2)]
            ys_t = [sb("ys_t%d" % i, [128, D], F32) for i in range(2)]
            Bn = {}

            def Bf(n):
                if n not in Bn:
                    Bn[n] = Buf(n)
                return Bn[n]

            V = lambda fn, r=(), w=(): S.op("vector", fn, reads=r, writes=w)
            A = lambda fn, r=(), w=(): S.op("scalar", fn, reads=r, writes=w)
            P = lambda fn, r=(), w=(): S.op("gpsimd", fn, reads=r, writes=w)
            TE = lambda fn, r=(), w=(): S.op("tensor", fn, reads=r, writes=w)
            dwu = [S.dsem("wu0"), S.dsem("wu1")]
            dwd = [S.dsem("wd0"), S.dsem("wd1")]
            dxs = [S.dsem("xs0"), S.dsem("xs1")]
            dys = [S.dsem("ys0"), S.dsem("ys1")]

            def load_w(e_):
                sl = e_ % 2
                if not cfg["repl"]:
                    S.op("sync", lambda e, sl=sl, e_=e_: e.dma_start(out=wu[sl][:, :, :], in_=wa_up[e_ * D:(e_ + 1) * D, :].rearrange("(kc p) f -> p kc f", p=128)),
                         writes=[Bf("wu%d" % sl)], dma_sem=dwu[sl])
                    S.op("sync", lambda e, sl=sl, e_=e_: e.dma_start(out=wd[sl][:, :, :], in_=wa_dn[e_ * D:(e_ + 1) * D, :].rearrange("(kc p) f -> p kc f", p=128)),
                         writes=[Bf("wd%d" % sl)], dma_sem=dwd[sl])
                    return
                nsp = 2 if KC >= 2 else 1
                kh = KC // nsp
                for h in range(nsp):
                    S.op("gpsimd", lambda e, h=h, sl=sl, e_=e_: e.dma_start(out=wu[sl][:, h * kh:(h + 1) * kh, :],
                                                                          in_=w_up[e_, h * kh * 128:(h + 1) * kh * 128, :].rearrange("(kc p) f -> p kc f", p=128)),
                         writes=[Bf("wu%d" % sl)], dma_sem=dwu[sl])
                S.op("gpsimd", lambda e, sl=sl, e_=e_: e.dma_start(out=wd[sl][:, :, :], in_=w_dn[e_, :, :].rearrange("(fc p) d -> p fc d", p=128)),
                     writes=[Bf("wd%d" % sl)], dma_sem=dwd[sl])

            def load_xs(e_):
                sl = e_ % 2
                S.op("sync", lambda e, sl=sl, e_=e_: e.dma_start(out=xs_t[sl][:, :, :], in_=xs_d[e_ * CAP:(e_ + 1) * CAP, :].rearrange("(b p) d -> p b d", p=128)),
                     reads=[B_xs], writes=[Bf("xs_t%d" % sl)], dma_sem=dxs[sl])

            load_w(0)
            load_xs(0)
            cnt = [0]
            for e_ in range(NE):
                sl = e_ % 2
                if e_ + 1 < NE:
                    load_w(e_ + 1)
                    load_xs(e_ + 1)
                Bwu, Bwd = Bf("wu%d" % sl), Bf("wd%d" % sl)
                for nb in range(NB):
                    b = 6 + nb % 2
                    pbf = ps[b][:, :].bitcast(BF16)
                    for kc in range(KC):
                        TE(lambda e, kc=kc, nb=nb, pbf=pbf, sl=sl: e.transpose(out=pbf[:, kc * 128:(kc + 1) * 128], in_=xs_t[sl][:, nb, kc * 128:(kc + 1) * 128], identity=ident_b[:, :]),
                           [Bf("xs_t%d" % sl)], [PB[b]])
                    cp = A if nb % 2 == 0 else V
                    if nb % 2 == 0:
                        A(lambda e, pbf=pbf, nb=nb, sl=sl: e.activation(out=xsT[sl][:, :, nb * 128:(nb + 1) * 128], in_=pbf[:, 0:KC * 128].rearrange("p (k t) -> p k t", k=KC), func=AF.Copy),
                          [PB[b]], [Bf("xsT%d" % sl)])
                    else:
                        V(lambda e, pbf=pbf, nb=nb, sl=sl: e.tensor_copy(out=xsT[sl][:, :, nb * 128:(nb + 1) * 128], in_=pbf[:, 0:KC * 128].rearrange("p (k t) -> p k t", k=KC)),
                          [PB[b]], [Bf("xsT%d" % sl)])
                for fp in range(FC):
                    cnt[0] += 1
                    pr = cnt[0] % 2
                    bG, bL = (0, 1) if pr == 0 else (2, 3)
                    for kc in range(KC):
                        TE(lambda e, kc=kc, fp=fp, bG=bG, sl=sl: e.matmul(ps[bG][:, 0:CAP], lhsT=wu[sl][:, kc, fp * 128:(fp + 1) * 128], rhs=xsT[sl][:, kc, :],
                                                                         start=(kc == 0), stop=(kc == KC - 1)), [Bwu, Bf("xsT%d" % sl)], [PB[bG]])
                    for kc in range(KC):
                        TE(lambda e, kc=kc, fp=fp, bL=bL, sl=sl: e.matmul(ps[bL][:, 0:CAP], lhsT=wu[sl][:, kc, D + fp * 128:D + (fp + 1) * 128], rhs=xsT[sl][:, kc, :],
                                                                         start=(kc == 0), stop=(kc == KC - 1)), [Bwu, Bf("xsT%d" % sl)], [PB[bL]])
                    V(lambda e, bG=bG, e_=e_, fp=fp, pr=pr: e.tensor_scalar(out=xg[pr][:, :], in0=ps[bG][:, 0:CAP], scalar1=bup[:, e_, fp:fp + 1], scalar2=7.0, op0=ALU.add, op1=ALU.min),
                      [PB[bG]], [Bf("xg%d" % pr)])
                    A(lambda e, pr=pr: e.activation(out=sg[pr][:, :], in_=xg[pr][:, :], func=AF.Sigmoid, scale=1.702), [Bf("xg%d" % pr)], [Bf("sg%d" % pr)])
                    V(lambda e, bL=bL, e_=e_, fp=fp, pr=pr: e.tensor_scalar(out=xl[pr][:, :], in0=ps[bL][:, 0:CAP], scalar1=bup[:, e_, FC + fp:FC + fp + 1], scalar2=7.0, op0=ALU.add, op1=ALU.min),
                      [PB[bL]], [Bf("xl%d" % pr)])
                    P(lambda e, pr=pr: e.tensor_scalar(out=xl[pr][:, :], in0=xl[pr][:, :], scalar1=7.0, scalar2=-7.0, op0=ALU.min, op1=ALU.max), [Bf("xl%d" % pr)], [Bf("xl%d" % pr)])
                    P(lambda e, pr=pr: e.tensor_tensor(out=xg[pr][:, :], in0=xg[pr][:, :], in1=sg[pr][:, :], op=ALU.mult), [Bf("xg%d" % pr), Bf("sg%d" % pr)], [Bf("xg%d" % pr)])
                    V(lambda e, pr=pr, fp=fp, sl=sl: e.scalar_tensor_tensor(out=actT[sl][:, fp, :], in0=xl[pr][:, :], scalar=1.0, in1=xg[pr][:, :], op0=ALU.add, op1=ALU.mult),
                      [Bf("xg%d" % pr), Bf("xl%d" % pr)], [Bf("actT%d" % sl)])
                for nb in range(NB):
                    ysl = (e_ * NB + nb) % 2
                    for h in range(NH2):
                        b = 4 + h % 2
                        for fc in range(FC):
                            TE(lambda e, fc=fc, nb=nb, h=h, b=b, sl=sl: e.matmul(ps[b][:, 0:W2], lhsT=actT[sl][:, fc, nb * 128:(nb + 1) * 128], rhs=wd[sl][:, fc, h * W2:(h + 1) * W2],
                                                                                start=(fc == 0), stop=False), [Bf("actT%d" % sl), Bwd], [PB[b]])
                        TE(lambda e, h=h, b=b, e_=e_: e.matmul(ps[b][:, 0:W2], lhsT=selT[:, e_, :], rhs=bdn_b[:, h * W2:(h + 1) * W2], start=False, stop=True),
                           [], [PB[b]])
                        if h % 2 == 0:
                            A(lambda e, b=b, h=h, ysl=ysl: e.activation(out=ys_t[ysl][:, h * W2:(h + 1) * W2], in_=ps[b][:, 0:W2], func=AF.Copy), [PB[b]], [Bf("ys_t%d" % ysl)])
                        else:
                            V(lambda e, b=b, h=h, ysl=ysl: e.tensor_copy(out=ys_t[ysl][:, h * W2:(h + 1) * W2], in_=ps[b][:, 0:W2]), [PB[b]], [Bf("ys_t%d" % ysl)])
                    r0 = e_ * CAP + nb * 128
                    S.op("sync", lambda e, ysl=ysl, r0=r0: e.dma_start(out=ys_d[r0:r0 + 128, :], in_=ys_t[ysl][:, :]), reads=[Bf("ys_t%d" % ysl)], writes=[], dma_sem=dys[ysl])
            S.emit()

        S = Sched(nc, "c")
        with ExitStack() as st:
            def sb(name, shape, dt):
                return st.enter_context(nc.sbuf_tensor(name, list(shape), dt))

            x1c = [sb("x1c%d" % i, [128, D], F32) for i in range(2)]
            yk = [[sb("yk%d_%d" % (i, k), [128, D], F32) for k in range(4)] for i in range(2)]
            accs = [sb("acc%d" % i, [128, D], F32) for i in range(2)]
            nrms = [sb("nrm2_%d" % i, [128, D], F32) for i in range(2)]
            res = [sb("res%d" % i, [128, D], F32) for i in range(2)]
            stt = sb("stt2", [128, max(NH2, 1) * 6], F32)
            mv = sb("mv2", [128, 2], F32)
            sm = sb("sm2", [128, 8], F32)
            Bn = {}

            def Bf(n):
                if n not in Bn:
                    Bn[n] = Buf(n)
                return Bn[n]

            V = lambda fn, r=(), w=(): S.op("vector", fn, reads=r, writes=w)
            A = lambda fn, r=(), w=(): S.op("scalar", fn, reads=r, writes=w)
            P = lambda fn, r=(), w=(): S.op("gpsimd", fn, reads=r, writes=w)
            S.op("sync", lambda e: e.dma_start(out=lnbc[:, :, :], in_=lnb[:, 2:4, :]), writes=[Bf("lnbc")], dma_sem=S.dsem("ln2", group=True))
            dxc = [S.dsem("xc0"), S.dsem("xc1")]
            dyk = [S.dsem("yk0"), S.dsem("yk1")]
            dout = [S.dsem("o0"), S.dsem("o1")]
            for blk in range(NBLK):
                sl = blk % 2
                t0 = blk * 128
                S.op("sync", lambda e, sl=sl, t0=t0: e.dma_start(out=x1c[sl][:, :], in_=x1_d[t0:t0 + 128, :]), writes=[Bf("x1c%d" % sl)], dma_sem=dxc[sl])
                for k in range(4):
                    S.op("gpsimd", lambda e, k=k, sl=sl, blk=blk: e.indirect_dma_start(
                        out=yk[sl][k][:, :], out_offset=None, in_=ys_d[:, :], in_offset=bass.IndirectOffsetOnAxis(ap=dest_i[:, blk, k:k + 1], axis=0),
                        bounds_check=breg(e, 3), oob_is_err=False), writes=[Bf("yk%d_%d" % (sl, k))], dma_sem=dyk[sl])
                acc, nrm = accs[sl], nrms[sl]
                BA, BN = Bf("acc%d" % sl), Bf("nrm%d" % sl)
                V(lambda e, sl=sl, acc=acc: e.tensor_scalar(out=acc[:, :], in0=x1c[sl][:, :], scalar1=alpha, scalar2=None, op0=ALU.mult), [Bf("x1c%d" % sl)], [BA])
                for k in range(4):
                    V(lambda e, k=k, sl=sl, blk=blk, acc=acc: e.scalar_tensor_tensor(out=acc[:, :], in0=yk[sl][k][:, :], scalar=gate[:, blk, k:k + 1], in1=acc[:, :], op0=ALU.mult, op1=ALU.add),
                      [Bf("yk%d_%d" % (sl, k)), BA], [BA])
                for h in range(NH2):
                    V(lambda e, h=h, acc=acc: e.bn_stats(out=stt[:, h * 6:(h + 1) * 6], in_=acc[:, h * W2:(h + 1) * W2]), [BA], [Bf("stt")])
                V(lambda e: e.bn_aggr(out=mv[:, :], in_=stt[:, 0:NH2 * 6]), [Bf("stt")], [Bf("mv")])
                V(lambda e: e.tensor_scalar(out=sm[:, 0:1], in0=mv[:, 1:2], scalar1=EPS, scalar2=None, op0=ALU.add), [Bf("mv")], [Bf("sm")])
                A(lambda e: e.activation(out=sm[:, 1:2], in_=sm[:, 0:1], func=AF.Sqrt), [Bf("sm")], [Bf("sm")])
                V(lambda e: e.reciprocal(out=sm[:, 2:3], in_=sm[:, 1:2]), [Bf("sm")], [Bf("sm")])
                V(lambda e: e.scalar_tensor_tensor(out=sm[:, 3:4], in0=mv[:, 0:1], scalar=-1.0, in1=sm[:, 2:3], op0=ALU.mult, op1=ALU.mult), [Bf("mv"), Bf("sm")], [Bf("sm")])
                A(lambda e, acc=acc, nrm=nrm: e.activation(out=nrm[:, :], in_=acc[:, :], func=AF.Identity, scale=sm[:, 2:3], bias=sm[:, 3:4]), [BA, Bf("sm")], [BN])
                P(lambda e, nrm=nrm: e.tensor_tensor(out=nrm[:, :], in0=nrm[:, :], in1=lnbc[:, 0, :], op=ALU.mult), [BN, Bf("lnbc")], [BN])
                P(lambda e, sl=sl, nrm=nrm: e.tensor_tensor(out=res[sl][:, :], in0=nrm[:, :], in1=lnbc[:, 1, :], op=ALU.add), [BN, Bf("lnbc")], [Bf("res%d" % sl)])
                S.op("sync", lambda e, sl=sl, t0=t0: e.dma_start(out=out[t0:t0 + 128, :], in_=res[sl][:, :]), reads=[Bf("res%d" % sl)], writes=[], dma_sem=dout[sl])
            S.emit()
    return nc


def prep_inputs(cfg, x, w_in, b_in, attn_sinks, w_attn_br, conv_w, conv_b, conv_ln_g, conv_ln_b,
                w_conv_br, b_conv_br, w_o, ln1_g, ln1_b, w_router, b_router, w_up, b_up,
                w_down, b_down, ln2_g, ln2_b):
    D, NH, CC, NE, T, CAP = (cfg[k] for k in ["D", "NH", "CC", "NE", "T", "CAP"])
    G, AW, QE, KE, VE, CE, INW, KC, CCH, FC = (cfg[k] for k in ["G", "AW", "QE", "KE", "VE", "CE", "INW", "KC", "CCH", "FC"])
    NC_ = cfg["NCORES"]
    CPB = NC_ // cfg["B"]
    f = lambda a: np.ascontiguousarray(np.asarray(a, dtype=np.float32))
    x = f(x)
    w_in0, b_in0 = f(w_in)[0], f(b_in)[0]
    qperm = np.array([(g * G + c) * 64 + j for c in range(G) for g in range(2) for j in range(64)])
    perm = np.concatenate([qperm, np.arange(QE, INW)])
    w_in_p = np.ascontiguousarray(w_in0[:, perm])
    b_in_p = b_in0[perm]
    b_in_t = np.ascontiguousarray(b_in_p.reshape(INW // 128, 128).T)
    b_v = np.ascontiguousarray(b_in_p[KE:VE].reshape(1, 128))
    sinks_b = np.ascontiguousarray(np.broadcast_to(f(attn_sinks)[0][None, :], (128, NH)))
    cw_t = np.ascontiguousarray(f(conv_w)[0].T.reshape(CCH, 128, 31).transpose(1, 0, 2))
    pp = lambda v: v.reshape(-1, 128).T
    cvec = np.ascontiguousarray(np.stack([pp(f(conv_b)[0]), pp(f(conv_ln_g)[0]), pp(f(conv_ln_b)[0])], axis=1))
    bcb_t = np.ascontiguousarray(pp(f(b_conv_br)[0]))
    lnb = np.ascontiguousarray(np.broadcast_to(np.stack([f(ln1_g)[0], f(ln1_b)[0], f(ln2_g)[0], f(ln2_b)[0]])[None], (128, 4, D)))
    w_up0 = f(w_up)[0]
    w_up_p = np.concatenate([w_up0[:, :, 0::2], w_up0[:, :, 1::2]], axis=2)
    b_up0 = f(b_up)[0]
    b_up_p = np.concatenate([b_up0[:, 0::2], b_up0[:, 1::2]], axis=1)
    b_up_t = np.ascontiguousarray(b_up_p.reshape(NE, 2 * FC, 128).transpose(2, 0, 1))
    w_dn0 = f(w_down)[0]
    b_dn = np.ascontiguousarray(f(b_down)[0])
    ident = np.eye(128, dtype=np.float32)
    ltri = np.triu(np.ones((128, 128), np.float32), 1)
    consts = np.ascontiguousarray(np.stack([ident, ltri, np.ones((128, 128), np.float32)], axis=1))
    erow = np.ascontiguousarray(np.broadcast_to(np.stack([np.arange(NE, dtype=np.float32), np.arange(NE, dtype=np.float32) * CAP])[None], (128, 2, NE)))
    kk = np.arange(128)[:, None]
    qq = np.arange(128)[None, :]
    m_cur = (kk <= qq).astype(np.float32)
    m_prev = (kk > qq).astype(np.float32)
    shared = dict(w_in=w_in_p, b_in_t=b_in_t, b_v=b_v, sinks_b=sinks_b, w_ab=f(w_attn_br)[0], cw_t=cw_t, cvec=cvec,
                  w_cb=f(w_conv_br)[0], bcb_t=bcb_t, w_o=f(w_o)[0], lnb=lnb, w_r=f(w_router)[0], b_r=f(b_router)[0].reshape(1, NE),
                  b_up_t=b_up_t, b_dn=b_dn, consts=consts, erow=erow)
    maps = []
    for c in range(NC_):
        b, h = c // CPB, c % CPB
        st = h * T
        xT = np.zeros((D, 128 + T), np.float32)
        xT[:, 128:] = x[b, st:st + T].T
        if h > 0:
            xT[:, :128] = x[b, st - 128:st].T
        fl = 1.0 if h > 0 else 0.0
        masks = np.stack([np.tile(m_cur, (1, G)), np.tile(m_prev, (1, G)), np.tile(m_prev * fl, (1, G))], axis=1)
        m = dict(shared)
        m.update(xT=xT, xtok=np.ascontiguousarray(x[b, st:st + T]), masks=np.ascontiguousarray(masks),
                 flag=np.full((128, 1), fl, np.float32))
        if cfg["repl"]:
            m.update(w_up=w_up_p, w_dn=w_dn0)
        else:
            E = cfg["EPC"]
            m.update(w_up=np.ascontiguousarray(w_up_p[c * E:(c + 1) * E]), w_dn=np.ascontiguousarray(w_dn0[c * E:(c + 1) * E]))
        maps.append(m)
    return maps


def run_cfg(cfg, inputs, trace=False):
    maps = prep_inputs(cfg, **inputs)
    nc = build(cfg)
    res = run_bass_kernel_spmd(nc, maps, core_ids=list(range(cfg["NCORES"])), trace=trace)
    T, D, B = cfg["T"], cfg["D"], cfg["B"]
    CPB = cfg["NCORES"] // B
    outv = np.zeros((B, CPB * T, D), np.float32)
    for c in range(cfg["NCORES"]):
        b, h = c // CPB, c % CPB
        outv[b, h * T:(h + 1) * T] = res.results[c]["out"]
    return outv, res


def kernel(**inputs):
    cfg = make_cfg()
    outv, _ = run_cfg(cfg, inputs)
    return outv
```

```python
from contextlib import ExitStack
import numpy as np
import concourse.bass as bass
import concourse.mybir as mybir
from concourse.bass_utils import run_bass_kernel_spmd

F32 = mybir.dt.float32
BF16 = mybir.dt.bfloat16
I32 = mybir.dt.int32
U32 = mybir.dt.uint32
AF = mybir.ActivationFunctionType
ALU = mybir.AluOpType
ENGS = ["tensor", "vector", "scalar", "gpsimd", "sync"]


class Buf:
    __slots__ = ("name", "writers", "readers")

    def __init__(self, name):
        self.name = name
        self.writers = []
        self.readers = []


class Op:
    __slots__ = ("eng", "fn", "deps", "marked", "is_dma", "sem", "val")

    def __init__(self, eng, fn, is_dma=False, sem=None):
        self.eng = eng
        self.fn = fn
        self.deps = []
        self.marked = False
        self.is_dma = is_dma
        self.sem = sem
        self.val = None


class DSem:
    def __init__(self, name, group=False, inc=16):
        self.name = name
        self.count = 0
        self.handle = None
        self.group = group
        self.inc = inc


def _prune(lst, op):
    if op.is_dma:
        out = [o for o in lst if not (o.is_dma and o.sem is op.sem)]
    else:
        out = [o for o in lst if o.is_dma or o.eng != op.eng]
    out.append(op)
    return out


class Sched:
    def __init__(self, nc, tag):
        self.nc = nc
        self.tag = tag
        self.ops = {e: [] for e in ENGS}
        self.dsems = []
        self.dma_ops = []

    def dsem(self, name, group=False, inc=16):
        s = DSem(name, group, inc)
        self.dsems.append(s)
        return s

    def op(self, eng, fn, reads=(), writes=(), dma_sem=None):
        o = Op(eng, fn, is_dma=dma_sem is not None, sem=dma_sem)
        deps = []
        for b in reads:
            deps.extend(b.writers)
        for b in writes:
            deps.extend(b.writers)
            deps.extend(b.readers)
        seen = set()
        for d in deps:
            if id(d) in seen:
                continue
            seen.add(id(d))
            if (not d.is_dma) and (not o.is_dma) and d.eng == "tensor" and eng == "tensor":
                continue
            if d.is_dma and o.is_dma and d.sem is o.sem:
                continue
            o.deps.append(d)
            d.marked = True
        if o.is_dma:
            dma_sem.count += dma_sem.inc
            o.val = dma_sem.count
            o.marked = True
            self.dma_ops.append(o)
        for b in reads:
            b.readers = _prune(b.readers, o)
        for b in writes:
            b.writers = [o]
            b.readers = []
        self.ops[eng].append(o)
        return o

    def emit(self):
        nc = self.nc
        with ExitStack() as st:
            esem = {e: st.enter_context(nc.semaphore(self.tag + "_s_" + e)) for e in ENGS}
            for s in self.dsems:
                s.handle = st.enter_context(nc.semaphore(self.tag + "_d_" + s.name))
            for e in ENGS:
                c = 0
                for o in self.ops[e]:
                    if not o.is_dma and o.marked:
                        c += 1
                        o.val = c
            finals = {}
            for o in self.dma_ops:
                finals[id(o.sem)] = o
            block = st.enter_context(nc.Block())

            def run(e, eng):
                waited = {}
                for o in self.ops[e]:
                    for d in o.deps:
                        if d.is_dma:
                            key, h = id(d.sem), d.sem.handle
                            dv = d.sem.count if d.sem.group else d.val
                        else:
                            key, h = d.eng, esem[d.eng]
                            dv = d.val
                        if waited.get(key, 0) >= dv:
                            continue
                        waited[key] = dv
                        eng.wait_ge(h, dv)
                    inst = o.fn(eng)
                    if o.is_dma:
                        if o.sem.inc == 16:
                            inst.then_inc(o.sem.handle, 16)
                        else:
                            inst.then_inc(o.sem.handle)
                    elif o.marked:
                        inst.then_inc(esem[e], 1)
                if e == "sync":
                    for d in finals.values():
                        eng.wait_ge(d.sem.handle, d.val)

            @block.tensor
            def _(eng):
                run("tensor", eng)

            @block.vector
            def _(eng):
                run("vector", eng)

            @block.scalar
            def _(eng):
                run("scalar", eng)

            @block.gpsimd
            def _(eng):
                run("gpsimd", eng)

            @block.sync
            def _(eng):
                run("sync", eng)


def make_cfg(D=1024, NH=8, CC=512, NE=32, T=2048, CAP=384, ST=256, NCORES=8, B=4, repl=True,
             alpha=2 ** 0.25):
    c = dict(D=D, NH=NH, CC=CC, NE=NE, T=T, CAP=CAP, ST=ST, NCORES=NCORES, B=B, repl=repl, alpha=alpha)
    c["G"] = NH // 2
    c["AW"] = NH * 64
    c["QE"] = c["AW"]
    c["KE"] = c["QE"] + 128
    c["VE"] = c["KE"] + 128
    c["CE"] = c["VE"] + 2 * CC
    c["INW"] = c["CE"] + 2 * D
    c["KC"] = D // 128
    c["AC"] = c["AW"] // 128
    c["CCH"] = CC // 128
    c["FC"] = D // 128
    c["W2"] = min(D, 512)
    c["NH2"] = D // c["W2"]
    c["NBLK"] = T // 128
    c["NB"] = CAP // 128
    c["EPC"] = NE // NCORES
    c["SEQ"] = T * (NCORES // B)
    return c


def build(cfg):
    D, NH, CC, NE, T, CAP, ST = (cfg[k] for k in ["D", "NH", "CC", "NE", "T", "CAP", "ST"])
    G, AW, QE, KE, VE, CE, INW = (cfg[k] for k in ["G", "AW", "QE", "KE", "VE", "CE", "INW"])
    KC, AC, CCH, FC, W2, NH2, NBLK, NB = (cfg[k] for k in ["KC", "AC", "CCH", "FC", "W2", "NH2", "NBLK", "NB"])
    alpha = float(cfg["alpha"])
    NU = INW // 128
    GW = G * 128
    PADL = 32
    NEW = NE if cfg["repl"] else cfg["EPC"]
    BIG = 4.0e6
    EPS = 1e-5

    nc = bass.Bass("TRN2", target_bir_lowering=False)

    def din(name, shape, dt=F32):
        return nc.dram_tensor(name, list(shape), dt, kind="ExternalInput").ap()

    xT = din("xT", [D, 128 + T])
    xtok = din("xtok", [T, D])
    masks = din("masks", [128, 3, GW])
    flag = din("flag", [128, 1])
    w_in = din("w_in", [D, INW])
    b_in_t = din("b_in_t", [128, NU])
    b_v = din("b_v", [1, 128])
    sinks_b = din("sinks_b", [128, NH])
    w_ab = din("w_ab", [AW, D])
    cw_t = din("cw_t", [128, CCH, 31])
    cvec = din("cvec", [128, 3, CCH])
    w_cb = din("w_cb", [CC, D])
    bcb_t = din("bcb_t", [128, KC])
    w_o = din("w_o", [D, D])
    lnb = din("lnb", [128, 4, D])
    w_r = din("w_r", [D, NE])
    b_r = din("b_r", [1, NE])
    w_up = din("w_up", [NEW, D, 2 * D])
    b_up_t = din("b_up_t", [128, NE, 2 * FC])
    w_dn = din("w_dn", [NEW, D, D])
    b_dn = din("b_dn", [NE, D])
    consts = din("consts", [128, 3, 128])
    erow = din("erow", [128, 2, NE])
    out = nc.dram_tensor("out", [T, D], F32, kind="ExternalOutput").ap()
    xs_d = nc.dram_tensor("xs_d", [NE * CAP, D], BF16, kind="Internal").ap()
    ys_d = nc.dram_tensor("ys_d", [NE * CAP, D], F32, kind="Internal").ap()
    x1_d = nc.dram_tensor("x1_d", [T, D], F32, kind="Internal").ap()
    if not cfg["repl"]:
        EPC = cfg["EPC"]
        wl_up_t = nc.dram_tensor("wl_up", [EPC * D, 2 * D], BF16)
        wa_up_t = nc.dram_tensor("wa_up", [NE * D, 2 * D], BF16)
        wl_dn_t = nc.dram_tensor("wl_dn", [EPC * D, D], BF16)
        wa_dn_t = nc.dram_tensor("wa_dn", [NE * D, D], BF16)
        wl_up, wa_up, wl_dn, wa_dn = wl_up_t.ap(), wa_up_t.ap(), wl_dn_t.ap(), wa_dn_t.ap()

    regs = {}

    def breg(e, phase):
        if phase not in regs:
            regs[phase] = e.to_reg(NE * CAP - 1)
        return regs[phase]

    with ExitStack() as pst:
        def sbp(name, shape, dt):
            return pst.enter_context(nc.sbuf_tensor(name, list(shape), dt))

        dest_i = sbp("dest_i", [128, NBLK, 4], I32)
        gate = sbp("gate", [128, NBLK, 4], F32)
        ident_f = sbp("ident_f", [128, 128], F32)
        ident_b = sbp("ident_b", [128, 128], BF16)
        ones_b = sbp("ones_b", [128, 128], BF16)
        ones_f = sbp("ones_f", [128, 128], F32)
        lnbc = sbp("lnbc", [128, 2, D], F32)
        bdn_b = sbp("bdn_b", [NE, D], BF16)
        selT = sbp("selT", [NE, NE, 128], BF16)
        bup = sbp("bup", [128, NE, 2 * FC], F32)
        ps = [pst.enter_context(nc.psum_tensor("ps%d" % i, [128, 512], F32)) for i in range(8)]
        B_dest, B_gate, B_xs, B_ys, B_x1d = Buf("dest"), Buf("gate"), Buf("xs"), Buf("ys"), Buf("x1d")
        B_const = Buf("const")

        S = Sched(nc, "m")
        PB = [Buf("ps%d" % i) for i in range(8)]
        with ExitStack() as st:
            def sb(name, shape, dt):
                return st.enter_context(nc.sbuf_tensor(name, list(shape), dt))

            w_in_bf = sb("w_in_bf", [128, KC, INW], BF16)
            w_ab_bf = sb("w_ab_bf", [128, AC, D], BF16)
            w_cb_bf = sb("w_cb_bf", [128, CCH, D], BF16)
            w_o_bf = sb("w_o_bf", [128, KC, D], BF16)
            diag2 = [sb("diag%d" % i, [128, 31, 128], BF16) for i in range(2)]
            w_r_f = sb("w_r_f", [128, KC, NE], F32)
            b_r_f = sb("b_r_f", [1, NE], F32)
            bin_t = sb("bin_t", [128, NU], F32)
            bv_b = sb("bv_b", [1, 128], BF16)
            esink = sb("esink", [128, NH], F32)
            cw = sb("cw", [128, CCH, 31], F32)
            cv = sb("cv", [128, 3, CCH], F32)
            bcb = sb("bcb", [128, KC], F32)
            mk = sb("mk", [128, 3, GW], BF16)
            flg = sb("flg", [128, 1], F32)
            cst = sb("cst", [128, 3, 128], F32)
            ltri_b = sb("ltri_b", [128, 128], BF16)
            onesm_f = sb("onesm_f", [128, 128], F32)
            er = sb("er", [128, 2, NE], F32)
            run_c = sb("run_c", [128, NE], F32)
            xT_bf = [sb("xT_bf%d" % i, [128, KC, ST], BF16) for i in range(2)]
            qT = sb("qT", [128, AC, ST], BF16)
            kT = sb("kT", [128, 128 + ST], BF16)
            Vr = sb("Vr", [128, 1 + ST // 128, 2, 65], BF16)
            aT = sb("aT", [128, CCH, PADL + ST], BF16)
            sgt = sb("sgt", [128, ST], F32)
            Pp = [sb("Pp%d" % i, [128, GW], BF16) for i in range(4)]
            Pc = [sb("Pc%d" % i, [128, GW], BF16) for i in range(4)]
            dens = [sb("den%d" % i, [128, G], F32) for i in range(2)]
            o_n = sb("o_n", [128, AW], BF16)
            oT = sb("oT", [128, AC, ST], BF16)
            yb = sb("yb", [128, CCH, ST], F32)
            sq = sb("sq", [128, CCH, ST], F32)
            mean_sb = sb("mean_sb", [128, ST], F32)
            var_sb = sb("var_sb", [128, ST], F32)
            tmpc = sb("tmpc", [128, ST], F32)
            sT = sb("sT", [128, CCH, ST], BF16)
            ga = [sb("ga%d" % i, [128, ST], F32) for i in range(2)]
            gb = [sb("gb%d" % i, [128, ST], F32) for i in range(2)]
            t1s = [sb("t1_%d" % i, [128, ST], F32) for i in range(1)] * 2
            t2s = [sb("t2_%d" % i, [128, ST], F32) for i in range(1)] * 2
            mg = sb("mg", [128, KC, ST], BF16)
            xt = [sb("xt%d" % i, [128, D], F32) for i in range(1)]
            z = sb("z", [128, D], F32)
            x1 = [sb("x1_%d" % i, [128, D], F32) for i in range(1)]
            x1b = [sb("x1b%d" % i, [128, D], BF16) for i in range(1)]
            x1T = sb("x1T", [128, KC, 128], F32)
            stt = sb("stt", [128, max(NH2, 1) * 6], F32)
            mv = sb("mv", [128, 2], F32)
            sm = sb("sm", [128, 8], F32)
            lg = sb("lg", [128, NE], F32)
            top8 = sb("top8", [128, 8], F32)
            tidx = sb("tidx", [128, 8], U32)
            ef = sb("ef", [128, 4], F32)
            ex4 = sb("ex4", [128, 4], F32)
            mskb = sb("mskb", [128, NE], BF16)
            pos = sb("pos", [128, NE], F32)
            ovf = sb("ovf", [128, NE], F32)
            junk = sb("junk", [128, NE], F32)
            dest_f = sb("dest_f", [128, 4], F32)

            Bn = {}

            def Bf(n):
                if n not in Bn:
                    Bn[n] = Buf(n)
                return Bn[n]

            dc = S.dsem("c", group=True)
            dw = S.dsem("w", group=True)

            def ld(eng, o_ap, i_ap, bufname, sem=dc, **kw):
                S.op(eng, lambda e: e.dma_start(out=o_ap, in_=i_ap, **kw), writes=[Bf(bufname)], dma_sem=sem)

            ld("sync", cst[:, :, :], consts, "cst")
            ld("sync", er[:, :, :], erow, "er")
            ld("sync", lnbc[:, :, :], lnb[:, 0:2, :], "lnbc")
            ld("sync", bup[:, :, :], b_up_t, "bup")
            ld("sync", w_r_f[:, :, :], w_r.rearrange("(kc p) n -> p kc n", p=128), "w_r")
            ld("sync", b_r_f[:, :], b_r, "b_r")
            ld("sync", bin_t[:, :], b_in_t, "bin")
            ld("sync", esink[:, :], sinks_b, "esink")
            ld("sync", cw[:, :, :], cw_t, "cw")
            ld("sync", cv[:, :, :], cvec, "cv")
            ld("sync", bcb[:, :], bcb_t, "bcb")
            ld("gpsimd", mk[:, :, :], masks, "mk")
            ld("sync", flg[:, :], flag, "flg")
            ld("gpsimd", bv_b[:, :], b_v, "bv")
            ld("gpsimd", bdn_b[:, :], b_dn, "bdn")
            HW = INW // 2
            for kc in range(KC):
                for hh in range(2):
                    ld("gpsimd", w_in_bf[:, kc, hh * HW:(hh + 1) * HW], w_in[kc * 128:(kc + 1) * 128, hh * HW:(hh + 1) * HW], "w_in", sem=dw)
            for ac in range(AC):
                ld("gpsimd", w_ab_bf[:, ac, :], w_ab[ac * 128:(ac + 1) * 128, :], "w_ab", sem=dw)
            for c in range(CCH):
                ld("gpsimd", w_cb_bf[:, c, :], w_cb[c * 128:(c + 1) * 128, :], "w_cb", sem=dw)
            for kc in range(KC):
                ld("gpsimd", w_o_bf[:, kc, :], w_o[kc * 128:(kc + 1) * 128, :], "w_o", sem=dw)

            V = lambda fn, r=(), w=(): S.op("vector", fn, reads=r, writes=w)
            A = lambda fn, r=(), w=(): S.op("scalar", fn, reads=r, writes=w)
            P = lambda fn, r=(), w=(): S.op("gpsimd", fn, reads=r, writes=w)
            TE = lambda fn, r=(), w=(): S.op("tensor", fn, reads=r, writes=w)

            cast_jobs = []
            if not cfg["repl"]:
                dwl = S.dsem("wl", group=True)
                dag = S.dsem("ag", group=True, inc=1)
                for i in range(EPC):
                    for kc in range(KC):
                        cast_jobs.append((0, i, kc))
                        cast_jobs.append((1, i, kc))

            def issue_cast(job, jn):
                which, i, kc = job
                r0 = i * D + kc * 128
                if which == 0:
                    S.op("gpsimd", lambda e: e.dma_start(out=wl_up[r0:r0 + 128, :], in_=w_up[i, kc * 128:(kc + 1) * 128, :]), writes=[Bf("wl")], dma_sem=dwl)
                else:
                    S.op("gpsimd", lambda e: e.dma_start(out=wl_dn[r0:r0 + 128, :], in_=w_dn[i, kc * 128:(kc + 1) * 128, :]), writes=[Bf("wl")], dma_sem=dwl)

            def issue_ag():
                grp = [list(range(cfg["NCORES"]))]
                S.op("gpsimd", lambda e: e.collective_compute("AllGather", ALU.bypass, replica_groups=grp, ins=[wl_up_t.ap().opt()], outs=[wa_up_t.ap().opt()]),
                     reads=[Bf("wl")], writes=[Bf("wa")], dma_sem=dag)
                S.op("gpsimd", lambda e: e.collective_compute("AllGather", ALU.bypass, replica_groups=grp, ins=[wl_dn_t.ap().opt()], outs=[wa_dn_t.ap().opt()]),
                     reads=[Bf("wl")], writes=[Bf("wa")], dma_sem=dag)

            V(lambda e: e.tensor_copy(out=ident_f[:, :], in_=cst[:, 0, :]), [Bf("cst")], [Bf("ident_f")])
            V(lambda e: e.tensor_copy(out=ident_b[:, :], in_=cst[:, 0, :]), [Bf("cst")], [Bf("ident_b")])
            V(lambda e: e.tensor_copy(out=ltri_b[:, :], in_=cst[:, 1, :]), [Bf("cst")], [Bf("ltri")])
            V(lambda e: e.tensor_copy(out=ones_b[:, :], in_=cst[:, 2, :]), [Bf("cst")], [Bf("ones_b")])
            V(lambda e: e.tensor_copy(out=ones_f[:, :], in_=cst[:, 2, :]), [Bf("cst")], [Bf("ones_f")])
            V(lambda e: e.tensor_scalar(out=onesm_f[:, :], in0=cst[:, 2, :], scalar1=1.0 / CC, scalar2=None, op0=ALU.mult),
              [Bf("cst")], [Bf("onesm")])
            V(lambda e: e.tensor_copy(out=selT[:, :, :], in_=cst[0:NE, 0, 0:NE].unsqueeze(2).to_broadcast([NE, NE, 128])), [Bf("cst")], [Bf("selT")])
            A(lambda e: e.activation(out=esink[:, :], in_=esink[:, :], func=AF.Exp), [Bf("esink")], [Bf("esink")])
            P(lambda e: e.memset(Vr[:, :, :, :], 1.0), [], [Bf("Vr")])
            P(lambda e: e.memset(run_c[:, :], 0.0), [], [Bf("run")])
            P(lambda e: e.memset(aT[:, :, :], 0.0), [], [Bf("aT")])
            P(lambda e: e.memset(kT[:, :], 0.0), [], [Bf("kT")])

            rot = [0]

            def nbank(pool=(0, 1, 2, 3)):
                rot[0] += 1
                return pool[rot[0] % len(pool)]

            dx = [S.dsem("x0"), S.dsem("x1")]
            dxt = [S.dsem("xt0"), S.dsem("xt1")]
            dx1 = [S.dsem("x1s0"), S.dsem("x1s1")]
            dsc = [S.dsem("sc0"), S.dsem("sc1")]

            NST = T // ST
            sizes = [128] + [ST] * NST
            tau0 = 0
            def body(s, n, tau0):
                    xs_ = s % 2
                    XB = xT_bf[xs_]
                    BX = Bf("xT_bf%d" % xs_)
                    for kc in range(KC):
                        S.op("gpsimd", lambda e, kc=kc, XB=XB, tau0=tau0, n=n: e.dma_start(
                            out=XB[:, kc, 0:n], in_=xT[kc * 128:(kc + 1) * 128, tau0:tau0 + n]), writes=[BX], dma_sem=dx[xs_])

                    def proj(ch, n=n, XB=XB, BX=BX):
                        b = nbank()
                        for kc in range(KC):
                            TE(lambda e, kc=kc, b=b, ch=ch: e.matmul(ps[b][:, 0:n], lhsT=w_in_bf[:, kc, ch * 128:(ch + 1) * 128],
                                                                     rhs=XB[:, kc, 0:n], start=(kc == 0), stop=(kc == KC - 1)),
                               [Bf("w_in"), BX], [PB[b]])
                        return b

                    if s > 0:
                        for c in range(AC):
                            b = proj(c)
                            A(lambda e, b=b, c=c, n=n: e.activation(out=qT[:, c, 0:n], in_=ps[b][:, 0:n], func=AF.Identity,
                                                                     bias=bin_t[:, c:c + 1], scale=1.0), [PB[b], Bf("bin")], [Bf("qT")])
                    b = proj(AC)
                    A(lambda e, b=b, n=n: e.activation(out=kT[:, 128:128 + n], in_=ps[b][:, 0:n], func=AF.Identity,
                                                       bias=bin_t[:, AC:AC + 1], scale=1.0), [PB[b], Bf("bin")], [Bf("kT")])
                    for bb in range(n // 128):
                        b = nbank()
                        for kc in range(KC):
                            TE(lambda e, kc=kc, b=b, bb=bb, XB=XB: e.matmul(ps[b][:, 0:128], lhsT=XB[:, kc, bb * 128:(bb + 1) * 128],
                                                                            rhs=w_in_bf[:, kc, KE:VE], start=(kc == 0), stop=False),
                               [Bf("w_in"), BX], [PB[b]])
                        TE(lambda e, b=b: e.matmul(ps[b][:, 0:128], lhsT=ones_b[0:1, :], rhs=bv_b[0:1, :], start=False, stop=True),
                           [Bf("ones_b"), Bf("bv")], [PB[b]])
                        A(lambda e, b=b, bb=bb: e.activation(out=Vr[:, 1 + bb, :, 0:64], in_=ps[b][:, 0:128].rearrange("p (g d) -> p g d", g=2), func=AF.Copy),
                          [PB[b]], [Bf("Vr")])
                    for c in range(CCH):
                        bg = proj(AC + 2 + CCH + c)
                        A(lambda e, bg=bg, c=c, n=n: e.activation(out=sgt[:, 0:n], in_=ps[bg][:, 0:n], func=AF.Sigmoid,
                                                                   bias=bin_t[:, AC + 2 + CCH + c:AC + 3 + CCH + c], scale=1.0),
                          [PB[bg], Bf("bin")], [Bf("sgt")])
                        ba = proj(AC + 2 + c)
                        V(lambda e, ba=ba, c=c, n=n: e.scalar_tensor_tensor(out=aT[:, c, PADL:PADL + n], in0=ps[ba][:, 0:n],
                                                                            scalar=bin_t[:, AC + 2 + c:AC + 3 + c], in1=sgt[:, 0:n],
                                                                            op0=ALU.add, op1=ALU.mult),
                          [PB[ba], Bf("bin"), Bf("sgt")], [Bf("aT")])

                    yield "A1"
                    if s > 0:
                        nqb = n // 128
                        for qb in range(nqb):
                            gq = (s - 1) * (ST // 128) + qb
                            mprev = 2 if gq == 0 else 1
                            for g in range(2):
                                rows = slice(64 * g, 64 * g + 64)
                                bP, bC = 2 + 2 * g, 3 + 2 * g
                                pi = (qb % 2) * 2 + g
                                pp, pc = Pp[pi], Pc[pi]
                                TE(lambda e, rows=rows, bP=bP, qb=qb: e.matmul(ps[bP][:, 0:GW], lhsT=kT[rows, qb * 128:qb * 128 + 128],
                                                                              rhs=qT[rows, :, qb * 128:(qb + 1) * 128], start=True, stop=True),
                                   [Bf("kT"), Bf("qT")], [PB[bP]])
                                TE(lambda e, rows=rows, bC=bC, qb=qb: e.matmul(ps[bC][:, 0:GW], lhsT=kT[rows, 128 + qb * 128:256 + qb * 128],
                                                                              rhs=qT[rows, :, qb * 128:(qb + 1) * 128], start=True, stop=True),
                                   [Bf("kT"), Bf("qT")], [PB[bC]])
                                A(lambda e, bP=bP, pp=pp: e.activation(out=pp[:, :], in_=ps[bP][:, 0:GW], func=AF.Exp, scale=0.125),
                                  [PB[bP]], [Bf("Pp%d" % pi)])
                                A(lambda e, bC=bC, pc=pc: e.activation(out=pc[:, :], in_=ps[bC][:, 0:GW], func=AF.Exp, scale=0.125),
                                  [PB[bC]], [Bf("Pc%d" % pi)])
                                P(lambda e, pp=pp, mprev=mprev: e.tensor_tensor(out=pp[:, :], in0=pp[:, :], in1=mk[:, mprev, :], op=ALU.mult),
                                  [Bf("Pp%d" % pi), Bf("mk")], [Bf("Pp%d" % pi)])
                                P(lambda e, pc=pc: e.tensor_tensor(out=pc[:, :], in0=pc[:, :], in1=mk[:, 0, :], op=ALU.mult),
                                  [Bf("Pc%d" % pi), Bf("mk")], [Bf("Pc%d" % pi)])
                        for qb in range(nqb):
                            for g in range(2):
                                bO = 6 + g
                                pi = (qb % 2) * 2 + g
                                pp, pc = Pp[pi], Pc[pi]
                                for c in range(G):
                                    TE(lambda e, c=c, bO=bO, pp=pp, qb=qb, g=g: e.matmul(ps[bO][:, c * 65:(c + 1) * 65], lhsT=pp[:, c * 128:(c + 1) * 128],
                                                                                        rhs=Vr[:, qb, g, :], start=True, stop=False),
                                       [Bf("Pp%d" % pi), Bf("Vr")], [PB[bO]])
                                    TE(lambda e, c=c, bO=bO, pc=pc, qb=qb, g=g: e.matmul(ps[bO][:, c * 65:(c + 1) * 65], lhsT=pc[:, c * 128:(c + 1) * 128],
                                                                                        rhs=Vr[:, qb + 1, g, :], start=False, stop=True),
                                       [Bf("Pc%d" % pi), Bf("Vr")], [PB[bO]])
                                o3 = ps[bO][:, 0:G * 65].rearrange("p (c d) -> p c d", c=G)
                                dn = dens[g]
                                V(lambda e, o3=o3, g=g, dn=dn: e.tensor_tensor(out=dn[:, :], in0=o3[:, :, 64], in1=esink[:, g * G:(g + 1) * G], op=ALU.add),
                                  [PB[bO], Bf("esink")], [Bf("den%d" % g)])
                                V(lambda e, dn=dn: e.reciprocal(out=dn[:, :], in_=dn[:, :]), [Bf("den%d" % g)], [Bf("den%d" % g)])
                                V(lambda e, o3=o3, g=g, dn=dn: e.tensor_tensor(out=o_n[:, g * G * 64:(g + 1) * G * 64].rearrange("p (c d) -> p c d", c=G),
                                                                               in0=o3[:, :, 0:64], in1=dn[:, :].unsqueeze(2).to_broadcast([128, G, 64]), op=ALU.mult),
                                  [PB[bO], Bf("den%d" % g)], [Bf("o_n")])
                            b = nbank((0, 1))
                            pbf = ps[b][:, :].bitcast(BF16)
                            for ac in range(AC):
                                TE(lambda e, ac=ac, pbf=pbf: e.transpose(out=pbf[:, ac * 128:(ac + 1) * 128], in_=o_n[:, ac * 128:(ac + 1) * 128], identity=ident_b[:, :]),
                                   [Bf("o_n"), Bf("ident_b")], [PB[b]])
                            A(lambda e, pbf=pbf, qb=qb: e.activation(out=oT[:, :, qb * 128:(qb + 1) * 128], in_=pbf[:, 0:AC * 128].rearrange("p (a t) -> p a t", a=AC),
                                                                     func=AF.Copy), [PB[b]], [Bf("oT")])
                    yield "A2"
                    if s > 0:
                        for c in range(CCH):
                            b = nbank((0, 1))
                            dg = diag2[c % 2]
                            for j in range(31):
                                eng_ = V if j % 3 == 0 else P
                                eng_(lambda e, j=j, c=c, dg=dg: e.tensor_scalar(out=dg[:, j, :], in0=cst[:, 0, :], scalar1=cw[:, c, j:j + 1],
                                                                                scalar2=1.0, op0=ALU.mult, op1=ALU.mult), [Bf("cst"), Bf("cw")], [Bf("diag%d_%d" % (c % 2, j))])
                            for j in range(31):
                                TE(lambda e, j=j, c=c, b=b, n=n, dg=dg: e.matmul(ps[b][:, 0:n], lhsT=dg[:, j, :], rhs=aT[:, c, PADL - 30 + j:PADL - 30 + j + n],
                                                                                 start=(j == 0), stop=(j == 30)), [Bf("diag%d_%d" % (c % 2, j)), Bf("aT")], [PB[b]])
                            A(lambda e, c=c, b=b, n=n: e.activation(out=yb[:, c, 0:n], in_=ps[b][:, 0:n], func=AF.Identity, bias=cv[:, 0, c:c + 1], scale=1.0),
                              [PB[b], Bf("cv")], [Bf("yb")])
                            A(lambda e, c=c, b=b, n=n: e.activation(out=sq[:, c, 0:n], in_=ps[b][:, 0:n], func=AF.Square, bias=cv[:, 0, c:c + 1], scale=1.0),
                              [PB[b], Bf("cv")], [Bf("sq")])
                        for c in range(CCH):
                            TE(lambda e, c=c, n=n: e.matmul(ps[2][:, 0:n], lhsT=onesm_f[:, :], rhs=yb[:, c, 0:n], start=(c == 0), stop=(c == CCH - 1)),
                               [Bf("onesm"), Bf("yb")], [PB[2]])
                        for c in range(CCH):
                            TE(lambda e, c=c, n=n: e.matmul(ps[3][:, 0:n], lhsT=onesm_f[:, :], rhs=sq[:, c, 0:n], start=(c == 0), stop=(c == CCH - 1)),
                               [Bf("onesm"), Bf("sq")], [PB[3]])
                        A(lambda e, n=n: e.activation(out=mean_sb[:, 0:n], in_=ps[2][:, 0:n], func=AF.Copy), [PB[2]], [Bf("mean")])
                        V(lambda e, n=n: e.tensor_tensor(out=var_sb[:, 0:n], in0=mean_sb[:, 0:n], in1=mean_sb[:, 0:n], op=ALU.mult), [Bf("mean")], [Bf("var")])
                        V(lambda e, n=n: e.tensor_tensor(out=var_sb[:, 0:n], in0=ps[3][:, 0:n], in1=var_sb[:, 0:n], op=ALU.subtract), [PB[3], Bf("var")], [Bf("var")])
                        V(lambda e, n=n: e.tensor_scalar(out=var_sb[:, 0:n], in0=var_sb[:, 0:n], scalar1=EPS, scalar2=None, op0=ALU.add), [Bf("var")], [Bf("var")])
                        A(lambda e, n=n: e.activation(out=var_sb[:, 0:n], in_=var_sb[:, 0:n], func=AF.Sqrt), [Bf("var")], [Bf("var")])
                        V(lambda e, n=n: e.reciprocal(out=var_sb[:, 0:n], in_=var_sb[:, 0:n]), [Bf("var")], [Bf("var")])
                        for c in range(CCH):
                            V(lambda e, c=c, n=n: e.tensor_tensor(out=tmpc[:, 0:n], in0=yb[:, c, 0:n], in1=mean_sb[:, 0:n], op=ALU.subtract),
                              [Bf("yb"), Bf("mean")], [Bf("tmpc")])
                            V(lambda e, n=n: e.tensor_tensor(out=tmpc[:, 0:n], in0=tmpc[:, 0:n], in1=var_sb[:, 0:n], op=ALU.mult),
                              [Bf("tmpc"), Bf("var")], [Bf("tmpc")])
                            A(lambda e, c=c, n=n: e.activation(out=sT[:, c, 0:n], in_=tmpc[:, 0:n], func=AF.Silu, scale=cv[:, 1, c:c + 1], bias=cv[:, 2, c:c + 1]),
                              [Bf("tmpc"), Bf("cv")], [Bf("sT")])
                    P(lambda e, n=n: e.tensor_copy(out=kT[:, 0:128], in_=kT[:, n:n + 128]), [Bf("kT")], [Bf("kT")])
                    P(lambda e, n=n: e.tensor_copy(out=Vr[:, 0, :, 0:64], in_=Vr[:, n // 128, :, 0:64]), [Bf("Vr")], [Bf("Vr")])
                    P(lambda e, n=n: e.tensor_copy(out=aT[:, :, 0:PADL], in_=aT[:, :, n:n + PADL]), [Bf("aT")], [Bf("aT")])
                    if s == 0:
                        P(lambda e: e.tensor_scalar(out=aT[:, :, 0:PADL], in0=aT[:, :, 0:PADL], scalar1=flg[:, 0:1], scalar2=None, op0=ALU.mult),
                          [Bf("aT"), Bf("flg")], [Bf("aT")])
                    yield "A"
                    if s == 0:
                        return
                    for j in range(KC):
                        bs = (0, 1, 2, 3) if j % 2 == 0 else (4, 5, 6, 7)
                        bA, bGA, bB, bGB = bs
                        gaj, gbj = ga[j % 2], gb[j % 2]
                        for ac in range(AC):
                            TE(lambda e, ac=ac, j=j, bA=bA, n=n: e.matmul(ps[bA][:, 0:n], lhsT=w_ab_bf[:, ac, j * 128:(j + 1) * 128], rhs=oT[:, ac, 0:n],
                                                                          start=(ac == 0), stop=(ac == AC - 1)), [Bf("w_ab"), Bf("oT")], [PB[bA]])
                        for kc in range(KC):
                            TE(lambda e, kc=kc, j=j, bGA=bGA, n=n, XB=XB: e.matmul(ps[bGA][:, 0:n], lhsT=w_in_bf[:, kc, CE + j * 128:CE + (j + 1) * 128], rhs=XB[:, kc, 0:n],
                                                                                  start=(kc == 0), stop=(kc == KC - 1)), [Bf("w_in"), BX], [PB[bGA]])
                        for c in range(CCH):
                            TE(lambda e, c=c, j=j, bB=bB, n=n: e.matmul(ps[bB][:, 0:n], lhsT=w_cb_bf[:, c, j * 128:(j + 1) * 128], rhs=sT[:, c, 0:n],
                                                                        start=(c == 0), stop=(c == CCH - 1)), [Bf("w_cb"), Bf("sT")], [PB[bB]])
                        for kc in range(KC):
                            TE(lambda e, kc=kc, j=j, bGB=bGB, n=n, XB=XB: e.matmul(ps[bGB][:, 0:n], lhsT=w_in_bf[:, kc, CE + D + j * 128:CE + D + (j + 1) * 128], rhs=XB[:, kc, 0:n],
                                                                                  start=(kc == 0), stop=(kc == KC - 1)), [Bf("w_in"), BX], [PB[bGB]])
                        ua = CE // 128 + j
                        ub = CE // 128 + KC + j
                        A(lambda e, bGA=bGA, gaj=gaj, ua=ua, n=n: e.activation(out=gaj[:, 0:n], in_=ps[bGA][:, 0:n], func=AF.Sigmoid, bias=bin_t[:, ua:ua + 1], scale=1.0),
                          [PB[bGA], Bf("bin")], [Bf("ga%d" % (j % 2))])
                        A(lambda e, bGB=bGB, gbj=gbj, ub=ub, n=n: e.activation(out=gbj[:, 0:n], in_=ps[bGB][:, 0:n], func=AF.Sigmoid, bias=bin_t[:, ub:ub + 1], scale=1.0),
                          [PB[bGB], Bf("bin")], [Bf("gb%d" % (j % 2))])
                        t1, t2 = t1s[j % 2], t2s[j % 2]
                        V(lambda e, bA=bA, gaj=gaj, n=n, t1=t1: e.tensor_tensor(out=t1[:, 0:n], in0=ps[bA][:, 0:n], in1=gaj[:, 0:n], op=ALU.mult),
                          [PB[bA], Bf("ga%d" % (j % 2))], [Bf("t1_0")])
                        V(lambda e, bB=bB, gbj=gbj, j=j, n=n, t2=t2: e.scalar_tensor_tensor(out=t2[:, 0:n], in0=ps[bB][:, 0:n], scalar=bcb[:, j:j + 1], in1=gbj[:, 0:n],
                                                                                           op0=ALU.add, op1=ALU.mult), [PB[bB], Bf("gb%d" % (j % 2)), Bf("bcb")], [Bf("t2_0")])
                        P(lambda e, j=j, n=n, t1=t1, t2=t2: e.tensor_tensor(out=mg[:, j, 0:n], in0=t1[:, 0:n], in1=t2[:, 0:n], op=ALU.add),
                          [Bf("t1_0"), Bf("t2_0")], [Bf("mg")])
                    yield "B"
                    for bb in range(n // 128):
                        blk = (s - 1) * (ST // 128) + bb
                        t0 = blk * 128
                        sl = 0
                        xtt, x1t, x1bt = xt[sl], x1[sl], x1b[sl]
                        S.op("sync", lambda e, xtt=xtt, t0=t0: e.dma_start(out=xtt[:, :], in_=xtok[t0:t0 + 128, :]), writes=[Bf("xt%d" % sl)], dma_sem=dxt[sl])
                        zb = (0, 1) if blk % 2 == 0 else (2, 3)
                        for h in range(NH2):
                            b = zb[h % 2]
                            for kc in range(KC):
                                TE(lambda e, kc=kc, b=b, h=h, bb=bb: e.matmul(ps[b][:, 0:W2], lhsT=mg[:, kc, bb * 128:(bb + 1) * 128], rhs=w_o_bf[:, kc, h * W2:(h + 1) * W2],
                                                                            start=(kc == 0), stop=(kc == KC - 1)), [Bf("mg"), Bf("w_o")], [PB[b]])
                            V(lambda e, b=b, h=h, xtt=xtt: e.scalar_tensor_tensor(out=z[:, h * W2:(h + 1) * W2], in0=xtt[:, h * W2:(h + 1) * W2], scalar=alpha,
                                                                                 in1=ps[b][:, 0:W2], op0=ALU.mult, op1=ALU.add), [PB[b], Bf("xt%d" % sl)], [Bf("z")])

                        def layer_norm(src, srcB, dst, dstB, gi):
                            for h in range(NH2):
                                V(lambda e, h=h: e.bn_stats(out=stt[:, h * 6:(h + 1) * 6], in_=src[:, h * W2:(h + 1) * W2]), [srcB], [Bf("stt")])
                            V(lambda e: e.bn_aggr(out=mv[:, :], in_=stt[:, 0:NH2 * 6]), [Bf("stt")], [Bf("mv")])
                            V(lambda e: e.tensor_scalar(out=sm[:, 0:1], in0=mv[:, 1:2], scalar1=EPS, scalar2=None, op0=ALU.add), [Bf("mv")], [Bf("sm")])
                            A(lambda e: e.activation(out=sm[:, 1:2], in_=sm[:, 0:1], func=AF.Sqrt), [Bf("sm")], [Bf("sm")])
                            V(lambda e: e.reciprocal(out=sm[:, 2:3], in_=sm[:, 1:2]), [Bf("sm")], [Bf("sm")])
                            V(lambda e: e.scalar_tensor_tensor(out=sm[:, 3:4], in0=mv[:, 0:1], scalar=-1.0, in1=sm[:, 2:3], op0=ALU.mult, op1=ALU.mult),
                              [Bf("mv"), Bf("sm")], [Bf("sm")])
                            A(lambda e: e.activation(out=src[:, :], in_=src[:, :], func=AF.Identity, scale=sm[:, 2:3], bias=sm[:, 3:4]), [srcB, Bf("sm")], [srcB])
                            P(lambda e: e.tensor_tensor(out=src[:, :], in0=src[:, :], in1=lnbc[:, gi, :], op=ALU.mult), [srcB, Bf("lnbc")], [srcB])
                            P(lambda e: e.tensor_tensor(out=dst[:, :], in0=src[:, :], in1=lnbc[:, gi + 1, :], op=ALU.add), [srcB, Bf("lnbc")], [dstB])

                        layer_norm(z, Bf("z"), x1t, Bf("x1_%d" % sl), 0)
                        S.op("sync", lambda e, x1t=x1t, t0=t0: e.dma_start(out=x1_d[t0:t0 + 128, :], in_=x1t[:, :]), reads=[Bf("x1_%d" % sl)], writes=[B_x1d], dma_sem=dx1[sl])
                        A(lambda e, x1t=x1t, x1bt=x1bt: e.activation(out=x1bt[:, :], in_=x1t[:, :], func=AF.Copy), [Bf("x1_%d" % sl)], [Bf("x1b%d" % sl)])
                        yield "C1"
                        for kc in range(KC):
                            b = 4 + (kc // 4) % 2
                            TE(lambda e, kc=kc, b=b, x1t=x1t: e.transpose(out=ps[b][:, (kc % 4) * 128:(kc % 4 + 1) * 128], in_=x1t[:, kc * 128:(kc + 1) * 128], identity=ident_f[:, :]),
                               [Bf("x1_%d" % sl), Bf("ident_f")], [PB[b]])
                            if kc % 4 == 3 or kc == KC - 1:
                                k0 = (kc // 4) * 4
                                nk = kc - k0 + 1
                                A(lambda e, b=b, k0=k0, nk=nk: e.activation(out=x1T[:, k0:k0 + nk, :], in_=ps[b][:, 0:nk * 128].rearrange("p (k t) -> p k t", k=nk), func=AF.Copy),
                                  [PB[b]], [Bf("x1T")])
                        for kc in range(KC):
                            TE(lambda e, kc=kc: e.matmul(ps[6][:, 0:NE], lhsT=x1T[:, kc, :], rhs=w_r_f[:, kc, :], start=(kc == 0), stop=False),
                               [Bf("x1T"), Bf("w_r")], [PB[6]])
                        TE(lambda e: e.matmul(ps[6][:, 0:NE], lhsT=ones_f[0:1, :], rhs=b_r_f[0:1, :], start=False, stop=True), [Bf("ones_f"), Bf("b_r")], [PB[6]])
                        V(lambda e: e.tensor_copy(out=lg[:, :], in_=ps[6][:, 0:NE]), [PB[6]], [Bf("lg")])
                        V(lambda e: e.max(out=top8[:, :], in_=lg[:, :]), [Bf("lg")], [Bf("top8")])
                        V(lambda e: e.max_index(out=tidx[:, :], in_max=top8[:, :], in_values=lg[:, :]), [Bf("lg"), Bf("top8")], [Bf("tidx")])
                        V(lambda e: e.tensor_scalar(out=sm[:, 4:5], in0=top8[:, 0:1], scalar1=-1.0, scalar2=None, op0=ALU.mult), [Bf("top8")], [Bf("sm2")])
                        A(lambda e: e.activation(out=ex4[:, :], in_=top8[:, 0:4], func=AF.Exp, bias=sm[:, 4:5], scale=1.0), [Bf("top8"), Bf("sm2")], [Bf("ex4")])
                        V(lambda e: e.tensor_reduce(out=sm[:, 5:6], in_=ex4[:, :], axis=mybir.AxisListType.X, op=ALU.add), [Bf("ex4")], [Bf("sm3")])
                        V(lambda e: e.reciprocal(out=sm[:, 6:7], in_=sm[:, 5:6]), [Bf("sm3")], [Bf("sm3")])
                        V(lambda e, blk=blk: e.tensor_scalar(out=gate[:, blk, :], in0=ex4[:, :], scalar1=sm[:, 6:7], scalar2=None, op0=ALU.mult),
                          [Bf("ex4"), Bf("sm3")], [B_gate])
                        V(lambda e: e.tensor_scalar(out=mskb[:, :], in0=lg[:, :], scalar1=top8[:, 3:4], scalar2=None, op0=ALU.is_ge), [Bf("lg"), Bf("top8")], [Bf("mskb")])
                        yield "C2"
                        TE(lambda e: e.matmul(ps[7][:, 0:NE], lhsT=ltri_b[:, :], rhs=mskb[:, :], start=True, stop=True), [Bf("ltri"), Bf("mskb")], [PB[7]])
                        TE(lambda e: e.matmul(ps[7][:, 64:64 + NE], lhsT=ones_b[:, :], rhs=mskb[:, :], start=True, stop=True), [Bf("ones_b"), Bf("mskb")], [PB[7]])
                        V(lambda e: e.tensor_tensor(out=pos[:, :], in0=ps[7][:, 0:NE], in1=run_c[:, :], op=ALU.add), [PB[7], Bf("run")], [Bf("pos")])
                        V(lambda e: e.tensor_tensor(out=run_c[:, :], in0=ps[7][:, 64:64 + NE], in1=run_c[:, :], op=ALU.add), [PB[7], Bf("run")], [Bf("run")])
                        V(lambda e: e.tensor_scalar(out=ovf[:, :], in0=pos[:, :], scalar1=float(CAP), scalar2=BIG, op0=ALU.is_ge, op1=ALU.mult), [Bf("pos")], [Bf("ovf")])
                        V(lambda e: e.tensor_tensor(out=pos[:, :], in0=pos[:, :], in1=er[:, 1, :], op=ALU.add), [Bf("pos"), Bf("er")], [Bf("pos")])
                        V(lambda e: e.tensor_tensor(out=pos[:, :], in0=pos[:, :], in1=ovf[:, :], op=ALU.add), [Bf("pos"), Bf("ovf")], [Bf("pos")])
                        V(lambda e: e.tensor_copy(out=ef[:, :], in_=tidx[:, 0:4]), [Bf("tidx")], [Bf("ef")])
                        for k in range(4):
                            V(lambda e, k=k: e.scalar_tensor_tensor(out=junk[:, :], in0=er[:, 0, :], scalar=ef[:, k:k + 1], in1=pos[:, :], op0=ALU.is_equal, op1=ALU.mult,
                                                                   accum_out=dest_f[:, k:k + 1]), [Bf("er"), Bf("ef"), Bf("pos")], [Bf("junk"), Bf("dest_f")])
                        V(lambda e, blk=blk: e.tensor_copy(out=dest_i[:, blk, :], in_=dest_f[:, :]), [Bf("dest_f")], [B_dest])
                        for k in range(4):
                            S.op("gpsimd", lambda e, k=k, blk=blk, x1bt=x1bt: e.indirect_dma_start(
                                out=xs_d[:, :], out_offset=bass.IndirectOffsetOnAxis(ap=dest_i[:, blk, k:k + 1], axis=0), in_=x1bt[:, :], in_offset=None,
                                bounds_check=breg(e, 1), oob_is_err=False), reads=[Bf("x1b%d" % sl), B_dest], writes=[], dma_sem=dsc[sl])

            gens = []
            tau0 = 0
            for s, n in enumerate(sizes):
                gens.append(body(s, n, tau0))
                tau0 += n

            def adv(i):
                if 0 <= i < len(gens):
                    try:
                        next(gens[i])
                    except StopIteration:
                        pass

            for s in range(len(sizes) + 3):
                adv(s)
                adv(s - 2)
                adv(s - 1)
                adv(s)
                adv(s - 1)
                adv(s)
                adv(s - 1)
                adv(s)
                adv(s - 1)
                if cast_jobs and s < len(sizes):
                    ncast = len(cast_jobs)
                    nsp = min(4, NST)
                    per = (ncast + nsp - 1) // nsp
                    if 1 <= s <= nsp:
                        for jn in range((s - 1) * per, min(s * per, ncast)):
                            issue_cast(cast_jobs[jn], jn)
                    if s == min(6, len(sizes) - 1):
                        issue_ag()
            for g_ in gens:
                for _ in g_:
                    pass
            S.emit()

        S = Sched(nc, "e")
        PB = [Buf("ps%d" % i) for i in range(8)]
        with ExitStack() as st:
            def sb(name, shape, dt):
                return st.enter_context(nc.sbuf_tensor(name, list(shape), dt))

            wu = [sb("wu%d" % i, [128, KC, 2 * D], BF16) for i in range(2)]
            wd = [sb("wd%d" % i, [128, FC, D], BF16) for i in range(2)]
            xs_t = [sb("xs_t%d" % i, [128, NB, D], BF16) for i in range(2)]
            xsT = [sb("xsT%d" % i, [128, KC, CAP], BF16) for i in range(2)]
            actT = [sb("actT%d" % i, [128, FC, CAP], BF16) for i in range(2)]
            xg = [sb("xg%d" % i, [128, CAP], F32) for i in range(2)]
            sg = [sb("sg%d" % i, [128, CAP], F32) for i in range(2)]
            xl = [sb("xl%d" % i, [128, CAP], F32) for i in range(2)]
            ys_t = [sb("ys_t%d" % i, [128, D], F32) for i in range(2)]
            Bn = {}

            def Bf(n):
                if n not in Bn:
                    Bn[n] = Buf(n)
                return Bn[n]

            V = lambda fn, r=(), w=(): S.op("vector", fn, reads=r, writes=w)
            A = lambda fn, r=(), w=(): S.op("scalar", fn, reads=r, writes=w)
            P = lambda fn, r=(), w=(): S.op("gpsimd", fn, reads=r, writes=w)
            TE = lambda fn, r=(), w=(): S.op("tensor", fn, reads=r, writes=w)
            dwu = [S.dsem("wu0"), S.dsem("wu1")]
            dwd = [S.dsem("wd0"), S.dsem("wd1")]
            dxs = [S.dsem("xs0"), S.dsem("xs1")]
            dys = [S.dsem("ys0"), S.dsem("ys1")]

            def load_w(e_):
                sl = e_ % 2
                if not cfg["repl"]:
                    S.op("sync", lambda e, sl=sl, e_=e_: e.dma_start(out=wu[sl][:, :, :], in_=wa_up[e_ * D:(e_ + 1) * D, :].rearrange("(kc p) f -> p kc f", p=128)),
                         writes=[Bf("wu%d" % sl)], dma_sem=dwu[sl])
                    S.op("sync", lambda e, sl=sl, e_=e_: e.dma_start(out=wd[sl][:, :, :], in_=wa_dn[e_ * D:(e_ + 1) * D, :].rearrange("(kc p) f -> p kc f", p=128)),
                         writes=[Bf("wd%d" % sl)], dma_sem=dwd[sl])
                    return
                nsp = 2 if KC >= 2 else 1
                kh = KC // nsp
                for h in range(nsp):
                    S.op("gpsimd", lambda e, h=h, sl=sl, e_=e_: e.dma_start(out=wu[sl][:, h * kh:(h + 1) * kh, :],
                                                                          in_=w_up[e_, h * kh * 128:(h + 1) * kh * 128, :].rearrange("(kc p) f -> p kc f", p=128)),
                         writes=[Bf("wu%d" % sl)], dma_sem=dwu[sl])
                S.op("gpsimd", lambda e, sl=sl, e_=e_: e.dma_start(out=wd[sl][:, :, :], in_=w_dn[e_, :, :].rearrange("(fc p) d -> p fc d", p=128)),
                     writes=[Bf("wd%d" % sl)], dma_sem=dwd[sl])

            def load_xs(e_):
                sl = e_ % 2
                S.op("sync", lambda e, sl=sl, e_=e_: e.dma_start(out=xs_t[sl][:, :, :], in_=xs_d[e_ * CAP:(e_ + 1) * CAP, :].rearrange("(b p) d -> p b d", p=128)),
                     reads=[B_xs], writes=[Bf("xs_t%d" % sl)], dma_sem=dxs[sl])

            load_w(0)
            load_xs(0)
            cnt = [0]
            for e_ in range(NE):
                sl = e_ % 2
                if e_ + 1 < NE:
                    load_w(e_ + 1)
                    load_xs(e_ + 1)
                Bwu, Bwd = Bf("wu%d" % sl), Bf("wd%d" % sl)
                for nb in range(NB):
                    b = 6 + nb % 2
                    pbf = ps[b][:, :].bitcast(BF16)
                    for kc in range(KC):
                        TE(lambda e, kc=kc, nb=nb, pbf=pbf, sl=sl: e.transpose(out=pbf[:, kc * 128:(kc + 1) * 128], in_=xs_t[sl][:, nb, kc * 128:(kc + 1) * 128], identity=ident_b[:, :]),
                           [Bf("xs_t%d" % sl)], [PB[b]])
                    cp = A if nb % 2 == 0 else V
                    if nb % 2 == 0:
                        A(lambda e, pbf=pbf, nb=nb, sl=sl: e.activation(out=xsT[sl][:, :, nb * 128:(nb + 1) * 128], in_=pbf[:, 0:KC * 128].rearrange("p (k t) -> p k t", k=KC), func=AF.Copy),
                          [PB[b]], [Bf("xsT%d" % sl)])
                    else:
                        V(lambda e, pbf=pbf, nb=nb, sl=sl: e.tensor_copy(out=xsT[sl][:, :, nb * 128:(nb + 1) * 128], in_=pbf[:, 0:KC * 128].rearrange("p (k t) -> p k t", k=KC)),
                          [PB[b]], [Bf("xsT%d" % sl)])
                for fp in range(FC):
                    cnt[0] += 1
                    pr = cnt[0] % 2
                    bG, bL = (0, 1) if pr == 0 else (2, 3)
                    for kc in range(KC):
                        TE(lambda e, kc=kc, fp=fp, bG=bG, sl=sl: e.matmul(ps[bG][:, 0:CAP], lhsT=wu[sl][:, kc, fp * 128:(fp + 1) * 128], rhs=xsT[sl][:, kc, :],
                                                                         start=(kc == 0), stop=(kc == KC - 1)), [Bwu, Bf("xsT%d" % sl)], [PB[bG]])
                    for kc in range(KC):
                        TE(lambda e, kc=kc, fp=fp, bL=bL, sl=sl: e.matmul(ps[bL][:, 0:CAP], lhsT=wu[sl][:, kc, D + fp * 128:D + (fp + 1) * 128], rhs=xsT[sl][:, kc, :],
                                                                         start=(kc == 0), stop=(kc == KC - 1)), [Bwu, Bf("xsT%d" % sl)], [PB[bL]])
                    V(lambda e, bG=bG, e_=e_, fp=fp, pr=pr: e.tensor_scalar(out=xg[pr][:, :], in0=ps[bG][:, 0:CAP], scalar1=bup[:, e_, fp:fp + 1], scalar2=7.0, op0=ALU.add, op1=ALU.min),
                      [PB[bG]], [Bf("xg%d" % pr)])
                    A(lambda e, pr=pr: e.activation(out=sg[pr][:, :], in_=xg[pr][:, :], func=AF.Sigmoid, scale=1.702), [Bf("xg%d" % pr)], [Bf("sg%d" % pr)])
                    V(lambda e, bL=bL, e_=e_, fp=fp, pr=pr: e.tensor_scalar(out=xl[pr][:, :], in0=ps[bL][:, 0:CAP], scalar1=bup[:, e_, FC + fp:FC + fp + 1], scalar2=7.0, op0=ALU.add, op1=ALU.min),
                      [PB[bL]], [Bf("xl%d" % pr)])
                    P(lambda e, pr=pr: e.tensor_scalar(out=xl[pr][:, :], in0=xl[pr][:, :], scalar1=7.0, scalar2=-7.0, op0=ALU.min, op1=ALU.max), [Bf("xl%d" % pr)], [Bf("xl%d" % pr)])
                    P(lambda e, pr=pr: e.tensor_tensor(out=xg[pr][:, :], in0=xg[pr][:, :], in1=sg[pr][:, :], op=ALU.mult), [Bf("xg%d" % pr), Bf("sg%d" % pr)], [Bf("xg%d" % pr)])
                    V(lambda e, pr=pr, fp=fp, sl=sl: e.scalar_tensor_tensor(out=actT[sl][:, fp, :], in0=xl[pr][:, :], scalar=1.0, in1=xg[pr][:, :], op0=ALU.add, op1=ALU.mult),
                      [Bf("xg%d" % pr), Bf("xl%d" % pr)], [Bf("actT%d" % sl)])
                for nb in range(NB):
                    ysl = (e_ * NB + nb) % 2
                    for h in range(NH2):
                        b = 4 + h % 2
                        for fc in range(FC):
                            TE(lambda e, fc=fc, nb=nb, h=h, b=b, sl=sl: e.matmul(ps[b][:, 0:W2], lhsT=actT[sl][:, fc, nb * 128:(nb + 1) * 128], rhs=wd[sl][:, fc, h * W2:(h + 1) * W2],
                                                                                start=(fc == 0), stop=False), [Bf("actT%d" % sl), Bwd], [PB[b]])
                        TE(lambda e, h=h, b=b, e_=e_: e.matmul(ps[b][:, 0:W2], lhsT=selT[:, e_, :], rhs=bdn_b[:, h * W2:(h + 1) * W2], start=False, stop=True),
                           [], [PB[b]])
                        if h % 2 == 0:
                            A(lambda e, b=b, h=h, ysl=ysl: e.activation(out=ys_t[ysl][:, h * W2:(h + 1) * W2], in_=ps[b][:, 0:W2], func=AF.Copy), [PB[b]], [Bf("ys_t%d" % ysl)])
                        else:
                            V(lambda e, b=b, h=h, ysl=ysl: e.tensor_copy(out=ys_t[ysl][:, h * W2:(h + 1) * W2], in_=ps[b][:, 0:W2]), [PB[b]], [Bf("ys_t%d" % ysl)])
                    r0 = e_ * CAP + nb * 128
                    S.op("sync", lambda e, ysl=ysl, r0=r0: e.dma_start(out=ys_d[r0:r0 + 128, :], in_=ys_t[ysl][:, :]), reads=[Bf("ys_t%d" % ysl)], writes=[], dma_sem=dys[ysl])
            S.emit()

        S = Sched(nc, "c")
        with ExitStack() as st:
            def sb(name, shape, dt):
                return st.enter_context(nc.sbuf_tensor(name, list(shape), dt))

            x1c = [sb("x1c%d" % i, [128, D], F32) for i in range(2)]
            yk = [[sb("yk%d_%d" % (i, k), [128, D], F32) for k in range(4)] for i in range(2)]
            accs = [sb("acc%d" % i, [128, D], F32) for i in range(2)]
            nrms = [sb("nrm2_%d" % i, [128, D], F32) for i in range(2)]
            res = [sb("res%d" % i, [128, D], F32) for i in range(2)]
            stt = sb("stt2", [128, max(NH2, 1) * 6], F32)
            mv = sb("mv2", [128, 2], F32)
            sm = sb("sm2", [128, 8], F32)
            Bn = {}

            def Bf(n):
                if n not in Bn:
                    Bn[n] = Buf(n)
                return Bn[n]

            V = lambda fn, r=(), w=(): S.op("vector", fn, reads=r, writes=w)
            A = lambda fn, r=(), w=(): S.op("scalar", fn, reads=r, writes=w)
            P = lambda fn, r=(), w=(): S.op("gpsimd", fn, reads=r, writes=w)
            S.op("sync", lambda e: e.dma_start(out=lnbc[:, :, :], in_=lnb[:, 2:4, :]), writes=[Bf("lnbc")], dma_sem=S.dsem("ln2", group=True))
            dxc = [S.dsem("xc0"), S.dsem("xc1")]
            dyk = [S.dsem("yk0"), S.dsem("yk1")]
            dout = [S.dsem("o0"), S.dsem("o1")]
            def fetch(blk):
                sl = blk % 2
                t0 = blk * 128
                S.op("sync", lambda e, sl=sl, t0=t0: e.dma_start(out=x1c[sl][:, :], in_=x1_d[t0:t0 + 128, :]), writes=[Bf("x1c%d" % sl)], dma_sem=dxc[sl])
                for k in range(4):
                    S.op("gpsimd", lambda e, k=k, sl=sl, blk=blk: e.indirect_dma_start(
                        out=yk[sl][k][:, :], out_offset=None, in_=ys_d[:, :], in_offset=bass.IndirectOffsetOnAxis(ap=dest_i[:, blk, k:k + 1], axis=0),
                        bounds_check=breg(e, 3), oob_is_err=False), writes=[Bf("yk%d_%d" % (sl, k))], dma_sem=dyk[sl])

            fetch(0)
            for blk in range(NBLK):
                sl = blk % 2
                t0 = blk * 128
                if blk + 1 < NBLK:
                    fetch(blk + 1)
                acc, nrm = accs[sl], nrms[sl]
                BA, BN = Bf("acc%d" % sl), Bf("nrm%d" % sl)
                V(lambda e, sl=sl, acc=acc: e.tensor_scalar(out=acc[:, :], in0=x1c[sl][:, :], scalar1=alpha, scalar2=None, op0=ALU.mult), [Bf("x1c%d" % sl)], [BA])
                for k in range(4):
                    V(lambda e, k=k, sl=sl, blk=blk, acc=acc: e.scalar_tensor_tensor(out=acc[:, :], in0=yk[sl][k][:, :], scalar=gate[:, blk, k:k + 1], in1=acc[:, :], op0=ALU.mult, op1=ALU.add),
                      [Bf("yk%d_%d" % (sl, k)), BA], [BA])
                for h in range(NH2):
                    V(lambda e, h=h, acc=acc: e.bn_stats(out=stt[:, h * 6:(h + 1) * 6], in_=acc[:, h * W2:(h + 1) * W2]), [BA], [Bf("stt")])
                V(lambda e: e.bn_aggr(out=mv[:, :], in_=stt[:, 0:NH2 * 6]), [Bf("stt")], [Bf("mv")])
                V(lambda e: e.tensor_scalar(out=sm[:, 0:1], in0=mv[:, 1:2], scalar1=EPS, scalar2=None, op0=ALU.add), [Bf("mv")], [Bf("sm")])
                A(lambda e: e.activation(out=sm[:, 1:2], in_=sm[:, 0:1], func=AF.Sqrt), [Bf("sm")], [Bf("sm")])
                V(lambda e: e.reciprocal(out=sm[:, 2:3], in_=sm[:, 1:2]), [Bf("sm")], [Bf("sm")])
                V(lambda e: e.scalar_tensor_tensor(out=sm[:, 3:4], in0=mv[:, 0:1], scalar=-1.0, in1=sm[:, 2:3], op0=ALU.mult, op1=ALU.mult), [Bf("mv"), Bf("sm")], [Bf("sm")])
                A(lambda e, acc=acc, nrm=nrm: e.activation(out=nrm[:, :], in_=acc[:, :], func=AF.Identity, scale=sm[:, 2:3], bias=sm[:, 3:4]), [BA, Bf("sm")], [BN])
                P(lambda e, nrm=nrm: e.tensor_tensor(out=nrm[:, :], in0=nrm[:, :], in1=lnbc[:, 0, :], op=ALU.mult), [BN, Bf("lnbc")], [BN])
                P(lambda e, sl=sl, nrm=nrm: e.tensor_tensor(out=res[sl][:, :], in0=nrm[:, :], in1=lnbc[:, 1, :], op=ALU.add), [BN, Bf("lnbc")], [Bf("res%d" % sl)])
                S.op("sync", lambda e, sl=sl, t0=t0: e.dma_start(out=out[t0:t0 + 128, :], in_=res[sl][:, :]), reads=[Bf("res%d" % sl)], writes=[], dma_sem=dout[sl])
            S.emit()
    return nc


def prep_inputs(cfg, x, w_in, b_in, attn_sinks, w_attn_br, conv_w, conv_b, conv_ln_g, conv_ln_b,
                w_conv_br, b_conv_br, w_o, ln1_g, ln1_b, w_router, b_router, w_up, b_up,
                w_down, b_down, ln2_g, ln2_b):
    D, NH, CC, NE, T, CAP = (cfg[k] for k in ["D", "NH", "CC", "NE", "T", "CAP"])
    G, AW, QE, KE, VE, CE, INW, KC, CCH, FC = (cfg[k] for k in ["G", "AW", "QE", "KE", "VE", "CE", "INW", "KC", "CCH", "FC"])
    NC_ = cfg["NCORES"]
    CPB = NC_ // cfg["B"]
    f = lambda a: np.ascontiguousarray(np.asarray(a, dtype=np.float32))
    x = f(x)
    w_in0, b_in0 = f(w_in)[0], f(b_in)[0]
    qperm = np.array([(g * G + c) * 64 + j for c in range(G) for g in range(2) for j in range(64)])
    perm = np.concatenate([qperm, np.arange(QE, INW)])
    w_in_p = np.ascontiguousarray(w_in0[:, perm])
    b_in_p = b_in0[perm]
    b_in_t = np.ascontiguousarray(b_in_p.reshape(INW // 128, 128).T)
    b_v = np.ascontiguousarray(b_in_p[KE:VE].reshape(1, 128))
    sinks_b = np.ascontiguousarray(np.broadcast_to(f(attn_sinks)[0][None, :], (128, NH)))
    cw_t = np.ascontiguousarray(f(conv_w)[0].T.reshape(CCH, 128, 31).transpose(1, 0, 2))
    pp = lambda v: v.reshape(-1, 128).T
    cvec = np.ascontiguousarray(np.stack([pp(f(conv_b)[0]), pp(f(conv_ln_g)[0]), pp(f(conv_ln_b)[0])], axis=1))
    bcb_t = np.ascontiguousarray(pp(f(b_conv_br)[0]))
    lnb = np.ascontiguousarray(np.broadcast_to(np.stack([f(ln1_g)[0], f(ln1_b)[0], f(ln2_g)[0], f(ln2_b)[0]])[None], (128, 4, D)))
    w_up0 = f(w_up)[0]
    w_up_p = np.concatenate([w_up0[:, :, 0::2], w_up0[:, :, 1::2]], axis=2)
    b_up0 = f(b_up)[0]
    b_up_p = np.concatenate([b_up0[:, 0::2], b_up0[:, 1::2]], axis=1)
    b_up_t = np.ascontiguousarray(b_up_p.reshape(NE, 2 * FC, 128).transpose(2, 0, 1))
    w_dn0 = f(w_down)[0]
    b_dn = np.ascontiguousarray(f(b_down)[0])
    ident = np.eye(128, dtype=np.float32)
    ltri = np.triu(np.ones((128, 128), np.float32), 1)
    consts = np.ascontiguousarray(np.stack([ident, ltri, np.ones((128, 128), np.float32)], axis=1))
    erow = np.ascontiguousarray(np.broadcast_to(np.stack([np.arange(NE, dtype=np.float32), np.arange(NE, dtype=np.float32) * CAP])[None], (128, 2, NE)))
    kk = np.arange(128)[:, None]
    qq = np.arange(128)[None, :]
    m_cur = (kk <= qq).astype(np.float32)
    m_prev = (kk > qq).astype(np.float32)
    shared = dict(w_in=w_in_p, b_in_t=b_in_t, b_v=b_v, sinks_b=sinks_b, w_ab=f(w_attn_br)[0], cw_t=cw_t, cvec=cvec,
                  w_cb=f(w_conv_br)[0], bcb_t=bcb_t, w_o=f(w_o)[0], lnb=lnb, w_r=f(w_router)[0], b_r=f(b_router)[0].reshape(1, NE),
                  b_up_t=b_up_t, b_dn=b_dn, consts=consts, erow=erow)
    maps = []
    for c in range(NC_):
        b, h = c // CPB, c % CPB
        st = h * T
        xT = np.zeros((D, 128 + T), np.float32)
        xT[:, 128:] = x[b, st:st + T].T
        if h > 0:
            xT[:, :128] = x[b, st - 128:st].T
        fl = 1.0 if h > 0 else 0.0
        masks = np.stack([np.tile(m_cur, (1, G)), np.tile(m_prev, (1, G)), np.tile(m_prev * fl, (1, G))], axis=1)
        m = dict(shared)
        m.update(xT=xT, xtok=np.ascontiguousarray(x[b, st:st + T]), masks=np.ascontiguousarray(masks),
                 flag=np.full((128, 1), fl, np.float32))
        if cfg["repl"]:
            m.update(w_up=w_up_p, w_dn=w_dn0)
        else:
            E = cfg["EPC"]
            m.update(w_up=np.ascontiguousarray(w_up_p[c * E:(c + 1) * E]), w_dn=np.ascontiguousarray(w_dn0[c * E:(c + 1) * E]))
        maps.append(m)
    return maps


def run_cfg(cfg, inputs, trace=False):
    maps = prep_inputs(cfg, **inputs)
    nc = build(cfg)
    res = run_bass_kernel_spmd(nc, maps, core_ids=list(range(cfg["NCORES"])), trace=trace)
    T, D, B = cfg["T"], cfg["D"], cfg["B"]
    CPB = cfg["NCORES"] // B
    outv = np.zeros((B, CPB * T, D), np.float32)
    for c in range(cfg["NCORES"]):
        b, h = c // CPB, c % CPB
        outv[b, h * T:(h + 1) * T] = res.results[c]["out"]
    return outv, res


def kernel(**inputs):
    cfg = make_cfg()
    outv, _ = run_cfg(cfg, inputs)
    return outv
```

```python
from contextlib import ExitStack
import numpy as np
import concourse.bass as bass
import concourse.mybir as mybir
from concourse.bass_utils import run_bass_kernel_spmd

F32 = mybir.dt.float32
BF16 = mybir.dt.bfloat16
I32 = mybir.dt.int32
U32 = mybir.dt.uint32
AF = mybir.ActivationFunctionType
ALU = mybir.AluOpType
ENGS = ["tensor", "vector", "scalar", "gpsimd", "sync"]


class Buf:
    __slots__ = ("name", "writers", "readers")

    def __init__(self, name):
        self.name = name
        self.writers = []
        self.readers = []


class Op:
    __slots__ = ("eng", "fn", "deps", "marked", "is_dma", "sem", "val")

    def __init__(self, eng, fn, is_dma=False, sem=None):
        self.eng = eng
        self.fn = fn
        self.deps = []
        self.marked = False
        self.is_dma = is_dma
        self.sem = sem
        self.val = None


class DSem:
    def __init__(self, name, group=False, inc=16):
        self.name = name
        self.count = 0
        self.handle = None
        self.group = group
        self.inc = inc


def _prune(lst, op):
    if op.is_dma:
        out = [o for o in lst if not (o.is_dma and o.sem is op.sem)]
    else:
        out = [o for o in lst if o.is_dma or o.eng != op.eng]
    out.append(op)
    return out


class Sched:
    def __init__(self, nc, tag):
        self.nc = nc
        self.tag = tag
        self.ops = {e: [] for e in ENGS}
        self.dsems = []
        self.dma_ops = []

    def dsem(self, name, group=False, inc=16):
        s = DSem(name, group, inc)
        self.dsems.append(s)
        return s

    def op(self, eng, fn, reads=(), writes=(), dma_sem=None):
        o = Op(eng, fn, is_dma=dma_sem is not None, sem=dma_sem)
        deps = []
        for b in reads:
            deps.extend(b.writers)
        for b in writes:
            deps.extend(b.writers)
            deps.extend(b.readers)
        seen = set()
        for d in deps:
            if id(d) in seen:
                continue
            seen.add(id(d))
            if (not d.is_dma) and (not o.is_dma) and d.eng == "tensor" and eng == "tensor":
                continue
            if d.is_dma and o.is_dma and d.sem is o.sem:
                continue
            o.deps.append(d)
            d.marked = True
        if o.is_dma:
            dma_sem.count += dma_sem.inc
            o.val = dma_sem.count
            o.marked = True
            self.dma_ops.append(o)
        for b in reads:
            b.readers = _prune(b.readers, o)
        for b in writes:
            b.writers = [o]
            b.readers = []
        self.ops[eng].append(o)
        return o

    def emit(self):
        nc = self.nc
        with ExitStack() as st:
            esem = {e: st.enter_context(nc.semaphore(self.tag + "_s_" + e)) for e in ENGS}
            for s in self.dsems:
                s.handle = st.enter_context(nc.semaphore(self.tag + "_d_" + s.name))
            for e in ENGS:
                c = 0
                for o in self.ops[e]:
                    if not o.is_dma and o.marked:
                        c += 1
                        o.val = c
            finals = {}
            for o in self.dma_ops:
                finals[id(o.sem)] = o
            block = st.enter_context(nc.Block())

            def run(e, eng):
                waited = {}
                for o in self.ops[e]:
                    for d in o.deps:
                        if d.is_dma:
                            key, h = id(d.sem), d.sem.handle
                            dv = d.sem.count if d.sem.group else d.val
                        else:
                            key, h = d.eng, esem[d.eng]
                            dv = d.val
                        if waited.get(key, 0) >= dv:
                            continue
                        waited[key] = dv
                        eng.wait_ge(h, dv)
                    inst = o.fn(eng)
                    if o.is_dma:
                        if o.sem.inc == 16:
                            inst.then_inc(o.sem.handle, 16)
                        else:
                            inst.then_inc(o.sem.handle)
                    elif o.marked:
                        inst.then_inc(esem[e], 1)
                if e == "sync":
                    for d in finals.values():
                        eng.wait_ge(d.sem.handle, d.val)

            @block.tensor
            def _(eng):
                run("tensor", eng)

            @block.vector
            def _(eng):
                run("vector", eng)

            @block.scalar
            def _(eng):
                run("scalar", eng)

            @block.gpsimd
            def _(eng):
                run("gpsimd", eng)

            @block.sync
            def _(eng):
                run("sync", eng)


def make_cfg(D=1024, NH=8, CC=512, NE=32, T=2048, CAP=384, ST=256, NCORES=8, B=4, repl=True,
             alpha=2 ** 0.25):
    c = dict(D=D, NH=NH, CC=CC, NE=NE, T=T, CAP=CAP, ST=ST, NCORES=NCORES, B=B, repl=repl, alpha=alpha)
    c["G"] = NH // 2
    c["AW"] = NH * 64
    c["QE"] = c["AW"]
    c["KE"] = c["QE"] + 128
    c["VE"] = c["KE"] + 128
    c["CE"] = c["VE"] + 2 * CC
    c["INW"] = c["CE"] + 2 * D
    c["KC"] = D // 128
    c["AC"] = c["AW"] // 128
    c["CCH"] = CC // 128
    c["FC"] = D // 128
    c["W2"] = min(D, 512)
    c["NH2"] = D // c["W2"]
    c["NBLK"] = T // 128
    c["NB"] = CAP // 128
    c["EPC"] = NE // NCORES
    c["SEQ"] = T * (NCORES // B)
    return c


def build(cfg):
    D, NH, CC, NE, T, CAP, ST = (cfg[k] for k in ["D", "NH", "CC", "NE", "T", "CAP", "ST"])
    G, AW, QE, KE, VE, CE, INW = (cfg[k] for k in ["G", "AW", "QE", "KE", "VE", "CE", "INW"])
    KC, AC, CCH, FC, W2, NH2, NBLK, NB = (cfg[k] for k in ["KC", "AC", "CCH", "FC", "W2", "NH2", "NBLK", "NB"])
    alpha = float(cfg["alpha"])
    NU = INW // 128
    GW = G * 128
    PADL = 32
    NEW = NE if cfg["repl"] else cfg["EPC"]
    BIG = 4.0e6
    EPS = 1e-5

    nc = bass.Bass("TRN2", target_bir_lowering=False)

    def din(name, shape, dt=F32):
        return nc.dram_tensor(name, list(shape), dt, kind="ExternalInput").ap()

    xT = din("xT", [D, 128 + T])
    xtok = din("xtok", [T, D])
    masks = din("masks", [128, 3, GW])
    flag = din("flag", [128, 1])
    w_in = din("w_in", [D, INW])
    b_in_t = din("b_in_t", [128, NU])
    b_v = din("b_v", [1, 128])
    sinks_b = din("sinks_b", [128, NH])
    w_ab = din("w_ab", [AW, D])
    cw_t = din("cw_t", [128, CCH, 31])
    cvec = din("cvec", [128, 3, CCH])
    w_cb = din("w_cb", [CC, D])
    bcb_t = din("bcb_t", [128, KC])
    w_o = din("w_o", [D, D])
    lnb = din("lnb", [128, 4, D])
    w_r = din("w_r", [D, NE])
    b_r = din("b_r", [1, NE])
    w_up = din("w_up", [NEW, D, 2 * D])
    b_up_t = din("b_up_t", [128, NE, 2 * FC])
    w_dn = din("w_dn", [NEW, D, D])
    b_dn = din("b_dn", [NE, D])
    consts = din("consts", [128, 3, 128])
    erow = din("erow", [128, 2, NE])
    out = nc.dram_tensor("out", [T, D], F32, kind="ExternalOutput").ap()
    xs_d = nc.dram_tensor("xs_d", [NE * CAP, D], BF16, kind="Internal").ap()
    ys_d = nc.dram_tensor("ys_d", [NE * CAP, D], F32, kind="Internal").ap()
    x1_d = nc.dram_tensor("x1_d", [T, D], F32, kind="Internal").ap()
    if not cfg["repl"]:
        EPC = cfg["EPC"]
        wl_up_t = nc.dram_tensor("wl_up", [EPC * D, 2 * D], BF16)
        wa_up_t = nc.dram_tensor("wa_up", [NE * D, 2 * D], BF16)
        wl_dn_t = nc.dram_tensor("wl_dn", [EPC * D, D], BF16)
        wa_dn_t = nc.dram_tensor("wa_dn", [NE * D, D], BF16)
        wl_up, wa_up, wl_dn, wa_dn = wl_up_t.ap(), wa_up_t.ap(), wl_dn_t.ap(), wa_dn_t.ap()

    regs = {}

    def breg(e, phase):
        if phase not in regs:
            regs[phase] = e.to_reg(NE * CAP - 1)
        return regs[phase]

    with ExitStack() as pst:
        def sbp(name, shape, dt):
            return pst.enter_context(nc.sbuf_tensor(name, list(shape), dt))

        dest_i = sbp("dest_i", [128, NBLK, 4], I32)
        gate = sbp("gate", [128, NBLK, 4], F32)
        ident_f = sbp("ident_f", [128, 128], F32)
        ident_b = sbp("ident_b", [128, 128], BF16)
        ones_b = sbp("ones_b", [128, 128], BF16)
        ones_f = sbp("ones_f", [128, 128], F32)
        lnbc = sbp("lnbc", [128, 2, D], F32)
        bdn_b = sbp("bdn_b", [NE, D], BF16)
        selT = sbp("selT", [NE, NE, 128], BF16)
        bup = sbp("bup", [128, NE, 2 * FC], F32)
        ps = [pst.enter_context(nc.psum_tensor("ps%d" % i, [128, 512], F32)) for i in range(8)]
        B_dest, B_gate, B_xs, B_ys, B_x1d = Buf("dest"), Buf("gate"), Buf("xs"), Buf("ys"), Buf("x1d")
        B_const = Buf("const")

        S = Sched(nc, "m")
        PB = [Buf("ps%d" % i) for i in range(8)]
        with ExitStack() as st:
            def sb(name, shape, dt):
                return st.enter_context(nc.sbuf_tensor(name, list(shape), dt))

            w_in_bf = sb("w_in_bf", [128, KC, INW], BF16)
            w_ab_bf = sb("w_ab_bf", [128, AC, D], BF16)
            w_cb_bf = sb("w_cb_bf", [128, CCH, D], BF16)
            w_o_bf = sb("w_o_bf", [128, KC, D], BF16)
            diag2 = [sb("diag%d" % i, [128, 31, 128], BF16) for i in range(2)]
            w_r_f = sb("w_r_f", [128, KC, NE], F32)
            b_r_f = sb("b_r_f", [1, NE], F32)
            bin_t = sb("bin_t", [128, NU], F32)
            bv_b = sb("bv_b", [1, 128], BF16)
            esink = sb("esink", [128, NH], F32)
            cw = sb("cw", [128, CCH, 31], F32)
            cv = sb("cv", [128, 3, CCH], F32)
            bcb = sb("bcb", [128, KC], F32)
            mk = sb("mk", [128, 3, GW], BF16)
            flg = sb("flg", [128, 1], F32)
            cst = sb("cst", [128, 3, 128], F32)
            ltri_b = sb("ltri_b", [128, 128], BF16)
            onesm_f = sb("onesm_f", [128, 128], F32)
            er = sb("er", [128, 2, NE], F32)
            run_c = sb("run_c", [128, NE], F32)
            xT_bf = [sb("xT_bf%d" % i, [128, KC, ST], BF16) for i in range(2)]
            qT = sb("qT", [128, AC, ST], BF16)
            kT = sb("kT", [128, 128 + ST], BF16)
            Vr = sb("Vr", [128, 1 + ST // 128, 2, 65], BF16)
            aT = sb("aT", [128, CCH, PADL + ST], BF16)
            sgt = sb("sgt", [128, ST], F32)
            Pp = [sb("Pp%d" % i, [128, GW], BF16) for i in range(4)]
            Pc = [sb("Pc%d" % i, [128, GW], BF16) for i in range(4)]
            dens = [sb("den%d" % i, [128, G], F32) for i in range(2)]
            o_n = sb("o_n", [128, AW], BF16)
            oT = sb("oT", [128, AC, ST], BF16)
            yb = sb("yb", [128, CCH, ST], F32)
            sq = sb("sq", [128, CCH, ST], F32)
            mean_sb = sb("mean_sb", [128, ST], F32)
            var_sb = sb("var_sb", [128, ST], F32)
            tmpc = sb("tmpc", [128, ST], F32)
            sT = sb("sT", [128, CCH, ST], BF16)
            ga = [sb("ga%d" % i, [128, ST], F32) for i in range(2)]
            gb = [sb("gb%d" % i, [128, ST], F32) for i in range(2)]
            t1s = [sb("t1_%d" % i, [128, ST], F32) for i in range(1)] * 2
            t2s = [sb("t2_%d" % i, [128, ST], F32) for i in range(1)] * 2
            mg = sb("mg", [128, KC, ST], BF16)
            xt = [sb("xt%d" % i, [128, D], F32) for i in range(1)]
            z = sb("z", [128, D], F32)
            x1 = [sb("x1_%d" % i, [128, D], F32) for i in range(1)]
            x1b = [sb("x1b%d" % i, [128, D], BF16) for i in range(1)]
            x1T = sb("x1T", [128, KC, 128], F32)
            stt = sb("stt", [128, max(NH2, 1) * 6], F32)
            mv = sb("mv", [128, 2], F32)
            sm = sb("sm", [128, 8], F32)
            lg = sb("lg", [128, NE], F32)
            top8 = sb("top8", [128, 8], F32)
            tidx = sb("tidx", [128, 8], U32)
            ef = sb("ef", [128, 4], F32)
            ex4 = sb("ex4", [128, 4], F32)
            mskb = sb("mskb", [128, NE], BF16)
            pos = sb("pos", [128, NE], F32)
            ovf = sb("ovf", [128, NE], F32)
            junk = sb("junk", [128, NE], F32)
            dest_f = sb("dest_f", [128, 4], F32)

            Bn = {}

            def Bf(n):
                if n not in Bn:
                    Bn[n] = Buf(n)
                return Bn[n]

            dc = S.dsem("c", group=True)
            dw = S.dsem("w", group=True)

            def ld(eng, o_ap, i_ap, bufname, sem=dc, **kw):
                S.op(eng, lambda e: e.dma_start(out=o_ap, in_=i_ap, **kw), writes=[Bf(bufname)], dma_sem=sem)

            ld("sync", cst[:, :, :], consts, "cst")
            ld("sync", er[:, :, :], erow, "er")
            ld("sync", lnbc[:, :, :], lnb[:, 0:2, :], "lnbc")
            ld("sync", bup[:, :, :], b_up_t, "bup")
            ld("sync", w_r_f[:, :, :], w_r.rearrange("(kc p) n -> p kc n", p=128), "w_r")
            ld("sync", b_r_f[:, :], b_r, "b_r")
            ld("sync", bin_t[:, :], b_in_t, "bin")
            ld("sync", esink[:, :], sinks_b, "esink")
            ld("sync", cw[:, :, :], cw_t, "cw")
            ld("sync", cv[:, :, :], cvec, "cv")
            ld("sync", bcb[:, :], bcb_t, "bcb")
            ld("gpsimd", mk[:, :, :], masks, "mk")
            ld("sync", flg[:, :], flag, "flg")
            ld("gpsimd", bv_b[:, :], b_v, "bv")
            ld("gpsimd", bdn_b[:, :], b_dn, "bdn")
            HW = INW // 2
            for kc in range(KC):
                for hh in range(2):
                    ld("gpsimd", w_in_bf[:, kc, hh * HW:(hh + 1) * HW], w_in[kc * 128:(kc + 1) * 128, hh * HW:(hh + 1) * HW], "w_in", sem=dw)
            for ac in range(AC):
                ld("gpsimd", w_ab_bf[:, ac, :], w_ab[ac * 128:(ac + 1) * 128, :], "w_ab", sem=dw)
            for c in range(CCH):
                ld("gpsimd", w_cb_bf[:, c, :], w_cb[c * 128:(c + 1) * 128, :], "w_cb", sem=dw)
            for kc in range(KC):
                ld("gpsimd", w_o_bf[:, kc, :], w_o[kc * 128:(kc + 1) * 128, :], "w_o", sem=dw)

            V = lambda fn, r=(), w=(): S.op("vector", fn, reads=r, writes=w)
            A = lambda fn, r=(), w=(): S.op("scalar", fn, reads=r, writes=w)
            P = lambda fn, r=(), w=(): S.op("gpsimd", fn, reads=r, writes=w)
            TE = lambda fn, r=(), w=(): S.op("tensor", fn, reads=r, writes=w)

            cast_jobs = []
            if not cfg["repl"]:
                dwl = S.dsem("wl", group=True)
                dag = S.dsem("ag", group=True, inc=1)
                for i in range(EPC):
                    for kc in range(KC):
                        cast_jobs.append((0, i, kc))
                        cast_jobs.append((1, i, kc))

            def issue_cast(job, jn):
                which, i, kc = job
                r0 = i * D + kc * 128
                if which == 0:
                    S.op("gpsimd", lambda e: e.dma_start(out=wl_up[r0:r0 + 128, :], in_=w_up[i, kc * 128:(kc + 1) * 128, :]), writes=[Bf("wl")], dma_sem=dwl)
                else:
                    S.op("gpsimd", lambda e: e.dma_start(out=wl_dn[r0:r0 + 128, :], in_=w_dn[i, kc * 128:(kc + 1) * 128, :]), writes=[Bf("wl")], dma_sem=dwl)

            def issue_ag():
                grp = [list(range(cfg["NCORES"]))]
                S.op("gpsimd", lambda e: e.collective_compute("AllGather", ALU.bypass, replica_groups=grp, ins=[wl_up_t.ap().opt()], outs=[wa_up_t.ap().opt()]),
                     reads=[Bf("wl")], writes=[Bf("wa")], dma_sem=dag)
                S.op("gpsimd", lambda e: e.collective_compute("AllGather", ALU.bypass, replica_groups=grp, ins=[wl_dn_t.ap().opt()], outs=[wa_dn_t.ap().opt()]),
                     reads=[Bf("wl")], writes=[Bf("wa")], dma_sem=dag)

            V(lambda e: e.tensor_copy(out=ident_f[:, :], in_=cst[:, 0, :]), [Bf("cst")], [Bf("ident_f")])
            V(lambda e: e.tensor_copy(out=ident_b[:, :], in_=cst[:, 0, :]), [Bf("cst")], [Bf("ident_b")])
            V(lambda e: e.tensor_copy(out=ltri_b[:, :], in_=cst[:, 1, :]), [Bf("cst")], [Bf("ltri")])
            V(lambda e: e.tensor_copy(out=ones_b[:, :], in_=cst[:, 2, :]), [Bf("cst")], [Bf("ones_b")])
            V(lambda e: e.tensor_copy(out=ones_f[:, :], in_=cst[:, 2, :]), [Bf("cst")], [Bf("ones_f")])
            V(lambda e: e.tensor_scalar(out=onesm_f[:, :], in0=cst[:, 2, :], scalar1=1.0 / CC, scalar2=None, op0=ALU.mult),
              [Bf("cst")], [Bf("onesm")])
            V(lambda e: e.tensor_copy(out=selT[:, :, :], in_=cst[0:NE, 0, 0:NE].unsqueeze(2).to_broadcast([NE, NE, 128])), [Bf("cst")], [Bf("selT")])
            A(lambda e: e.activation(out=esink[:, :], in_=esink[:, :], func=AF.Exp), [Bf("esink")], [Bf("esink")])
            P(lambda e: e.memset(Vr[:, :, :, :], 1.0), [], [Bf("Vr")])
            P(lambda e: e.memset(run_c[:, :], 0.0), [], [Bf("run")])
            P(lambda e: e.memset(aT[:, :, :], 0.0), [], [Bf("aT")])
            P(lambda e: e.memset(kT[:, :], 0.0), [], [Bf("kT")])

            rot = [0]

            def nbank(pool=(0, 1, 2, 3)):
                rot[0] += 1
                return pool[rot[0] % len(pool)]

            dx = [S.dsem("x0"), S.dsem("x1")]
            dxt = [S.dsem("xt0"), S.dsem("xt1")]
            dx1 = [S.dsem("x1s0"), S.dsem("x1s1")]
            dsc = [S.dsem("sc0"), S.dsem("sc1")]

            NST = T // ST
            sizes = [128] + [ST] * NST
            tau0 = 0
            def body(s, n, tau0):
                    xs_ = s % 2
                    XB = xT_bf[xs_]
                    BX = Bf("xT_bf%d" % xs_)
                    for kc in range(KC):
                        S.op("gpsimd", lambda e, kc=kc, XB=XB, tau0=tau0, n=n: e.dma_start(
                            out=XB[:, kc, 0:n], in_=xT[kc * 128:(kc + 1) * 128, tau0:tau0 + n]), writes=[BX], dma_sem=dx[xs_])

                    def proj(ch, n=n, XB=XB, BX=BX):
                        b = nbank()
                        for kc in range(KC):
                            TE(lambda e, kc=kc, b=b, ch=ch: e.matmul(ps[b][:, 0:n], lhsT=w_in_bf[:, kc, ch * 128:(ch + 1) * 128],
                                                                     rhs=XB[:, kc, 0:n], start=(kc == 0), stop=(kc == KC - 1)),
                               [Bf("w_in"), BX], [PB[b]])
                        return b

                    if s > 0:
                        for c in range(AC):
                            b = proj(c)
                            A(lambda e, b=b, c=c, n=n: e.activation(out=qT[:, c, 0:n], in_=ps[b][:, 0:n], func=AF.Identity,
                                                                     bias=bin_t[:, c:c + 1], scale=1.0), [PB[b], Bf("bin")], [Bf("qT")])
                    b = proj(AC)
                    A(lambda e, b=b, n=n: e.activation(out=kT[:, 128:128 + n], in_=ps[b][:, 0:n], func=AF.Identity,
                                                       bias=bin_t[:, AC:AC + 1], scale=1.0), [PB[b], Bf("bin")], [Bf("kT")])
                    for bb in range(n // 128):
                        b = nbank()
                        for kc in range(KC):
                            TE(lambda e, kc=kc, b=b, bb=bb, XB=XB: e.matmul(ps[b][:, 0:128], lhsT=XB[:, kc, bb * 128:(bb + 1) * 128],
                                                                            rhs=w_in_bf[:, kc, KE:VE], start=(kc == 0), stop=False),
                               [Bf("w_in"), BX], [PB[b]])
                        TE(lambda e, b=b: e.matmul(ps[b][:, 0:128], lhsT=ones_b[0:1, :], rhs=bv_b[0:1, :], start=False, stop=True),
                           [Bf("ones_b"), Bf("bv")], [PB[b]])
                        A(lambda e, b=b, bb=bb: e.activation(out=Vr[:, 1 + bb, :, 0:64], in_=ps[b][:, 0:128].rearrange("p (g d) -> p g d", g=2), func=AF.Copy),
                          [PB[b]], [Bf("Vr")])
                    for c in range(CCH):
                        bg = proj(AC + 2 + CCH + c)
                        A(lambda e, bg=bg, c=c, n=n: e.activation(out=sgt[:, 0:n], in_=ps[bg][:, 0:n], func=AF.Sigmoid,
                                                                   bias=bin_t[:, AC + 2 + CCH + c:AC + 3 + CCH + c], scale=1.0),
                          [PB[bg], Bf("bin")], [Bf("sgt")])
                        ba = proj(AC + 2 + c)
                        V(lambda e, ba=ba, c=c, n=n: e.scalar_tensor_tensor(out=aT[:, c, PADL:PADL + n], in0=ps[ba][:, 0:n],
                                                                            scalar=bin_t[:, AC + 2 + c:AC + 3 + c], in1=sgt[:, 0:n],
                                                                            op0=ALU.add, op1=ALU.mult),
                          [PB[ba], Bf("bin"), Bf("sgt")], [Bf("aT")])

                    yield "A1"
                    if s > 0:
                        nqb = n // 128
                        for qb in range(nqb):
                            gq = (s - 1) * (ST // 128) + qb
                            mprev = 2 if gq == 0 else 1
                            for g in range(2):
                                rows = slice(64 * g, 64 * g + 64)
                                bP, bC = 2 + 2 * g, 3 + 2 * g
                                pi = (qb % 2) * 2 + g
                                pp, pc = Pp[pi], Pc[pi]
                                TE(lambda e, rows=rows, bP=bP, qb=qb: e.matmul(ps[bP][:, 0:GW], lhsT=kT[rows, qb * 128:qb * 128 + 128],
                                                                              rhs=qT[rows, :, qb * 128:(qb + 1) * 128], start=True, stop=True),
                                   [Bf("kT"), Bf("qT")], [PB[bP]])
                                TE(lambda e, rows=rows, bC=bC, qb=qb: e.matmul(ps[bC][:, 0:GW], lhsT=kT[rows, 128 + qb * 128:256 + qb * 128],
                                                                              rhs=qT[rows, :, qb * 128:(qb + 1) * 128], start=True, stop=True),
                                   [Bf("kT"), Bf("qT")], [PB[bC]])
                                A(lambda e, bP=bP, pp=pp: e.activation(out=pp[:, :], in_=ps[bP][:, 0:GW], func=AF.Exp, scale=0.125),
                                  [PB[bP]], [Bf("Pp%d" % pi)])
                                A(lambda e, bC=bC, pc=pc: e.activation(out=pc[:, :], in_=ps[bC][:, 0:GW], func=AF.Exp, scale=0.125),
                                  [PB[bC]], [Bf("Pc%d" % pi)])
                                P(lambda e, pp=pp, mprev=mprev: e.tensor_tensor(out=pp[:, :], in0=pp[:, :], in1=mk[:, mprev, :], op=ALU.mult),
                                  [Bf("Pp%d" % pi), Bf("mk")], [Bf("Pp%d" % pi)])
                                P(lambda e, pc=pc: e.tensor_tensor(out=pc[:, :], in0=pc[:, :], in1=mk[:, 0, :], op=ALU.mult),
                                  [Bf("Pc%d" % pi), Bf("mk")], [Bf("Pc%d" % pi)])
                        for qb in range(nqb):
                            for g in range(2):
                                bO = 6 + g
                                pi = (qb % 2) * 2 + g
                                pp, pc = Pp[pi], Pc[pi]
                                for c in range(G):
                                    TE(lambda e, c=c, bO=bO, pp=pp, qb=qb, g=g: e.matmul(ps[bO][:, c * 65:(c + 1) * 65], lhsT=pp[:, c * 128:(c + 1) * 128],
                                                                                        rhs=Vr[:, qb, g, :], start=True, stop=False),
                                       [Bf("Pp%d" % pi), Bf("Vr")], [PB[bO]])
                                    TE(lambda e, c=c, bO=bO, pc=pc, qb=qb, g=g: e.matmul(ps[bO][:, c * 65:(c + 1) * 65], lhsT=pc[:, c * 128:(c + 1) * 128],
                                                                                        rhs=Vr[:, qb + 1, g, :], start=False, stop=True),
                                       [Bf("Pc%d" % pi), Bf("Vr")], [PB[bO]])
                                o3 = ps[bO][:, 0:G * 65].rearrange("p (c d) -> p c d", c=G)
                                dn = dens[g]
                                V(lambda e, o3=o3, g=g, dn=dn: e.tensor_tensor(out=dn[:, :], in0=o3[:, :, 64], in1=esink[:, g * G:(g + 1) * G], op=ALU.add),
                                  [PB[bO], Bf("esink")], [Bf("den%d" % g)])
                                V(lambda e, dn=dn: e.reciprocal(out=dn[:, :], in_=dn[:, :]), [Bf("den%d" % g)], [Bf("den%d" % g)])
                                V(lambda e, o3=o3, g=g, dn=dn: e.tensor_tensor(out=o_n[:, g * G * 64:(g + 1) * G * 64].rearrange("p (c d) -> p c d", c=G),
                                                                               in0=o3[:, :, 0:64], in1=dn[:, :].unsqueeze(2).to_broadcast([128, G, 64]), op=ALU.mult),
                                  [PB[bO], Bf("den%d" % g)], [Bf("o_n")])
                            b = nbank((0, 1))
                            pbf = ps[b][:, :].bitcast(BF16)
                            for ac in range(AC):
                                TE(lambda e, ac=ac, pbf=pbf: e.transpose(out=pbf[:, ac * 128:(ac + 1) * 128], in_=o_n[:, ac * 128:(ac + 1) * 128], identity=ident_b[:, :]),
                                   [Bf("o_n"), Bf("ident_b")], [PB[b]])
                            A(lambda e, pbf=pbf, qb=qb: e.activation(out=oT[:, :, qb * 128:(qb + 1) * 128], in_=pbf[:, 0:AC * 128].rearrange("p (a t) -> p a t", a=AC),
                                                                     func=AF.Copy), [PB[b]], [Bf("oT")])
                    yield "A2"
                    if s > 0:
                        for c in range(CCH):
                            b = nbank((0, 1))
                            dg = diag2[c % 2]
                            for j in range(31):
                                eng_ = V if j % 3 == 0 else P
                                eng_(lambda e, j=j, c=c, dg=dg: e.tensor_scalar(out=dg[:, j, :], in0=cst[:, 0, :], scalar1=cw[:, c, j:j + 1],
                                                                                scalar2=1.0, op0=ALU.mult, op1=ALU.mult), [Bf("cst"), Bf("cw")], [Bf("diag%d_%d" % (c % 2, j))])
                            for j in range(31):
                                TE(lambda e, j=j, c=c, b=b, n=n, dg=dg: e.matmul(ps[b][:, 0:n], lhsT=dg[:, j, :], rhs=aT[:, c, PADL - 30 + j:PADL - 30 + j + n],
                                                                                 start=(j == 0), stop=(j == 30)), [Bf("diag%d_%d" % (c % 2, j)), Bf("aT")], [PB[b]])
                            A(lambda e, c=c, b=b, n=n: e.activation(out=yb[:, c, 0:n], in_=ps[b][:, 0:n], func=AF.Identity, bias=cv[:, 0, c:c + 1], scale=1.0),
                              [PB[b], Bf("cv")], [Bf("yb")])
                            A(lambda e, c=c, b=b, n=n: e.activation(out=sq[:, c, 0:n], in_=ps[b][:, 0:n], func=AF.Square, bias=cv[:, 0, c:c + 1], scale=1.0),
                              [PB[b], Bf("cv")], [Bf("sq")])
                        for c in range(CCH):
                            TE(lambda e, c=c, n=n: e.matmul(ps[2][:, 0:n], lhsT=onesm_f[:, :], rhs=yb[:, c, 0:n], start=(c == 0), stop=(c == CCH - 1)),
                               [Bf("onesm"), Bf("yb")], [PB[2]])
                        for c in range(CCH):
                            TE(lambda e, c=c, n=n: e.matmul(ps[3][:, 0:n], lhsT=onesm_f[:, :], rhs=sq[:, c, 0:n], start=(c == 0), stop=(c == CCH - 1)),
                               [Bf("onesm"), Bf("sq")], [PB[3]])
                        A(lambda e, n=n: e.activation(out=mean_sb[:, 0:n], in_=ps[2][:, 0:n], func=AF.Copy), [PB[2]], [Bf("mean")])
                        V(lambda e, n=n: e.tensor_tensor(out=var_sb[:, 0:n], in0=mean_sb[:, 0:n], in1=mean_sb[:, 0:n], op=ALU.mult), [Bf("mean")], [Bf("var")])
                        V(lambda e, n=n: e.tensor_tensor(out=var_sb[:, 0:n], in0=ps[3][:, 0:n], in1=var_sb[:, 0:n], op=ALU.subtract), [PB[3], Bf("var")], [Bf("var")])
                        V(lambda e, n=n: e.tensor_scalar(out=var_sb[:, 0:n], in0=var_sb[:, 0:n], scalar1=EPS, scalar2=None, op0=ALU.add), [Bf("var")], [Bf("var")])
                        A(lambda e, n=n: e.activation(out=var_sb[:, 0:n], in_=var_sb[:, 0:n], func=AF.Sqrt), [Bf("var")], [Bf("var")])
                        V(lambda e, n=n: e.reciprocal(out=var_sb[:, 0:n], in_=var_sb[:, 0:n]), [Bf("var")], [Bf("var")])
                        for c in range(CCH):
                            V(lambda e, c=c, n=n: e.tensor_tensor(out=tmpc[:, 0:n], in0=yb[:, c, 0:n], in1=mean_sb[:, 0:n], op=ALU.subtract),
                              [Bf("yb"), Bf("mean")], [Bf("tmpc")])
                            V(lambda e, n=n: e.tensor_tensor(out=tmpc[:, 0:n], in0=tmpc[:, 0:n], in1=var_sb[:, 0:n], op=ALU.mult),
                              [Bf("tmpc"), Bf("var")], [Bf("tmpc")])
                            A(lambda e, c=c, n=n: e.activation(out=sT[:, c, 0:n], in_=tmpc[:, 0:n], func=AF.Silu, scale=cv[:, 1, c:c + 1], bias=cv[:, 2, c:c + 1]),
                              [Bf("tmpc"), Bf("cv")], [Bf("sT")])
                    P(lambda e, n=n: e.tensor_copy(out=kT[:, 0:128], in_=kT[:, n:n + 128]), [Bf("kT")], [Bf("kT")])
                    P(lambda e, n=n: e.tensor_copy(out=Vr[:, 0, :, 0:64], in_=Vr[:, n // 128, :, 0:64]), [Bf("Vr")], [Bf("Vr")])
                    P(lambda e, n=n: e.tensor_copy(out=aT[:, :, 0:PADL], in_=aT[:, :, n:n + PADL]), [Bf("aT")], [Bf("aT")])
                    if s == 0:
                        P(lambda e: e.tensor_scalar(out=aT[:, :, 0:PADL], in0=aT[:, :, 0:PADL], scalar1=flg[:, 0:1], scalar2=None, op0=ALU.mult),
                          [Bf("aT"), Bf("flg")], [Bf("aT")])
                    yield "A"
                    if s == 0:
                        return
                    for j in range(KC):
                        bs = (0, 1, 2, 3) if j % 2 == 0 else (4, 5, 6, 7)
                        bA, bGA, bB, bGB = bs
                        gaj, gbj = ga[j % 2], gb[j % 2]
                        for ac in range(AC):
                            TE(lambda e, ac=ac, j=j, bA=bA, n=n: e.matmul(ps[bA][:, 0:n], lhsT=w_ab_bf[:, ac, j * 128:(j + 1) * 128], rhs=oT[:, ac, 0:n],
                                                                          start=(ac == 0), stop=(ac == AC - 1)), [Bf("w_ab"), Bf("oT")], [PB[bA]])
                        for kc in range(KC):
                            TE(lambda e, kc=kc, j=j, bGA=bGA, n=n, XB=XB: e.matmul(ps[bGA][:, 0:n], lhsT=w_in_bf[:, kc, CE + j * 128:CE + (j + 1) * 128], rhs=XB[:, kc, 0:n],
                                                                                  start=(kc == 0), stop=(kc == KC - 1)), [Bf("w_in"), BX], [PB[bGA]])
                        for c in range(CCH):
                            TE(lambda e, c=c, j=j, bB=bB, n=n: e.matmul(ps[bB][:, 0:n], lhsT=w_cb_bf[:, c, j * 128:(j + 1) * 128], rhs=sT[:, c, 0:n],
                                                                        start=(c == 0), stop=(c == CCH - 1)), [Bf("w_cb"), Bf("sT")], [PB[bB]])
                        for kc in range(KC):
                            TE(lambda e, kc=kc, j=j, bGB=bGB, n=n, XB=XB: e.matmul(ps[bGB][:, 0:n], lhsT=w_in_bf[:, kc, CE + D + j * 128:CE + D + (j + 1) * 128], rhs=XB[:, kc, 0:n],
                                                                                  start=(kc == 0), stop=(kc == KC - 1)), [Bf("w_in"), BX], [PB[bGB]])
                        ua = CE // 128 + j
                        ub = CE // 128 + KC + j
                        A(lambda e, bGA=bGA, gaj=gaj, ua=ua, n=n: e.activation(out=gaj[:, 0:n], in_=ps[bGA][:, 0:n], func=AF.Sigmoid, bias=bin_t[:, ua:ua + 1], scale=1.0),
                          [PB[bGA], Bf("bin")], [Bf("ga%d" % (j % 2))])
                        A(lambda e, bGB=bGB, gbj=gbj, ub=ub, n=n: e.activation(out=gbj[:, 0:n], in_=ps[bGB][:, 0:n], func=AF.Sigmoid, bias=bin_t[:, ub:ub + 1], scale=1.0),
                          [PB[bGB], Bf("bin")], [Bf("gb%d" % (j % 2))])
                        t1, t2 = t1s[j % 2], t2s[j % 2]
                        V(lambda e, bA=bA, gaj=gaj, n=n, t1=t1: e.tensor_tensor(out=t1[:, 0:n], in0=ps[bA][:, 0:n], in1=gaj[:, 0:n], op=ALU.mult),
                          [PB[bA], Bf("ga%d" % (j % 2))], [Bf("t1_0")])
                        V(lambda e, bB=bB, gbj=gbj, j=j, n=n, t2=t2: e.scalar_tensor_tensor(out=t2[:, 0:n], in0=ps[bB][:, 0:n], scalar=bcb[:, j:j + 1], in1=gbj[:, 0:n],
                                                                                           op0=ALU.add, op1=ALU.mult), [PB[bB], Bf("gb%d" % (j % 2)), Bf("bcb")], [Bf("t2_0")])
                        P(lambda e, j=j, n=n, t1=t1, t2=t2: e.tensor_tensor(out=mg[:, j, 0:n], in0=t1[:, 0:n], in1=t2[:, 0:n], op=ALU.add),
                          [Bf("t1_0"), Bf("t2_0")], [Bf("mg")])
                    yield "B"
                    for bb in range(n // 128):
                        blk = (s - 1) * (ST // 128) + bb
                        t0 = blk * 128
                        sl = 0
                        xtt, x1t, x1bt = xt[sl], x1[sl], x1b[sl]
                        S.op("sync", lambda e, xtt=xtt, t0=t0: e.dma_start(out=xtt[:, :], in_=xtok[t0:t0 + 128, :]), writes=[Bf("xt%d" % sl)], dma_sem=dxt[sl])
                        zb = (0, 1) if blk % 2 == 0 else (2, 3)
                        for h in range(NH2):
                            b = zb[h % 2]
                            for kc in range(KC):
                                TE(lambda e, kc=kc, b=b, h=h, bb=bb: e.matmul(ps[b][:, 0:W2], lhsT=mg[:, kc, bb * 128:(bb + 1) * 128], rhs=w_o_bf[:, kc, h * W2:(h + 1) * W2],
                                                                            start=(kc == 0), stop=(kc == KC - 1)), [Bf("mg"), Bf("w_o")], [PB[b]])
                            V(lambda e, b=b, h=h, xtt=xtt: e.scalar_tensor_tensor(out=z[:, h * W2:(h + 1) * W2], in0=xtt[:, h * W2:(h + 1) * W2], scalar=alpha,
                                                                                 in1=ps[b][:, 0:W2], op0=ALU.mult, op1=ALU.add), [PB[b], Bf("xt%d" % sl)], [Bf("z")])

                        def layer_norm(src, srcB, dst, dstB, gi):
                            for h in range(NH2):
                                V(lambda e, h=h: e.bn_stats(out=stt[:, h * 6:(h + 1) * 6], in_=src[:, h * W2:(h + 1) * W2]), [srcB], [Bf("stt")])
                            V(lambda e: e.bn_aggr(out=mv[:, :], in_=stt[:, 0:NH2 * 6]), [Bf("stt")], [Bf("mv")])
                            V(lambda e: e.tensor_scalar(out=sm[:, 0:1], in0=mv[:, 1:2], scalar1=EPS, scalar2=None, op0=ALU.add), [Bf("mv")], [Bf("sm")])
                            A(lambda e: e.activation(out=sm[:, 1:2], in_=sm[:, 0:1], func=AF.Sqrt), [Bf("sm")], [Bf("sm")])
                            V(lambda e: e.reciprocal(out=sm[:, 2:3], in_=sm[:, 1:2]), [Bf("sm")], [Bf("sm")])
                            V(lambda e: e.scalar_tensor_tensor(out=sm[:, 3:4], in0=mv[:, 0:1], scalar=-1.0, in1=sm[:, 2:3], op0=ALU.mult, op1=ALU.mult),
                              [Bf("mv"), Bf("sm")], [Bf("sm")])
                            A(lambda e: e.activation(out=src[:, :], in_=src[:, :], func=AF.Identity, scale=sm[:, 2:3], bias=sm[:, 3:4]), [srcB, Bf("sm")], [srcB])
                            P(lambda e: e.tensor_tensor(out=src[:, :], in0=src[:, :], in1=lnbc[:, gi, :], op=ALU.mult), [srcB, Bf("lnbc")], [srcB])
                            P(lambda e: e.tensor_tensor(out=dst[:, :], in0=src[:, :], in1=lnbc[:, gi + 1, :], op=ALU.add), [srcB, Bf("lnbc")], [dstB])

                        layer_norm(z, Bf("z"), x1t, Bf("x1_%d" % sl), 0)
                        S.op("sync", lambda e, x1t=x1t, t0=t0: e.dma_start(out=x1_d[t0:t0 + 128, :], in_=x1t[:, :]), reads=[Bf("x1_%d" % sl)], writes=[B_x1d], dma_sem=dx1[sl])
                        A(lambda e, x1t=x1t, x1bt=x1bt: e.activation(out=x1bt[:, :], in_=x1t[:, :], func=AF.Copy), [Bf("x1_%d" % sl)], [Bf("x1b%d" % sl)])
                        yield "C1"
                        for kc in range(KC):
                            b = 4 + (kc // 4) % 2
                            TE(lambda e, kc=kc, b=b, x1t=x1t: e.transpose(out=ps[b][:, (kc % 4) * 128:(kc % 4 + 1) * 128], in_=x1t[:, kc * 128:(kc + 1) * 128], identity=ident_f[:, :]),
                               [Bf("x1_%d" % sl), Bf("ident_f")], [PB[b]])
                            if kc % 4 == 3 or kc == KC - 1:
                                k0 = (kc // 4) * 4
                                nk = kc - k0 + 1
                                A(lambda e, b=b, k0=k0, nk=nk: e.activation(out=x1T[:, k0:k0 + nk, :], in_=ps[b][:, 0:nk * 128].rearrange("p (k t) -> p k t", k=nk), func=AF.Copy),
                                  [PB[b]], [Bf("x1T")])
                        for kc in range(KC):
                            TE(lambda e, kc=kc: e.matmul(ps[6][:, 0:NE], lhsT=x1T[:, kc, :], rhs=w_r_f[:, kc, :], start=(kc == 0), stop=False),
                               [Bf("x1T"), Bf("w_r")], [PB[6]])
                        TE(lambda e: e.matmul(ps[6][:, 0:NE], lhsT=ones_f[0:1, :], rhs=b_r_f[0:1, :], start=False, stop=True), [Bf("ones_f"), Bf("b_r")], [PB[6]])
                        V(lambda e: e.tensor_copy(out=lg[:, :], in_=ps[6][:, 0:NE]), [PB[6]], [Bf("lg")])
                        V(lambda e: e.max(out=top8[:, :], in_=lg[:, :]), [Bf("lg")], [Bf("top8")])
                        V(lambda e: e.max_index(out=tidx[:, :], in_max=top8[:, :], in_values=lg[:, :]), [Bf("lg"), Bf("top8")], [Bf("tidx")])
                        V(lambda e: e.tensor_scalar(out=sm[:, 4:5], in0=top8[:, 0:1], scalar1=-1.0, scalar2=None, op0=ALU.mult), [Bf("top8")], [Bf("sm2")])
                        A(lambda e: e.activation(out=ex4[:, :], in_=top8[:, 0:4], func=AF.Exp, bias=sm[:, 4:5], scale=1.0), [Bf("top8"), Bf("sm2")], [Bf("ex4")])
                        V(lambda e: e.tensor_reduce(out=sm[:, 5:6], in_=ex4[:, :], axis=mybir.AxisListType.X, op=ALU.add), [Bf("ex4")], [Bf("sm3")])
                        V(lambda e: e.reciprocal(out=sm[:, 6:7], in_=sm[:, 5:6]), [Bf("sm3")], [Bf("sm3")])
                        V(lambda e, blk=blk: e.tensor_scalar(out=gate[:, blk, :], in0=ex4[:, :], scalar1=sm[:, 6:7], scalar2=None, op0=ALU.mult),
                          [Bf("ex4"), Bf("sm3")], [B_gate])
                        V(lambda e: e.tensor_scalar(out=mskb[:, :], in0=lg[:, :], scalar1=top8[:, 3:4], scalar2=None, op0=ALU.is_ge), [Bf("lg"), Bf("top8")], [Bf("mskb")])
                        yield "C2"
                        TE(lambda e: e.matmul(ps[7][:, 0:NE], lhsT=ltri_b[:, :], rhs=mskb[:, :], start=True, stop=True), [Bf("ltri"), Bf("mskb")], [PB[7]])
                        TE(lambda e: e.matmul(ps[7][:, 64:64 + NE], lhsT=ones_b[:, :], rhs=mskb[:, :], start=True, stop=True), [Bf("ones_b"), Bf("mskb")], [PB[7]])
                        V(lambda e: e.tensor_tensor(out=pos[:, :], in0=ps[7][:, 0:NE], in1=run_c[:, :], op=ALU.add), [PB[7], Bf("run")], [Bf("pos")])
                        V(lambda e: e.tensor_tensor(out=run_c[:, :], in0=ps[7][:, 64:64 + NE], in1=run_c[:, :], op=ALU.add), [PB[7], Bf("run")], [Bf("run")])
                        V(lambda e: e.tensor_scalar(out=ovf[:, :], in0=pos[:, :], scalar1=float(CAP), scalar2=BIG, op0=ALU.is_ge, op1=ALU.mult), [Bf("pos")], [Bf("ovf")])
                        V(lambda e: e.tensor_tensor(out=pos[:, :], in0=pos[:, :], in1=er[:, 1, :], op=ALU.add), [Bf("pos"), Bf("er")], [Bf("pos")])
                        V(lambda e: e.tensor_tensor(out=pos[:, :], in0=pos[:, :], in1=ovf[:, :], op=ALU.add), [Bf("pos"), Bf("ovf")], [Bf("pos")])
                        V(lambda e: e.tensor_copy(out=ef[:, :], in_=tidx[:, 0:4]), [Bf("tidx")], [Bf("ef")])
                        for k in range(4):
                            V(lambda e, k=k: e.scalar_tensor_tensor(out=junk[:, :], in0=er[:, 0, :], scalar=ef[:, k:k + 1], in1=pos[:, :], op0=ALU.is_equal, op1=ALU.mult,
                                                                   accum_out=dest_f[:, k:k + 1]), [Bf("er"), Bf("ef"), Bf("pos")], [Bf("junk"), Bf("dest_f")])
                        V(lambda e, blk=blk: e.tensor_copy(out=dest_i[:, blk, :], in_=dest_f[:, :]), [Bf("dest_f")], [B_dest])
                        for k in range(4):
                            S.op("gpsimd", lambda e, k=k, blk=blk, x1bt=x1bt: e.indirect_dma_start(
                                out=xs_d[:, :], out_offset=bass.IndirectOffsetOnAxis(ap=dest_i[:, blk, k:k + 1], axis=0), in_=x1bt[:, :], in_offset=None,
                                bounds_check=breg(e, 1), oob_is_err=False), reads=[Bf("x1b%d" % sl), B_dest], writes=[], dma_sem=dsc[sl])

            gens = []
            tau0 = 0
            for s, n in enumerate(sizes):
                gens.append(body(s, n, tau0))
                tau0 += n

            def adv(i):
                if 0 <= i < len(gens):
                    try:
                        next(gens[i])
                    except StopIteration:
                        pass

            for s in range(len(sizes) + 3):
                adv(s)
                adv(s - 2)
                adv(s - 1)
                adv(s)
                adv(s - 1)
                adv(s)
                adv(s - 1)
                adv(s)
                adv(s - 1)
                if cast_jobs and s < len(sizes):
                    ncast = len(cast_jobs)
                    nsp = min(4, NST)
                    per = (ncast + nsp - 1) // nsp
                    if 1 <= s <= nsp:
                        for jn in range((s - 1) * per, min(s * per, ncast)):
                            issue_cast(cast_jobs[jn], jn)
                    if s == min(6, len(sizes) - 1):
                        issue_ag()
            for g_ in gens:
                for _ in g_:
                    pass
            S.emit()

        S = Sched(nc, "e")
        PB = [Buf("ps%d" % i) for i in range(8)]
        with ExitStack() as st:
            def sb(name, shape, dt):
                return st.enter_context(nc.sbuf_tensor(name, list(shape), dt))

            wu = [sb("wu%d" % i, [128, KC, 2 * D], BF16) for i in range(2)]
            wd = [sb("wd%d" % i, [128, FC, D], BF16) for i in range(2)]
            xs_t = [sb("xs_t%d" % i, [128, NB, D], BF16) for i in range(2)]
            xsT = [sb("xsT%d" % i, [128, KC, CAP], BF16) for i in range(2)]
            actT = [sb("actT%d" % i, [128, FC, CAP], BF16) for i in range(2)]
            xg = [sb("xg%d" % i, [128, CAP], F32) for i in range(2)]
            sg = [sb("sg%d" % i, [128, CAP], F32) for i in range(2)]
            xl = [sb("xl%d" % i, [128, CAP], F32) for i in range(2)]
            ys_t = [sb("ys_t%d" % i, [128, D], F32) for i in range(2)]
            Bn = {}

            def Bf(n):
                if n not in Bn:
                    Bn[n] = Buf(n)
                return Bn[n]

            V = lambda fn, r=(), w=(): S.op("vector", fn, reads=r, writes=w)
            A = lambda fn, r=(), w=(): S.op("scalar", fn, reads=r, writes=w)
            P = lambda fn, r=(), w=(): S.op("gpsimd", fn, reads=r, writes=w)
            TE = lambda fn, r=(), w=(): S.op("tensor", fn, reads=r, writes=w)
            dwu = [S.dsem("wu0"), S.dsem("wu1")]
            dwd = [S.dsem("wd0"), S.dsem("wd1")]
            dxs = [S.dsem("xs0"), S.dsem("xs1")]
            dys = [S.dsem("ys0"), S.dsem("ys1")]

            def load_wu(e_):
                sl = e_ % 2
                if not cfg["repl"]:
                    S.op("sync", lambda e, sl=sl, e_=e_: e.dma_start(out=wu[sl][:, :, :], in_=wa_up[e_ * D:(e_ + 1) * D, :].rearrange("(kc p) f -> p kc f", p=128)),
                         writes=[Bf("wu%d" % sl)], dma_sem=dwu[sl])
                    return
                nsp = 2 if KC >= 2 else 1
                kh = KC // nsp
                for h in range(nsp):
                    S.op("gpsimd", lambda e, h=h, sl=sl, e_=e_: e.dma_start(out=wu[sl][:, h * kh:(h + 1) * kh, :],
                                                                          in_=w_up[e_, h * kh * 128:(h + 1) * kh * 128, :].rearrange("(kc p) f -> p kc f", p=128)),
                         writes=[Bf("wu%d" % sl)], dma_sem=dwu[sl])

            def load_wd(e_):
                sl = e_ % 2
                if not cfg["repl"]:
                    S.op("sync", lambda e, sl=sl, e_=e_: e.dma_start(out=wd[sl][:, :, :], in_=wa_dn[e_ * D:(e_ + 1) * D, :].rearrange("(kc p) f -> p kc f", p=128)),
                         writes=[Bf("wd%d" % sl)], dma_sem=dwd[sl])
                    return
                S.op("gpsimd", lambda e, sl=sl, e_=e_: e.dma_start(out=wd[sl][:, :, :], in_=w_dn[e_, :, :].rearrange("(fc p) d -> p fc d", p=128)),
                     writes=[Bf("wd%d" % sl)], dma_sem=dwd[sl])

            def load_xs(e_):
                sl = e_ % 2
                S.op("sync", lambda e, sl=sl, e_=e_: e.dma_start(out=xs_t[sl][:, :, :], in_=xs_d[e_ * CAP:(e_ + 1) * CAP, :].rearrange("(b p) d -> p b d", p=128)),
                     reads=[B_xs], writes=[Bf("xs_t%d" % sl)], dma_sem=dxs[sl])

            load_wu(0)
            load_wd(0)
            load_xs(0)
            def down_proj(e_):
                sl = e_ % 2
                Bwd = Bf("wd%d" % sl)
                for nb in range(NB):
                    ysl = (e_ * NB + nb) % 2
                    for h in range(NH2):
                        b = 4 + h % 2
                        for fc in range(FC):
                            TE(lambda e, fc=fc, nb=nb, h=h, b=b, sl=sl: e.matmul(ps[b][:, 0:W2], lhsT=actT[sl][:, fc, nb * 128:(nb + 1) * 128], rhs=wd[sl][:, fc, h * W2:(h + 1) * W2],
                                                                                start=(fc == 0), stop=False), [Bf("actT%d" % sl), Bwd], [PB[b]])
                        TE(lambda e, h=h, b=b, e_=e_: e.matmul(ps[b][:, 0:W2], lhsT=selT[:, e_, :], rhs=bdn_b[:, h * W2:(h + 1) * W2], start=False, stop=True),
                           [], [PB[b]])
                        if h % 2 == 0:
                            A(lambda e, b=b, h=h, ysl=ysl: e.activation(out=ys_t[ysl][:, h * W2:(h + 1) * W2], in_=ps[b][:, 0:W2], func=AF.Copy), [PB[b]], [Bf("ys_t%d" % ysl)])
                        else:
                            V(lambda e, b=b, h=h, ysl=ysl: e.tensor_copy(out=ys_t[ysl][:, h * W2:(h + 1) * W2], in_=ps[b][:, 0:W2]), [PB[b]], [Bf("ys_t%d" % ysl)])
                    r0 = e_ * CAP + nb * 128
                    S.op("sync", lambda e, ysl=ysl, r0=r0: e.dma_start(out=ys_d[r0:r0 + 128, :], in_=ys_t[ysl][:, :]), reads=[Bf("ys_t%d" % ysl)], writes=[], dma_sem=dys[ysl])

            cnt = [0]
            for e_ in range(NE):
                sl = e_ % 2
                if e_ + 1 < NE:
                    load_wu(e_ + 1)
                    load_xs(e_ + 1)
                Bwu, Bwd = Bf("wu%d" % sl), Bf("wd%d" % sl)
                for nb in range(NB):
                    b = 6 + nb % 2
                    pbf = ps[b][:, :].bitcast(BF16)
                    for kc in range(KC):
                        TE(lambda e, kc=kc, nb=nb, pbf=pbf, sl=sl: e.transpose(out=pbf[:, kc * 128:(kc + 1) * 128], in_=xs_t[sl][:, nb, kc * 128:(kc + 1) * 128], identity=ident_b[:, :]),
                           [Bf("xs_t%d" % sl)], [PB[b]])
                    cp = A if nb % 2 == 0 else V
                    if nb % 2 == 0:
                        A(lambda e, pbf=pbf, nb=nb, sl=sl: e.activation(out=xsT[sl][:, :, nb * 128:(nb + 1) * 128], in_=pbf[:, 0:KC * 128].rearrange("p (k t) -> p k t", k=KC), func=AF.Copy),
                          [PB[b]], [Bf("xsT%d" % sl)])
                    else:
                        V(lambda e, pbf=pbf, nb=nb, sl=sl: e.tensor_copy(out=xsT[sl][:, :, nb * 128:(nb + 1) * 128], in_=pbf[:, 0:KC * 128].rearrange("p (k t) -> p k t", k=KC)),
                          [PB[b]], [Bf("xsT%d" % sl)])
                for fp in range(FC):
                    cnt[0] += 1
                    pr = cnt[0] % 2
                    bG, bL = (0, 1) if pr == 0 else (2, 3)
                    for kc in range(KC):
                        TE(lambda e, kc=kc, fp=fp, bG=bG, sl=sl: e.matmul(ps[bG][:, 0:CAP], lhsT=wu[sl][:, kc, fp * 128:(fp + 1) * 128], rhs=xsT[sl][:, kc, :],
                                                                         start=(kc == 0), stop=(kc == KC - 1)), [Bwu, Bf("xsT%d" % sl)], [PB[bG]])
                    for kc in range(KC):
                        TE(lambda e, kc=kc, fp=fp, bL=bL, sl=sl: e.matmul(ps[bL][:, 0:CAP], lhsT=wu[sl][:, kc, D + fp * 128:D + (fp + 1) * 128], rhs=xsT[sl][:, kc, :],
                                                                         start=(kc == 0), stop=(kc == KC - 1)), [Bwu, Bf("xsT%d" % sl)], [PB[bL]])
                    V(lambda e, bG=bG, e_=e_, fp=fp, pr=pr: e.tensor_scalar(out=xg[pr][:, :], in0=ps[bG][:, 0:CAP], scalar1=bup[:, e_, fp:fp + 1], scalar2=7.0, op0=ALU.add, op1=ALU.min),
                      [PB[bG]], [Bf("xg%d" % pr)])
                    A(lambda e, pr=pr: e.activation(out=sg[pr][:, :], in_=xg[pr][:, :], func=AF.Sigmoid, scale=1.702), [Bf("xg%d" % pr)], [Bf("sg%d" % pr)])
                    V(lambda e, bL=bL, e_=e_, fp=fp, pr=pr: e.tensor_scalar(out=xl[pr][:, :], in0=ps[bL][:, 0:CAP], scalar1=bup[:, e_, FC + fp:FC + fp + 1], scalar2=7.0, op0=ALU.add, op1=ALU.min),
                      [PB[bL]], [Bf("xl%d" % pr)])
                    P(lambda e, pr=pr: e.tensor_scalar(out=xl[pr][:, :], in0=xl[pr][:, :], scalar1=7.0, scalar2=-7.0, op0=ALU.min, op1=ALU.max), [Bf("xl%d" % pr)], [Bf("xl%d" % pr)])
                    P(lambda e, pr=pr: e.tensor_tensor(out=xg[pr][:, :], in0=xg[pr][:, :], in1=sg[pr][:, :], op=ALU.mult), [Bf("xg%d" % pr), Bf("sg%d" % pr)], [Bf("xg%d" % pr)])
                    V(lambda e, pr=pr, fp=fp, sl=sl: e.scalar_tensor_tensor(out=actT[sl][:, fp, :], in0=xl[pr][:, :], scalar=1.0, in1=xg[pr][:, :], op0=ALU.add, op1=ALU.mult),
                      [Bf("xg%d" % pr), Bf("xl%d" % pr)], [Bf("actT%d" % sl)])
                if e_ > 0:
                    down_proj(e_ - 1)
                if e_ + 1 < NE:
                    load_wd(e_ + 1)
            down_proj(NE - 1)
            S.emit()

        S = Sched(nc, "c")
        with ExitStack() as st:
            def sb(name, shape, dt):
                return st.enter_context(nc.sbuf_tensor(name, list(shape), dt))

            x1c = [sb("x1c%d" % i, [128, D], F32) for i in range(2)]
            yk = [[sb("yk%d_%d" % (i, k), [128, D], F32) for k in range(4)] for i in range(2)]
            accs = [sb("acc%d" % i, [128, D], F32) for i in range(2)]
            nrms = [sb("nrm2_%d" % i, [128, D], F32) for i in range(2)]
            res = [sb("res%d" % i, [128, D], F32) for i in range(2)]
            stt = sb("stt2", [128, max(NH2, 1) * 6], F32)
            mv = sb("mv2", [128, 2], F32)
            sm = sb("sm2", [128, 8], F32)
            Bn = {}

            def Bf(n):
                if n not in Bn:
                    Bn[n] = Buf(n)
                return Bn[n]

            V = lambda fn, r=(), w=(): S.op("vector", fn, reads=r, writes=w)
            A = lambda fn, r=(), w=(): S.op("scalar", fn, reads=r, writes=w)
            P = lambda fn, r=(), w=(): S.op("gpsimd", fn, reads=r, writes=w)
            S.op("sync", lambda e: e.dma_start(out=lnbc[:, :, :], in_=lnb[:, 2:4, :]), writes=[Bf("lnbc")], dma_sem=S.dsem("ln2", group=True))
            dxc = [S.dsem("xc0"), S.dsem("xc1")]
            dyk = [S.dsem("yk0"), S.dsem("yk1")]
            dout = [S.dsem("o0"), S.dsem("o1")]
            def fetch(blk):
                sl = blk % 2
                t0 = blk * 128
                S.op("sync", lambda e, sl=sl, t0=t0: e.dma_start(out=x1c[sl][:, :], in_=x1_d[t0:t0 + 128, :]), writes=[Bf("x1c%d" % sl)], dma_sem=dxc[sl])
                for k in range(4):
                    S.op("gpsimd", lambda e, k=k, sl=sl, blk=blk: e.indirect_dma_start(
                        out=yk[sl][k][:, :], out_offset=None, in_=ys_d[:, :], in_offset=bass.IndirectOffsetOnAxis(ap=dest_i[:, blk, k:k + 1], axis=0),
                        bounds_check=breg(e, 3), oob_is_err=False), writes=[Bf("yk%d_%d" % (sl, k))], dma_sem=dyk[sl])

            fetch(0)
            for blk in range(NBLK):
                sl = blk % 2
                t0 = blk * 128
                if blk + 1 < NBLK:
                    fetch(blk + 1)
                acc, nrm = accs[sl], nrms[sl]
                BA, BN = Bf("acc%d" % sl), Bf("nrm%d" % sl)
                V(lambda e, sl=sl, acc=acc: e.tensor_scalar(out=acc[:, :], in0=x1c[sl][:, :], scalar1=alpha, scalar2=None, op0=ALU.mult), [Bf("x1c%d" % sl)], [BA])
                for k in range(4):
                    V(lambda e, k=k, sl=sl, blk=blk, acc=acc: e.scalar_tensor_tensor(out=acc[:, :], in0=yk[sl][k][:, :], scalar=gate[:, blk, k:k + 1], in1=acc[:, :], op0=ALU.mult, op1=ALU.add),
                      [Bf("yk%d_%d" % (sl, k)), BA], [BA])
                for h in range(NH2):
                    V(lambda e, h=h, acc=acc: e.bn_stats(out=stt[:, h * 6:(h + 1) * 6], in_=acc[:, h * W2:(h + 1) * W2]), [BA], [Bf("stt")])
                V(lambda e: e.bn_aggr(out=mv[:, :], in_=stt[:, 0:NH2 * 6]), [Bf("stt")], [Bf("mv")])
                V(lambda e: e.tensor_scalar(out=sm[:, 0:1], in0=mv[:, 1:2], scalar1=EPS, scalar2=None, op0=ALU.add), [Bf("mv")], [Bf("sm")])
                A(lambda e: e.activation(out=sm[:, 1:2], in_=sm[:, 0:1], func=AF.Sqrt), [Bf("sm")], [Bf("sm")])
                V(lambda e: e.reciprocal(out=sm[:, 2:3], in_=sm[:, 1:2]), [Bf("sm")], [Bf("sm")])
                V(lambda e: e.scalar_tensor_tensor(out=sm[:, 3:4], in0=mv[:, 0:1], scalar=-1.0, in1=sm[:, 2:3], op0=ALU.mult, op1=ALU.mult), [Bf("mv"), Bf("sm")], [Bf("sm")])
                A(lambda e, acc=acc, nrm=nrm: e.activation(out=nrm[:, :], in_=acc[:, :], func=AF.Identity, scale=sm[:, 2:3], bias=sm[:, 3:4]), [BA, Bf("sm")], [BN])
                P(lambda e, nrm=nrm: e.tensor_tensor(out=nrm[:, :], in0=nrm[:, :], in1=lnbc[:, 0, :], op=ALU.mult), [BN, Bf("lnbc")], [BN])
                P(lambda e, sl=sl, nrm=nrm: e.tensor_tensor(out=res[sl][:, :], in0=nrm[:, :], in1=lnbc[:, 1, :], op=ALU.add), [BN, Bf("lnbc")], [Bf("res%d" % sl)])
                S.op("sync", lambda e, sl=sl, t0=t0: e.dma_start(out=out[t0:t0 + 128, :], in_=res[sl][:, :]), reads=[Bf("res%d" % sl)], writes=[], dma_sem=dout[sl])
            S.emit()
    return nc


def prep_inputs(cfg, x, w_in, b_in, attn_sinks, w_attn_br, conv_w, conv_b, conv_ln_g, conv_ln_b,
                w_conv_br, b_conv_br, w_o, ln1_g, ln1_b, w_router, b_router, w_up, b_up,
                w_down, b_down, ln2_g, ln2_b):
    D, NH, CC, NE, T, CAP = (cfg[k] for k in ["D", "NH", "CC", "NE", "T", "CAP"])
    G, AW, QE, KE, VE, CE, INW, KC, CCH, FC = (cfg[k] for k in ["G", "AW", "QE", "KE", "VE", "CE", "INW", "KC", "CCH", "FC"])
    NC_ = cfg["NCORES"]
    CPB = NC_ // cfg["B"]
    f = lambda a: np.ascontiguousarray(np.asarray(a, dtype=np.float32))
    x = f(x)
    w_in0, b_in0 = f(w_in)[0], f(b_in)[0]
    qperm = np.array([(g * G + c) * 64 + j for c in range(G) for g in range(2) for j in range(64)])
    perm = np.concatenate([qperm, np.arange(QE, INW)])
    w_in_p = np.ascontiguousarray(w_in0[:, perm])
    b_in_p = b_in0[perm]
    b_in_t = np.ascontiguousarray(b_in_p.reshape(INW // 128, 128).T)
    b_v = np.ascontiguousarray(b_in_p[KE:VE].reshape(1, 128))
    sinks_b = np.ascontiguousarray(np.broadcast_to(f(attn_sinks)[0][None, :], (128, NH)))
    cw_t = np.ascontiguousarray(f(conv_w)[0].T.reshape(CCH, 128, 31).transpose(1, 0, 2))
    pp = lambda v: v.reshape(-1, 128).T
    cvec = np.ascontiguousarray(np.stack([pp(f(conv_b)[0]), pp(f(conv_ln_g)[0]), pp(f(conv_ln_b)[0])], axis=1))
    bcb_t = np.ascontiguousarray(pp(f(b_conv_br)[0]))
    lnb = np.ascontiguousarray(np.broadcast_to(np.stack([f(ln1_g)[0], f(ln1_b)[0], f(ln2_g)[0], f(ln2_b)[0]])[None], (128, 4, D)))
    w_up0 = f(w_up)[0]
    w_up_p = np.concatenate([w_up0[:, :, 0::2], w_up0[:, :, 1::2]], axis=2)
    b_up0 = f(b_up)[0]
    b_up_p = np.concatenate([b_up0[:, 0::2], b_up0[:, 1::2]], axis=1)
    b_up_t = np.ascontiguousarray(b_up_p.reshape(NE, 2 * FC, 128).transpose(2, 0, 1))
    w_dn0 = f(w_down)[0]
    b_dn = np.ascontiguousarray(f(b_down)[0])
    ident = np.eye(128, dtype=np.float32)
    ltri = np.triu(np.ones((128, 128), np.float32), 1)
    consts = np.ascontiguousarray(np.stack([ident, ltri, np.ones((128, 128), np.float32)], axis=1))
    erow = np.ascontiguousarray(np.broadcast_to(np.stack([np.arange(NE, dtype=np.float32), np.arange(NE, dtype=np.float32) * CAP])[None], (128, 2, NE)))
    kk = np.arange(128)[:, None]
    qq = np.arange(128)[None, :]
    m_cur = (kk <= qq).astype(np.float32)
    m_prev = (kk > qq).astype(np.float32)
    shared = dict(w_in=w_in_p, b_in_t=b_in_t, b_v=b_v, sinks_b=sinks_b, w_ab=f(w_attn_br)[0], cw_t=cw_t, cvec=cvec,
                  w_cb=f(w_conv_br)[0], bcb_t=bcb_t, w_o=f(w_o)[0], lnb=lnb, w_r=f(w_router)[0], b_r=f(b_router)[0].reshape(1, NE),
                  b_up_t=b_up_t, b_dn=b_dn, consts=consts, erow=erow)
    maps = []
    for c in range(NC_):
        b, h = c // CPB, c % CPB
        st = h * T
        xT = np.zeros((D, 128 + T), np.float32)
        xT[:, 128:] = x[b, st:st + T].T
        if h > 0:
            xT[:, :128] = x[b, st - 128:st].T
        fl = 1.0 if h > 0 else 0.0
        masks = np.stack([np.tile(m_cur, (1, G)), np.tile(m_prev, (1, G)), np.tile(m_prev * fl, (1, G))], axis=1)
        m = dict(shared)
        m.update(xT=xT, xtok=np.ascontiguousarray(x[b, st:st + T]), masks=np.ascontiguousarray(masks),
                 flag=np.full((128, 1), fl, np.float32))
        if cfg["repl"]:
            m.update(w_up=w_up_p, w_dn=w_dn0)
        else:
            E = cfg["EPC"]
            m.update(w_up=np.ascontiguousarray(w_up_p[c * E:(c + 1) * E]), w_dn=np.ascontiguousarray(w_dn0[c * E:(c + 1) * E]))
        maps.append(m)
    return maps


def run_cfg(cfg, inputs, trace=False):
    maps = prep_inputs(cfg, **inputs)
    nc = build(cfg)
    res = run_bass_kernel_spmd(nc, maps, core_ids=list(range(cfg["NCORES"])), trace=trace)
    T, D, B = cfg["T"], cfg["D"], cfg["B"]
    CPB = cfg["NCORES"] // B
    outv = np.zeros((B, CPB * T, D), np.float32)
    for c in range(cfg["NCORES"]):
        b, h = c // CPB, c % CPB
        outv[b, h * T:(h + 1) * T] = res.results[c]["out"]
    return outv, res


def kernel(**inputs):
    cfg = make_cfg()
    outv, _ = run_cfg(cfg, inputs)
    return outv
```
